# Optimizing a Trainium2 kernel written in Bass

```python
import math
import jax
import jax.numpy as jnp
from jax import lax
import numpy as np

D_MODEL = 2048
BATCH = 4
SEQ = 4096
DEPTH = 1

GRID_W = 64
CTX_LEN = 256
EPS = 1e-6

N_Q_HEADS = 8
N_KV_HEADS = 2
Q_PER_KV = N_Q_HEADS // N_KV_HEADS
HEAD_DIM = 128
ATTN_WIDTH = N_Q_HEADS * HEAD_DIM
KV_WIDTH = N_KV_HEADS * HEAD_DIM
WINDOW = 128
ATTN_BLOCK = 128
ROPE_THETA = 10000.0
ROPE_AXIS_DIM = HEAD_DIM // 2

SSD_HEADS = 16
SSD_HEAD_DIM = 64
SSD_WIDTH = SSD_HEADS * SSD_HEAD_DIM
SSD_GROUPS = 4
SSD_HEADS_PER_GROUP = SSD_HEADS // SSD_GROUPS
SSD_STATE = 128
SSD_CONV = 5
SSD_CHUNK = 128
N_DIRS = 2
XBC_WIDTH = SSD_WIDTH + 2 * SSD_GROUPS * SSD_STATE

MIX_WIDTH = ATTN_WIDTH + SSD_WIDTH
IN_SPLITS = (ATTN_WIDTH, KV_WIDTH, KV_WIDTH, SSD_WIDTH, XBC_WIDTH, N_DIRS * SSD_HEADS)
IN_WIDTH = ATTN_WIDTH + 2 * KV_WIDTH + SSD_WIDTH + XBC_WIDTH + N_DIRS * SSD_HEADS

N_EXPERTS = 64
N_EXPERT_GROUPS = 8
EXPERTS_PER_GROUP = N_EXPERTS // N_EXPERT_GROUPS
TOPK_GROUPS = 4
TOP_K = 8
EXPERT_DIM = 512
SHARED_DIM = 512
ROUTED_SCALE = 2.5
MOE_BLOCK = 128

kernel_name = "hybrid_attn_ssd_moe_dit_layer"


def rmsnorm(x, g):
    xf = x.astype(jnp.float32)
    y = xf * lax.rsqrt(jnp.mean(xf * xf, axis=-1, keepdims=True) + EPS)
    return (y * g.astype(jnp.float32)).astype(x.dtype)


def modulate(h, shift, scale):
    return h * (1 + scale) + shift


def split_cols(p):
    idx = np.cumsum(IN_SPLITS)[:-1].tolist()
    return jnp.split(p, idx, axis=-1)


def rope_tables(pos, dim):
    inv = ROPE_THETA ** (-jnp.arange(0, dim, 2, dtype=jnp.float32) / dim)
    ang = pos.astype(jnp.float32)[:, None] * inv[None, :]
    return jnp.cos(ang), jnp.sin(ang)


def rotate_half_axis(x, cos, sin):
    half = x.shape[-1] // 2
    shape = (1, cos.shape[0]) + (1,) * (x.ndim - 3) + (half,)
    c = cos.reshape(shape).astype(x.dtype)
    s = sin.reshape(shape).astype(x.dtype)
    x1, x2 = x[..., :half], x[..., half:]
    return jnp.concatenate([x1 * c - x2 * s, x1 * s + x2 * c], axis=-1)


def axial_rope(x, rows, cols):
    cr, sr = rope_tables(rows, ROPE_AXIS_DIM)
    cc, sc = rope_tables(cols, ROPE_AXIS_DIM)
    return jnp.concatenate([rotate_half_axis(x[..., :ROPE_AXIS_DIM], cr, sr),
                            rotate_half_axis(x[..., ROPE_AXIS_DIM:], cc, sc)], axis=-1)


def windowed_attention(q, k, v, k_ctx, v_ctx, sink):
    b, n = q.shape[:2]
    nb = n // ATTN_BLOCK
    scale = HEAD_DIM ** -0.5
    qb = q.reshape(b, nb, ATTN_BLOCK, N_KV_HEADS, Q_PER_KV, HEAD_DIM)

    def band(t):
        tp = jnp.pad(t, ((0, 0), (ATTN_BLOCK, ATTN_BLOCK), (0, 0), (0, 0)))
        tp = tp.reshape(b, nb + 2, ATTN_BLOCK, N_KV_HEADS, HEAD_DIM)
        return jnp.concatenate([tp[:, :-2], tp[:, 1:-1], tp[:, 2:]], axis=2)

    kw, vw = band(k), band(v)
    s_loc = jnp.einsum('bnqhgd,bnkhd->bnhgqk', qb, kw).astype(jnp.float32) * scale
    blk = jnp.arange(nb)[:, None, None]
    qpos = blk * ATTN_BLOCK + jnp.arange(ATTN_BLOCK)[None, :, None]
    kpos = (blk - 1) * ATTN_BLOCK + jnp.arange(3 * ATTN_BLOCK)[None, None, :]
    valid = (jnp.abs(qpos - kpos) <= WINDOW) & (kpos >= 0) & (kpos < n)
    s_loc = jnp.where(valid[None, :, None, None], s_loc, -jnp.inf)
    s_ctx = jnp.einsum('bnqhgd,bchd->bnhgqc', qb, k_ctx).astype(jnp.float32) * scale
    s_sink = jnp.broadcast_to(sink.astype(jnp.float32)[None, None, :, :, None, None],
                              s_loc.shape[:-1] + (1,))
    p = jax.nn.softmax(jnp.concatenate([s_loc, s_ctx, s_sink], axis=-1), axis=-1)
    n_loc = 3 * ATTN_BLOCK
    p_loc = p[..., :n_loc].astype(v.dtype)
    p_ctx = p[..., n_loc:n_loc + k_ctx.shape[1]].astype(v.dtype)
    o = (jnp.einsum('bnhgqk,bnkhd->bnqhgd', p_loc, vw)
         + jnp.einsum('bnhgqc,bchd->bnqhgd', p_ctx, v_ctx))
    return o.reshape(b, n, ATTN_WIDTH)


def context_attention(q, k, v, sink):
    b, n = q.shape[:2]
    s = jnp.einsum('bqhgd,bkhd->bhgqk', q, k).astype(jnp.float32) * (HEAD_DIM ** -0.5)
    s_sink = jnp.broadcast_to(sink.astype(jnp.float32)[None, :, :, None, None], s.shape[:-1] + (1,))
    p = jax.nn.softmax(jnp.concatenate([s, s_sink], axis=-1), axis=-1)[..., :-1]
    o = jnp.einsum('bhgqk,bkhd->bqhgd', p.astype(v.dtype), v)
    return o.reshape(b, n, ATTN_WIDTH)


def dwconv_centred(u, w, bias):
    out = lax.conv_general_dilated(u, w[:, None, :], window_strides=(1,),
                                   padding=[(SSD_CONV // 2, SSD_CONV // 2)],
                                   dimension_numbers=('NWC', 'WIO', 'NWC'),
                                   feature_group_count=u.shape[-1])
    return out + bias


def conv_split(xbc, conv_w, conv_b):
    b, n, _ = xbc.shape
    u = jax.nn.silu(dwconv_centred(xbc, conv_w, conv_b))
    xs, bm, cm = jnp.split(u, [SSD_WIDTH, SSD_WIDTH + SSD_GROUPS * SSD_STATE], axis=-1)
    return (xs.reshape(b, n, SSD_GROUPS, SSD_HEADS_PER_GROUP, SSD_HEAD_DIM),
            bm.reshape(b, n, SSD_GROUPS, SSD_STATE),
            cm.reshape(b, n, SSD_GROUPS, SSD_STATE))


def ssd_chunked(x, dt, a, bm, cm, h0):
    b, n = x.shape[:2]
    nc, q = n // SSD_CHUNK, SSD_CHUNK
    x = x.reshape(b, nc, q, SSD_GROUPS, SSD_HEADS_PER_GROUP, SSD_HEAD_DIM)
    dt = dt.reshape(b, nc, q, SSD_GROUPS, SSD_HEADS_PER_GROUP)
    bm = bm.reshape(b, nc, q, SSD_GROUPS, SSD_STATE)
    cm = cm.reshape(b, nc, q, SSD_GROUPS, SSD_STATE)
    a_cum = jnp.cumsum(dt * a, axis=2)
    tril = jnp.tril(jnp.ones((q, q), dtype=bool))[None, None, :, :, None, None]
    seg = a_cum[:, :, :, None] - a_cum[:, :, None, :]
    decay_in = jnp.exp(jnp.where(tril, seg, -jnp.inf))
    xd = x * dt[..., None]
    cb = jnp.einsum('bclgn,bcsgn->bclsg', cm, bm)
    y_diag = jnp.einsum('bclsgr,bcsgrp->bclgrp', cb[..., None] * decay_in, xd)
    decay_out = jnp.exp(a_cum[:, :, -1:] - a_cum)
    states = jnp.einsum('bcsgn,bcsgrp->bcgrpn', bm, xd * decay_out[..., None])
    chunk_decay = jnp.exp(a_cum[:, :, -1])

    def step(h, inp):
        dec, st = inp
        return h * dec[..., None, None] + st, h

    h_last, h_prev = lax.scan(step, h0.astype(states.dtype),
                              (jnp.moveaxis(chunk_decay, 1, 0), jnp.moveaxis(states, 1, 0)))
    h_prev = jnp.moveaxis(h_prev, 0, 1)
    y_off = jnp.einsum('bclgn,bcgrpn->bclgrp', cm, h_prev) * jnp.exp(a_cum)[..., None]
    y = (y_diag + y_off).reshape(b, n, SSD_GROUPS, SSD_HEADS_PER_GROUP, SSD_HEAD_DIM)
    return y, h_last


def seq_flip(t, rev):
    return jnp.flip(t, axis=1) if rev else t


def bidirectional_ssd(xbc_c, dt_c, xbc_l, dt_l, conv_w, conv_b, dt_bias, a_log, d_skip):
    xs_c, b_c, c_c = conv_split(xbc_c, conv_w, conv_b)
    xs_l, b_l, c_l = conv_split(xbc_l, conv_w, conv_b)
    bsz, n_l = xbc_l.shape[:2]
    n_c = xbc_c.shape[1]
    gr = (SSD_GROUPS, SSD_HEADS_PER_GROUP)
    dt_c = dt_c.reshape((bsz, n_c, N_DIRS) + gr).astype(jnp.float32)
    dt_l = dt_l.reshape((bsz, n_l, N_DIRS) + gr).astype(jnp.float32)
    skip = d_skip.reshape(gr)[..., None]
    y_c = skip * xs_c
    y_l = skip * xs_l
    h0 = jnp.zeros((bsz,) + gr + (SSD_HEAD_DIM, SSD_STATE), jnp.float32)
    for d in range(N_DIRS):
        rev = d == 1
        a = -jnp.exp(a_log[d].astype(jnp.float32)).reshape(gr)
        bias = dt_bias[d].astype(jnp.float32).reshape(gr)
        dtc = jax.nn.softplus(dt_c[:, :, d] + bias)
        dtl = jax.nn.softplus(dt_l[:, :, d] + bias)
        yc, hc = ssd_chunked(seq_flip(xs_c, rev), seq_flip(dtc, rev), a,
                             seq_flip(b_c, rev), seq_flip(c_c, rev), h0)
        yl, _ = ssd_chunked(seq_flip(xs_l, rev), seq_flip(dtl, rev), a,
                            seq_flip(b_l, rev), seq_flip(c_l, rev), hc)
        y_c = y_c + seq_flip(yc, rev)
        y_l = y_l + seq_flip(yl, rev)
    return (y_l.reshape(bsz, n_l, SSD_WIDTH).astype(xbc_l.dtype),
            y_c.reshape(bsz, n_c, SSD_WIDTH).astype(xbc_c.dtype))


def token_mixer(h_lat, h_ctx, rows, cols, w_in, attn_sink, conv_w, conv_b, dt_bias, a_log,
                d_skip, ssd_norm_g, w_out, with_ctx_out):
    b, n, _ = h_lat.shape
    n_c = h_ctx.shape[1]
    q_l, k_l, v_l, z_l, xbc_l, dt_l = split_cols(h_lat @ w_in)
    q_c, k_c, v_c, z_c, xbc_c, dt_c = split_cols(h_ctx @ w_in)
    sink = attn_sink.reshape(N_KV_HEADS, Q_PER_KV)
    q_l = axial_rope(q_l.reshape(b, n, N_KV_HEADS, Q_PER_KV, HEAD_DIM), rows, cols)
    k_l = axial_rope(k_l.reshape(b, n, N_KV_HEADS, HEAD_DIM), rows, cols)
    v_l = v_l.reshape(b, n, N_KV_HEADS, HEAD_DIM)
    k_c = k_c.reshape(b, n_c, N_KV_HEADS, HEAD_DIM)
    v_c = v_c.reshape(b, n_c, N_KV_HEADS, HEAD_DIM)
    attn_l = windowed_attention(q_l, k_l, v_l, k_c, v_c, sink)
    ssd_l, ssd_c = bidirectional_ssd(xbc_c, dt_c, xbc_l, dt_l, conv_w, conv_b, dt_bias, a_log, d_skip)
    ssd_l = rmsnorm(ssd_l * jax.nn.silu(z_l), ssd_norm_g)
    out_l = jnp.concatenate([attn_l, ssd_l], axis=-1) @ w_out
    if not with_ctx_out:
        return out_l, None
    attn_c = context_attention(q_c.reshape(b, n_c, N_KV_HEADS, Q_PER_KV, HEAD_DIM), k_c, v_c, sink)
    ssd_c = rmsnorm(ssd_c * jax.nn.silu(z_c), ssd_norm_g)
    out_c = jnp.concatenate([attn_c, ssd_c], axis=-1) @ w_out
    return out_l, out_c


def moe_ffn(h, router_w, router_bias, w_gate, w_up, w_down, sw_gate, sw_up, sw_down):
    t, d = h.shape
    scores = jax.nn.sigmoid((h @ router_w).astype(jnp.float32))
    sel = scores + router_bias.astype(jnp.float32)
    grp_score = lax.top_k(sel.reshape(t, N_EXPERT_GROUPS, EXPERTS_PER_GROUP), 2)[0].sum(-1)
    _, grp_idx = lax.top_k(grp_score, TOPK_GROUPS)
    grp_keep = jnp.any(grp_idx[:, :, None] == jnp.arange(N_EXPERT_GROUPS)[None, None, :], axis=1)
    sel = jnp.where(jnp.repeat(grp_keep, EXPERTS_PER_GROUP, axis=1), sel, -jnp.inf)
    _, top_idx = lax.top_k(sel, TOP_K)
    top_s = jnp.take_along_axis(scores, top_idx, axis=1)
    top_w = ROUTED_SCALE * top_s / jnp.sum(top_s, axis=-1, keepdims=True)

    n_assign = t * TOP_K
    eid = top_idx.reshape(-1)
    tid = jnp.repeat(jnp.arange(t, dtype=jnp.int32), TOP_K)
    wts = top_w.reshape(-1)
    order = jnp.argsort(eid)
    eid_s, tid_s, w_s = eid[order], tid[order], wts[order]
    counts = jnp.bincount(eid, length=N_EXPERTS)
    padded = (counts + MOE_BLOCK - 1) // MOE_BLOCK * MOE_BLOCK
    raw_start = jnp.cumsum(counts) - counts
    pad_end = jnp.cumsum(padded)
    pad_start = pad_end - padded
    dest = pad_start[eid_s] + jnp.arange(n_assign, dtype=jnp.int32) - raw_start[eid_s]
    n_slots = -(-(n_assign + N_EXPERTS * (MOE_BLOCK - 1)) // MOE_BLOCK) * MOE_BLOCK
    n_blocks = n_slots // MOE_BLOCK
    slot_tok = jnp.full((n_slots,), t, jnp.int32).at[dest].set(tid_s)
    slot_w = jnp.zeros((n_slots,), jnp.float32).at[dest].set(w_s)
    blk_start = jnp.arange(n_blocks, dtype=jnp.int32) * MOE_BLOCK
    blk_exp = jnp.minimum(jnp.sum(blk_start[:, None] >= pad_end[None, :], axis=1), N_EXPERTS - 1)
    h_pad = jnp.concatenate([h, jnp.zeros((1, d), h.dtype)], axis=0)

    def block_step(y, inp):
        tok, w, e = inp
        xb = h_pad[tok]
        act = jax.nn.silu(xb @ w_gate[e]) * (xb @ w_up[e])
        out = (act @ w_down[e]) * w[:, None].astype(act.dtype)
        return y.at[tok].add(out.astype(y.dtype)), None

    y, _ = lax.scan(block_step, jnp.zeros((t + 1, d), h.dtype),
                    (slot_tok.reshape(n_blocks, MOE_BLOCK), slot_w.reshape(n_blocks, MOE_BLOCK), blk_exp))
    shared = (jax.nn.silu(h @ sw_gate) * (h @ sw_up)) @ sw_down
    return y[:t] + shared


def setup_inputs(seed: int = 0) -> dict:
    key = jax.random.key(seed)
    ks = jax.random.split(key, 26)
    f32 = jnp.float32

    def nrm(k, shape, scale):
        return jax.random.normal(k, shape, f32) * scale

    def gain(k, shape):
        return 1.0 + 0.01 * jax.random.normal(k, shape, f32)

    dt0 = jnp.exp(jax.random.uniform(ks[12], (DEPTH, N_DIRS, SSD_HEADS), f32,
                                     math.log(1e-3), math.log(1e-1)))
    return {
        "x": nrm(ks[0], (BATCH, SEQ, D_MODEL), 1.0),
        "c": nrm(ks[1], (BATCH, D_MODEL), 1.0),
        "ctx": nrm(ks[2], (BATCH, CTX_LEN, D_MODEL), 1.0),
        "c_ctx": nrm(ks[3], (D_MODEL,), 1.0),
        "w_ada": nrm(ks[4], (DEPTH, D_MODEL, 6 * D_MODEL), 0.5 * D_MODEL ** -0.5),
        "b_ada": nrm(ks[5], (DEPTH, 6 * D_MODEL), 0.01),
        "norm1_g": gain(ks[6], (DEPTH, D_MODEL)),
        "norm2_g": gain(ks[7], (DEPTH, D_MODEL)),
        "w_in": nrm(ks[8], (DEPTH, D_MODEL, IN_WIDTH), D_MODEL ** -0.5),
        "attn_sink": nrm(ks[9], (DEPTH, N_Q_HEADS), 0.5),
        "conv_w": nrm(ks[10], (DEPTH, SSD_CONV, XBC_WIDTH), SSD_CONV ** -0.5),
        "conv_b": nrm(ks[11], (DEPTH, XBC_WIDTH), 0.01),
        "dt_bias": dt0 + jnp.log(-jnp.expm1(-dt0)),
        "a_log": jnp.log(jax.random.uniform(ks[13], (DEPTH, N_DIRS, SSD_HEADS), f32, 1.0, 16.0)),
        "d_skip": gain(ks[14], (DEPTH, SSD_HEADS)),
        "ssd_norm_g": gain(ks[15], (DEPTH, SSD_WIDTH)),
        "w_out": nrm(ks[16], (DEPTH, MIX_WIDTH, D_MODEL), MIX_WIDTH ** -0.5),
        "router_w": nrm(ks[17], (DEPTH, D_MODEL, N_EXPERTS), D_MODEL ** -0.5),
        "router_bias": nrm(ks[18], (DEPTH, N_EXPERTS), 0.01),
        "expert_w_gate": nrm(ks[19], (DEPTH, N_EXPERTS, D_MODEL, EXPERT_DIM), D_MODEL ** -0.5),
        "expert_w_up": nrm(ks[20], (DEPTH, N_EXPERTS, D_MODEL, EXPERT_DIM), D_MODEL ** -0.5),
        "expert_w_down": nrm(ks[21], (DEPTH, N_EXPERTS, EXPERT_DIM, D_MODEL), EXPERT_DIM ** -0.5),
        "shared_w_gate": nrm(ks[22], (DEPTH, D_MODEL, SHARED_DIM), D_MODEL ** -0.5),
        "shared_w_up": nrm(ks[23], (DEPTH, D_MODEL, SHARED_DIM), D_MODEL ** -0.5),
        "shared_w_down": nrm(ks[24], (DEPTH, SHARED_DIM, D_MODEL), SHARED_DIM ** -0.5),
        "final_norm_g": gain(ks[25], (D_MODEL,)),
    }


def reference(x, c, ctx, c_ctx, w_ada, b_ada, norm1_g, norm2_g, w_in, attn_sink, conv_w, conv_b,
              dt_bias, a_log, d_skip, ssd_norm_g, w_out, router_w, router_bias, expert_w_gate,
              expert_w_up, expert_w_down, shared_w_gate, shared_w_up, shared_w_down, final_norm_g):
    b, n, _ = x.shape
    n_rows = n // GRID_W
    rows = jnp.broadcast_to(jnp.arange(n_rows, dtype=jnp.int32)[:, None], (n_rows, GRID_W)).reshape(-1)
    cols = jnp.broadcast_to(jnp.arange(GRID_W, dtype=jnp.int32)[None, :], (n_rows, GRID_W)).reshape(-1)
    xc = ctx
    for l in range(DEPTH):
        update_ctx = l < DEPTH - 1
        mod = jax.nn.silu(c) @ w_ada[l] + b_ada[l]
        mod_c = jax.nn.silu(c_ctx) @ w_ada[l] + b_ada[l]
        sh1, sc1, g1, sh2, sc2, g2 = jnp.split(mod[:, None, :], 6, axis=-1)
        sh1c, sc1c, g1c, sh2c, sc2c, g2c = jnp.split(mod_c, 6, axis=-1)
        h = modulate(rmsnorm(x, norm1_g[l]), sh1, sc1)
        hc = modulate(rmsnorm(xc, norm1_g[l]), sh1c, sc1c)
        m_l, m_c = token_mixer(h, hc, rows, cols, w_in[l], attn_sink[l], conv_w[l], conv_b[l],
                               dt_bias[l], a_log[l], d_skip[l], ssd_norm_g[l], w_out[l], update_ctx)
        x = x + g1 * m_l
        h2 = modulate(rmsnorm(x, norm2_g[l]), sh2, sc2)
        ffn = moe_ffn(h2.reshape(-1, D_MODEL), router_w[l], router_bias[l], expert_w_gate[l],
                      expert_w_up[l], expert_w_down[l], shared_w_gate[l], shared_w_up[l], shared_w_down[l])
        x = x + g2 * ffn.reshape(b, n, D_MODEL)
        if update_ctx:
            xc = xc + g1c * m_c
            hc2 = modulate(rmsnorm(xc, norm2_g[l]), sh2c, sc2c)
            ffn_c = moe_ffn(hc2.reshape(-1, D_MODEL), router_w[l], router_bias[l], expert_w_gate[l],
                            expert_w_up[l], expert_w_down[l], shared_w_gate[l], shared_w_up[l],
                            shared_w_down[l])
            xc = xc + g2c * ffn_c.reshape(xc.shape)
    return rmsnorm(x, final_norm_g)
```

```python
from contextlib import ExitStack

import numpy as np
import concourse.bass as bass
import concourse.mybir as mybir
from concourse.bass_utils import run_bass_kernel_spmd

F32 = mybir.dt.float32
BF16 = mybir.dt.bfloat16
AF = mybir.ActivationFunctionType
ALU = mybir.AluOpType
AX = mybir.AxisListType

D = 2048
SEQ = 4096
NB = 4
CTX = 256
OWN = 2048
NT = 16
INW = 4640
NEXP = 64
EPS = 1e-6
NEG = -30000.0
NSLOT = 8


class Tok:
    __slots__ = ("sem", "val", "ek")

    def __init__(self, sem, val, ek):
        self.sem, self.val, self.ek = sem, val, ek


class Buf:
    def __init__(self, name=""):
        self.name = name
        self.w = None
        self.r = {}


class Eng:
    def __init__(self, key, obj, sem):
        self.key, self.obj, self.sem = key, obj, sem
        self.count = 0
        self.pending = False
        self.seen = {}
        self.slots = []
        self.nslot = 0


class KB:
    def __init__(self, nc, es):
        self.nc = nc
        self.eng = {}
        for key, obj in (("pe", nc.tensor), ("dve", nc.vector), ("act", nc.scalar),
                         ("pool", nc.gpsimd), ("sp", nc.sync)):
            sem = es.enter_context(nc.semaphore("s_" + key))
            self.eng[key] = Eng(key, obj, sem)
        for key in ("sp", "pool", "act"):
            e = self.eng[key]
            for i in range(NSLOT):
                e.slots.append([es.enter_context(nc.semaphore("d_%s%d" % (key, i))), 0])

    def _wait(self, e, tok):
        if tok is None:
            return
        sid = id(tok.sem)
        if e.seen.get(sid, 0) >= tok.val:
            return
        e.obj.wait_ge(tok.sem, tok.val)
        e.seen[sid] = tok.val

    def _deps(self, e, reads, writes):
        for b in reads:
            if b.w is not None and not (b.w.ek == e.key and e.key == "pe"):
                self._wait(e, b.w)
        for b in writes:
            if b.w is not None and not (b.w.ek == e.key and e.key == "pe"):
                self._wait(e, b.w)
            for ek, t in b.r.items():
                if ek != e.key:
                    self._wait(e, t)

    def op(self, ek, fn, reads=(), writes=(), inc=True):
        e = self.eng[ek]
        self._deps(e, reads, writes)
        ins = fn(e.obj)
        if inc:
            ins.then_inc(e.sem, 1)
            e.count += 1
            e.pending = False
            tok = Tok(e.sem, e.count, ek)
        else:
            e.pending = True
            tok = Tok(e.sem, e.count + 1, ek)
        for b in writes:
            b.w = tok
            b.r = {}
        for b in reads:
            b.r[ek] = tok
        return ins

    def dma(self, qk, out, in_, reads=(), writes=(), **kw):
        e = self.eng[qk]
        slot = e.slots[e.nslot % NSLOT]
        e.nslot += 1
        if slot[1] > 0:
            self._wait(e, Tok(slot[0], 16 * slot[1], "dma"))
        self._deps(e, reads, writes)
        ins = e.obj.dma_start(out=out, in_=in_, **kw)
        ins.then_inc(slot[0], 16)
        slot[1] += 1
        tok = Tok(slot[0], 16 * slot[1], "dma_" + qk + str(id(slot[0])))
        for b in writes:
            b.w = tok
            b.r = {}
        for b in reads:
            b.r[tok.ek] = tok
        return ins

    def coll(self, fn, reads=(), writes=()):
        e = self.eng["pool"]
        slot = e.slots[e.nslot % NSLOT]
        e.nslot += 1
        if slot[1] > 0:
            self._wait(e, Tok(slot[0], 16 * slot[1], "dma"))
        self._deps(e, reads, writes)
        ins = fn(e.obj)
        ins.then_inc(slot[0], 16)
        slot[1] += 1
        tok = Tok(slot[0], 16 * slot[1], "dma_coll")
        for b in writes:
            b.w = tok
            b.r = {}
        for b in reads:
            b.r[tok.ek] = tok
        return ins

    def barrier(self):
        toks = []
        for e in self.eng.values():
            assert not e.pending, e.key
            if e.count:
                toks.append(Tok(e.sem, e.count, e.key))
            for s in e.slots:
                if s[1]:
                    toks.append(Tok(s[0], 16 * s[1], "dma"))
        for e in self.eng.values():
            for t in toks:
                if t.ek == e.key:
                    continue
                self._wait(e, t)


def bcast_row(ap_row, n=128):
    return ap_row.partition_broadcast(n)


def build(stop_after=None, dbg=(), ncores=8, cut=99, cfg_nexp=NEXP + 1):
    nc = bass.Bass("TRN2", target_bir_lowering=False)
    dbg = set(dbg)

    class _Lazy:
        def __init__(self, name, shape, dt):
            self.name, self.shape, self.dt, self._ap = name, shape, dt, None

        @property
        def ap(self):
            if self._ap is None:
                self._ap = nc.dram_tensor(self.name, list(self.shape), self.dt, kind="ExternalInput").ap()
                used_inputs.append(self.name)
            return self._ap

        def __getitem__(self, key):
            return self.ap[key]

        def rearrange(self, *a, **k):
            return self.ap.rearrange(*a, **k)

    used_inputs = []
    nc._used_inputs = used_inputs

    def din(name, shape, dt=F32):
        return _Lazy(name, shape, dt)

    def dscr(name, shape, dt=F32):
        kind = "ExternalOutput" if name in dbg else "Internal"
        return nc.dram_tensor(name, list(shape), dt, kind=kind).ap()

    x_own = din("x_own", [OWN, D])
    x_halo = din("x_halo", [2, 128, D])
    ctx_in = din("ctx_b", [CTX, D])
    c_lay = din("c_lay", [128, 32])
    w_ada = din("w_ada", [D, 6 * D])
    b_ada = din("b_ada", [1, 6 * D])
    norm1_g = din("norm1_g", [1, D])
    norm2_g = din("norm2_g", [1, D])
    w_in = din("w_in", [D, INW])
    w_in_sw = din("w_in_sw", [D, 1280])
    attn_sink = din("attn_sink", [1, 8])
    conv_wl = din("conv_wl", [128, 16, 5])
    conv_bl = din("conv_bl", [128, 16])
    dt_bias = din("dt_bias", [1, 32])
    a_log = din("a_log", [1, 32])
    d_skip = din("d_skip", [1, 16])
    ssd_norm_g = din("ssd_norm_g", [1, 1024])
    w_out = din("w_out", [D, D])
    router_w = din("router_w", [D, NEXP])
    router_bias = din("router_bias", [1, NEXP])
    ew_gate = din("ew_gate", [NEXP + 1, D, 512])
    ew_up = din("ew_up", [NEXP + 1, D, 512])
    ew_down = din("ew_down", [NEXP + 1, 512, D])
    final_g = din("final_g", [1, D])
    rope_c = din("rope_c", [128, 2304])
    rope_s = din("rope_s", [128, 2304])
    cmask = din("cmask", [4, 128, 512], BF16)
    flags = din("flags", [128, 4])
    consts = din("consts", [6, 128, 128])
    x_oth = din("x_oth", [OWN, D])
    x_adj = din("x_adj", [128, D])
    w_dt_sel = din("w_dt_sel", [D, 16])
    dtb_sel = din("dtb_sel", [1, 16])
    alog_sel = din("alog_sel", [1, 16])
    tri_sel = din("tri_sel", [128, 128])

    out_d = nc.dram_tensor("out", [OWN, D], F32, kind="ExternalOutput").ap()

    mod_d = dscr("mod_d", [8, D])
    qT_d = dscr("qT_d", [8, 128, OWN], BF16)
    kT_d = dscr("kT_d", [2, 128, 2304], BF16)
    kcT_d = dscr("kcT_d", [2, 128, CTX], BF16)
    v_d = dscr("v_d", [20, 128, 256], BF16)
    zs_d = dscr("zs_d", [NT, 128, 1024])
    dtraw_d = dscr("dtraw_d", [18, 128, 32])
    uT_d = dscr("uT_d", [16, 128, OWN], BF16)
    uTc_d = dscr("uTc_d", [16, 128, CTX], BF16)
    mixT_d = dscr("mixT_d", [16, 128, OWN], BF16)
    uT2_d = dscr("uT2_d", [12, 128, OWN], BF16)
    dtraw2_d = dscr("dtraw2_d", [NT, 128, 16])
    h0_d = dscr("h0_d", [2, NT, 128, 1024])
    xch_in = dscr("xch_in", [128, 2112])
    xch_out = dscr("xch_out", [256, 2112])
    x1_d = dscr("x1_d", [OWN, D])
    h2T_d = dscr("h2T_d", [16, 128, OWN], BF16)
    gate_d = dscr("gate_d", [NT, 128, NEXP + 1])

    es = ExitStack()
    with es:
        kb = KB(nc, es)

        def sbt(st, name, shape, dt=F32):
            return st.enter_context(nc.sbuf_tensor(name, list(shape), dt))

        def pst(st, name, shape, dt=F32):
            return st.enter_context(nc.psum_tensor(name, list(shape), dt))

        ident_f = sbt(es, "ident_f", [128, 128]); ones_f = sbt(es, "ones_f", [128, 128])
        ident_b = sbt(es, "ident_b", [128, 128], BF16); ones_b = sbt(es, "ones_b", [128, 128], BF16)
        flg = sbt(es, "flg", [128, 4])
        B_const = Buf("const")
        for i, t in ((0, ident_f), (3, ones_f)):
            kb.dma("sp", t[:], consts[i], writes=[B_const])
        kb.dma("sp", flg[:], flags[:, :], writes=[B_const])
        kb.op("dve", lambda e: e.tensor_copy(out=ident_b[:], in_=ident_f[:]), reads=[B_const], writes=[B_const])
        kb.op("dve", lambda e: e.tensor_copy(out=ones_b[:], in_=ones_f[:]), reads=[B_const], writes=[B_const])
        kb.barrier()

        with ExitStack() as ph:
            cl = sbt(ph, "cl", [128, 32]); cs = sbt(ph, "cs", [128, 32])
            LC = sbt(ph, "LC", [128, 32, 128], BF16)
            Wb = [sbt(ph, "Wa%d" % i, [128, 16, 512], BF16) for i in range(2)]
            bb = [sbt(ph, "ba%d" % i, [128, 512]) for i in range(2)]
            mo = [sbt(ph, "mo%d" % i, [128, 512]) for i in range(4)]
            psA = [pst(ph, "psA%d" % i, [128, 512]) for i in range(4)]
            B_cl, B_LC = Buf(), Buf()
            B_W = [Buf(), Buf()]; B_b = [Buf(), Buf()]; B_mo = [Buf() for _ in range(4)]
            B_ps = [Buf() for _ in range(4)]
            kb.dma("sp", cl[:], c_lay[:, :], writes=[B_cl])
            kb.op("act", lambda e: e.activation(out=cs[:], in_=cl[:], func=AF.Silu), reads=[B_cl], writes=[B_cl])
            for k in range(32):
                kb.op("dve", lambda e, k=k: e.tensor_copy(out=LC[:, k, :], in_=cs[:, k:k + 1].to_broadcast([128, 128])),
                      reads=[B_cl], writes=[B_LC])
            wv = w_ada.rearrange("(k p) n -> p k n", p=128)
            nblk = 24
            it = 0
            for j in range(nblk):
                bi = j % 2
                kb.dma("pool", Wb[bi][:], wv[:, :, j * 512:(j + 1) * 512], writes=[B_W[bi]])
                kb.dma("sp", bb[bi][:], bcast_row(b_ada[0:1, j * 512:(j + 1) * 512]), writes=[B_b[bi]])
                chunk = j // 4
                for which in range(2 if j < 8 else 1):
                    pi = it % 4
                    it += 1
                    for k in range(16):
                        kb.op("pe", lambda e, k=k, pi=pi, which=which, bi=bi: e.matmul(
                            psA[pi][:], lhsT=LC[:, which * 16 + k, :], rhs=Wb[bi][:, k, :],
                            start=(k == 0), stop=(k == 15)),
                            reads=[B_LC, B_W[bi]], writes=[B_ps[pi]], inc=(k == 15))
                    if chunk in (1, 4):
                        kb.op("dve", lambda e, pi=pi, bi=bi: e.scalar_tensor_tensor(
                            out=mo[pi][:], in0=psA[pi][:], scalar=1.0, in1=bb[bi][:], op0=ALU.add, op1=ALU.add),
                            reads=[B_ps[pi], B_b[bi]], writes=[B_mo[pi]])
                    else:
                        kb.op("dve", lambda e, pi=pi, bi=bi: e.tensor_tensor(
                            out=mo[pi][:], in0=psA[pi][:], in1=bb[bi][:], op=ALU.add),
                            reads=[B_ps[pi], B_b[bi]], writes=[B_mo[pi]])
                    row = chunk if which == 0 else 6 + chunk
                    c0 = (j % 4) * 512
                    kb.dma("sp", mod_d[row:row + 1, c0:c0 + 512], mo[pi][0:1, :], reads=[B_mo[pi]])
            kb.barrier()
        if stop_after == "A":
            return nc

        def load_bc(st, qk, name, row_ap, width, buf):
            t = sbt(st, name, [128, width])
            kb.dma(qk, t[:], bcast_row(row_ap), writes=[buf])
            return t

        def rms_mod_tile(xt, B_x, G, shv, B_mod, junk, B_junk, ssq, B_ss, tmp, B_tmp, outb, B_out, width=D):
            kb.op("act", lambda e: e.activation(out=junk[:], in_=xt, func=AF.Square, accum_out=ssq[:, 0:1]),
                  reads=[B_x], writes=[B_junk, B_ss])
            kb.op("dve", lambda e: e.tensor_scalar(out=ssq[:, 1:2], in0=ssq[:, 0:1], scalar1=1.0 / width, scalar2=EPS,
                                                   op0=ALU.mult, op1=ALU.add), reads=[B_ss], writes=[B_ss])
            kb.op("act", lambda e: e.sqrt(out=ssq[:, 3:4], in_=ssq[:, 1:2]), reads=[B_ss], writes=[B_ss])
            kb.op("dve", lambda e: e.reciprocal(out=ssq[:, 2:3], in_=ssq[:, 3:4]), reads=[B_ss], writes=[B_ss])
            kb.op("dve", lambda e: e.scalar_tensor_tensor(out=tmp[:], in0=xt, scalar=ssq[:, 2:3], in1=G,
                                                          op0=ALU.mult, op1=ALU.mult),
                  reads=[B_x, B_ss, B_mod], writes=[B_tmp])
            if shv is not None:
                kb.op("dve", lambda e: e.tensor_tensor(out=outb, in0=tmp[:], in1=shv, op=ALU.add),
                      reads=[B_tmp, B_mod], writes=[B_out])

        def phase_BC(tag, cfg):
          with ExitStack() as phBC:
              ntile = cfg["ntile"]
              hT = sbt(phBC, "hT" + tag, [128, ntile, 16, 128], BF16)
              B_hT = [Buf("hT%d" % i) for i in range(ntile)]
              with ExitStack() as ph:
                  B_mod = Buf("mod1")
                  g1n = load_bc(ph, "sp", "g1n" + tag, norm1_g[0:1, :], D, B_mod)
                  GL = load_bc(ph, "sp", "GL" + tag, mod_d[1:2, :], D, B_mod)
                  SHL = load_bc(ph, "sp", "SHL" + tag, mod_d[0:1, :], D, B_mod)
                  GC = load_bc(ph, "sp", "GC" + tag, mod_d[7:8, :], D, B_mod)
                  SHC = load_bc(ph, "sp", "SHC" + tag, mod_d[6:7, :], D, B_mod)
                  kb.op("dve", lambda e: e.tensor_tensor(out=GL[:], in0=GL[:], in1=g1n[:], op=ALU.mult),
                        reads=[B_mod], writes=[B_mod])
                  kb.op("dve", lambda e: e.tensor_tensor(out=GC[:], in0=GC[:], in1=g1n[:], op=ALU.mult),
                        reads=[B_mod], writes=[B_mod])
                  xt = [sbt(ph, "xt%d" % i + tag, [128, D]) for i in range(2)]
                  B_x = [Buf(), Buf()]
                  junk = sbt(ph, "junk" + tag, [128, D], BF16); B_junk = Buf()
                  tmp = sbt(ph, "tmpB" + tag, [128, D]); B_tmp = Buf()
                  hb = [sbt(ph, "hb%d" % i + tag, [128, D], BF16) for i in range(2)]
                  B_hb = [Buf(), Buf()]
                  ssq = [sbt(ph, "ssq%d" % i + tag, [128, 4]) for i in range(2)]
                  B_ss = [Buf(), Buf()]
                  psT = [pst(ph, "psT%d" % i + tag, [128, 8, 128], BF16) for i in range(2)]
                  B_psT = [Buf(), Buf()]

                  src_of = cfg["src_of"]

                  kb.dma("sp", xt[0][:], src_of(0), writes=[B_x[0]])
                  for t in range(ntile):
                      bi = t % 2
                      if t + 1 < ntile:
                          kb.dma("sp", xt[1 - bi][:], src_of(t + 1), writes=[B_x[1 - bi]])
                      G, SHv = (GC, SHC) if t < cfg["nctx"] else (GL, SHL)
                      rms_mod_tile(xt[bi][:], B_x[bi], G[:], SHv[:], B_mod, junk, B_junk, ssq[bi], B_ss[bi],
                                   tmp, B_tmp, hb[bi][:], B_hb[bi])
                      for half in range(2):
                          for kk in range(8):
                              k = half * 8 + kk
                              kb.op("pe", lambda e, k=k, kk=kk, half=half, bi=bi: e.transpose(
                                  out=psT[half][:, kk, :], in_=hb[bi][:, k * 128:(k + 1) * 128], identity=ident_b[:]),
                                  reads=[B_hb[bi]], writes=[B_psT[half]], inc=(kk == 7))
                          eng = "act" if half == 0 else "dve"
                          kb.op(eng, lambda e, t=t, half=half: e.tensor_copy(out=hT[:, t, half * 8:(half + 1) * 8, :],
                                                                            in_=psT[half][:]) if eng != "act" else
                                e.copy(out=hT[:, t, half * 8:(half + 1) * 8, :], in_=psT[half][:]),
                                reads=[B_psT[half]], writes=[B_hT[t]])
                  kb.barrier()
              with ExitStack() as ph:
                  wv = w_in.rearrange("(k p) n -> p k n", p=128)
                  wsv = w_in_sw.rearrange("(k p) n -> p k n", p=128)
                  Wn = [sbt(ph, "Wn%d" % i + tag, [128, 16, 512], BF16) for i in range(2)]
                  Ws = [sbt(ph, "Ws%d" % i + tag, [128, 16, 128], BF16) for i in range(2)]
                  B_Wn = [Buf(), Buf()]; B_Ws = [Buf(), Buf()]
                  B_cst = Buf()
                  rc = sbt(ph, "rc" + tag, [128, 2304]); rs = sbt(ph, "rs" + tag, [128, 2304])
                  kb.dma("sp", rc[:], rope_c[:, :], writes=[B_cst])
                  kb.dma("sp", rs[:], rope_s[:, :], writes=[B_cst])
                  cw = sbt(ph, "cw" + tag, [128, 16, 5]); cb = sbt(ph, "cb" + tag, [128, 16])
                  kb.dma("sp", cw[:], conv_wl[:, :, :], writes=[B_cst])
                  kb.dma("sp", cb[:], conv_bl[:, :], writes=[B_cst])
                  psC = [pst(ph, "psC%d" % i + tag, [128, 512]) for i in range(4)]
                  B_psC = [Buf() for _ in range(4)]
                  ev = [sbt(ph, "ev%d" % i + tag, [128, 512]) for i in range(4)]
                  B_ev = [Buf() for _ in range(4)]
                  evb = [sbt(ph, "evb%d" % i + tag, [128, 512], BF16) for i in range(2)]
                  B_evb = [Buf(), Buf()]
                  v_all = sbt(ph, "v_all" + tag, [128, 20, 256], BF16); B_vall = Buf()
                  dt_all = sbt(ph, "dt_all" + tag, [128, 18, 32]); B_dtall = Buf()
                  xraw = [sbt(ph, "xraw%d" % i + tag, [128, 2052]) for i in range(2)]
                  B_xraw = [Buf(), Buf()]
                  xrawc = [sbt(ph, "xrawc%d" % i + tag, [128, 260]) for i in range(2)]
                  B_xrawc = [Buf(), Buf()]
                  acc0 = sbt(ph, "acc0" + tag, [128, 2048])
                  acc = [acc0, acc0]
                  B_acc0 = Buf()
                  B_acc = [B_acc0, B_acc0]
                  accc = sbt(ph, "accc" + tag, [128, 256]); B_accc = Buf()
                  uo = [sbt(ph, "uo%d" % i + tag, [128, 2048], BF16) for i in range(2)]
                  B_uo = [Buf(), Buf()]
                  uoc = [sbt(ph, "uoc%d" % i + tag, [128, 256], BF16) for i in range(2)]
                  B_uoc = [Buf(), Buf()]
                  for i in range(2):
                      kb.op("pool", lambda e, i=i: e.memset(xrawc[i][:], 0.0), writes=[B_xrawc[i]])

                  jobs = cfg["jobs"]

                  def load_job(ji):
                      kind, c0, ncol, idx = jobs[ji]
                      bi = ji % 2
                      wsrc = cfg["wdt"] if (kind == "dt" and cfg.get("wdt") is not None) else wv[:, :, c0:c0 + ncol]
                      kb.dma("pool", Wn[bi][:, :, 0:ncol], wsrc, writes=[B_Wn[bi]])
                      if kind in ("q", "k"):
                          kb.dma("pool", Ws[bi][:], wsv[:, :, c0:c0 + 128], writes=[B_Ws[bi]])

                  pctr = [0]

                  def fm_mm(W, B_W, tile0, ntile, tok_lo=0, tok_hi=128):
                      pi = pctr[0] % 4
                      pctr[0] += 1
                      n = ntile * (tok_hi - tok_lo)
                      for k in range(16):
                          kb.op("pe", lambda e, k=k: e.matmul(
                              psC[pi][:, 0:n], lhsT=W[:, k, 0:128], rhs=hT[:, tile0:tile0 + ntile, k, tok_lo:tok_hi],
                              start=(k == 0), stop=(k == 15)),
                              reads=[B_W] + B_hT[tile0:tile0 + ntile], writes=[B_psC[pi]], inc=(k == 15))
                      return pi

                  def tm_mm(W, B_W, t, ncol):
                      pi = pctr[0] % 4
                      pctr[0] += 1
                      for k in range(16):
                          kb.op("pe", lambda e, k=k: e.matmul(
                              psC[pi][:, 0:ncol], lhsT=hT[:, t, k, :], rhs=W[:, k, 0:ncol],
                              start=(k == 0), stop=(k == 15)),
                              reads=[B_W, B_hT[t]], writes=[B_psC[pi]], inc=(k == 15))
                      return pi

                  ectr = [0]
                  load_job(0)
                  for ji, (kind, c0, ncol, idx) in enumerate(jobs):
                      if ji + 1 < len(jobs):
                          load_job(ji + 1)
                      bi = ji % 2
                      W, BW = Wn[bi], B_Wn[bi]
                      if kind in ("q", "k"):
                          if kind == "q":
                              groups = [(3 + 4 * g, 4, 128 + g * 512, g * 512) for g in range(4)]
                              dest = qT_d[idx]
                          else:
                              groups = [(2 + 4 * g, 4, g * 512, g * 512) for g in range(4)] + [(18, 2, 2048, 2048)]
                              dest = kT_d[idx]
                          for (t0, ntl, roff, doff) in groups:
                              n = ntl * 128
                              pa = fm_mm(W, BW, t0, ntl)
                              pb = fm_mm(Ws[bi], B_Ws[bi], t0, ntl)
                              e0, e1 = (ectr[0] % 2) * 2, (ectr[0] % 2) * 2 + 1
                              eb = ectr[0] % 2
                              ectr[0] += 1
                              kb.op("dve", lambda e: e.tensor_tensor(out=ev[e0][:, 0:n], in0=psC[pa][:, 0:n],
                                                                     in1=rc[:, roff:roff + n], op=ALU.mult),
                                    reads=[B_psC[pa], B_cst], writes=[B_ev[e0]])
                              kb.op("dve", lambda e: e.tensor_tensor(out=ev[e1][:, 0:n], in0=psC[pb][:, 0:n],
                                                                     in1=rs[:, roff:roff + n], op=ALU.mult),
                                    reads=[B_psC[pb], B_cst], writes=[B_ev[e1]])
                              kb.op("pool", lambda e: e.tensor_tensor(out=evb[eb][:, 0:n], in0=ev[e0][:, 0:n],
                                                                      in1=ev[e1][:, 0:n], op=ALU.add),
                                    reads=[B_ev[e0], B_ev[e1]], writes=[B_evb[eb]])
                              kb.dma("sp", dest[:, doff:doff + n], evb[eb][:, 0:n], reads=[B_evb[eb]])
                          if kind == "k":
                              pa = fm_mm(W, BW, 0, 2)
                              eb = ectr[0] % 2
                              ectr[0] += 1
                              kb.op("act", lambda e: e.copy(out=evb[eb][:, 0:256], in_=psC[pa][:, 0:256]),
                                    reads=[B_psC[pa]], writes=[B_evb[eb]])
                              kb.dma("sp", kcT_d[idx], evb[eb][:, 0:256], reads=[B_evb[eb]])
                      elif kind == "v":
                          for t in range(20):
                              pa = tm_mm(W, BW, t, 256)
                              kb.op("act", lambda e, t=t: e.copy(out=v_all[:, t, :], in_=psC[pa][:, 0:256]),
                                    reads=[B_psC[pa]], writes=[B_vall])
                          kb.dma("sp", v_d.rearrange("t p c -> p t c"), v_all[:], reads=[B_vall])
                      elif kind == "z":
                          for t in range(NT):
                              pa = tm_mm(W, BW, 3 + t, 512)
                              e0 = ectr[0] % 4
                              ectr[0] += 1
                              kb.op("act", lambda e: e.activation(out=ev[e0][:], in_=psC[pa][:], func=AF.Silu),
                                    reads=[B_psC[pa]], writes=[B_ev[e0]])
                              kb.dma("sp", zs_d[t][:, idx * 512:(idx + 1) * 512], ev[e0][:], reads=[B_ev[e0]])
                      elif kind == "dt":
                          dtt = cfg["dt_tiles"]
                          for i, t in enumerate(dtt):
                              pa = tm_mm(W, BW, t, ncol)
                              kb.op("act", lambda e, i=i: e.copy(out=dt_all[:, i, 0:ncol], in_=psC[pa][:, 0:ncol]),
                                    reads=[B_psC[pa]], writes=[B_dtall])
                          kb.dma("sp", cfg["dt_dst"].rearrange("t p c -> p t c"), dt_all[:, 0:len(dtt), 0:ncol], reads=[B_dtall])
                      else:
                          j = idx
                          xi = j % 2
                          xr, Bxr = xraw[xi], B_xraw[xi]
                          for g in range(4):
                              pa = fm_mm(W, BW, cfg["own0"] + 4 * g, 4)
                              kb.op("act", lambda e, g=g: e.copy(out=xr[:, 2 + g * 512:2 + (g + 1) * 512], in_=psC[pa][:]),
                                    reads=[B_psC[pa]], writes=[Bxr])
                          pa = fm_mm(W, BW, cfg["halo_lo"], 1, 126, 128)
                          kb.op("dve", lambda e: e.tensor_scalar(out=xr[:, 0:2], in0=psC[pa][:, 0:2], scalar1=cfg["fl_lo"],
                                                                 scalar2=None, op0=ALU.mult),
                                reads=[B_psC[pa]], writes=[Bxr])
                          pa = fm_mm(W, BW, cfg["halo_hi"], 1, 0, 2)
                          kb.op("dve", lambda e: e.tensor_scalar(out=xr[:, 2050:2052], in0=psC[pa][:, 0:2],
                                                                 scalar1=cfg["fl_hi"], scalar2=None, op0=ALU.mult),
                                reads=[B_psC[pa]], writes=[Bxr])
                          if cfg["nctx"]:
                              pa = fm_mm(W, BW, 0, 2)
                              kb.op("act", lambda e: e.copy(out=xrawc[xi][:, 2:258], in_=psC[pa][:, 0:256]),
                                    reads=[B_psC[pa]], writes=[B_xrawc[xi]])
                          convs = [(xr, Bxr, acc[xi], B_acc[xi], 2048, uo[xi], B_uo[xi], cfg["uT_dst"][j])]
                          if cfg["nctx"]:
                              convs.append((xrawc[xi], B_xrawc[xi], accc, B_accc, 256, uoc[xi], B_uoc[xi], uTc_d[j]))
                          for (src, Bs, dst, Bd, n, o, Bo, dd) in convs:
                              kb.op("dve", lambda e: e.tensor_scalar(out=dst[:, 0:n], in0=src[:, 0:n], scalar1=cw[:, j, 0:1],
                                                                     scalar2=None, op0=ALU.mult),
                                    reads=[Bs, B_cst], writes=[Bd])
                              for tap in range(1, 5):
                                  kb.op("dve", lambda e, tap=tap: e.scalar_tensor_tensor(
                                      out=dst[:, 0:n], in0=src[:, tap:tap + n], scalar=cw[:, j, tap:tap + 1],
                                      in1=dst[:, 0:n], op0=ALU.mult, op1=ALU.add),
                                      reads=[Bs, B_cst, Bd], writes=[Bd])
                              kb.op("act", lambda e: e.activation(out=o[:, 0:n], in_=dst[:, 0:n], func=AF.Silu,
                                                                  bias=cb[:, j:j + 1]),
                                    reads=[Bd, B_cst], writes=[Bo])
                              kb.dma("sp", dd, o[:, 0:n], reads=[Bo])
                  kb.barrier()
        def main_src(t):
            if t < 2:
                return ctx_in[t * 128:(t + 1) * 128, :]
            if t == 2:
                return x_halo[0]
            if t == 19:
                return x_halo[1]
            return x_own[(t - 3) * 128:(t - 2) * 128, :]

        jobs_main = []
        for h in range(8):
            jobs_main.append(("q", h * 128, 128, h))
        for kv in range(2):
            jobs_main.append(("k", 1024 + kv * 128, 128, kv))
        jobs_main.append(("v", 1280, 256, 0))
        jobs_main.append(("z", 1536, 512, 0))
        jobs_main.append(("z", 2048, 512, 1))
        jobs_main.append(("dt", 4608, 32, 0))
        for j in range(16):
            jobs_main.append(("x", 2560 + j * 128, 128, j))
        phase_BC("m", dict(ntile=20, nctx=2, src_of=main_src, jobs=jobs_main, own0=3, halo_lo=2, halo_hi=19,
                           fl_lo=flg[:, 2:3], fl_hi=flg[:, 3:4], uT_dst=uT_d, dt_tiles=[0, 1] + list(range(3, 19)),
                           dt_dst=dtraw_d, wdt=None))
        if stop_after == "C":
            return nc

        def oth_src(t):
            if t == 0:
                return x_adj[:, :]
            return x_oth[(t - 1) * 128:t * 128, :]

        jobs_oth = [("dt", 0, 16, 0)] + [("x", 2560 + j * 128, 128, j) for j in range(12)]
        phase_BC("o", dict(ntile=17, nctx=0, src_of=oth_src, jobs=jobs_oth, own0=1, halo_lo=0, halo_hi=0,
                           fl_lo=flg[:, 3:4], fl_hi=flg[:, 2:3], uT_dst=uT2_d, dt_tiles=list(range(1, 17)),
                           dt_dst=dtraw2_d, wdt=w_dt_sel.rearrange("(k p) n -> p k n", p=128)))
        if stop_after == "C2":
            return nc

        with ExitStack() as ph:
            qT = sbt(ph, "qT", [128, 8, OWN], BF16)
            kT = sbt(ph, "kT", [128, 2, 2304], BF16)
            kcT = sbt(ph, "kcT", [128, 2, CTX], BF16)
            vv = sbt(ph, "vv", [128, 20, 256], BF16)
            msk = sbt(ph, "msk", [128, 4, 512], BF16)
            esk = sbt(ph, "esk", [128, 8])
            B_in = Buf()
            kb.dma("sp", qT[:], qT_d.rearrange("h p t -> p h t"), writes=[B_in])
            kb.dma("sp", kT[:], kT_d.rearrange("h p t -> p h t"), writes=[B_in])
            kb.dma("sp", kcT[:], kcT_d.rearrange("h p t -> p h t"), writes=[B_in])
            kb.dma("sp", vv[:], v_d.rearrange("t p c -> p t c"), writes=[B_in])
            kb.dma("sp", msk[:], cmask.rearrange("m p c -> p m c"), writes=[B_in])
            kb.dma("sp", esk[:], bcast_row(attn_sink[0:1, :]), writes=[B_in])
            kb.op("act", lambda e: e.activation(out=esk[:], in_=esk[:], func=AF.Exp), reads=[B_in], writes=[B_in])
            psS = [pst(ph, "psS%d" % i, [128, 512]) for i in range(2)]
            psO = [pst(ph, "psO%d" % i, [128, 512]) for i in range(2)]
            psD = [pst(ph, "psD%d" % i, [128, 512]) for i in range(2)]
            B_psS = [Buf(), Buf()]; B_psO = [Buf(), Buf()]; B_psD = [Buf(), Buf()]
            Eb = [sbt(ph, "Eb%d" % i, [128, 512], BF16) for i in range(3)]
            B_E = [Buf() for _ in range(3)]
            den = [sbt(ph, "den%d" % i, [128, 512]) for i in range(2)]
            B_den = [Buf(), Buf()]
            ob = [sbt(ph, "ob%d" % i, [128, 512], BF16) for i in range(2)]
            B_ob = [Buf(), Buf()]
            scale = 128 ** -0.5
            sctr = 0
            ectr_ = 0
            for i in range(NT):
                for kv in range(2):
                    pi = (i * 2 + kv) % 2
                    rhs_q = qT[:, 4 * kv:4 * kv + 4, i * 128:(i + 1) * 128]
                    keyt = []
                    for j in range(3):
                        m = None
                        if j == 0:
                            m = 2 if i == 0 else 0
                        if j == 2:
                            m = 3 if i == NT - 1 else 1
                        keyt.append((kT[:, kv, (i + j) * 128:(i + j + 1) * 128], 2 + i + j, m))
                    for cc in range(2):
                        keyt.append((kcT[:, kv, cc * 128:(cc + 1) * 128], cc, None))
                    for n, (kap, vt, m) in enumerate(keyt):
                        si = sctr % 2
                        sctr += 1
                        ei = ectr_ % 3
                        ectr_ += 1
                        kb.op("pe", lambda e: e.matmul(psS[si][:], lhsT=kap, rhs=rhs_q, start=True, stop=(m is None)),
                              reads=[B_in], writes=[B_psS[si]], inc=(m is None))
                        if m is not None:
                            kb.op("pe", lambda e: e.matmul(psS[si][:], lhsT=ident_b[:], rhs=msk[:, m, :], start=False, stop=True),
                                  reads=[B_in], writes=[B_psS[si]])
                        kb.op("act", lambda e: e.activation(out=Eb[ei][:], in_=psS[si][:], func=AF.Exp, scale=scale),
                              reads=[B_psS[si]], writes=[B_E[ei]])
                        kb.op("pe", lambda e: e.matmul(psO[pi][:], lhsT=vv[:, vt, kv * 128:(kv + 1) * 128], rhs=Eb[ei][:],
                                                       start=(n == 0), stop=(n == 4)),
                              reads=[B_in, B_E[ei]], writes=[B_psO[pi]], inc=False)
                        kb.op("pe", lambda e: e.matmul(psD[pi][:], lhsT=ones_b[:], rhs=Eb[ei][:],
                                                       start=(n == 0), stop=(n == 4)),
                              reads=[B_E[ei]], writes=[B_psD[pi]], inc=True)
                    kb.op("dve", lambda e: e.tensor_tensor(
                        out=den[pi][:].rearrange("p (h q) -> p h q", h=4),
                        in0=psD[pi][:].rearrange("p (h q) -> p h q", h=4),
                        in1=esk[:, 4 * kv:4 * kv + 4].unsqueeze(2).to_broadcast([128, 4, 128]), op=ALU.add),
                        reads=[B_psD[pi], B_in], writes=[B_den[pi]])
                    kb.op("dve", lambda e: e.reciprocal(out=den[pi][:], in_=den[pi][:]), reads=[B_den[pi]], writes=[B_den[pi]])
                    kb.op("dve", lambda e: e.tensor_tensor(out=ob[pi][:], in0=psO[pi][:], in1=den[pi][:], op=ALU.mult),
                          reads=[B_psO[pi], B_den[pi]], writes=[B_ob[pi]])
                    kb.dma("sp", mixT_d[4 * kv:4 * kv + 4, :, i * 128:(i + 1) * 128].rearrange("h p t -> p h t"),
                           ob[pi][:].rearrange("p (h q) -> p h q", h=4), reads=[B_ob[pi]])
            kb.barrier()
        if stop_after == "D":
            return nc

        with ExitStack() as phE:
            uTbc = sbt(phE, "uTbc", [128, 8, OWN], BF16)
            B_u = Buf()
            tri_f = sbt(phE, "tri_f", [128, 128]); triT_f = sbt(phE, "triT_f", [128, 128])
            nmf_f = sbt(phE, "nmf_f", [128, 128]); nmb_f = sbt(phE, "nmb_f", [128, 128])
            B_cE = Buf()
            for i, t in ((1, tri_f), (2, triT_f), (4, nmf_f), (5, nmb_f)):
                kb.dma("sp", t[:], consts[i], writes=[B_cE])
            for j in range(8):
                kb.dma("sp", uTbc[:, j, :], uT_d[8 + j], writes=[B_u])
            dtr = sbt(phE, "dtr", [128, 18, 32]); dtv = sbt(phE, "dtv", [128, 18, 32]); dtA = sbt(phE, "dtA", [128, 18, 32])
            dtb = sbt(phE, "dtb", [128, 32]); alg = sbt(phE, "alg", [128, 32]); dsk = sbt(phE, "dsk", [128, 16])
            gssd = sbt(phE, "gssd", [128, 1024])
            B_dt = Buf()
            kb.dma("sp", dtr[:], dtraw_d.rearrange("t p c -> p t c"), writes=[B_dt])
            kb.dma("sp", dtb[:], bcast_row(dt_bias[0:1, :]), writes=[B_dt])
            kb.dma("sp", alg[:], bcast_row(a_log[0:1, :]), writes=[B_dt])
            kb.dma("sp", dsk[:], bcast_row(d_skip[0:1, :]), writes=[B_dt])
            kb.dma("sp", gssd[:], bcast_row(ssd_norm_g[0:1, :]), writes=[B_dt])
            kb.op("dve", lambda e: e.tensor_tensor(out=dtr[:], in0=dtr[:], in1=dtb[:].unsqueeze(1).to_broadcast([128, 18, 32]),
                                                   op=ALU.add), reads=[B_dt, B_cE], writes=[B_dt])
            kb.op("act", lambda e: e.activation(out=dtr[:], in_=dtr[:], func=AF.Exp), reads=[B_dt], writes=[B_dt])
            kb.op("act", lambda e: e.activation(out=dtv[:], in_=dtr[:], func=AF.Ln, bias=ones_f[:, 0:1]), reads=[B_dt], writes=[B_dt])
            kb.op("act", lambda e: e.activation(out=alg[:], in_=alg[:], func=AF.Exp), reads=[B_dt], writes=[B_dt])
            kb.op("dve", lambda e: e.scalar_tensor_tensor(out=dtA[:], in0=dtv[:], scalar=-1.0,
                                                          in1=alg[:].unsqueeze(1).to_broadcast([128, 18, 32]),
                                                          op0=ALU.mult, op1=ALU.mult), reads=[B_dt], writes=[B_dt])
            xs_tok = sbt(phE, "xs_tok", [128, 18, 1024], BF16)
            B_xs = [Buf() for _ in range(18)]
            eac = sbt(phE, "eac", [128, NT, 32]); B_eac = Buf()
            Pall = sbt(phE, "Pall", [128, 2, 18, 16]); B_P = Buf()
            hc = sbt(phE, "hc", [128, 2, 1024]); B_hc = Buf()
            hinit = sbt(phE, "hinit", [128, 2, 1024]); B_hi = Buf()
            B_h0 = [[Buf() for _ in range(NT)] for _ in range(2)]

            with ExitStack() as ph:
                uTx = sbt(ph, "uTx", [128, 8, OWN], BF16)
                uTc = sbt(ph, "uTc", [128, 16, CTX], BF16)
                bm_tok = sbt(ph, "bm_tok", [128, 18, 512], BF16)
                for j in range(8):
                    kb.dma("sp", uTx[:, j, :], uT_d[j], writes=[B_u])
                kb.dma("sp", uTc[:], uTc_d.rearrange("j p t -> p j t"), writes=[B_u])
                psT1 = pst(ph, "psTE1", [128, 8, 128], BF16); B_psT1 = Buf()
                psT2 = pst(ph, "psTE2", [128, 4, 128], BF16); B_psT2 = Buf()
                sm_ps = pst(ph, "sm_ps", [128, 32]); B_smps = Buf()
                S_ps = pst(ph, "S_ps", [128, 1024]); B_Sps = Buf()
                sm = sbt(ph, "sm", [128, 64]); B_sm = Buf()
                xdd = [sbt(ph, "xdd%d" % i, [128, 16, 64], BF16) for i in range(2)]
                B_xdd = [Buf(), Buf()]
                Hb = [sbt(ph, "Hb%d" % i, [128, 1024]) for i in range(2)]
                B_H = [Buf(), Buf()]
                for ti in range(18):
                    if ti < 2:
                        srcs = lambda j, ti=ti: uTc[:, j, ti * 128:(ti + 1) * 128]
                    else:
                        srcs = lambda j, ti=ti: (uTx[:, j, (ti - 2) * 128:(ti - 1) * 128] if j < 8
                                                 else uTbc[:, j - 8, (ti - 2) * 128:(ti - 1) * 128])
                    for j in range(8):
                        kb.op("pe", lambda e, j=j: e.transpose(out=psT1[:, j, :], in_=srcs(j), identity=ident_b[:]),
                              reads=[B_u], writes=[B_psT1], inc=(j == 7))
                    kb.op("act", lambda e, ti=ti: e.copy(out=xs_tok[:, ti, :], in_=psT1[:].rearrange("p a b -> p (a b)")),
                          reads=[B_psT1], writes=[B_xs[ti]])
                    for j in range(4):
                        kb.op("pe", lambda e, j=j: e.transpose(out=psT2[:, j, :], in_=srcs(8 + j), identity=ident_b[:]),
                              reads=[B_u], writes=[B_psT2], inc=(j == 3))
                    kb.op("dve", lambda e, ti=ti: e.tensor_copy(out=bm_tok[:, ti, :], in_=psT2[:].rearrange("p a b -> p (a b)")),
                          reads=[B_psT2], writes=[B_xs[ti]])

                hctr = [0]

                def chunk_S(tr, dtA_ap, dtv_ap, xs_ap, bm_of, rd, own_c=None, d=0, tot_ap=None):
                    kb.op("pe", lambda e: e.matmul(sm_ps[:, 0:16], lhsT=tr[:], rhs=dtA_ap, start=True, stop=True),
                          reads=rd, writes=[B_smps], inc=False)
                    kb.op("pe", lambda e: e.matmul(sm_ps[:, 16:32], lhsT=ones_f[:], rhs=dtA_ap, start=True, stop=True),
                          reads=rd, writes=[B_smps])
                    kb.op("act", lambda e: e.copy(out=sm[:, 0:32], in_=sm_ps[:, 0:32]), reads=[B_smps], writes=[B_sm])
                    kb.op("dve", lambda e: e.tensor_tensor(out=sm[:, 32:48], in0=sm[:, 16:32], in1=sm[:, 0:16], op=ALU.subtract),
                          reads=[B_sm], writes=[B_sm])
                    kb.op("act", lambda e: e.activation(out=sm[:, 32:48], in_=sm[:, 32:48], func=AF.Exp), reads=[B_sm], writes=[B_sm])
                    kb.op("act", lambda e: e.activation(out=sm[:, 48:64], in_=sm[:, 16:32], func=AF.Exp), reads=[B_sm], writes=[B_sm])
                    if own_c is not None:
                        kb.op("act", lambda e: e.activation(out=eac[:, own_c, d * 16:(d + 1) * 16], in_=sm[:, 0:16], func=AF.Exp),
                              reads=[B_sm], writes=[B_eac])
                    kb.op("dve", lambda e: e.tensor_tensor(out=sm[:, 32:48], in0=sm[:, 32:48], in1=dtv_ap, op=ALU.mult),
                          reads=[B_sm] + rd, writes=[B_sm])
                    xi = hctr[0] % 2
                    hctr[0] += 1
                    kb.op("dve", lambda e: e.tensor_tensor(
                        out=xdd[xi][:], in0=xs_ap.rearrange("p (h q) -> p h q", h=16),
                        in1=sm[:, 32:48].unsqueeze(2).to_broadcast([128, 16, 64]), op=ALU.mult),
                        reads=[B_sm] + rd, writes=[B_xdd[xi]])
                    for g in range(4):
                        kb.op("pe", lambda e, g=g: e.matmul(S_ps[:, g * 256:(g + 1) * 256], lhsT=bm_of(g),
                                                            rhs=xdd[xi][:, 4 * g:4 * g + 4, :], start=True, stop=True),
                              reads=rd + [B_xdd[xi]], writes=[B_Sps], inc=(g == 3))

                def chunk_state(ti, d, Hin, B_Hin, Hout, B_Hout, own_c=None):
                    tr = tri_f if d == 0 else triT_f
                    chunk_S(tr, dtA[:, ti, d * 16:(d + 1) * 16], dtv[:, ti, d * 16:(d + 1) * 16], xs_tok[:, ti, :],
                            lambda g: bm_tok[:, ti, g * 128:(g + 1) * 128], [B_dt, B_xs[ti]], own_c=own_c, d=d)
                    if Hin is None:
                        kb.op("dve", lambda e: e.tensor_copy(out=Hout, in_=S_ps[:]), reads=[B_Sps], writes=[B_Hout])
                    else:
                        kb.op("dve", lambda e: e.tensor_tensor(
                            out=Hout.rearrange("p (h q) -> p h q", h=16), in0=Hin.rearrange("p (h q) -> p h q", h=16),
                            in1=sm[:, 48:64].unsqueeze(2).to_broadcast([128, 16, 64]), op=ALU.mult),
                            reads=[B_Hin, B_sm], writes=[B_Hout])
                        kb.op("dve", lambda e: e.tensor_tensor(out=Hout, in0=Hout, in1=S_ps[:], op=ALU.add),
                              reads=[B_Sps, B_Hout], writes=[B_Hout])

                for d in range(2):
                    order = [0, 1] if d == 0 else [1, 0]
                    chunk_state(order[0], d, None, None, Hb[0][:], B_H[0])
                    chunk_state(order[1], d, Hb[0][:], B_H[0], hc[:, d, :], B_hc)
                kb.op("pool", lambda e: e.memset(Pall[:], 1.0), writes=[B_P])
                for d in range(2):
                    order = list(range(NT)) if d == 0 else list(range(NT - 1, -1, -1))
                    kb.op("pool", lambda e: e.memset(Hb[0][:], 0.0), writes=[B_H[0]])
                    cur = 0
                    for n, c in enumerate(order):
                        kb.dma("sp", h0_d[d, c], Hb[cur][:], reads=[B_H[cur]], writes=[B_h0[d][c]])
                        if n < NT - 1:
                            chunk_state(2 + c, d, Hb[cur][:], B_H[cur], Hb[1 - cur][:], B_H[1 - cur], own_c=c)
                        else:
                            chunk_state(2 + c, d, Hb[cur][:], B_H[cur], Hb[1 - cur][:], B_H[1 - cur], own_c=c)
                        pin = Pall[:, d, c, :] if d == 0 else Pall[:, d, c + 1, :]
                        pout = Pall[:, d, c + 1, :] if d == 0 else Pall[:, d, c, :]
                        kb.op("dve", lambda e: e.tensor_tensor(out=pout, in0=pin, in1=sm[:, 48:64], op=ALU.mult),
                              reads=[B_sm, B_P], writes=[B_P])
                        cur = 1 - cur

                dt2r = sbt(ph, "dt2r", [128, NT, 16]); dt2 = sbt(ph, "dt2", [128, NT, 16]); dtA2 = sbt(ph, "dtA2", [128, NT, 16])
                dtb2 = sbt(ph, "dtb2", [128, 16]); alg2 = sbt(ph, "alg2", [128, 16]); tris = sbt(ph, "tris", [128, 128])
                B_d2 = Buf()
                kb.dma("sp", dt2r[:], dtraw2_d.rearrange("t p c -> p t c"), writes=[B_d2])
                kb.dma("sp", dtb2[:], bcast_row(dtb_sel[0:1, :]), writes=[B_d2])
                kb.dma("sp", alg2[:], bcast_row(alog_sel[0:1, :]), writes=[B_d2])
                kb.dma("sp", tris[:], tri_sel[:, :], writes=[B_d2])
                kb.op("dve", lambda e: e.tensor_tensor(out=dt2r[:], in0=dt2r[:], in1=dtb2[:].unsqueeze(1).to_broadcast([128, NT, 16]),
                                                       op=ALU.add), reads=[B_d2], writes=[B_d2])
                kb.op("act", lambda e: e.activation(out=dt2r[:], in_=dt2r[:], func=AF.Exp), reads=[B_d2], writes=[B_d2])
                kb.op("act", lambda e: e.activation(out=dt2[:], in_=dt2r[:], func=AF.Ln, bias=ones_f[:, 0:1]), reads=[B_d2], writes=[B_d2])
                kb.op("act", lambda e: e.activation(out=alg2[:], in_=alg2[:], func=AF.Exp), reads=[B_d2], writes=[B_d2])
                kb.op("dve", lambda e: e.scalar_tensor_tensor(out=dtA2[:], in0=dt2[:], scalar=-1.0,
                                                              in1=alg2[:].unsqueeze(1).to_broadcast([128, NT, 16]),
                                                              op0=ALU.mult, op1=ALU.mult), reads=[B_d2], writes=[B_d2])
                tot2_ps = pst(ph, "tot2_ps", [128, 256]); B_t2ps = Buf()
                kb.op("pe", lambda e: e.matmul(tot2_ps[:], lhsT=ones_f[:], rhs=dtA2[:], start=True, stop=True),
                      reads=[B_d2], writes=[B_t2ps])
                tot2 = sbt(ph, "tot2", [128, NT, 16]); pre = sbt(ph, "pre", [128, NT + 1, 16]); suf = sbt(ph, "suf", [128, NT + 1, 16])
                wch = sbt(ph, "wch", [128, NT + 1, 16]); B_w = Buf()
                kb.op("act", lambda e: e.copy(out=tot2[:].rearrange("p a b -> p (a b)"), in_=tot2_ps[:]), reads=[B_t2ps], writes=[B_w])
                kb.op("pool", lambda e: e.memset(pre[:], 0.0), writes=[B_w])
                kb.op("pool", lambda e: e.memset(suf[:], 0.0), writes=[B_w])
                for c in range(NT):
                    kb.op("dve", lambda e, c=c: e.tensor_tensor(out=pre[:, c + 1, :], in0=pre[:, c, :], in1=tot2[:, c, :], op=ALU.add),
                          reads=[B_w], writes=[B_w])
                for c in range(NT - 1, 0, -1):
                    kb.op("dve", lambda e, c=c: e.tensor_tensor(out=suf[:, c - 1, :], in0=suf[:, c, :], in1=tot2[:, c, :], op=ALU.add),
                          reads=[B_w], writes=[B_w])
                kb.op("dve", lambda e: e.tensor_scalar(out=wch[:], in0=suf[:], scalar1=flg[:, 0:1], scalar2=None, op0=ALU.mult),
                      reads=[B_w], writes=[B_w])
                kb.op("dve", lambda e: e.scalar_tensor_tensor(out=wch[:], in0=pre[:], scalar=flg[:, 1:2], in1=wch[:],
                                                              op0=ALU.mult, op1=ALU.add), reads=[B_w], writes=[B_w])
                kb.op("dve", lambda e: e.tensor_copy(out=wch[:, NT, :], in_=pre[:, NT, :]), reads=[B_w], writes=[B_w])
                kb.op("act", lambda e: e.activation(out=wch[:], in_=wch[:], func=AF.Exp), reads=[B_w], writes=[B_w])
                u2 = [sbt(ph, "u2_%d" % i, [128, 12, 128], BF16) for i in range(2)]
                B_u2 = [Buf(), Buf()]
                xs2 = [sbt(ph, "xs2_%d" % i, [128, 1024], BF16) for i in range(2)]
                bm2 = [sbt(ph, "bm2_%d" % i, [128, 512], BF16) for i in range(2)]
                B_x2 = [Buf(), Buf()]
                Hacc = Hb[0]; B_Hacc = B_H[0]
                stmp = Hb[1]; B_stmp = B_H[1]
                kb.op("pool", lambda e: e.memset(Hacc[:], 0.0), writes=[B_Hacc])
                u2v = uT2_d.rearrange("j p t -> p j t")
                kb.dma("sp", u2[0][:], u2v[:, :, 0:128], writes=[B_u2[0]])
                for c in range(NT):
                    bi = c % 2
                    if c + 1 < NT:
                        kb.dma("sp", u2[1 - bi][:], u2v[:, :, (c + 1) * 128:(c + 2) * 128], writes=[B_u2[1 - bi]])
                    for j in range(8):
                        kb.op("pe", lambda e, j=j: e.transpose(out=psT1[:, j, :], in_=u2[bi][:, j, :], identity=ident_b[:]),
                              reads=[B_u2[bi]], writes=[B_psT1], inc=(j == 7))
                    kb.op("act", lambda e: e.copy(out=xs2[bi][:], in_=psT1[:].rearrange("p a b -> p (a b)")),
                          reads=[B_psT1], writes=[B_x2[bi]])
                    for j in range(4):
                        kb.op("pe", lambda e, j=j: e.transpose(out=psT2[:, j, :], in_=u2[bi][:, 8 + j, :], identity=ident_b[:]),
                              reads=[B_u2[bi]], writes=[B_psT2], inc=(j == 3))
                    kb.op("dve", lambda e: e.tensor_copy(out=bm2[bi][:], in_=psT2[:].rearrange("p a b -> p (a b)")),
                          reads=[B_psT2], writes=[B_x2[bi]])
                    chunk_S(tris, dtA2[:, c, :], dt2[:, c, :], xs2[bi][:], lambda g: bm2[bi][:, g * 128:(g + 1) * 128],
                            [B_d2, B_x2[bi]])
                    kb.op("dve", lambda e: e.tensor_tensor(
                        out=stmp[:].rearrange("p (h q) -> p h q", h=16), in0=S_ps[:].rearrange("p (h q) -> p h q", h=16),
                        in1=wch[:, c, :].unsqueeze(2).to_broadcast([128, 16, 64]), op=ALU.mult),
                        reads=[B_Sps, B_w], writes=[B_stmp])
                    kb.op("pool", lambda e: e.tensor_tensor(out=Hacc[:], in0=Hacc[:], in1=stmp[:], op=ALU.add),
                          reads=[B_stmp, B_Hacc], writes=[B_Hacc])
                hsel = stmp; B_hsel = B_stmp
                kb.op("dve", lambda e: e.tensor_scalar(out=hsel[:], in0=hc[:, 0, :], scalar1=flg[:, 0:1], scalar2=None, op0=ALU.mult),
                      reads=[B_hc], writes=[B_hsel])
                kb.op("dve", lambda e: e.scalar_tensor_tensor(out=hsel[:], in0=hc[:, 1, :], scalar=flg[:, 1:2], in1=hsel[:],
                                                              op0=ALU.mult, op1=ALU.add), reads=[B_hc, B_hsel], writes=[B_hsel])
                kb.op("dve", lambda e: e.tensor_tensor(
                    out=hsel[:].rearrange("p (h q) -> p h q", h=16), in0=hsel[:].rearrange("p (h q) -> p h q", h=16),
                    in1=wch[:, NT, :].unsqueeze(2).to_broadcast([128, 16, 64]), op=ALU.mult),
                    reads=[B_hsel, B_w], writes=[B_hsel])
                kb.op("dve", lambda e: e.tensor_tensor(out=hsel[:], in0=hsel[:], in1=Hacc[:], op=ALU.add),
                      reads=[B_hsel, B_Hacc], writes=[B_hsel])
                for d in range(2):
                    fa, fb = (flg[:, 0:1], flg[:, 1:2]) if d == 0 else (flg[:, 1:2], flg[:, 0:1])
                    kb.op("dve", lambda e, d=d, fa=fa: e.tensor_scalar(out=hinit[:, d, :], in0=hsel[:], scalar1=fa, scalar2=None,
                                                                       op0=ALU.mult), reads=[B_hsel], writes=[B_hi])
                    kb.op("dve", lambda e, d=d, fb=fb: e.scalar_tensor_tensor(out=hinit[:, d, :], in0=hc[:, d, :], scalar=fb,
                                                                              in1=hinit[:, d, :], op0=ALU.mult, op1=ALU.add),
                          reads=[B_hc, B_hi], writes=[B_hi])
                kb.barrier()
            if stop_after == "E1":
                dbg_h = dscr("dbg_h", [128, 4, 1024])
                kb.dma("sp", dbg_h[:, 0:2, :], hc[:], reads=[B_hc])
                kb.dma("sp", dbg_h[:, 2:4, :], hinit[:], reads=[B_hi])
                kb.barrier()
                return nc

            with ExitStack() as ph:
                seg_ps = [pst(ph, "seg_ps%d" % i, [128, 512]) for i in range(2)]; B_seg = [Buf(), Buf()]
                cb_ps = pst(ph, "cb_ps", [128, 512]); B_cbps = Buf()
                yd_ps = pst(ph, "yd_ps", [128, 1024]); B_ydps = Buf()
                yo_ps = pst(ph, "yo_ps", [128, 1024]); B_yops = Buf()
                psT3 = pst(ph, "psTE3", [128, 8, 128], BF16); B_psT3 = Buf()
                negones = sbt(ph, "negones", [128, 128]); nm4 = sbt(ph, "nm4", [128, 2, 4, 128], BF16); B_c2 = Buf()
                kb.op("pool", lambda e: e.memset(negones[:], -1.0), writes=[B_c2])
                for d, nmx in enumerate((nmf_f, nmb_f)):
                    kb.op("dve", lambda e, d=d, nmx=nmx: e.tensor_copy(out=nm4[:, d, :, :],
                                                                       in_=nmx[:].unsqueeze(1).to_broadcast([128, 4, 128])),
                          writes=[B_c2])
                Rf = sbt(ph, "Rf", [128, 32, 128]); B_R = Buf()
                Mb = sbt(ph, "Mb", [128, 32, 128], BF16); B_M = Buf()
                LT = sbt(ph, "LT", [128, 32, 128], BF16); B_LT = Buf()
                cbT = sbt(ph, "cbT", [128, 4, 128], BF16); B_cbT = Buf()
                xd = sbt(ph, "xd", [128, 32, 64], BF16); B_xd2 = Buf()
                h0t = [sbt(ph, "h0t%d" % i, [128, 1024]) for i in range(2)]; B_h0t = [Buf(), Buf()]
                hp = [sbt(ph, "hp%d" % i, [128, 16, 64], BF16) for i in range(2)]; B_hp = [Buf(), Buf()]
                htmp = sbt(ph, "htmp", [128, 1024]); B_htmp = Buf()
                ya = sbt(ph, "ya", [128, 1024]); B_ya = Buf()
                yb = sbt(ph, "yb", [128, 1024]); B_yb = Buf()
                yy = sbt(ph, "yy", [128, 1024]); B_yy = Buf()
                zt = [sbt(ph, "zt%d" % i, [128, 1024]) for i in range(2)]; B_zt = [Buf(), Buf()]
                junk2 = sbt(ph, "junk2", [128, 1024], BF16); B_j2 = Buf()
                ssq2 = sbt(ph, "ssq2", [128, 4]); B_ss2 = Buf()
                ynb = sbt(ph, "ynb", [128, 1024], BF16); B_ynb = Buf()
                mo2 = [sbt(ph, "mo2_%d" % i, [128, 8, 128], BF16) for i in range(2)]; B_mo2 = [Buf(), Buf()]
                sctr2 = 0
                for c in range(NT):
                    ti = 2 + c
                    tk = slice(c * 128, (c + 1) * 128)
                    kb.dma("sp", zt[c % 2][:], zs_d[c], writes=[B_zt[c % 2]])
                    for d in range(2):
                        kb.dma("sp", h0t[d][:], h0_d[d, c], reads=[B_h0[d][c]], writes=[B_h0t[d]])
                    for d, tr in enumerate((tri_f, triT_f)):
                        kb.op("pool", lambda e, d=d, tr=tr: e.tensor_tensor(
                            out=Rf[:, d * 16:(d + 1) * 16, :], in0=tr[:].unsqueeze(1).to_broadcast([128, 16, 128]),
                            in1=dtA[:, ti, d * 16:(d + 1) * 16].unsqueeze(2).to_broadcast([128, 16, 128]), op=ALU.mult),
                            reads=[B_dt], writes=[B_R])
                    if cut <= 1:
                        continue
                    for g in range(4):
                        kb.op("pe", lambda e, g=g: e.matmul(cb_ps[:, g * 128:(g + 1) * 128], lhsT=uTbc[:, g, tk], rhs=uTbc[:, 4 + g, tk],
                                                            start=True, stop=True), reads=[B_u], writes=[B_cbps], inc=(g == 3))
                    kb.op("act", lambda e: e.copy(out=cbT[:].rearrange("p a b -> p (a b)"), in_=cb_ps[:]), reads=[B_cbps], writes=[B_cbT])
                    for q4 in range(8):
                        d = q4 // 4
                        si = sctr2 % 2
                        sctr2 += 1
                        kb.op("pe", lambda e: e.matmul(seg_ps[si][:], lhsT=ones_f[:], rhs=Rf[:, 4 * q4:4 * q4 + 4, :],
                                                       start=True, stop=False), reads=[B_R], writes=[B_seg[si]], inc=False)
                        for r in range(4):
                            kb.op("pe", lambda e, r=r: e.matmul(seg_ps[si][:, r * 128:(r + 1) * 128], lhsT=Rf[:, 4 * q4 + r, :],
                                                                rhs=negones[:], start=False, stop=False),
                                  reads=[B_R, B_c2], writes=[B_seg[si]], inc=False)
                        kb.op("pe", lambda e: e.matmul(seg_ps[si][:], lhsT=ident_b[:], rhs=nm4[:, d, :, :],
                                                       start=False, stop=True), reads=[B_c2], writes=[B_seg[si]])
                        kb.op("act", lambda e: e.activation(out=Mb[:, 4 * q4:4 * q4 + 4, :], in_=seg_ps[si][:], func=AF.Exp),
                              reads=[B_seg[si]], writes=[B_M])
                    if cut <= 2:
                        continue
                    for d in range(2):
                        kb.op("dve", lambda e, d=d: e.tensor_tensor(
                            out=LT[:, d * 16:(d + 1) * 16, :].rearrange("p (g r) l -> p g r l", g=4),
                            in0=Mb[:, d * 16:(d + 1) * 16, :].rearrange("p (g r) l -> p g r l", g=4),
                            in1=cbT[:].unsqueeze(2).to_broadcast([128, 4, 4, 128]), op=ALU.mult),
                            reads=[B_M, B_cbT], writes=[B_LT])
                        kb.op("pool", lambda e, d=d: e.tensor_tensor(
                            out=xd[:, d * 16:(d + 1) * 16, :], in0=xs_tok[:, ti, :].rearrange("p (h q) -> p h q", h=16),
                            in1=dtv[:, ti, d * 16:(d + 1) * 16].unsqueeze(2).to_broadcast([128, 16, 64]), op=ALU.mult),
                            reads=[B_xs[ti], B_dt], writes=[B_xd2])
                    if cut <= 3:
                        continue
                    for h in range(16):
                        for d in range(2):
                            kb.op("pe", lambda e, h=h, d=d: e.matmul(yd_ps[:, h * 64:(h + 1) * 64], lhsT=LT[:, d * 16 + h, :],
                                                                     rhs=xd[:, d * 16 + h, :], start=(d == 0), stop=(d == 1)),
                                  reads=[B_LT, B_xd2], writes=[B_ydps], inc=(h == 15 and d == 1))
                    if cut <= 4:
                        continue
                    for d in range(2):
                        pin = Pall[:, d, c, :] if d == 0 else Pall[:, d, c + 1, :]
                        kb.op("dve", lambda e, d=d, pin=pin: e.tensor_tensor(
                            out=htmp[:].rearrange("p (h q) -> p h q", h=16), in0=hinit[:, d, :].rearrange("p (h q) -> p h q", h=16),
                            in1=pin.unsqueeze(2).to_broadcast([128, 16, 64]), op=ALU.mult),
                            reads=[B_hi, B_P], writes=[B_htmp])
                        kb.op("dve", lambda e, d=d: e.tensor_tensor(out=hp[d][:].rearrange("p h q -> p (h q)"), in0=htmp[:],
                                                                    in1=h0t[d][:], op=ALU.add),
                              reads=[B_htmp, B_h0t[d]], writes=[B_hp[d]])
                        for g in range(4):
                            kb.op("pe", lambda e, g=g, d=d: e.matmul(yo_ps[:, g * 256:(g + 1) * 256], lhsT=uTbc[:, 4 + g, tk],
                                                                     rhs=hp[d][:, 4 * g:4 * g + 4, :], start=True, stop=True),
                                  reads=[B_u, B_hp[d]], writes=[B_yops], inc=(g == 3))
                        dst, Bd = (ya, B_ya) if d == 0 else (yb, B_yb)
                        kb.op("dve", lambda e, d=d, dst=dst: e.tensor_tensor(
                            out=dst[:].rearrange("p (h q) -> p h q", h=16), in0=yo_ps[:].rearrange("p (h q) -> p h q", h=16),
                            in1=eac[:, c, d * 16:(d + 1) * 16].unsqueeze(2).to_broadcast([128, 16, 64]), op=ALU.mult),
                            reads=[B_yops, B_eac], writes=[Bd])
                    if cut <= 5:
                        continue
                    kb.op("dve", lambda e: e.tensor_tensor(out=yy[:], in0=yd_ps[:], in1=ya[:], op=ALU.add),
                          reads=[B_ydps, B_ya], writes=[B_yy])
                    kb.op("pool", lambda e: e.tensor_tensor(out=yy[:], in0=yy[:], in1=yb[:], op=ALU.add),
                          reads=[B_yy, B_yb], writes=[B_yy])
                    kb.op("pool", lambda e: e.tensor_tensor(
                        out=ya[:].rearrange("p (h q) -> p h q", h=16), in0=xs_tok[:, ti, :].rearrange("p (h q) -> p h q", h=16),
                        in1=dsk[:].unsqueeze(2).to_broadcast([128, 16, 64]), op=ALU.mult),
                        reads=[B_xs[ti], B_dt], writes=[B_ya])
                    kb.op("pool", lambda e: e.tensor_tensor(out=yy[:], in0=yy[:], in1=ya[:], op=ALU.add),
                          reads=[B_yy, B_ya], writes=[B_yy])
                    if cut <= 6:
                        continue
                    kb.op("dve", lambda e: e.tensor_tensor(out=yy[:], in0=yy[:], in1=zt[c % 2][:], op=ALU.mult),
                          reads=[B_yy, B_zt[c % 2]], writes=[B_yy])
                    rms_mod_tile(yy[:], B_yy, gssd[:], None, B_dt, junk2, B_j2, ssq2, B_ss2, yb, B_yb, None, None, width=1024)
                    if cut <= 7:
                        continue
                    kb.op("act", lambda e: e.copy(out=ynb[:], in_=yb[:]), reads=[B_yb], writes=[B_ynb])
                    for j in range(8):
                        kb.op("pe", lambda e, j=j: e.transpose(out=psT3[:, j, :], in_=ynb[:, j * 128:(j + 1) * 128], identity=ident_b[:]),
                              reads=[B_ynb], writes=[B_psT3], inc=(j == 7))
                    kb.op("act", lambda e: e.copy(out=mo2[c % 2][:], in_=psT3[:]), reads=[B_psT3], writes=[B_mo2[c % 2]])
                    kb.dma("sp", mixT_d[8:16, :, tk].rearrange("h p t -> p h t"), mo2[c % 2][:], reads=[B_mo2[c % 2]])
                kb.barrier()
        if stop_after == "E":
            return nc

        with ExitStack() as ph:
            Wo = sbt(ph, "Wo", [128, 16, D], BF16); B_Wo = Buf()
            wov = w_out.rearrange("(k p) n -> p k n", p=128)
            for cbk in range(4):
                kb.dma("pool", Wo[:, :, cbk * 512:(cbk + 1) * 512], wov[:, :, cbk * 512:(cbk + 1) * 512], writes=[B_Wo])
            B_m2 = Buf()
            g1b = load_bc(ph, "sp", "g1b", mod_d[2:3, :], D, B_m2)
            G2 = load_bc(ph, "sp", "G2", mod_d[4:5, :], D, B_m2)
            SH2 = load_bc(ph, "sp", "SH2", mod_d[3:4, :], D, B_m2)
            g2n = load_bc(ph, "sp", "g2n", norm2_g[0:1, :], D, B_m2)
            kb.op("dve", lambda e: e.tensor_tensor(out=G2[:], in0=G2[:], in1=g2n[:], op=ALU.mult), reads=[B_m2], writes=[B_m2])
            rw = sbt(ph, "rw", [128, 16, NEXP]); rbias = sbt(ph, "rbias", [128, NEXP])
            kb.dma("sp", rw[:], router_w.rearrange("(k p) e -> p k e", p=128), writes=[B_m2])
            kb.dma("sp", rbias[:], bcast_row(router_bias[0:1, :]), writes=[B_m2])
            mix = [sbt(ph, "mix%d" % i, [128, 16, 128], BF16) for i in range(2)]; B_mix = [Buf(), Buf()]
            xtF = [sbt(ph, "xtF%d" % i, [128, D]) for i in range(2)]; B_xtF = [Buf(), Buf()]
            x1t = sbt(ph, "x1t", [128, D]); B_x1t = Buf()
            tmpF = sbt(ph, "tmpF", [128, D]); B_tmpF = Buf()
            h2f = g2n; B_h2f = Buf()
            h2b = sbt(ph, "h2b", [128, D], BF16); B_h2b = Buf()
            junkF = sbt(ph, "junkF", [128, D], BF16); B_junkF = Buf()
            ssqF = sbt(ph, "ssqF", [128, 4]); B_ssF = Buf()
            h2To = [sbt(ph, "h2To%d" % i, [128, 16, 128], BF16) for i in range(2)]; B_h2To = [Buf(), Buf()]
            h2Tf = sbt(ph, "h2Tf", [128, 16, 128]); B_h2Tf = Buf()
            psF = [pst(ph, "psF%d" % i, [128, 512]) for i in range(2)]; B_psF = [Buf(), Buf()]
            psTF = [pst(ph, "psTF%d" % i, [128, 8, 128], BF16) for i in range(2)]; B_psTF = [Buf(), Buf()]
            psR = [pst(ph, "psR%d" % i, [128, 4, 128]) for i in range(2)]; B_psR = [Buf(), Buf()]
            psL = pst(ph, "psL", [128, NEXP]); B_psL = Buf()
            sc_ = sbt(ph, "sc_", [128, NEXP]); sel = sbt(ph, "sel", [128, NEXP]); sel2 = sbt(ph, "sel2", [128, NEXP])
            m1 = sbt(ph, "m1", [128, 8]); m2 = sbt(ph, "m2", [128, 8]); gs = sbt(ph, "gs", [128, 8])
            cmpt = sbt(ph, "cmpt", [128, 8, 8]); rank = sbt(ph, "rank", [128, 8]); km = sbt(ph, "km", [128, 8])
            mx8 = sbt(ph, "mx8", [128, 8]); thr = sbt(ph, "thr", [128, 2])
            gate = [sbt(ph, "gate%d" % i, [128, NEXP + 1]) for i in range(2)]; B_gate = [Buf(), Buf()]
            B_r = Buf()
            for i in range(2):
                kb.op("pool", lambda e, i=i: e.memset(gate[i][:], 1.0), writes=[B_gate[i]])
            mixv = mixT_d.rearrange("k p t -> p k t")
            h2Tv = h2T_d.rearrange("k p t -> p k t")
            kb.dma("sp", mix[0][:], mixv[:, :, 0:128], writes=[B_mix[0]])
            kb.dma("sp", xtF[0][:], x_own[0:128, :], writes=[B_xtF[0]])
            pctrF = 0
            for t in range(NT):
                bi = t % 2
                tk = slice(t * 128, (t + 1) * 128)
                if t + 1 < NT:
                    kb.dma("sp", mix[1 - bi][:], mixv[:, :, (t + 1) * 128:(t + 2) * 128], writes=[B_mix[1 - bi]])
                    kb.dma("sp", xtF[1 - bi][:], x_own[(t + 1) * 128:(t + 2) * 128, :], writes=[B_xtF[1 - bi]])
                for cbk in range(4):
                    pi = pctrF % 2
                    pctrF += 1
                    cs_ = slice(cbk * 512, (cbk + 1) * 512)
                    for k in range(16):
                        kb.op("pe", lambda e, k=k: e.matmul(psF[pi][:], lhsT=mix[bi][:, k, :], rhs=Wo[:, k, cs_],
                                                            start=(k == 0), stop=(k == 15)),
                              reads=[B_mix[bi], B_Wo], writes=[B_psF[pi]], inc=(k == 15))
                    kb.op("dve", lambda e: e.tensor_tensor(out=tmpF[:, cs_], in0=psF[pi][:], in1=g1b[:, cs_], op=ALU.mult),
                          reads=[B_psF[pi], B_m2], writes=[B_tmpF])
                    kb.op("pool", lambda e: e.tensor_tensor(out=x1t[:, cs_], in0=tmpF[:, cs_], in1=xtF[bi][:, cs_], op=ALU.add),
                          reads=[B_tmpF, B_xtF[bi]], writes=[B_x1t])
                kb.dma("sp", x1_d[tk, :], x1t[:], reads=[B_x1t])
                rms_mod_tile(x1t[:], B_x1t, G2[:], SH2[:], B_m2, junkF, B_junkF, ssqF, B_ssF, tmpF, B_tmpF, h2f[:], B_h2f)
                kb.op("act", lambda e: e.copy(out=h2b[:], in_=h2f[:]), reads=[B_h2f], writes=[B_h2b])
                for half in range(2):
                    for kk in range(8):
                        k = half * 8 + kk
                        kb.op("pe", lambda e, k=k, kk=kk: e.transpose(out=psTF[half][:, kk, :], in_=h2b[:, k * 128:(k + 1) * 128],
                                                                     identity=ident_b[:]),
                              reads=[B_h2b], writes=[B_psTF[half]], inc=(kk == 7))
                    if half == 0:
                        kb.op("act", lambda e: e.copy(out=h2To[bi][:, 0:8, :], in_=psTF[0][:]), reads=[B_psTF[0]], writes=[B_h2To[bi]])
                    else:
                        kb.op("dve", lambda e: e.tensor_copy(out=h2To[bi][:, 8:16, :], in_=psTF[1][:]), reads=[B_psTF[1]],
                              writes=[B_h2To[bi]])
                kb.dma("sp", h2Tv[:, :, tk], h2To[bi][:], reads=[B_h2To[bi]])
                for q in range(4):
                    ri = q % 2
                    for kk in range(4):
                        k = q * 4 + kk
                        kb.op("pe", lambda e, k=k, kk=kk: e.matmul(psR[ri][:, kk, :], lhsT=h2f[:, k * 128:(k + 1) * 128], rhs=ident_f[:],
                                                                  start=True, stop=True),
                              reads=[B_h2f], writes=[B_psR[ri]], inc=(kk == 3))
                    kb.op("act", lambda e, q=q: e.copy(out=h2Tf[:, q * 4:(q + 1) * 4, :], in_=psR[ri][:]), reads=[B_psR[ri]],
                          writes=[B_h2Tf])
                for k in range(16):
                    kb.op("pe", lambda e, k=k: e.matmul(psL[:], lhsT=h2Tf[:, k, :], rhs=rw[:, k, :], start=(k == 0), stop=(k == 15)),
                          reads=[B_h2Tf, B_m2], writes=[B_psL], inc=(k == 15))
                R_ = [B_r]
                kb.op("act", lambda e: e.activation(out=sc_[:], in_=psL[:], func=AF.Sigmoid), reads=[B_psL], writes=R_)
                kb.op("dve", lambda e: e.tensor_tensor(out=sel[:], in0=sc_[:], in1=rbias[:], op=ALU.add), reads=R_ + [B_m2], writes=R_)
                s3 = lambda a: a[:].rearrange("p (g e) -> p g e", g=8)
                kb.op("dve", lambda e: e.tensor_reduce(out=m1[:], in_=s3(sel), axis=AX.X, op=ALU.max), reads=R_, writes=R_)
                kb.op("dve", lambda e: e.tensor_tensor(out=s3(sel2), in0=s3(sel), in1=m1[:].unsqueeze(2).to_broadcast([128, 8, 8]),
                                                       op=ALU.is_equal), reads=R_, writes=R_)
                kb.op("dve", lambda e: e.scalar_tensor_tensor(out=sel2[:], in0=sel2[:], scalar=-1e9, in1=sel[:], op0=ALU.mult,
                                                              op1=ALU.add), reads=R_, writes=R_)
                kb.op("dve", lambda e: e.tensor_reduce(out=m2[:], in_=s3(sel2), axis=AX.X, op=ALU.max), reads=R_, writes=R_)
                kb.op("dve", lambda e: e.tensor_tensor(out=gs[:], in0=m1[:], in1=m2[:], op=ALU.add), reads=R_, writes=R_)
                kb.op("dve", lambda e: e.tensor_tensor(out=cmpt[:], in0=gs[:].unsqueeze(1).to_broadcast([128, 8, 8]),
                                                       in1=gs[:].unsqueeze(2).to_broadcast([128, 8, 8]), op=ALU.is_gt),
                      reads=R_, writes=R_)
                kb.op("dve", lambda e: e.tensor_reduce(out=rank[:], in_=cmpt[:], axis=AX.X, op=ALU.add), reads=R_, writes=R_)
                kb.op("dve", lambda e: e.tensor_scalar(out=km[:], in0=rank[:], scalar1=3.5, scalar2=1e9, op0=ALU.is_lt, op1=ALU.mult),
                      reads=R_, writes=R_)
                kb.op("dve", lambda e: e.tensor_scalar(out=km[:], in0=km[:], scalar1=-1e9, scalar2=None, op0=ALU.add),
                      reads=R_, writes=R_)
                kb.op("dve", lambda e: e.tensor_tensor(out=s3(sel2), in0=s3(sel), in1=km[:].unsqueeze(2).to_broadcast([128, 8, 8]),
                                                       op=ALU.add), reads=R_, writes=R_)
                kb.op("dve", lambda e: e.max(out=mx8[:], in_=sel2[:]), reads=R_, writes=R_)
                kb.op("dve", lambda e: e.tensor_reduce(out=thr[:, 0:1], in_=mx8[:], axis=AX.X, op=ALU.min), reads=R_, writes=R_)
                kb.op("dve", lambda e: e.tensor_scalar(out=sel2[:], in0=sel2[:], scalar1=thr[:, 0:1], scalar2=None, op0=ALU.is_ge),
                      reads=R_, writes=R_)
                kb.op("dve", lambda e: e.tensor_tensor(out=sel2[:], in0=sel2[:], in1=sc_[:], op=ALU.mult), reads=R_, writes=R_)
                kb.op("dve", lambda e: e.tensor_reduce(out=thr[:, 1:2], in_=sel2[:], axis=AX.X, op=ALU.add), reads=R_, writes=R_)
                kb.op("dve", lambda e: e.reciprocal(out=thr[:, 1:2], in_=thr[:, 1:2]), reads=R_, writes=R_)
                kb.op("dve", lambda e: e.tensor_scalar(out=gate[bi][:, 0:NEXP], in0=sel2[:], scalar1=thr[:, 1:2], scalar2=2.5,
                                                       op0=ALU.mult, op1=ALU.mult), reads=R_, writes=[B_gate[bi]])
                kb.dma("sp", gate_d[t], gate[bi][:], reads=[B_gate[bi]])
            kb.barrier()
        if stop_after == "F":
            return nc

        for half in range(2):
            with ExitStack() as ph:
                yacc = sbt(ph, "yacc%d" % half, [128, 8, D]); B_y = [Buf() for _ in range(8)]
                gts = sbt(ph, "gts%d" % half, [128, 8, NEXP + 1]); B_g = Buf()
                kb.dma("sp", gts[:], gate_d[half * 8:(half + 1) * 8].rearrange("t p e -> p t e"), writes=[B_g])
                for tl in range(8):
                    kb.op("pool", lambda e, tl=tl: e.memset(yacc[:, tl, :], 0.0), writes=[B_y[tl]])
                with ExitStack() as ph2:
                    h2T = sbt(ph2, "h2T%d" % half, [128, 16, 1024], BF16); B_h2T = Buf()
                    h2Tv = h2T_d.rearrange("k p t -> p k t")
                    for q in range(4):
                        kb.dma("sp", h2T[:, q * 4:(q + 1) * 4, :], h2Tv[:, q * 4:(q + 1) * 4, half * 1024:(half + 1) * 1024],
                               writes=[B_h2T])
                    Wg = [sbt(ph2, "Wg%d_%d" % (half, i), [128, 16, 512], BF16) for i in range(2)]
                    Wu = [sbt(ph2, "Wu%d_%d" % (half, i), [128, 16, 512], BF16) for i in range(2)]
                    Wd = [sbt(ph2, "Wd%d_%d" % (half, i), [128, 4, D], BF16) for i in range(2)]
                    B_We = [Buf(), Buf()]
                    sg0 = sbt(ph2, "sg%d" % half, [128, 512]); B_sg0 = Buf()
                    sg = [sg0, sg0]; B_sg = [B_sg0, B_sg0]
                    actT = [sbt(ph2, "actT%d_%d" % (half, i), [128, 4, 512], BF16) for i in range(2)]; B_aT = [Buf(), Buf()]
                    psg = [pst(ph2, "psg%d_%d" % (half, i), [128, 512]) for i in range(2)]; B_psg = [Buf(), Buf()]
                    psu = [pst(ph2, "psu%d_%d" % (half, i), [128, 512]) for i in range(2)]; B_psu = [Buf(), Buf()]
                    psy = [pst(ph2, "psy%d_%d" % (half, i), [128, 512]) for i in range(4)]; B_psy = [Buf() for _ in range(4)]

                    def load_e(e_):
                        bi = e_ % 2
                        kb.dma("pool", Wg[bi][:], ew_gate[e_].rearrange("(k p) f -> p k f", p=128), writes=[B_We[bi]])
                        kb.dma("pool", Wu[bi][:], ew_up[e_].rearrange("(k p) f -> p k f", p=128), writes=[B_We[bi]])
                        kb.dma("pool", Wd[bi][:], ew_down[e_].rearrange("(k p) d -> p k d", p=128), writes=[B_We[bi]])

                    NE = cfg_nexp
                    load_e(0)
                    gctr = 0
                    yctr = 0
                    for e_ in range(NE):
                        if e_ + 1 < NE:
                            load_e(e_ + 1)
                        bi = e_ % 2
                        for grp in range(2):
                            ai = gctr % 2
                            gctr += 1
                            ts_ = slice(grp * 512, (grp + 1) * 512)
                            for fc in range(4):
                                pi = fc % 2
                                fs = slice(fc * 128, (fc + 1) * 128)
                                for k in range(16):
                                    kb.op("pe", lambda e, k=k: e.matmul(psg[pi][:], lhsT=Wg[bi][:, k, fs], rhs=h2T[:, k, ts_],
                                                                        start=(k == 0), stop=(k == 15)),
                                          reads=[B_We[bi], B_h2T], writes=[B_psg[pi]], inc=(k == 15))
                                for k in range(16):
                                    kb.op("pe", lambda e, k=k: e.matmul(psu[pi][:], lhsT=Wu[bi][:, k, fs], rhs=h2T[:, k, ts_],
                                                                        start=(k == 0), stop=(k == 15)),
                                          reads=[B_We[bi], B_h2T], writes=[B_psu[pi]], inc=(k == 15))
                                kb.op("act", lambda e: e.activation(out=sg[pi][:], in_=psg[pi][:], func=AF.Silu),
                                      reads=[B_psg[pi]], writes=[B_sg[pi]])
                                kb.op("dve", lambda e, fc=fc: e.tensor_tensor(out=actT[ai][:, fc, :], in0=psu[pi][:], in1=sg[pi][:],
                                                                             op=ALU.mult),
                                      reads=[B_psu[pi], B_sg[pi]], writes=[B_aT[ai]])
                            for tt in range(4):
                                tl = grp * 4 + tt
                                for dc in range(4):
                                    yi = yctr % 4
                                    yctr += 1
                                    ds_ = slice(dc * 512, (dc + 1) * 512)
                                    for fc in range(4):
                                        kb.op("pe", lambda e, fc=fc: e.matmul(psy[yi][:], lhsT=actT[ai][:, fc, tt * 128:(tt + 1) * 128],
                                                                              rhs=Wd[bi][:, fc, ds_], start=(fc == 0), stop=(fc == 3)),
                                              reads=[B_aT[ai], B_We[bi]], writes=[B_psy[yi]], inc=(fc == 3))
                                    kb.op("dve", lambda e: e.scalar_tensor_tensor(out=yacc[:, tl, ds_], in0=psy[yi][:],
                                                                                  scalar=gts[:, tl, e_:e_ + 1], in1=yacc[:, tl, ds_],
                                                                                  op0=ALU.mult, op1=ALU.add),
                                          reads=[B_psy[yi], B_g, B_y[tl]], writes=[B_y[tl]])
                    kb.barrier()
                with ExitStack() as ph3:
                    B_m3 = Buf()
                    g2b = load_bc(ph3, "sp", "g2b%d" % half, mod_d[5:6, :], D, B_m3)
                    fgb = load_bc(ph3, "sp", "fgb%d" % half, final_g[0:1, :], D, B_m3)
                    x1l = [sbt(ph3, "x1l%d_%d" % (half, i), [128, D]) for i in range(2)]; B_x1l = [Buf(), Buf()]
                    junk3 = sbt(ph3, "junk3_%d" % half, [128, D], BF16); B_j3 = Buf()
                    ssq3 = sbt(ph3, "ssq3_%d" % half, [128, 4]); B_s3 = Buf()
                    ot = [sbt(ph3, "ot%d_%d" % (half, i), [128, D]) for i in range(2)]; B_ot = [Buf(), Buf()]
                    for tl in range(8):
                        t = half * 8 + tl
                        bi = tl % 2
                        kb.dma("sp", x1l[bi][:], x1_d[t * 128:(t + 1) * 128, :], writes=[B_x1l[bi]])
                        kb.op("dve", lambda e, tl=tl: e.tensor_tensor(out=yacc[:, tl, :], in0=yacc[:, tl, :], in1=g2b[:], op=ALU.mult),
                              reads=[B_y[tl], B_m3], writes=[B_y[tl]])
                        kb.op("pool", lambda e, tl=tl: e.tensor_tensor(out=yacc[:, tl, :], in0=yacc[:, tl, :], in1=x1l[bi][:], op=ALU.add),
                              reads=[B_y[tl], B_x1l[bi]], writes=[B_y[tl]])
                        rms_mod_tile(yacc[:, tl, :], B_y[tl], fgb[:], None, B_m3, junk3, B_j3, ssq3, B_s3, ot[bi], B_ot[bi], None, None)
                        kb.dma("sp", out_d[t * 128:(t + 1) * 128, :], ot[bi][:], reads=[B_ot[bi]])
                    kb.barrier()
        return nc

        return nc


def _host_consts():
    t = np.arange(128)
    ident = np.eye(128, dtype=np.float32)
    tri = (t[:, None] <= t[None, :]).astype(np.float32)
    triT = (t[:, None] >= t[None, :]).astype(np.float32)
    ones = np.ones((128, 128), np.float32)
    nmf = np.where(t[None, :] >= t[:, None], 0.0, NEG).astype(np.float32)
    nmb = np.where(t[None, :] <= t[:, None], 0.0, NEG).astype(np.float32)
    return np.stack([ident, tri, triT, ones, nmf, nmb])


def _rope_tables(s):
    pos = np.arange(-128, OWN + 128) + s * OWN
    pos = np.clip(pos, 0, SEQ - 1)
    rows = pos // 64
    cols = pos % 64
    inv = (10000.0 ** (-np.arange(0, 64, 2, dtype=np.float32) / 64)).astype(np.float32)
    ar = rows.astype(np.float32)[None, :] * inv[:, None]
    ac = cols.astype(np.float32)[None, :] * inv[:, None]
    cr, sr, cc, sc = np.cos(ar), np.sin(ar), np.cos(ac), np.sin(ac)
    C = np.concatenate([cr, cr, cc, cc], 0).astype(np.float32)
    S = np.concatenate([-sr, sr, -sc, sc], 0).astype(np.float32)
    return C, S


def _masks(s):
    import ml_dtypes
    j = np.arange(128)[:, None]
    r = np.arange(128)[None, :]
    lo = np.where(j >= r, 0.0, NEG).astype(np.float32)
    hi = np.where(j <= r, 0.0, NEG).astype(np.float32)
    allneg = np.full((128, 128), NEG, np.float32)
    lo0 = allneg if s == 0 else lo
    hi15 = allneg if s == 1 else hi
    m = np.stack([np.tile(a, (1, 4)) for a in (lo, hi, lo0, hi15)])
    return m.astype(ml_dtypes.bfloat16)


def _prep_inputs(inp):
    f = lambda a: np.ascontiguousarray(np.asarray(a, dtype=np.float32))
    x = f(inp["x"]); c = f(inp["c"]); ctx = f(inp["ctx"]); c_ctx = f(inp["c_ctx"])
    w_in = f(inp["w_in"][0])
    perm1 = np.concatenate([np.arange(32, 64), np.arange(0, 32), np.arange(96, 128), np.arange(64, 96)])
    perm = np.concatenate([h * 128 + perm1 for h in range(10)])
    w_in_sw = np.ascontiguousarray(w_in[:, :1280][:, perm])
    conv_w = f(inp["conv_w"][0])
    conv_wl = np.ascontiguousarray(conv_w.reshape(5, 16, 128).transpose(2, 1, 0))
    conv_bl = np.ascontiguousarray(f(inp["conv_b"][0]).reshape(16, 128).T)
    ew_gate = np.concatenate([f(inp["expert_w_gate"][0]), f(inp["shared_w_gate"][0])[None]], 0)
    ew_up = np.concatenate([f(inp["expert_w_up"][0]), f(inp["shared_w_up"][0])[None]], 0)
    ew_down = np.concatenate([f(inp["expert_w_down"][0]), f(inp["shared_w_down"][0])[None]], 0)
    consts = _host_consts()
    shared = dict(
        w_ada=f(inp["w_ada"][0]), b_ada=f(inp["b_ada"]), norm1_g=f(inp["norm1_g"]), norm2_g=f(inp["norm2_g"]),
        w_in=w_in, w_in_sw=w_in_sw, attn_sink=f(inp["attn_sink"]), conv_wl=conv_wl, conv_bl=conv_bl,
        dt_bias=f(inp["dt_bias"]).reshape(1, 32), a_log=f(inp["a_log"]).reshape(1, 32), d_skip=f(inp["d_skip"]),
        ssd_norm_g=f(inp["ssd_norm_g"]), w_out=f(inp["w_out"][0]), router_w=f(inp["router_w"][0]),
        router_bias=f(inp["router_bias"]), ew_gate=ew_gate, ew_up=ew_up, ew_down=ew_down,
        final_g=f(inp["final_norm_g"]).reshape(1, D), consts=consts)
    maps = []
    for core in range(8):
        b, s = core // 2, core % 2
        xo = x[b, s * OWN:(s + 1) * OWN]
        halo = np.zeros((2, 128, D), np.float32)
        if s == 1:
            halo[0] = x[b, OWN - 128:OWN]
        else:
            halo[1] = x[b, OWN:OWN + 128]
        cl = np.concatenate([c[b].reshape(16, 128).T, c_ctx.reshape(16, 128).T], 1)
        C, S = _rope_tables(s)
        fl = np.zeros((128, 4), np.float32)
        fl[:, 0] = s; fl[:, 1] = 1 - s; fl[:, 2] = 1.0 if s == 1 else 0.0; fl[:, 3] = 1.0 if s == 0 else 0.0
        dsel = 0 if s == 1 else 1
        xoth = x[b, (1 - s) * OWN:(2 - s) * OWN]
        xadj = xo[0:128] if s == 1 else xo[OWN - 128:OWN]
        tri, triT = consts[1], consts[2]
        extra = dict(x_oth=np.ascontiguousarray(xoth), x_adj=np.ascontiguousarray(xadj),
                     w_dt_sel=np.ascontiguousarray(w_in[:, 4608 + dsel * 16:4608 + dsel * 16 + 16]),
                     dtb_sel=np.ascontiguousarray(shared["dt_bias"][:, dsel * 16:(dsel + 1) * 16]),
                     alog_sel=np.ascontiguousarray(shared["a_log"][:, dsel * 16:(dsel + 1) * 16]),
                     tri_sel=np.ascontiguousarray(tri if dsel == 0 else triT))
        m = dict(shared)
        m.update(extra)
        m.update(x_own=np.ascontiguousarray(xo), x_halo=halo, ctx_b=np.ascontiguousarray(ctx[b]),
                 c_lay=np.ascontiguousarray(cl), rope_c=C, rope_s=S, cmask=_masks(s), flags=fl)
        maps.append(m)
    return maps


def kernel(**inputs):
    maps = _prep_inputs(inputs)
    nc = build()
    res = run_bass_kernel_spmd(nc, maps, core_ids=list(range(8)))
    out = np.zeros((NB, SEQ, D), np.float32)
    for core in range(8):
        b, s = core // 2, core % 2
        out[b, s * OWN:(s + 1) * OWN] = res.results[core]["out"]
    return out
```

```python
from contextlib import ExitStack

import numpy as np
import concourse.bass as bass
import concourse.mybir as mybir
from concourse.bass_utils import run_bass_kernel_spmd

F32 = mybir.dt.float32
BF16 = mybir.dt.bfloat16
AF = mybir.ActivationFunctionType
ALU = mybir.AluOpType
AX = mybir.AxisListType

D = 2048
SEQ = 4096
NB = 4
CTX = 256
OWN = 2048
NT = 16
INW = 4640
NEXP = 64
EPS = 1e-6
NEG = -30000.0
NSLOT = 8


class Tok:
    __slots__ = ("sem", "val", "ek")

    def __init__(self, sem, val, ek):
        self.sem, self.val, self.ek = sem, val, ek


class Buf:
    def __init__(self, name=""):
        self.name = name
        self.w = None
        self.r = {}


class Eng:
    def __init__(self, key, obj, sem):
        self.key, self.obj, self.sem = key, obj, sem
        self.count = 0
        self.pending = False
        self.seen = {}
        self.slots = []
        self.nslot = 0


class KB:
    def __init__(self, nc, es):
        self.nc = nc
        self.eng = {}
        for key, obj in (("pe", nc.tensor), ("dve", nc.vector), ("act", nc.scalar),
                         ("pool", nc.gpsimd), ("sp", nc.sync)):
            sem = es.enter_context(nc.semaphore("s_" + key))
            self.eng[key] = Eng(key, obj, sem)
        for key in ("sp", "pool", "act"):
            e = self.eng[key]
            for i in range(NSLOT):
                e.slots.append([es.enter_context(nc.semaphore("d_%s%d" % (key, i))), 0])

    def _wait(self, e, tok):
        if tok is None:
            return
        sid = id(tok.sem)
        if e.seen.get(sid, 0) >= tok.val:
            return
        e.obj.wait_ge(tok.sem, tok.val)
        e.seen[sid] = tok.val

    def _deps(self, e, reads, writes):
        for b in reads:
            if b.w is not None and not (b.w.ek == e.key and e.key == "pe"):
                self._wait(e, b.w)
        for b in writes:
            if b.w is not None and not (b.w.ek == e.key and e.key == "pe"):
                self._wait(e, b.w)
            for ek, t in b.r.items():
                if ek != e.key:
                    self._wait(e, t)

    def op(self, ek, fn, reads=(), writes=(), inc=True):
        e = self.eng[ek]
        self._deps(e, reads, writes)
        ins = fn(e.obj)
        if inc:
            ins.then_inc(e.sem, 1)
            e.count += 1
            e.pending = False
            tok = Tok(e.sem, e.count, ek)
        else:
            e.pending = True
            tok = Tok(e.sem, e.count + 1, ek)
        for b in writes:
            b.w = tok
            b.r = {}
        for b in reads:
            b.r[ek] = tok
        return ins

    def dma(self, qk, out, in_, reads=(), writes=(), **kw):
        e = self.eng[qk]
        slot = e.slots[e.nslot % NSLOT]
        e.nslot += 1
        if slot[1] > 0:
            self._wait(e, Tok(slot[0], 16 * slot[1], "dma"))
        self._deps(e, reads, writes)
        ins = e.obj.dma_start(out=out, in_=in_, **kw)
        ins.then_inc(slot[0], 16)
        slot[1] += 1
        tok = Tok(slot[0], 16 * slot[1], "dma_" + qk + str(id(slot[0])))
        for b in writes:
            b.w = tok
            b.r = {}
        for b in reads:
            b.r[tok.ek] = tok
        return ins

    def coll(self, fn, reads=(), writes=()):
        e = self.eng["pool"]
        slot = e.slots[e.nslot % NSLOT]
        e.nslot += 1
        if slot[1] > 0:
            self._wait(e, Tok(slot[0], 16 * slot[1], "dma"))
        self._deps(e, reads, writes)
        ins = fn(e.obj)
        ins.then_inc(slot[0], 16)
        slot[1] += 1
        tok = Tok(slot[0], 16 * slot[1], "dma_coll")
        for b in writes:
            b.w = tok
            b.r = {}
        for b in reads:
            b.r[tok.ek] = tok
        return ins

    def snapshot(self):
        for e in self.eng.values():
            assert not e.pending
        return {k: (e.count, [s[1] for s in e.slots], dict(e.seen)) for k, e in self.eng.items()}

    def compensate(self, snap):
        for k, e in self.eng.items():
            c0, uses0, seen0 = snap[k]
            assert not e.pending
            if e.count > c0:
                if c0 > 0:
                    e.obj.wait_ge(e.sem, c0)
                e.obj.sem_inc(e.sem, e.count - c0)
            for s, u0 in zip(e.slots, uses0):
                if s[1] > u0:
                    if u0 > 0:
                        e.obj.wait_ge(s[0], 16 * u0)
                    e.obj.sem_inc(s[0], 16 * (s[1] - u0))
            e.seen = dict(seen0)

    def barrier(self):
        toks = []
        for e in self.eng.values():
            assert not e.pending, e.key
            if e.count:
                toks.append(Tok(e.sem, e.count, e.key))
            for s in e.slots:
                if s[1]:
                    toks.append(Tok(s[0], 16 * s[1], "dma"))
        for e in self.eng.values():
            for t in toks:
                if t.ek == e.key:
                    continue
                self._wait(e, t)


def bcast_row(ap_row, n=128):
    return ap_row.partition_broadcast(n)


def build(stop_after=None, dbg=(), ncores=8, cut=99, cfg_nexp=NEXP + 1):
    nc = bass.Bass("TRN2", target_bir_lowering=False)
    dbg = set(dbg)

    class _Lazy:
        def __init__(self, name, shape, dt):
            self.name, self.shape, self.dt, self._ap = name, shape, dt, None

        @property
        def ap(self):
            if self._ap is None:
                self._ap = nc.dram_tensor(self.name, list(self.shape), self.dt, kind="ExternalInput").ap()
                used_inputs.append(self.name)
            return self._ap

        def __getitem__(self, key):
            return self.ap[key]

        def rearrange(self, *a, **k):
            return self.ap.rearrange(*a, **k)

    used_inputs = []
    nc._used_inputs = used_inputs

    def din(name, shape, dt=F32):
        return _Lazy(name, shape, dt)

    def dscr(name, shape, dt=F32):
        kind = "ExternalOutput" if name in dbg else "Internal"
        return nc.dram_tensor(name, list(shape), dt, kind=kind).ap()

    x_own = din("x_own", [OWN, D])
    x_halo = din("x_halo", [2, 128, D])
    ctx_in = din("ctx_b", [CTX, D])
    c_lay = din("c_lay", [128, 32])
    w_ada = din("w_ada", [D, 6 * D])
    b_ada = din("b_ada", [1, 6 * D])
    norm1_g = din("norm1_g", [1, D])
    norm2_g = din("norm2_g", [1, D])
    w_in = din("w_in", [D, INW])
    w_in_sw = din("w_in_sw", [D, 1280])
    attn_sink = din("attn_sink", [1, 8])
    conv_wl = din("conv_wl", [128, 16, 5])
    conv_bl = din("conv_bl", [128, 16])
    dt_bias = din("dt_bias", [1, 32])
    a_log = din("a_log", [1, 32])
    d_skip = din("d_skip", [1, 16])
    ssd_norm_g = din("ssd_norm_g", [1, 1024])
    w_out = din("w_out", [D, D])
    router_w = din("router_w", [D, NEXP])
    router_bias = din("router_bias", [1, NEXP])
    ew_gate = din("ew_gate", [NEXP + 1, D, 512])
    ew_up = din("ew_up", [NEXP + 1, D, 512])
    ew_down = din("ew_down", [NEXP + 1, 512, D])
    final_g = din("final_g", [1, D])
    rope_c = din("rope_c", [128, 2304])
    rope_s = din("rope_s", [128, 2304])
    cmask = din("cmask", [4, 128, 512], BF16)
    flags = din("flags", [128, 4])
    consts = din("consts", [6, 128, 128])
    x_oth = din("x_oth", [OWN, D])
    x_adj = din("x_adj", [128, D])
    w_dt_sel = din("w_dt_sel", [D, 16])
    dtb_sel = din("dtb_sel", [1, 16])
    alog_sel = din("alog_sel", [1, 16])
    tri_sel = din("tri_sel", [128, 128])
    ebase = din("ebase", [128, 2 * NEXP])
    lstrict = din("lstrict", [128, 128])
    oh2 = din("oh2", [128, NEXP + 1])

    out_d = nc.dram_tensor("out", [OWN, D], F32, kind="ExternalOutput").ap()

    mod_d = dscr("mod_d", [8, D])
    qT_d = dscr("qT_d", [8, 128, OWN], BF16)
    kT_d = dscr("kT_d", [2, 128, 2304], BF16)
    kcT_d = dscr("kcT_d", [2, 128, CTX], BF16)
    v_d = dscr("v_d", [20, 128, 256], BF16)
    zs_d = dscr("zs_d", [NT, 128, 1024])
    dtraw_d = dscr("dtraw_d", [18, 128, 32])
    uT_d = dscr("uT_d", [16, 128, OWN], BF16)
    uTc_d = dscr("uTc_d", [16, 128, CTX], BF16)
    mixT_d = dscr("mixT_d", [16, 128, OWN], BF16)
    uT2_d = dscr("uT2_d", [12, 128, OWN], BF16)
    dtraw2_d = dscr("dtraw2_d", [NT, 128, 16])
    h0_d = dscr("h0_d", [2, NT, 128, 1024])
    xch_in = dscr("xch_in", [128, 2112])
    xch_out = dscr("xch_out", [256, 2112])
    x1_d = dscr("x1_d", [OWN, D])
    h2T_d = dscr("h2T_d", [16, 128, OWN], BF16)
    gate_d = dscr("gate_d", [NT, 128, NEXP + 1])
    I32 = mybir.dt.int32
    NSL = NEXP * OWN
    h2tok_d = dscr("h2tok_d", [OWN + 128, D], BF16)
    idxtab_d = dscr("idxtab_d", [NSL, 1], I32)
    dest_d = dscr("dest_d", [NT, 128, 8], I32)
    w8_d = dscr("w8_d", [NT, 128, 8])
    nblk_d = dscr("nblk_d", [1, NEXP], I32)
    NSLC = 192 * 128
    SHR0 = NSLC + OWN
    slot_d = dscr("slot_d", [SHR0 + OWN, D])
    cbase_d = dscr("cbase_d", [1, NEXP], I32)
    rowtab_d = dscr("rowtab_d", [NSL, 1], I32)

    es = ExitStack()
    with es:
        kb = KB(nc, es)

        def sbt(st, name, shape, dt=F32):
            return st.enter_context(nc.sbuf_tensor(name, list(shape), dt))

        def pst(st, name, shape, dt=F32):
            return st.enter_context(nc.psum_tensor(name, list(shape), dt))

        ident_f = sbt(es, "ident_f", [128, 128]); ones_f = sbt(es, "ones_f", [128, 128])
        ident_b = sbt(es, "ident_b", [128, 128], BF16); ones_b = sbt(es, "ones_b", [128, 128], BF16)
        flg = sbt(es, "flg", [128, 4])
        B_const = Buf("const")
        for i, t in ((0, ident_f), (3, ones_f)):
            kb.dma("sp", t[:], consts[i], writes=[B_const])
        kb.dma("sp", flg[:], flags[:, :], writes=[B_const])
        kb.op("dve", lambda e: e.tensor_copy(out=ident_b[:], in_=ident_f[:]), reads=[B_const], writes=[B_const])
        kb.op("dve", lambda e: e.tensor_copy(out=ones_b[:], in_=ones_f[:]), reads=[B_const], writes=[B_const])
        kb.barrier()

        with ExitStack() as ph:
            cl = sbt(ph, "cl", [128, 32]); cs = sbt(ph, "cs", [128, 32])
            LC = sbt(ph, "LC", [128, 32, 128], BF16)
            Wb = [sbt(ph, "Wa%d" % i, [128, 16, 512], BF16) for i in range(2)]
            bb = [sbt(ph, "ba%d" % i, [128, 512]) for i in range(2)]
            mo = [sbt(ph, "mo%d" % i, [128, 512]) for i in range(4)]
            psA = [pst(ph, "psA%d" % i, [128, 512]) for i in range(4)]
            B_cl, B_LC = Buf(), Buf()
            B_W = [Buf(), Buf()]; B_b = [Buf(), Buf()]; B_mo = [Buf() for _ in range(4)]
            B_ps = [Buf() for _ in range(4)]
            kb.dma("sp", cl[:], c_lay[:, :], writes=[B_cl])
            kb.op("act", lambda e: e.activation(out=cs[:], in_=cl[:], func=AF.Silu), reads=[B_cl], writes=[B_cl])
            for k in range(32):
                kb.op("dve", lambda e, k=k: e.tensor_copy(out=LC[:, k, :], in_=cs[:, k:k + 1].to_broadcast([128, 128])),
                      reads=[B_cl], writes=[B_LC])
            wv = w_ada.rearrange("(k p) n -> p k n", p=128)
            nblk = 24
            it = 0
            for j in range(nblk):
                bi = j % 2
                kb.dma("pool", Wb[bi][:], wv[:, :, j * 512:(j + 1) * 512], writes=[B_W[bi]])
                kb.dma("sp", bb[bi][:], bcast_row(b_ada[0:1, j * 512:(j + 1) * 512]), writes=[B_b[bi]])
                chunk = j // 4
                for which in range(2 if j < 8 else 1):
                    pi = it % 4
                    it += 1
                    for k in range(16):
                        kb.op("pe", lambda e, k=k, pi=pi, which=which, bi=bi: e.matmul(
                            psA[pi][:], lhsT=LC[:, which * 16 + k, :], rhs=Wb[bi][:, k, :],
                            start=(k == 0), stop=(k == 15)),
                            reads=[B_LC, B_W[bi]], writes=[B_ps[pi]], inc=(k == 15))
                    if chunk in (1, 4):
                        kb.op("dve", lambda e, pi=pi, bi=bi: e.scalar_tensor_tensor(
                            out=mo[pi][:], in0=psA[pi][:], scalar=1.0, in1=bb[bi][:], op0=ALU.add, op1=ALU.add),
                            reads=[B_ps[pi], B_b[bi]], writes=[B_mo[pi]])
                    else:
                        kb.op("dve", lambda e, pi=pi, bi=bi: e.tensor_tensor(
                            out=mo[pi][:], in0=psA[pi][:], in1=bb[bi][:], op=ALU.add),
                            reads=[B_ps[pi], B_b[bi]], writes=[B_mo[pi]])
                    row = chunk if which == 0 else 6 + chunk
                    c0 = (j % 4) * 512
                    kb.dma("sp", mod_d[row:row + 1, c0:c0 + 512], mo[pi][0:1, :], reads=[B_mo[pi]])
            kb.barrier()
        if stop_after == "A":
            return nc

        def load_bc(st, qk, name, row_ap, width, buf):
            t = sbt(st, name, [128, width])
            kb.dma(qk, t[:], bcast_row(row_ap), writes=[buf])
            return t

        def rms_mod_tile(xt, B_x, G, shv, B_mod, junk, B_junk, ssq, B_ss, tmp, B_tmp, outb, B_out, width=D):
            kb.op("act", lambda e: e.activation(out=junk[:], in_=xt, func=AF.Square, accum_out=ssq[:, 0:1]),
                  reads=[B_x], writes=[B_junk, B_ss])
            kb.op("dve", lambda e: e.tensor_scalar(out=ssq[:, 1:2], in0=ssq[:, 0:1], scalar1=1.0 / width, scalar2=EPS,
                                                   op0=ALU.mult, op1=ALU.add), reads=[B_ss], writes=[B_ss])
            kb.op("act", lambda e: e.sqrt(out=ssq[:, 3:4], in_=ssq[:, 1:2]), reads=[B_ss], writes=[B_ss])
            kb.op("dve", lambda e: e.reciprocal(out=ssq[:, 2:3], in_=ssq[:, 3:4]), reads=[B_ss], writes=[B_ss])
            kb.op("dve", lambda e: e.scalar_tensor_tensor(out=tmp[:], in0=xt, scalar=ssq[:, 2:3], in1=G,
                                                          op0=ALU.mult, op1=ALU.mult),
                  reads=[B_x, B_ss, B_mod], writes=[B_tmp])
            if shv is not None:
                kb.op("dve", lambda e: e.tensor_tensor(out=outb, in0=tmp[:], in1=shv, op=ALU.add),
                      reads=[B_tmp, B_mod], writes=[B_out])

        def phase_BC(tag, cfg):
          with ExitStack() as phBC:
              ntile = cfg["ntile"]
              hT = sbt(phBC, "hT" + tag, [128, ntile, 16, 128], BF16)
              B_hT = [Buf("hT%d" % i) for i in range(ntile)]
              with ExitStack() as ph:
                  B_mod = Buf("mod1")
                  g1n = load_bc(ph, "sp", "g1n" + tag, norm1_g[0:1, :], D, B_mod)
                  GL = load_bc(ph, "sp", "GL" + tag, mod_d[1:2, :], D, B_mod)
                  SHL = load_bc(ph, "sp", "SHL" + tag, mod_d[0:1, :], D, B_mod)
                  GC = load_bc(ph, "sp", "GC" + tag, mod_d[7:8, :], D, B_mod)
                  SHC = load_bc(ph, "sp", "SHC" + tag, mod_d[6:7, :], D, B_mod)
                  kb.op("dve", lambda e: e.tensor_tensor(out=GL[:], in0=GL[:], in1=g1n[:], op=ALU.mult),
                        reads=[B_mod], writes=[B_mod])
                  kb.op("dve", lambda e: e.tensor_tensor(out=GC[:], in0=GC[:], in1=g1n[:], op=ALU.mult),
                        reads=[B_mod], writes=[B_mod])
                  xt = [sbt(ph, "xt%d" % i + tag, [128, D]) for i in range(2)]
                  B_x = [Buf(), Buf()]
                  junk = sbt(ph, "junk" + tag, [128, D], BF16); B_junk = Buf()
                  tmp = sbt(ph, "tmpB" + tag, [128, D]); B_tmp = Buf()
                  hb = [sbt(ph, "hb%d" % i + tag, [128, D], BF16) for i in range(2)]
                  B_hb = [Buf(), Buf()]
                  ssq = [sbt(ph, "ssq%d" % i + tag, [128, 4]) for i in range(2)]
                  B_ss = [Buf(), Buf()]
                  psT = [pst(ph, "psT%d" % i + tag, [128, 8, 128], BF16) for i in range(2)]
                  B_psT = [Buf(), Buf()]

                  src_of = cfg["src_of"]

                  kb.dma("sp", xt[0][:], src_of(0), writes=[B_x[0]])
                  for t in range(ntile):
                      bi = t % 2
                      if t + 1 < ntile:
                          kb.dma("sp", xt[1 - bi][:], src_of(t + 1), writes=[B_x[1 - bi]])
                      G, SHv = (GC, SHC) if t < cfg["nctx"] else (GL, SHL)
                      rms_mod_tile(xt[bi][:], B_x[bi], G[:], SHv[:], B_mod, junk, B_junk, ssq[bi], B_ss[bi],
                                   tmp, B_tmp, hb[bi][:], B_hb[bi])
                      for half in range(2):
                          for kk in range(8):
                              k = half * 8 + kk
                              kb.op("pe", lambda e, k=k, kk=kk, half=half, bi=bi: e.transpose(
                                  out=psT[half][:, kk, :], in_=hb[bi][:, k * 128:(k + 1) * 128], identity=ident_b[:]),
                                  reads=[B_hb[bi]], writes=[B_psT[half]], inc=(kk == 7))
                          eng = "act" if half == 0 else "dve"
                          kb.op(eng, lambda e, t=t, half=half: e.tensor_copy(out=hT[:, t, half * 8:(half + 1) * 8, :],
                                                                            in_=psT[half][:]) if eng != "act" else
                                e.copy(out=hT[:, t, half * 8:(half + 1) * 8, :], in_=psT[half][:]),
                                reads=[B_psT[half]], writes=[B_hT[t]])
                  kb.barrier()
              with ExitStack() as ph:
                  wv = w_in.rearrange("(k p) n -> p k n", p=128)
                  wsv = w_in_sw.rearrange("(k p) n -> p k n", p=128)
                  Wn = [sbt(ph, "Wn%d" % i + tag, [128, 16, 512], BF16) for i in range(2)]
                  Ws = [sbt(ph, "Ws%d" % i + tag, [128, 16, 128], BF16) for i in range(2)]
                  B_Wn = [Buf(), Buf()]; B_Ws = [Buf(), Buf()]
                  B_cst = Buf()
                  rc = sbt(ph, "rc" + tag, [128, 2304]); rs = sbt(ph, "rs" + tag, [128, 2304])
                  kb.dma("sp", rc[:], rope_c[:, :], writes=[B_cst])
                  kb.dma("sp", rs[:], rope_s[:, :], writes=[B_cst])
                  cw = sbt(ph, "cw" + tag, [128, 16, 5]); cb = sbt(ph, "cb" + tag, [128, 16])
                  kb.dma("sp", cw[:], conv_wl[:, :, :], writes=[B_cst])
                  kb.dma("sp", cb[:], conv_bl[:, :], writes=[B_cst])
                  psC = [pst(ph, "psC%d" % i + tag, [128, 512]) for i in range(4)]
                  B_psC = [Buf() for _ in range(4)]
                  ev = [sbt(ph, "ev%d" % i + tag, [128, 512]) for i in range(4)]
                  B_ev = [Buf() for _ in range(4)]
                  evb = [sbt(ph, "evb%d" % i + tag, [128, 512], BF16) for i in range(2)]
                  B_evb = [Buf(), Buf()]
                  v_all = sbt(ph, "v_all" + tag, [128, 20, 256], BF16); B_vall = Buf()
                  dt_all = sbt(ph, "dt_all" + tag, [128, 18, 32]); B_dtall = Buf()
                  xraw = [sbt(ph, "xraw%d" % i + tag, [128, 2052]) for i in range(2)]
                  B_xraw = [Buf(), Buf()]
                  xrawc = [sbt(ph, "xrawc%d" % i + tag, [128, 260]) for i in range(2)]
                  B_xrawc = [Buf(), Buf()]
                  acc0 = sbt(ph, "acc0" + tag, [128, 2048])
                  acc = [acc0, acc0]
                  B_acc0 = Buf()
                  B_acc = [B_acc0, B_acc0]
                  accc = sbt(ph, "accc" + tag, [128, 256]); B_accc = Buf()
                  uo = [sbt(ph, "uo%d" % i + tag, [128, 2048], BF16) for i in range(2)]
                  B_uo = [Buf(), Buf()]
                  uoc = [sbt(ph, "uoc%d" % i + tag, [128, 256], BF16) for i in range(2)]
                  B_uoc = [Buf(), Buf()]
                  for i in range(2):
                      kb.op("pool", lambda e, i=i: e.memset(xrawc[i][:], 0.0), writes=[B_xrawc[i]])

                  jobs = cfg["jobs"]

                  def load_job(ji):
                      kind, c0, ncol, idx = jobs[ji]
                      bi = ji % 2
                      wsrc = cfg["wdt"] if (kind == "dt" and cfg.get("wdt") is not None) else wv[:, :, c0:c0 + ncol]
                      kb.dma("pool", Wn[bi][:, :, 0:ncol], wsrc, writes=[B_Wn[bi]])
                      if kind in ("q", "k"):
                          kb.dma("pool", Ws[bi][:], wsv[:, :, c0:c0 + 128], writes=[B_Ws[bi]])

                  pctr = [0]

                  def fm_mm(W, B_W, tile0, ntile, tok_lo=0, tok_hi=128):
                      pi = pctr[0] % 4
                      pctr[0] += 1
                      n = ntile * (tok_hi - tok_lo)
                      for k in range(16):
                          kb.op("pe", lambda e, k=k: e.matmul(
                              psC[pi][:, 0:n], lhsT=W[:, k, 0:128], rhs=hT[:, tile0:tile0 + ntile, k, tok_lo:tok_hi],
                              start=(k == 0), stop=(k == 15)),
                              reads=[B_W] + B_hT[tile0:tile0 + ntile], writes=[B_psC[pi]], inc=(k == 15))
                      return pi

                  def tm_mm(W, B_W, t, ncol):
                      pi = pctr[0] % 4
                      pctr[0] += 1
                      for k in range(16):
                          kb.op("pe", lambda e, k=k: e.matmul(
                              psC[pi][:, 0:ncol], lhsT=hT[:, t, k, :], rhs=W[:, k, 0:ncol],
                              start=(k == 0), stop=(k == 15)),
                              reads=[B_W, B_hT[t]], writes=[B_psC[pi]], inc=(k == 15))
                      return pi

                  ectr = [0]
                  load_job(0)
                  for ji, (kind, c0, ncol, idx) in enumerate(jobs):
                      if ji + 1 < len(jobs):
                          load_job(ji + 1)
                      bi = ji % 2
                      W, BW = Wn[bi], B_Wn[bi]
                      if kind in ("q", "k"):
                          if kind == "q":
                              groups = [(3 + 4 * g, 4, 128 + g * 512, g * 512) for g in range(4)]
                              dest = qT_d[idx]
                          else:
                              groups = [(2 + 4 * g, 4, g * 512, g * 512) for g in range(4)] + [(18, 2, 2048, 2048)]
                              dest = kT_d[idx]
                          for (t0, ntl, roff, doff) in groups:
                              n = ntl * 128
                              pa = fm_mm(W, BW, t0, ntl)
                              pb = fm_mm(Ws[bi], B_Ws[bi], t0, ntl)
                              e0, e1 = (ectr[0] % 2) * 2, (ectr[0] % 2) * 2 + 1
                              eb = ectr[0] % 2
                              ectr[0] += 1
                              kb.op("dve", lambda e: e.tensor_tensor(out=ev[e0][:, 0:n], in0=psC[pa][:, 0:n],
                                                                     in1=rc[:, roff:roff + n], op=ALU.mult),
                                    reads=[B_psC[pa], B_cst], writes=[B_ev[e0]])
                              kb.op("dve", lambda e: e.tensor_tensor(out=ev[e1][:, 0:n], in0=psC[pb][:, 0:n],
                                                                     in1=rs[:, roff:roff + n], op=ALU.mult),
                                    reads=[B_psC[pb], B_cst], writes=[B_ev[e1]])
                              kb.op("pool", lambda e: e.tensor_tensor(out=evb[eb][:, 0:n], in0=ev[e0][:, 0:n],
                                                                      in1=ev[e1][:, 0:n], op=ALU.add),
                                    reads=[B_ev[e0], B_ev[e1]], writes=[B_evb[eb]])
                              kb.dma("sp", dest[:, doff:doff + n], evb[eb][:, 0:n], reads=[B_evb[eb]])
                          if kind == "k":
                              pa = fm_mm(W, BW, 0, 2)
                              eb = ectr[0] % 2
                              ectr[0] += 1
                              kb.op("act", lambda e: e.copy(out=evb[eb][:, 0:256], in_=psC[pa][:, 0:256]),
                                    reads=[B_psC[pa]], writes=[B_evb[eb]])
                              kb.dma("sp", kcT_d[idx], evb[eb][:, 0:256], reads=[B_evb[eb]])
                      elif kind == "v":
                          for t in range(20):
                              pa = tm_mm(W, BW, t, 256)
                              kb.op("act", lambda e, t=t: e.copy(out=v_all[:, t, :], in_=psC[pa][:, 0:256]),
                                    reads=[B_psC[pa]], writes=[B_vall])
                          kb.dma("sp", v_d.rearrange("t p c -> p t c"), v_all[:], reads=[B_vall])
                      elif kind == "z":
                          for t in range(NT):
                              pa = tm_mm(W, BW, 3 + t, 512)
                              e0 = ectr[0] % 4
                              ectr[0] += 1
                              kb.op("act", lambda e: e.activation(out=ev[e0][:], in_=psC[pa][:], func=AF.Silu),
                                    reads=[B_psC[pa]], writes=[B_ev[e0]])
                              kb.dma("sp", zs_d[t][:, idx * 512:(idx + 1) * 512], ev[e0][:], reads=[B_ev[e0]])
                      elif kind == "dt":
                          dtt = cfg["dt_tiles"]
                          for i, t in enumerate(dtt):
                              pa = tm_mm(W, BW, t, ncol)
                              kb.op("act", lambda e, i=i: e.copy(out=dt_all[:, i, 0:ncol], in_=psC[pa][:, 0:ncol]),
                                    reads=[B_psC[pa]], writes=[B_dtall])
                          kb.dma("sp", cfg["dt_dst"].rearrange("t p c -> p t c"), dt_all[:, 0:len(dtt), 0:ncol], reads=[B_dtall])
                      else:
                          j = idx
                          xi = j % 2
                          xr, Bxr = xraw[xi], B_xraw[xi]
                          for g in range(4):
                              pa = fm_mm(W, BW, cfg["own0"] + 4 * g, 4)
                              kb.op("act", lambda e, g=g: e.copy(out=xr[:, 2 + g * 512:2 + (g + 1) * 512], in_=psC[pa][:]),
                                    reads=[B_psC[pa]], writes=[Bxr])
                          pa = fm_mm(W, BW, cfg["halo_lo"], 1, 126, 128)
                          kb.op("dve", lambda e: e.tensor_scalar(out=xr[:, 0:2], in0=psC[pa][:, 0:2], scalar1=cfg["fl_lo"],
                                                                 scalar2=None, op0=ALU.mult),
                                reads=[B_psC[pa]], writes=[Bxr])
                          pa = fm_mm(W, BW, cfg["halo_hi"], 1, 0, 2)
                          kb.op("dve", lambda e: e.tensor_scalar(out=xr[:, 2050:2052], in0=psC[pa][:, 0:2],
                                                                 scalar1=cfg["fl_hi"], scalar2=None, op0=ALU.mult),
                                reads=[B_psC[pa]], writes=[Bxr])
                          if cfg["nctx"]:
                              pa = fm_mm(W, BW, 0, 2)
                              kb.op("act", lambda e: e.copy(out=xrawc[xi][:, 2:258], in_=psC[pa][:, 0:256]),
                                    reads=[B_psC[pa]], writes=[B_xrawc[xi]])
                          convs = [(xr, Bxr, acc[xi], B_acc[xi], 2048, uo[xi], B_uo[xi], cfg["uT_dst"][j])]
                          if cfg["nctx"]:
                              convs.append((xrawc[xi], B_xrawc[xi], accc, B_accc, 256, uoc[xi], B_uoc[xi], uTc_d[j]))
                          for (src, Bs, dst, Bd, n, o, Bo, dd) in convs:
                              kb.op("dve", lambda e: e.tensor_scalar(out=dst[:, 0:n], in0=src[:, 0:n], scalar1=cw[:, j, 0:1],
                                                                     scalar2=None, op0=ALU.mult),
                                    reads=[Bs, B_cst], writes=[Bd])
                              for tap in range(1, 5):
                                  kb.op("dve", lambda e, tap=tap: e.scalar_tensor_tensor(
                                      out=dst[:, 0:n], in0=src[:, tap:tap + n], scalar=cw[:, j, tap:tap + 1],
                                      in1=dst[:, 0:n], op0=ALU.mult, op1=ALU.add),
                                      reads=[Bs, B_cst, Bd], writes=[Bd])
                              kb.op("act", lambda e: e.activation(out=o[:, 0:n], in_=dst[:, 0:n], func=AF.Silu,
                                                                  bias=cb[:, j:j + 1]),
                                    reads=[Bd, B_cst], writes=[Bo])
                              kb.dma("sp", dd, o[:, 0:n], reads=[Bo])
                  kb.barrier()
        def main_src(t):
            if t < 2:
                return ctx_in[t * 128:(t + 1) * 128, :]
            if t == 2:
                return x_halo[0]
            if t == 19:
                return x_halo[1]
            return x_own[(t - 3) * 128:(t - 2) * 128, :]

        jobs_main = []
        for h in range(8):
            jobs_main.append(("q", h * 128, 128, h))
        for kv in range(2):
            jobs_main.append(("k", 1024 + kv * 128, 128, kv))
        jobs_main.append(("v", 1280, 256, 0))
        jobs_main.append(("z", 1536, 512, 0))
        jobs_main.append(("z", 2048, 512, 1))
        jobs_main.append(("dt", 4608, 32, 0))
        for j in range(16):
            jobs_main.append(("x", 2560 + j * 128, 128, j))
        phase_BC("m", dict(ntile=20, nctx=2, src_of=main_src, jobs=jobs_main, own0=3, halo_lo=2, halo_hi=19,
                           fl_lo=flg[:, 2:3], fl_hi=flg[:, 3:4], uT_dst=uT_d, dt_tiles=[0, 1] + list(range(3, 19)),
                           dt_dst=dtraw_d, wdt=None))
        if stop_after == "C":
            return nc

        def oth_src(t):
            if t == 0:
                return x_adj[:, :]
            return x_oth[(t - 1) * 128:t * 128, :]

        jobs_oth = [("dt", 0, 16, 0)] + [("x", 2560 + j * 128, 128, j) for j in range(12)]
        phase_BC("o", dict(ntile=17, nctx=0, src_of=oth_src, jobs=jobs_oth, own0=1, halo_lo=0, halo_hi=0,
                           fl_lo=flg[:, 3:4], fl_hi=flg[:, 2:3], uT_dst=uT2_d, dt_tiles=list(range(1, 17)),
                           dt_dst=dtraw2_d, wdt=w_dt_sel.rearrange("(k p) n -> p k n", p=128)))
        if stop_after == "C2":
            return nc

        with ExitStack() as ph:
            qT = sbt(ph, "qT", [128, 8, OWN], BF16)
            kT = sbt(ph, "kT", [128, 2, 2304], BF16)
            kcT = sbt(ph, "kcT", [128, 2, CTX], BF16)
            vv = sbt(ph, "vv", [128, 20, 256], BF16)
            msk = sbt(ph, "msk", [128, 4, 512], BF16)
            esk = sbt(ph, "esk", [128, 8])
            B_in = Buf()
            kb.dma("sp", qT[:], qT_d.rearrange("h p t -> p h t"), writes=[B_in])
            kb.dma("sp", kT[:], kT_d.rearrange("h p t -> p h t"), writes=[B_in])
            kb.dma("sp", kcT[:], kcT_d.rearrange("h p t -> p h t"), writes=[B_in])
            kb.dma("sp", vv[:], v_d.rearrange("t p c -> p t c"), writes=[B_in])
            kb.dma("sp", msk[:], cmask.rearrange("m p c -> p m c"), writes=[B_in])
            kb.dma("sp", esk[:], bcast_row(attn_sink[0:1, :]), writes=[B_in])
            kb.op("act", lambda e: e.activation(out=esk[:], in_=esk[:], func=AF.Exp), reads=[B_in], writes=[B_in])
            psS = [pst(ph, "psS%d" % i, [128, 512]) for i in range(2)]
            psO = [pst(ph, "psO%d" % i, [128, 512]) for i in range(2)]
            psD = [pst(ph, "psD%d" % i, [128, 512]) for i in range(2)]
            B_psS = [Buf(), Buf()]; B_psO = [Buf(), Buf()]; B_psD = [Buf(), Buf()]
            Eb = [sbt(ph, "Eb%d" % i, [128, 512], BF16) for i in range(3)]
            B_E = [Buf() for _ in range(3)]
            den = [sbt(ph, "den%d" % i, [128, 512]) for i in range(2)]
            B_den = [Buf(), Buf()]
            ob = [sbt(ph, "ob%d" % i, [128, 512], BF16) for i in range(2)]
            B_ob = [Buf(), Buf()]
            scale = 128 ** -0.5
            sctr = 0
            ectr_ = 0
            for i in range(NT):
                for kv in range(2):
                    pi = (i * 2 + kv) % 2
                    rhs_q = qT[:, 4 * kv:4 * kv + 4, i * 128:(i + 1) * 128]
                    keyt = []
                    for j in range(3):
                        m = None
                        if j == 0:
                            m = 2 if i == 0 else 0
                        if j == 2:
                            m = 3 if i == NT - 1 else 1
                        keyt.append((kT[:, kv, (i + j) * 128:(i + j + 1) * 128], 2 + i + j, m))
                    for cc in range(2):
                        keyt.append((kcT[:, kv, cc * 128:(cc + 1) * 128], cc, None))
                    for n, (kap, vt, m) in enumerate(keyt):
                        si = sctr % 2
                        sctr += 1
                        ei = ectr_ % 3
                        ectr_ += 1
                        kb.op("pe", lambda e: e.matmul(psS[si][:], lhsT=kap, rhs=rhs_q, start=True, stop=(m is None)),
                              reads=[B_in], writes=[B_psS[si]], inc=(m is None))
                        if m is not None:
                            kb.op("pe", lambda e: e.matmul(psS[si][:], lhsT=ident_b[:], rhs=msk[:, m, :], start=False, stop=True),
                                  reads=[B_in], writes=[B_psS[si]])
                        kb.op("act", lambda e: e.activation(out=Eb[ei][:], in_=psS[si][:], func=AF.Exp, scale=scale),
                              reads=[B_psS[si]], writes=[B_E[ei]])
                        kb.op("pe", lambda e: e.matmul(psO[pi][:], lhsT=vv[:, vt, kv * 128:(kv + 1) * 128], rhs=Eb[ei][:],
                                                       start=(n == 0), stop=(n == 4)),
                              reads=[B_in, B_E[ei]], writes=[B_psO[pi]], inc=False)
                        kb.op("pe", lambda e: e.matmul(psD[pi][:], lhsT=ones_b[:], rhs=Eb[ei][:],
                                                       start=(n == 0), stop=(n == 4)),
                              reads=[B_E[ei]], writes=[B_psD[pi]], inc=True)
                    kb.op("dve", lambda e: e.tensor_tensor(
                        out=den[pi][:].rearrange("p (h q) -> p h q", h=4),
                        in0=psD[pi][:].rearrange("p (h q) -> p h q", h=4),
                        in1=esk[:, 4 * kv:4 * kv + 4].unsqueeze(2).to_broadcast([128, 4, 128]), op=ALU.add),
                        reads=[B_psD[pi], B_in], writes=[B_den[pi]])
                    kb.op("dve", lambda e: e.reciprocal(out=den[pi][:], in_=den[pi][:]), reads=[B_den[pi]], writes=[B_den[pi]])
                    kb.op("dve", lambda e: e.tensor_tensor(out=ob[pi][:], in0=psO[pi][:], in1=den[pi][:], op=ALU.mult),
                          reads=[B_psO[pi], B_den[pi]], writes=[B_ob[pi]])
                    kb.dma("sp", mixT_d[4 * kv:4 * kv + 4, :, i * 128:(i + 1) * 128].rearrange("h p t -> p h t"),
                           ob[pi][:].rearrange("p (h q) -> p h q", h=4), reads=[B_ob[pi]])
            kb.barrier()
        if stop_after == "D":
            return nc

        with ExitStack() as phE:
            uTbc = sbt(phE, "uTbc", [128, 8, OWN], BF16)
            B_u = Buf()
            tri_f = sbt(phE, "tri_f", [128, 128]); triT_f = sbt(phE, "triT_f", [128, 128])
            nmf_f = sbt(phE, "nmf_f", [128, 128]); nmb_f = sbt(phE, "nmb_f", [128, 128])
            B_cE = Buf()
            for i, t in ((1, tri_f), (2, triT_f), (4, nmf_f), (5, nmb_f)):
                kb.dma("sp", t[:], consts[i], writes=[B_cE])
            for j in range(8):
                kb.dma("sp", uTbc[:, j, :], uT_d[8 + j], writes=[B_u])
            dtr = sbt(phE, "dtr", [128, 18, 32]); dtv = sbt(phE, "dtv", [128, 18, 32]); dtA = sbt(phE, "dtA", [128, 18, 32])
            dtb = sbt(phE, "dtb", [128, 32]); alg = sbt(phE, "alg", [128, 32]); dsk = sbt(phE, "dsk", [128, 16])
            gssd = sbt(phE, "gssd", [128, 1024])
            B_dt = Buf()
            kb.dma("sp", dtr[:], dtraw_d.rearrange("t p c -> p t c"), writes=[B_dt])
            kb.dma("sp", dtb[:], bcast_row(dt_bias[0:1, :]), writes=[B_dt])
            kb.dma("sp", alg[:], bcast_row(a_log[0:1, :]), writes=[B_dt])
            kb.dma("sp", dsk[:], bcast_row(d_skip[0:1, :]), writes=[B_dt])
            kb.dma("sp", gssd[:], bcast_row(ssd_norm_g[0:1, :]), writes=[B_dt])
            kb.op("dve", lambda e: e.tensor_tensor(out=dtr[:], in0=dtr[:], in1=dtb[:].unsqueeze(1).to_broadcast([128, 18, 32]),
                                                   op=ALU.add), reads=[B_dt, B_cE], writes=[B_dt])
            kb.op("act", lambda e: e.activation(out=dtr[:], in_=dtr[:], func=AF.Exp), reads=[B_dt], writes=[B_dt])
            kb.op("act", lambda e: e.activation(out=dtv[:], in_=dtr[:], func=AF.Ln, bias=ones_f[:, 0:1]), reads=[B_dt], writes=[B_dt])
            kb.op("act", lambda e: e.activation(out=alg[:], in_=alg[:], func=AF.Exp), reads=[B_dt], writes=[B_dt])
            kb.op("dve", lambda e: e.scalar_tensor_tensor(out=dtA[:], in0=dtv[:], scalar=-1.0,
                                                          in1=alg[:].unsqueeze(1).to_broadcast([128, 18, 32]),
                                                          op0=ALU.mult, op1=ALU.mult), reads=[B_dt], writes=[B_dt])
            xs_tok = sbt(phE, "xs_tok", [128, 18, 1024], BF16)
            B_xs = [Buf() for _ in range(18)]
            eac = sbt(phE, "eac", [128, NT, 32]); B_eac = Buf()
            Pall = sbt(phE, "Pall", [128, 2, 18, 16]); B_P = Buf()
            hc = sbt(phE, "hc", [128, 2, 1024]); B_hc = Buf()
            hinit = sbt(phE, "hinit", [128, 2, 1024]); B_hi = Buf()
            B_h0 = [[Buf() for _ in range(NT)] for _ in range(2)]

            with ExitStack() as ph:
                uTx = sbt(ph, "uTx", [128, 8, OWN], BF16)
                uTc = sbt(ph, "uTc", [128, 16, CTX], BF16)
                bm_tok = sbt(ph, "bm_tok", [128, 18, 512], BF16)
                for j in range(8):
                    kb.dma("sp", uTx[:, j, :], uT_d[j], writes=[B_u])
                kb.dma("sp", uTc[:], uTc_d.rearrange("j p t -> p j t"), writes=[B_u])
                psT1 = pst(ph, "psTE1", [128, 8, 128], BF16); B_psT1 = Buf()
                psT2 = pst(ph, "psTE2", [128, 4, 128], BF16); B_psT2 = Buf()
                sm_ps = pst(ph, "sm_ps", [128, 32]); B_smps = Buf()
                S_ps = pst(ph, "S_ps", [128, 1024]); B_Sps = Buf()
                sm = sbt(ph, "sm", [128, 64]); B_sm = Buf()
                xdd = [sbt(ph, "xdd%d" % i, [128, 16, 64], BF16) for i in range(2)]
                B_xdd = [Buf(), Buf()]
                Hb = [sbt(ph, "Hb%d" % i, [128, 1024]) for i in range(2)]
                B_H = [Buf(), Buf()]
                for ti in range(18):
                    if ti < 2:
                        srcs = lambda j, ti=ti: uTc[:, j, ti * 128:(ti + 1) * 128]
                    else:
                        srcs = lambda j, ti=ti: (uTx[:, j, (ti - 2) * 128:(ti - 1) * 128] if j < 8
                                                 else uTbc[:, j - 8, (ti - 2) * 128:(ti - 1) * 128])
                    for j in range(8):
                        kb.op("pe", lambda e, j=j: e.transpose(out=psT1[:, j, :], in_=srcs(j), identity=ident_b[:]),
                              reads=[B_u], writes=[B_psT1], inc=(j == 7))
                    kb.op("act", lambda e, ti=ti: e.copy(out=xs_tok[:, ti, :], in_=psT1[:].rearrange("p a b -> p (a b)")),
                          reads=[B_psT1], writes=[B_xs[ti]])
                    for j in range(4):
                        kb.op("pe", lambda e, j=j: e.transpose(out=psT2[:, j, :], in_=srcs(8 + j), identity=ident_b[:]),
                              reads=[B_u], writes=[B_psT2], inc=(j == 3))
                    kb.op("dve", lambda e, ti=ti: e.tensor_copy(out=bm_tok[:, ti, :], in_=psT2[:].rearrange("p a b -> p (a b)")),
                          reads=[B_psT2], writes=[B_xs[ti]])

                hctr = [0]

                def chunk_S(tr, dtA_ap, dtv_ap, xs_ap, bm_of, rd, own_c=None, d=0, tot_ap=None):
                    kb.op("pe", lambda e: e.matmul(sm_ps[:, 0:16], lhsT=tr[:], rhs=dtA_ap, start=True, stop=True),
                          reads=rd, writes=[B_smps], inc=False)
                    kb.op("pe", lambda e: e.matmul(sm_ps[:, 16:32], lhsT=ones_f[:], rhs=dtA_ap, start=True, stop=True),
                          reads=rd, writes=[B_smps])
                    kb.op("act", lambda e: e.copy(out=sm[:, 0:32], in_=sm_ps[:, 0:32]), reads=[B_smps], writes=[B_sm])
                    kb.op("dve", lambda e: e.tensor_tensor(out=sm[:, 32:48], in0=sm[:, 16:32], in1=sm[:, 0:16], op=ALU.subtract),
                          reads=[B_sm], writes=[B_sm])
                    kb.op("act", lambda e: e.activation(out=sm[:, 32:48], in_=sm[:, 32:48], func=AF.Exp), reads=[B_sm], writes=[B_sm])
                    kb.op("act", lambda e: e.activation(out=sm[:, 48:64], in_=sm[:, 16:32], func=AF.Exp), reads=[B_sm], writes=[B_sm])
                    if own_c is not None:
                        kb.op("act", lambda e: e.activation(out=eac[:, own_c, d * 16:(d + 1) * 16], in_=sm[:, 0:16], func=AF.Exp),
                              reads=[B_sm], writes=[B_eac])
                    kb.op("dve", lambda e: e.tensor_tensor(out=sm[:, 32:48], in0=sm[:, 32:48], in1=dtv_ap, op=ALU.mult),
                          reads=[B_sm] + rd, writes=[B_sm])
                    xi = hctr[0] % 2
                    hctr[0] += 1
                    kb.op("dve", lambda e: e.tensor_tensor(
                        out=xdd[xi][:], in0=xs_ap.rearrange("p (h q) -> p h q", h=16),
                        in1=sm[:, 32:48].unsqueeze(2).to_broadcast([128, 16, 64]), op=ALU.mult),
                        reads=[B_sm] + rd, writes=[B_xdd[xi]])
                    for g in range(4):
                        kb.op("pe", lambda e, g=g: e.matmul(S_ps[:, g * 256:(g + 1) * 256], lhsT=bm_of(g),
                                                            rhs=xdd[xi][:, 4 * g:4 * g + 4, :], start=True, stop=True),
                              reads=rd + [B_xdd[xi]], writes=[B_Sps], inc=(g == 3))

                def chunk_state(ti, d, Hin, B_Hin, Hout, B_Hout, own_c=None):
                    tr = tri_f if d == 0 else triT_f
                    chunk_S(tr, dtA[:, ti, d * 16:(d + 1) * 16], dtv[:, ti, d * 16:(d + 1) * 16], xs_tok[:, ti, :],
                            lambda g: bm_tok[:, ti, g * 128:(g + 1) * 128], [B_dt, B_xs[ti]], own_c=own_c, d=d)
                    if Hin is None:
                        kb.op("dve", lambda e: e.tensor_copy(out=Hout, in_=S_ps[:]), reads=[B_Sps], writes=[B_Hout])
                    else:
                        kb.op("dve", lambda e: e.tensor_tensor(
                            out=Hout.rearrange("p (h q) -> p h q", h=16), in0=Hin.rearrange("p (h q) -> p h q", h=16),
                            in1=sm[:, 48:64].unsqueeze(2).to_broadcast([128, 16, 64]), op=ALU.mult),
                            reads=[B_Hin, B_sm], writes=[B_Hout])
                        kb.op("dve", lambda e: e.tensor_tensor(out=Hout, in0=Hout, in1=S_ps[:], op=ALU.add),
                              reads=[B_Sps, B_Hout], writes=[B_Hout])

                for d in range(2):
                    order = [0, 1] if d == 0 else [1, 0]
                    chunk_state(order[0], d, None, None, Hb[0][:], B_H[0])
                    chunk_state(order[1], d, Hb[0][:], B_H[0], hc[:, d, :], B_hc)
                kb.op("pool", lambda e: e.memset(Pall[:], 1.0), writes=[B_P])
                for d in range(2):
                    order = list(range(NT)) if d == 0 else list(range(NT - 1, -1, -1))
                    kb.op("pool", lambda e: e.memset(Hb[0][:], 0.0), writes=[B_H[0]])
                    cur = 0
                    for n, c in enumerate(order):
                        kb.dma("sp", h0_d[d, c], Hb[cur][:], reads=[B_H[cur]], writes=[B_h0[d][c]])
                        if n < NT - 1:
                            chunk_state(2 + c, d, Hb[cur][:], B_H[cur], Hb[1 - cur][:], B_H[1 - cur], own_c=c)
                        else:
                            chunk_state(2 + c, d, Hb[cur][:], B_H[cur], Hb[1 - cur][:], B_H[1 - cur], own_c=c)
                        pin = Pall[:, d, c, :] if d == 0 else Pall[:, d, c + 1, :]
                        pout = Pall[:, d, c + 1, :] if d == 0 else Pall[:, d, c, :]
                        kb.op("dve", lambda e: e.tensor_tensor(out=pout, in0=pin, in1=sm[:, 48:64], op=ALU.mult),
                              reads=[B_sm, B_P], writes=[B_P])
                        cur = 1 - cur

                dt2r = sbt(ph, "dt2r", [128, NT, 16]); dt2 = sbt(ph, "dt2", [128, NT, 16]); dtA2 = sbt(ph, "dtA2", [128, NT, 16])
                dtb2 = sbt(ph, "dtb2", [128, 16]); alg2 = sbt(ph, "alg2", [128, 16]); tris = sbt(ph, "tris", [128, 128])
                B_d2 = Buf()
                kb.dma("sp", dt2r[:], dtraw2_d.rearrange("t p c -> p t c"), writes=[B_d2])
                kb.dma("sp", dtb2[:], bcast_row(dtb_sel[0:1, :]), writes=[B_d2])
                kb.dma("sp", alg2[:], bcast_row(alog_sel[0:1, :]), writes=[B_d2])
                kb.dma("sp", tris[:], tri_sel[:, :], writes=[B_d2])
                kb.op("dve", lambda e: e.tensor_tensor(out=dt2r[:], in0=dt2r[:], in1=dtb2[:].unsqueeze(1).to_broadcast([128, NT, 16]),
                                                       op=ALU.add), reads=[B_d2], writes=[B_d2])
                kb.op("act", lambda e: e.activation(out=dt2r[:], in_=dt2r[:], func=AF.Exp), reads=[B_d2], writes=[B_d2])
                kb.op("act", lambda e: e.activation(out=dt2[:], in_=dt2r[:], func=AF.Ln, bias=ones_f[:, 0:1]), reads=[B_d2], writes=[B_d2])
                kb.op("act", lambda e: e.activation(out=alg2[:], in_=alg2[:], func=AF.Exp), reads=[B_d2], writes=[B_d2])
                kb.op("dve", lambda e: e.scalar_tensor_tensor(out=dtA2[:], in0=dt2[:], scalar=-1.0,
                                                              in1=alg2[:].unsqueeze(1).to_broadcast([128, NT, 16]),
                                                              op0=ALU.mult, op1=ALU.mult), reads=[B_d2], writes=[B_d2])
                tot2_ps = pst(ph, "tot2_ps", [128, 256]); B_t2ps = Buf()
                kb.op("pe", lambda e: e.matmul(tot2_ps[:], lhsT=ones_f[:], rhs=dtA2[:], start=True, stop=True),
                      reads=[B_d2], writes=[B_t2ps])
                tot2 = sbt(ph, "tot2", [128, NT, 16]); pre = sbt(ph, "pre", [128, NT + 1, 16]); suf = sbt(ph, "suf", [128, NT + 1, 16])
                wch = sbt(ph, "wch", [128, NT + 1, 16]); B_w = Buf()
                kb.op("act", lambda e: e.copy(out=tot2[:].rearrange("p a b -> p (a b)"), in_=tot2_ps[:]), reads=[B_t2ps], writes=[B_w])
                kb.op("pool", lambda e: e.memset(pre[:], 0.0), writes=[B_w])
                kb.op("pool", lambda e: e.memset(suf[:], 0.0), writes=[B_w])
                for c in range(NT):
                    kb.op("dve", lambda e, c=c: e.tensor_tensor(out=pre[:, c + 1, :], in0=pre[:, c, :], in1=tot2[:, c, :], op=ALU.add),
                          reads=[B_w], writes=[B_w])
                for c in range(NT - 1, 0, -1):
                    kb.op("dve", lambda e, c=c: e.tensor_tensor(out=suf[:, c - 1, :], in0=suf[:, c, :], in1=tot2[:, c, :], op=ALU.add),
                          reads=[B_w], writes=[B_w])
                kb.op("dve", lambda e: e.tensor_scalar(out=wch[:], in0=suf[:], scalar1=flg[:, 0:1], scalar2=None, op0=ALU.mult),
                      reads=[B_w], writes=[B_w])
                kb.op("dve", lambda e: e.scalar_tensor_tensor(out=wch[:], in0=pre[:], scalar=flg[:, 1:2], in1=wch[:],
                                                              op0=ALU.mult, op1=ALU.add), reads=[B_w], writes=[B_w])
                kb.op("dve", lambda e: e.tensor_copy(out=wch[:, NT, :], in_=pre[:, NT, :]), reads=[B_w], writes=[B_w])
                kb.op("act", lambda e: e.activation(out=wch[:], in_=wch[:], func=AF.Exp), reads=[B_w], writes=[B_w])
                u2 = [sbt(ph, "u2_%d" % i, [128, 12, 128], BF16) for i in range(2)]
                B_u2 = [Buf(), Buf()]
                xs2 = [sbt(ph, "xs2_%d" % i, [128, 1024], BF16) for i in range(2)]
                bm2 = [sbt(ph, "bm2_%d" % i, [128, 512], BF16) for i in range(2)]
                B_x2 = [Buf(), Buf()]
                Hacc = Hb[0]; B_Hacc = B_H[0]
                stmp = Hb[1]; B_stmp = B_H[1]
                kb.op("pool", lambda e: e.memset(Hacc[:], 0.0), writes=[B_Hacc])
                u2v = uT2_d.rearrange("j p t -> p j t")
                kb.dma("sp", u2[0][:], u2v[:, :, 0:128], writes=[B_u2[0]])
                for c in range(NT):
                    bi = c % 2
                    if c + 1 < NT:
                        kb.dma("sp", u2[1 - bi][:], u2v[:, :, (c + 1) * 128:(c + 2) * 128], writes=[B_u2[1 - bi]])
                    for j in range(8):
                        kb.op("pe", lambda e, j=j: e.transpose(out=psT1[:, j, :], in_=u2[bi][:, j, :], identity=ident_b[:]),
                              reads=[B_u2[bi]], writes=[B_psT1], inc=(j == 7))
                    kb.op("act", lambda e: e.copy(out=xs2[bi][:], in_=psT1[:].rearrange("p a b -> p (a b)")),
                          reads=[B_psT1], writes=[B_x2[bi]])
                    for j in range(4):
                        kb.op("pe", lambda e, j=j: e.transpose(out=psT2[:, j, :], in_=u2[bi][:, 8 + j, :], identity=ident_b[:]),
                              reads=[B_u2[bi]], writes=[B_psT2], inc=(j == 3))
                    kb.op("dve", lambda e: e.tensor_copy(out=bm2[bi][:], in_=psT2[:].rearrange("p a b -> p (a b)")),
                          reads=[B_psT2], writes=[B_x2[bi]])
                    chunk_S(tris, dtA2[:, c, :], dt2[:, c, :], xs2[bi][:], lambda g: bm2[bi][:, g * 128:(g + 1) * 128],
                            [B_d2, B_x2[bi]])
                    kb.op("dve", lambda e: e.tensor_tensor(
                        out=stmp[:].rearrange("p (h q) -> p h q", h=16), in0=S_ps[:].rearrange("p (h q) -> p h q", h=16),
                        in1=wch[:, c, :].unsqueeze(2).to_broadcast([128, 16, 64]), op=ALU.mult),
                        reads=[B_Sps, B_w], writes=[B_stmp])
                    kb.op("pool", lambda e: e.tensor_tensor(out=Hacc[:], in0=Hacc[:], in1=stmp[:], op=ALU.add),
                          reads=[B_stmp, B_Hacc], writes=[B_Hacc])
                hsel = stmp; B_hsel = B_stmp
                kb.op("dve", lambda e: e.tensor_scalar(out=hsel[:], in0=hc[:, 0, :], scalar1=flg[:, 0:1], scalar2=None, op0=ALU.mult),
                      reads=[B_hc], writes=[B_hsel])
                kb.op("dve", lambda e: e.scalar_tensor_tensor(out=hsel[:], in0=hc[:, 1, :], scalar=flg[:, 1:2], in1=hsel[:],
                                                              op0=ALU.mult, op1=ALU.add), reads=[B_hc, B_hsel], writes=[B_hsel])
                kb.op("dve", lambda e: e.tensor_tensor(
                    out=hsel[:].rearrange("p (h q) -> p h q", h=16), in0=hsel[:].rearrange("p (h q) -> p h q", h=16),
                    in1=wch[:, NT, :].unsqueeze(2).to_broadcast([128, 16, 64]), op=ALU.mult),
                    reads=[B_hsel, B_w], writes=[B_hsel])
                kb.op("dve", lambda e: e.tensor_tensor(out=hsel[:], in0=hsel[:], in1=Hacc[:], op=ALU.add),
                      reads=[B_hsel, B_Hacc], writes=[B_hsel])
                for d in range(2):
                    fa, fb = (flg[:, 0:1], flg[:, 1:2]) if d == 0 else (flg[:, 1:2], flg[:, 0:1])
                    kb.op("dve", lambda e, d=d, fa=fa: e.tensor_scalar(out=hinit[:, d, :], in0=hsel[:], scalar1=fa, scalar2=None,
                                                                       op0=ALU.mult), reads=[B_hsel], writes=[B_hi])
                    kb.op("dve", lambda e, d=d, fb=fb: e.scalar_tensor_tensor(out=hinit[:, d, :], in0=hc[:, d, :], scalar=fb,
                                                                              in1=hinit[:, d, :], op0=ALU.mult, op1=ALU.add),
                          reads=[B_hc, B_hi], writes=[B_hi])
                kb.barrier()
            if stop_after == "E1":
                dbg_h = dscr("dbg_h", [128, 4, 1024])
                kb.dma("sp", dbg_h[:, 0:2, :], hc[:], reads=[B_hc])
                kb.dma("sp", dbg_h[:, 2:4, :], hinit[:], reads=[B_hi])
                kb.barrier()
                return nc

            with ExitStack() as ph:
                seg_ps = [pst(ph, "seg_ps%d" % i, [128, 512]) for i in range(2)]; B_seg = [Buf(), Buf()]
                cb_ps = pst(ph, "cb_ps", [128, 512]); B_cbps = Buf()
                yd_ps = pst(ph, "yd_ps", [128, 1024]); B_ydps = Buf()
                yo_ps = pst(ph, "yo_ps", [128, 1024]); B_yops = Buf()
                psT3 = pst(ph, "psTE3", [128, 8, 128], BF16); B_psT3 = Buf()
                negones = sbt(ph, "negones", [128, 128]); nm4 = sbt(ph, "nm4", [128, 2, 4, 128], BF16); B_c2 = Buf()
                kb.op("pool", lambda e: e.memset(negones[:], -1.0), writes=[B_c2])
                for d, nmx in enumerate((nmf_f, nmb_f)):
                    kb.op("dve", lambda e, d=d, nmx=nmx: e.tensor_copy(out=nm4[:, d, :, :],
                                                                       in_=nmx[:].unsqueeze(1).to_broadcast([128, 4, 128])),
                          writes=[B_c2])
                Rf = sbt(ph, "Rf", [128, 32, 128]); B_R = Buf()
                Mb = sbt(ph, "Mb", [128, 32, 128], BF16); B_M = Buf()
                LT = sbt(ph, "LT", [128, 32, 128], BF16); B_LT = Buf()
                cbT = sbt(ph, "cbT", [128, 4, 128], BF16); B_cbT = Buf()
                xd = sbt(ph, "xd", [128, 32, 64], BF16); B_xd2 = Buf()
                h0t = [sbt(ph, "h0t%d" % i, [128, 1024]) for i in range(2)]; B_h0t = [Buf(), Buf()]
                hp = [sbt(ph, "hp%d" % i, [128, 16, 64], BF16) for i in range(2)]; B_hp = [Buf(), Buf()]
                htmp = sbt(ph, "htmp", [128, 1024]); B_htmp = Buf()
                ya = sbt(ph, "ya", [128, 1024]); B_ya = Buf()
                yb = sbt(ph, "yb", [128, 1024]); B_yb = Buf()
                yy = sbt(ph, "yy", [128, 1024]); B_yy = Buf()
                zt = [sbt(ph, "zt%d" % i, [128, 1024]) for i in range(2)]; B_zt = [Buf(), Buf()]
                junk2 = sbt(ph, "junk2", [128, 1024], BF16); B_j2 = Buf()
                ssq2 = sbt(ph, "ssq2", [128, 4]); B_ss2 = Buf()
                ynb = sbt(ph, "ynb", [128, 1024], BF16); B_ynb = Buf()
                mo2 = [sbt(ph, "mo2_%d" % i, [128, 8, 128], BF16) for i in range(2)]; B_mo2 = [Buf(), Buf()]
                sctr2 = 0
                for c in range(NT):
                    ti = 2 + c
                    tk = slice(c * 128, (c + 1) * 128)
                    kb.dma("sp", zt[c % 2][:], zs_d[c], writes=[B_zt[c % 2]])
                    for d in range(2):
                        kb.dma("sp", h0t[d][:], h0_d[d, c], reads=[B_h0[d][c]], writes=[B_h0t[d]])
                    for d, tr in enumerate((tri_f, triT_f)):
                        kb.op("pool", lambda e, d=d, tr=tr: e.tensor_tensor(
                            out=Rf[:, d * 16:(d + 1) * 16, :], in0=tr[:].unsqueeze(1).to_broadcast([128, 16, 128]),
                            in1=dtA[:, ti, d * 16:(d + 1) * 16].unsqueeze(2).to_broadcast([128, 16, 128]), op=ALU.mult),
                            reads=[B_dt], writes=[B_R])
                    if cut <= 1:
                        continue
                    for g in range(4):
                        kb.op("pe", lambda e, g=g: e.matmul(cb_ps[:, g * 128:(g + 1) * 128], lhsT=uTbc[:, g, tk], rhs=uTbc[:, 4 + g, tk],
                                                            start=True, stop=True), reads=[B_u], writes=[B_cbps], inc=(g == 3))
                    kb.op("act", lambda e: e.copy(out=cbT[:].rearrange("p a b -> p (a b)"), in_=cb_ps[:]), reads=[B_cbps], writes=[B_cbT])
                    for q4 in range(8):
                        d = q4 // 4
                        si = sctr2 % 2
                        sctr2 += 1
                        kb.op("pe", lambda e: e.matmul(seg_ps[si][:], lhsT=ones_f[:], rhs=Rf[:, 4 * q4:4 * q4 + 4, :],
                                                       start=True, stop=False), reads=[B_R], writes=[B_seg[si]], inc=False)
                        for r in range(4):
                            kb.op("pe", lambda e, r=r: e.matmul(seg_ps[si][:, r * 128:(r + 1) * 128], lhsT=Rf[:, 4 * q4 + r, :],
                                                                rhs=negones[:], start=False, stop=False),
                                  reads=[B_R, B_c2], writes=[B_seg[si]], inc=False)
                        kb.op("pe", lambda e: e.matmul(seg_ps[si][:], lhsT=ident_b[:], rhs=nm4[:, d, :, :],
                                                       start=False, stop=True), reads=[B_c2], writes=[B_seg[si]])
                        kb.op("act", lambda e: e.activation(out=Mb[:, 4 * q4:4 * q4 + 4, :], in_=seg_ps[si][:], func=AF.Exp),
                              reads=[B_seg[si]], writes=[B_M])
                    if cut <= 2:
                        continue
                    for d in range(2):
                        kb.op("dve", lambda e, d=d: e.tensor_tensor(
                            out=LT[:, d * 16:(d + 1) * 16, :].rearrange("p (g r) l -> p g r l", g=4),
                            in0=Mb[:, d * 16:(d + 1) * 16, :].rearrange("p (g r) l -> p g r l", g=4),
                            in1=cbT[:].unsqueeze(2).to_broadcast([128, 4, 4, 128]), op=ALU.mult),
                            reads=[B_M, B_cbT], writes=[B_LT])
                        kb.op("pool", lambda e, d=d: e.tensor_tensor(
                            out=xd[:, d * 16:(d + 1) * 16, :], in0=xs_tok[:, ti, :].rearrange("p (h q) -> p h q", h=16),
                            in1=dtv[:, ti, d * 16:(d + 1) * 16].unsqueeze(2).to_broadcast([128, 16, 64]), op=ALU.mult),
                            reads=[B_xs[ti], B_dt], writes=[B_xd2])
                    if cut <= 3:
                        continue
                    for h in range(16):
                        for d in range(2):
                            kb.op("pe", lambda e, h=h, d=d: e.matmul(yd_ps[:, h * 64:(h + 1) * 64], lhsT=LT[:, d * 16 + h, :],
                                                                     rhs=xd[:, d * 16 + h, :], start=(d == 0), stop=(d == 1)),
                                  reads=[B_LT, B_xd2], writes=[B_ydps], inc=(h == 15 and d == 1))
                    if cut <= 4:
                        continue
                    for d in range(2):
                        pin = Pall[:, d, c, :] if d == 0 else Pall[:, d, c + 1, :]
                        kb.op("dve", lambda e, d=d, pin=pin: e.tensor_tensor(
                            out=htmp[:].rearrange("p (h q) -> p h q", h=16), in0=hinit[:, d, :].rearrange("p (h q) -> p h q", h=16),
                            in1=pin.unsqueeze(2).to_broadcast([128, 16, 64]), op=ALU.mult),
                            reads=[B_hi, B_P], writes=[B_htmp])
                        kb.op("dve", lambda e, d=d: e.tensor_tensor(out=hp[d][:].rearrange("p h q -> p (h q)"), in0=htmp[:],
                                                                    in1=h0t[d][:], op=ALU.add),
                              reads=[B_htmp, B_h0t[d]], writes=[B_hp[d]])
                        for g in range(4):
                            kb.op("pe", lambda e, g=g, d=d: e.matmul(yo_ps[:, g * 256:(g + 1) * 256], lhsT=uTbc[:, 4 + g, tk],
                                                                     rhs=hp[d][:, 4 * g:4 * g + 4, :], start=True, stop=True),
                                  reads=[B_u, B_hp[d]], writes=[B_yops], inc=(g == 3))
                        dst, Bd = (ya, B_ya) if d == 0 else (yb, B_yb)
                        kb.op("dve", lambda e, d=d, dst=dst: e.tensor_tensor(
                            out=dst[:].rearrange("p (h q) -> p h q", h=16), in0=yo_ps[:].rearrange("p (h q) -> p h q", h=16),
                            in1=eac[:, c, d * 16:(d + 1) * 16].unsqueeze(2).to_broadcast([128, 16, 64]), op=ALU.mult),
                            reads=[B_yops, B_eac], writes=[Bd])
                    if cut <= 5:
                        continue
                    kb.op("dve", lambda e: e.tensor_tensor(out=yy[:], in0=yd_ps[:], in1=ya[:], op=ALU.add),
                          reads=[B_ydps, B_ya], writes=[B_yy])
                    kb.op("pool", lambda e: e.tensor_tensor(out=yy[:], in0=yy[:], in1=yb[:], op=ALU.add),
                          reads=[B_yy, B_yb], writes=[B_yy])
                    kb.op("pool", lambda e: e.tensor_tensor(
                        out=ya[:].rearrange("p (h q) -> p h q", h=16), in0=xs_tok[:, ti, :].rearrange("p (h q) -> p h q", h=16),
                        in1=dsk[:].unsqueeze(2).to_broadcast([128, 16, 64]), op=ALU.mult),
                        reads=[B_xs[ti], B_dt], writes=[B_ya])
                    kb.op("pool", lambda e: e.tensor_tensor(out=yy[:], in0=yy[:], in1=ya[:], op=ALU.add),
                          reads=[B_yy, B_ya], writes=[B_yy])
                    if cut <= 6:
                        continue
                    kb.op("dve", lambda e: e.tensor_tensor(out=yy[:], in0=yy[:], in1=zt[c % 2][:], op=ALU.mult),
                          reads=[B_yy, B_zt[c % 2]], writes=[B_yy])
                    rms_mod_tile(yy[:], B_yy, gssd[:], None, B_dt, junk2, B_j2, ssq2, B_ss2, yb, B_yb, None, None, width=1024)
                    if cut <= 7:
                        continue
                    kb.op("act", lambda e: e.copy(out=ynb[:], in_=yb[:]), reads=[B_yb], writes=[B_ynb])
                    for j in range(8):
                        kb.op("pe", lambda e, j=j: e.transpose(out=psT3[:, j, :], in_=ynb[:, j * 128:(j + 1) * 128], identity=ident_b[:]),
                              reads=[B_ynb], writes=[B_psT3], inc=(j == 7))
                    kb.op("act", lambda e: e.copy(out=mo2[c % 2][:], in_=psT3[:]), reads=[B_psT3], writes=[B_mo2[c % 2]])
                    kb.dma("sp", mixT_d[8:16, :, tk].rearrange("h p t -> p h t"), mo2[c % 2][:], reads=[B_mo2[c % 2]])
                kb.barrier()
        if stop_after == "E":
            return nc

        with ExitStack() as ph:
            Wo = sbt(ph, "Wo", [128, 16, D], BF16); B_Wo = Buf()
            wov = w_out.rearrange("(k p) n -> p k n", p=128)
            for cbk in range(4):
                kb.dma("pool", Wo[:, :, cbk * 512:(cbk + 1) * 512], wov[:, :, cbk * 512:(cbk + 1) * 512], writes=[B_Wo])
            B_m2 = Buf()
            g1b = load_bc(ph, "sp", "g1b", mod_d[2:3, :], D, B_m2)
            G2 = load_bc(ph, "sp", "G2", mod_d[4:5, :], D, B_m2)
            SH2 = load_bc(ph, "sp", "SH2", mod_d[3:4, :], D, B_m2)
            g2n = load_bc(ph, "sp", "g2n", norm2_g[0:1, :], D, B_m2)
            kb.op("dve", lambda e: e.tensor_tensor(out=G2[:], in0=G2[:], in1=g2n[:], op=ALU.mult), reads=[B_m2], writes=[B_m2])
            rw = sbt(ph, "rw", [128, 16, NEXP]); rbias = sbt(ph, "rbias", [128, NEXP])
            kb.dma("sp", rw[:], router_w.rearrange("(k p) e -> p k e", p=128), writes=[B_m2])
            kb.dma("sp", rbias[:], bcast_row(router_bias[0:1, :]), writes=[B_m2])
            mix = [sbt(ph, "mix%d" % i, [128, 16, 128], BF16) for i in range(2)]; B_mix = [Buf(), Buf()]
            xtF = [sbt(ph, "xtF%d" % i, [128, D]) for i in range(2)]; B_xtF = [Buf(), Buf()]
            x1t = sbt(ph, "x1t", [128, D]); B_x1t = Buf()
            tmpF = sbt(ph, "tmpF", [128, D]); B_tmpF = Buf()
            h2f = g2n; B_h2f = Buf()
            h2b = sbt(ph, "h2b", [128, D], BF16); B_h2b = Buf()
            junkF = sbt(ph, "junkF", [128, D], BF16); B_junkF = Buf()
            ssqF = sbt(ph, "ssqF", [128, 4]); B_ssF = Buf()
            h2To = [sbt(ph, "h2To%d" % i, [128, 16, 128], BF16) for i in range(2)]; B_h2To = [Buf(), Buf()]
            h2Tf = sbt(ph, "h2Tf", [128, 16, 128]); B_h2Tf = Buf()
            psF = [pst(ph, "psF%d" % i, [128, 512]) for i in range(2)]; B_psF = [Buf(), Buf()]
            psTF = [pst(ph, "psTF%d" % i, [128, 8, 128], BF16) for i in range(2)]; B_psTF = [Buf(), Buf()]
            psR = [pst(ph, "psR%d" % i, [128, 4, 128]) for i in range(2)]; B_psR = [Buf(), Buf()]
            psL = pst(ph, "psL", [128, NEXP]); B_psL = Buf()
            sc_ = sbt(ph, "sc_", [128, NEXP]); sel = sbt(ph, "sel", [128, NEXP]); sel2 = sbt(ph, "sel2", [128, NEXP])
            m1 = sbt(ph, "m1", [128, 8]); m2 = sbt(ph, "m2", [128, 8]); gs = sbt(ph, "gs", [128, 8])
            cmpt = sbt(ph, "cmpt", [128, 8, 8]); rank = sbt(ph, "rank", [128, 8]); km = sbt(ph, "km", [128, 8])
            mx8 = sbt(ph, "mx8", [128, 8]); thr = sbt(ph, "thr", [128, 2])
            gate = [sbt(ph, "gate%d" % i, [128, NEXP + 1]) for i in range(2)]; B_gate = [Buf(), Buf()]
            B_r = Buf()
            for i in range(2):
                kb.op("pool", lambda e, i=i: e.memset(gate[i][:], 1.0), writes=[B_gate[i]])
            mixv = mixT_d.rearrange("k p t -> p k t")
            h2Tv = h2T_d.rearrange("k p t -> p k t")
            kb.dma("sp", mix[0][:], mixv[:, :, 0:128], writes=[B_mix[0]])
            kb.dma("sp", xtF[0][:], x_own[0:128, :], writes=[B_xtF[0]])
            pctrF = 0
            for t in range(NT):
                bi = t % 2
                tk = slice(t * 128, (t + 1) * 128)
                if t + 1 < NT:
                    kb.dma("sp", mix[1 - bi][:], mixv[:, :, (t + 1) * 128:(t + 2) * 128], writes=[B_mix[1 - bi]])
                    kb.dma("sp", xtF[1 - bi][:], x_own[(t + 1) * 128:(t + 2) * 128, :], writes=[B_xtF[1 - bi]])
                for cbk in range(4):
                    pi = pctrF % 2
                    pctrF += 1
                    cs_ = slice(cbk * 512, (cbk + 1) * 512)
                    for k in range(16):
                        kb.op("pe", lambda e, k=k: e.matmul(psF[pi][:], lhsT=mix[bi][:, k, :], rhs=Wo[:, k, cs_],
                                                            start=(k == 0), stop=(k == 15)),
                              reads=[B_mix[bi], B_Wo], writes=[B_psF[pi]], inc=(k == 15))
                    kb.op("dve", lambda e: e.tensor_tensor(out=tmpF[:, cs_], in0=psF[pi][:], in1=g1b[:, cs_], op=ALU.mult),
                          reads=[B_psF[pi], B_m2], writes=[B_tmpF])
                    kb.op("pool", lambda e: e.tensor_tensor(out=x1t[:, cs_], in0=tmpF[:, cs_], in1=xtF[bi][:, cs_], op=ALU.add),
                          reads=[B_tmpF, B_xtF[bi]], writes=[B_x1t])
                kb.dma("sp", x1_d[tk, :], x1t[:], reads=[B_x1t])
                rms_mod_tile(x1t[:], B_x1t, G2[:], SH2[:], B_m2, junkF, B_junkF, ssqF, B_ssF, tmpF, B_tmpF, h2f[:], B_h2f)
                kb.op("act", lambda e: e.copy(out=h2b[:], in_=h2f[:]), reads=[B_h2f], writes=[B_h2b])
                kb.dma("sp", h2tok_d[tk, :], h2b[:], reads=[B_h2b])
                for half in range(2):
                    for kk in range(8):
                        k = half * 8 + kk
                        kb.op("pe", lambda e, k=k, kk=kk: e.transpose(out=psTF[half][:, kk, :], in_=h2b[:, k * 128:(k + 1) * 128],
                                                                     identity=ident_b[:]),
                              reads=[B_h2b], writes=[B_psTF[half]], inc=(kk == 7))
                    if half == 0:
                        kb.op("act", lambda e: e.copy(out=h2To[bi][:, 0:8, :], in_=psTF[0][:]), reads=[B_psTF[0]], writes=[B_h2To[bi]])
                    else:
                        kb.op("dve", lambda e: e.tensor_copy(out=h2To[bi][:, 8:16, :], in_=psTF[1][:]), reads=[B_psTF[1]],
                              writes=[B_h2To[bi]])
                kb.dma("sp", h2Tv[:, :, tk], h2To[bi][:], reads=[B_h2To[bi]])
                for q in range(4):
                    ri = q % 2
                    for kk in range(4):
                        k = q * 4 + kk
                        kb.op("pe", lambda e, k=k, kk=kk: e.matmul(psR[ri][:, kk, :], lhsT=h2f[:, k * 128:(k + 1) * 128], rhs=ident_f[:],
                                                                  start=True, stop=True),
                              reads=[B_h2f], writes=[B_psR[ri]], inc=(kk == 3))
                    kb.op("act", lambda e, q=q: e.copy(out=h2Tf[:, q * 4:(q + 1) * 4, :], in_=psR[ri][:]), reads=[B_psR[ri]],
                          writes=[B_h2Tf])
                for k in range(16):
                    kb.op("pe", lambda e, k=k: e.matmul(psL[:], lhsT=h2Tf[:, k, :], rhs=rw[:, k, :], start=(k == 0), stop=(k == 15)),
                          reads=[B_h2Tf, B_m2], writes=[B_psL], inc=(k == 15))
                R_ = [B_r]
                kb.op("act", lambda e: e.activation(out=sc_[:], in_=psL[:], func=AF.Sigmoid), reads=[B_psL], writes=R_)
                kb.op("dve", lambda e: e.tensor_tensor(out=sel[:], in0=sc_[:], in1=rbias[:], op=ALU.add), reads=R_ + [B_m2], writes=R_)
                s3 = lambda a: a[:].rearrange("p (g e) -> p g e", g=8)
                kb.op("dve", lambda e: e.tensor_reduce(out=m1[:], in_=s3(sel), axis=AX.X, op=ALU.max), reads=R_, writes=R_)
                kb.op("dve", lambda e: e.tensor_tensor(out=s3(sel2), in0=s3(sel), in1=m1[:].unsqueeze(2).to_broadcast([128, 8, 8]),
                                                       op=ALU.is_equal), reads=R_, writes=R_)
                kb.op("dve", lambda e: e.scalar_tensor_tensor(out=sel2[:], in0=sel2[:], scalar=-1e9, in1=sel[:], op0=ALU.mult,
                                                              op1=ALU.add), reads=R_, writes=R_)
                kb.op("dve", lambda e: e.tensor_reduce(out=m2[:], in_=s3(sel2), axis=AX.X, op=ALU.max), reads=R_, writes=R_)
                kb.op("dve", lambda e: e.tensor_tensor(out=gs[:], in0=m1[:], in1=m2[:], op=ALU.add), reads=R_, writes=R_)
                kb.op("dve", lambda e: e.tensor_tensor(out=cmpt[:], in0=gs[:].unsqueeze(1).to_broadcast([128, 8, 8]),
                                                       in1=gs[:].unsqueeze(2).to_broadcast([128, 8, 8]), op=ALU.is_gt),
                      reads=R_, writes=R_)
                kb.op("dve", lambda e: e.tensor_reduce(out=rank[:], in_=cmpt[:], axis=AX.X, op=ALU.add), reads=R_, writes=R_)
                kb.op("dve", lambda e: e.tensor_scalar(out=km[:], in0=rank[:], scalar1=3.5, scalar2=1e9, op0=ALU.is_lt, op1=ALU.mult),
                      reads=R_, writes=R_)
                kb.op("dve", lambda e: e.tensor_scalar(out=km[:], in0=km[:], scalar1=-1e9, scalar2=None, op0=ALU.add),
                      reads=R_, writes=R_)
                kb.op("dve", lambda e: e.tensor_tensor(out=s3(sel2), in0=s3(sel), in1=km[:].unsqueeze(2).to_broadcast([128, 8, 8]),
                                                       op=ALU.add), reads=R_, writes=R_)
                kb.op("dve", lambda e: e.max(out=mx8[:], in_=sel2[:]), reads=R_, writes=R_)
                kb.op("dve", lambda e: e.tensor_reduce(out=thr[:, 0:1], in_=mx8[:], axis=AX.X, op=ALU.min), reads=R_, writes=R_)
                kb.op("dve", lambda e: e.tensor_scalar(out=sel2[:], in0=sel2[:], scalar1=thr[:, 0:1], scalar2=None, op0=ALU.is_ge),
                      reads=R_, writes=R_)
                kb.op("dve", lambda e: e.tensor_tensor(out=sel2[:], in0=sel2[:], in1=sc_[:], op=ALU.mult), reads=R_, writes=R_)
                kb.op("dve", lambda e: e.tensor_reduce(out=thr[:, 1:2], in_=sel2[:], axis=AX.X, op=ALU.add), reads=R_, writes=R_)
                kb.op("dve", lambda e: e.reciprocal(out=thr[:, 1:2], in_=thr[:, 1:2]), reads=R_, writes=R_)
                kb.op("dve", lambda e: e.tensor_scalar(out=gate[bi][:, 0:NEXP], in0=sel2[:], scalar1=thr[:, 1:2], scalar2=2.5,
                                                       op0=ALU.mult, op1=ALU.mult), reads=R_, writes=[B_gate[bi]])
                kb.dma("sp", gate_d[t], gate[bi][:], reads=[B_gate[bi]])
            kb.barrier()
        if stop_after == "F":
            return nc

        PEe, DVEe, ACTe, POOLe, SPe = (mybir.EngineType.PE, mybir.EngineType.DVE, mybir.EngineType.Activation,
                                       mybir.EngineType.Pool, mybir.EngineType.SP)
        with ExitStack() as ph:
            Gt = sbt(ph, "Gt", [128, NT, NEXP + 1]); B_G = Buf()
            kb.dma("sp", Gt[:], gate_d.rearrange("t p e -> p t e"), writes=[B_G])
            eb = sbt(ph, "eb", [128, 2 * NEXP]); lsf = sbt(ph, "lsf", [128, 128]); lsb = sbt(ph, "lsb", [128, 128], BF16)
            kb.dma("sp", eb[:], ebase[:, :], writes=[B_G])
            kb.dma("sp", lsf[:], lstrict[:, :], writes=[B_G])
            kb.op("dve", lambda e: e.tensor_copy(out=lsb[:], in_=lsf[:]), reads=[B_G], writes=[B_G])
            Mb_ = sbt(ph, "Mb_", [128, NT, NEXP], BF16)
            kb.op("dve", lambda e: e.tensor_scalar(out=Mb_[:], in0=Gt[:, :, 0:NEXP], scalar1=0.0, scalar2=None, op0=ALU.is_gt),
                  reads=[B_G], writes=[B_G])
            zrow = sbt(ph, "zrow", [128, D], BF16); B_z = Buf()
            kb.op("pool", lambda e: e.memset(zrow[:], 0.0), writes=[B_z])
            kb.dma("sp", h2tok_d[OWN:OWN + 128, :], zrow[:], reads=[B_z])
            fill = sbt(ph, "fill", [128, NSL // 128], I32); B_fill = Buf(); B_tab = Buf()
            kb.op("pool", lambda e: e.memset(fill[:], OWN), writes=[B_fill])
            kb.dma("sp", idxtab_d.rearrange("(p f) o -> p (f o)", p=128), fill[:], reads=[B_fill], writes=[B_tab])
            tokid = sbt(ph, "tokid", [128, NT], I32); B_tok = Buf()
            kb.op("pool", lambda e: e.iota(tokid[:], pattern=[[128, NT]], base=0, channel_multiplier=1), writes=[B_tok])
            cnt_ps = pst(ph, "cnt_ps", [128, NEXP]); B_cps = Buf()
            pos_ps = [pst(ph, "pos_ps%d" % i, [128, NEXP]) for i in range(2)]; B_pps = [Buf(), Buf()]
            for t in range(NT):
                kb.op("pe", lambda e, t=t: e.matmul(cnt_ps[:], lhsT=ones_b[:], rhs=Mb_[:, t, :], start=(t == 0), stop=(t == NT - 1)),
                      reads=[B_G], writes=[B_cps], inc=(t == NT - 1))
            nbf = sbt(ph, "nbf", [128, NEXP]); nbm = sbt(ph, "nbm", [128, NEXP]); nbi = sbt(ph, "nbi", [128, NEXP], I32); B_nb = Buf()
            kb.op("dve", lambda e: e.tensor_scalar(out=nbf[:], in0=cnt_ps[:], scalar1=127.0, scalar2=None, op0=ALU.add),
                  reads=[B_cps], writes=[B_nb])
            nbr = sbt(ph, "nbr", [128, NEXP], I32)
            kb.op("dve", lambda e: e.tensor_copy(out=nbr[:], in_=nbf[:]), reads=[B_nb], writes=[B_nb])
            kb.op("dve", lambda e: e.tensor_single_scalar(out=nbi[:], in_=nbr[:], scalar=7, op=ALU.arith_shift_right),
                  reads=[B_nb], writes=[B_nb])
            kb.op("dve", lambda e: e.tensor_single_scalar(out=nbr[:], in_=nbi[:], scalar=7, op=ALU.logical_shift_left),
                  reads=[B_nb], writes=[B_nb])
            kb.op("dve", lambda e: e.tensor_copy(out=nbf[:], in_=nbr[:]), reads=[B_nb], writes=[B_nb])
            cs0 = sbt(ph, "cs0", [128, NEXP]); cs1 = sbt(ph, "cs1", [128, NEXP]); cbs = sbt(ph, "cbs", [128, NEXP])
            cbi = sbt(ph, "cbi", [128, NEXP], I32)
            kb.op("dve", lambda e: e.tensor_copy(out=cs0[:], in_=nbf[:]), reads=[B_nb], writes=[B_nb])
            cur_, oth_ = cs0, cs1
            for s_ in (1, 2, 4, 8, 16, 32):
                kb.op("dve", lambda e, cur_=cur_, oth_=oth_, s_=s_: e.tensor_copy(out=oth_[:, 0:s_], in_=cur_[:, 0:s_]),
                      reads=[B_nb], writes=[B_nb])
                kb.op("dve", lambda e, cur_=cur_, oth_=oth_, s_=s_: e.tensor_tensor(out=oth_[:, s_:NEXP], in0=cur_[:, s_:NEXP],
                                                                                   in1=cur_[:, 0:NEXP - s_], op=ALU.add),
                      reads=[B_nb], writes=[B_nb])
                cur_, oth_ = oth_, cur_
            kb.op("dve", lambda e, cur_=cur_: e.tensor_tensor(out=cbs[:], in0=cur_[:], in1=nbf[:], op=ALU.subtract),
                  reads=[B_nb], writes=[B_nb])
            kb.op("dve", lambda e: e.tensor_copy(out=cbi[:], in_=cbs[:]), reads=[B_nb], writes=[B_nb])
            kb.dma("sp", cbase_d[0:1, :], cbi[0:1, :], reads=[B_nb])
            oh = sbt(ph, "oh", [128, NEXP + 1]); cbp = sbt(ph, "cbp", [128, 2]); B_oh = Buf()
            kb.dma("sp", oh[:], oh2[:, :], writes=[B_oh])
            ohj = sbt(ph, "ohj", [128, NEXP])
            kb.op("dve", lambda e: e.tensor_tensor(out=ohj[:], in0=oh[:, 0:NEXP], in1=cbs[:], op=ALU.mult), reads=[B_oh, B_nb], writes=[B_oh])
            kb.op("dve", lambda e: e.tensor_reduce(out=cbp[:, 0:1], in_=ohj[:], axis=AX.X, op=ALU.add), reads=[B_oh], writes=[B_oh])
            kb.op("dve", lambda e: e.tensor_tensor(out=cbp[:, 1:2], in0=cbp[:, 0:1], in1=oh[:, NEXP:NEXP + 1], op=ALU.add),
                  reads=[B_oh], writes=[B_oh])
            rtf = sbt(ph, "rtf", [128, NSL // 128]); rti = sbt(ph, "rti", [128, NSL // 128], I32); B_rt = Buf()
            kb.op("pool", lambda e: e.iota(rti[:], pattern=[[1, NSL // 128]], base=0, channel_multiplier=0), writes=[B_rt])
            kb.op("dve", lambda e: e.tensor_copy(out=rtf[:], in_=rti[:]), reads=[B_rt], writes=[B_rt])
            kb.op("dve", lambda e: e.tensor_scalar(out=rtf[:], in0=rtf[:], scalar1=cbp[:, 1:2], scalar2=None, op0=ALU.add),
                  reads=[B_rt, B_oh], writes=[B_rt])
            kb.op("dve", lambda e: e.tensor_copy(out=rti[:], in_=rtf[:]), reads=[B_rt], writes=[B_rt])
            kb.dma("sp", rowtab_d.rearrange("(p f) o -> p (f o)", p=128), rti[:], reads=[B_rt])
            kb.dma("sp", nblk_d[0:1, :], nbi[0:1, :], reads=[B_nb])
            dc8i = sbt(ph, "dc8i", [128, NT, 8], I32); B_dc8 = Buf()
            key = sbt(ph, "key", [128, NEXP]); B_key = Buf()
            d8f = sbt(ph, "d8f", [128, 8]); d8i = sbt(ph, "d8i", [128, NT, 8], I32); w8 = sbt(ph, "w8", [128, NT, 8])
            B_d8 = Buf(); B_w8 = Buf()
            e8i = sbt(ph, "e8i", [128, 8], I32); e8f = sbt(ph, "e8f", [128, 8]); B_e8 = Buf()
            for t in range(NT):
                pi = t % 2
                for t2 in range(t):
                    kb.op("pe", lambda e, t2=t2: e.matmul(pos_ps[pi][:], lhsT=ones_b[:], rhs=Mb_[:, t2, :], start=(t2 == 0), stop=False),
                          reads=[B_G], writes=[B_pps[pi]], inc=False)
                kb.op("pe", lambda e, t=t: e.matmul(pos_ps[pi][:], lhsT=lsb[:], rhs=Mb_[:, t, :], start=(t == 0), stop=True),
                      reads=[B_G], writes=[B_pps[pi]])
                kb.op("dve", lambda e: e.tensor_tensor(out=key[:], in0=pos_ps[pi][:], in1=eb[:, 0:NEXP], op=ALU.add),
                      reads=[B_pps[pi], B_G], writes=[B_key])
                kb.op("dve", lambda e, t=t: e.tensor_tensor(out=key[:], in0=key[:], in1=Mb_[:, t, :], op=ALU.mult),
                      reads=[B_key, B_G], writes=[B_key])
                kb.op("dve", lambda e: e.max(out=d8f[:], in_=key[:]), reads=[B_key], writes=[B_key])
                kb.op("dve", lambda e: e.tensor_scalar(out=d8f[:], in0=d8f[:], scalar1=-1.0, scalar2=None, op0=ALU.add),
                      reads=[B_key], writes=[B_key])
                kb.op("dve", lambda e, t=t: e.tensor_copy(out=d8i[:, t, :], in_=d8f[:]), reads=[B_key], writes=[B_d8])
                kb.op("dve", lambda e: e.scalar_tensor_tensor(out=key[:], in0=pos_ps[pi][:], scalar=1.0, in1=cbs[:], op0=ALU.add,
                                                              op1=ALU.add), reads=[B_pps[pi], B_nb, B_key], writes=[B_key])
                kb.op("dve", lambda e, t=t: e.tensor_tensor(out=key[:], in0=key[:], in1=Mb_[:, t, :], op=ALU.mult),
                      reads=[B_key, B_G], writes=[B_key])
                kb.op("dve", lambda e: e.max(out=d8f[:], in_=key[:]), reads=[B_key], writes=[B_key])
                kb.op("dve", lambda e: e.tensor_scalar(out=d8f[:], in0=d8f[:], scalar1=-1.0, scalar2=None, op0=ALU.add),
                      reads=[B_key], writes=[B_key])
                kb.op("dve", lambda e, t=t: e.tensor_copy(out=dc8i[:, t, :], in_=d8f[:]), reads=[B_key], writes=[B_dc8])
                kb.op("dve", lambda e, t=t: e.tensor_tensor(out=key[:], in0=Gt[:, t, 0:NEXP], in1=eb[:, NEXP:2 * NEXP], op=ALU.add),
                      reads=[B_G, B_key], writes=[B_key])
                kb.op("dve", lambda e, t=t: e.tensor_tensor(out=key[:], in0=key[:], in1=Mb_[:, t, :], op=ALU.mult),
                      reads=[B_key, B_G], writes=[B_key])
                kb.op("dve", lambda e: e.max(out=d8f[:], in_=key[:]), reads=[B_key], writes=[B_key])
                kb.op("dve", lambda e, t=t: e.tensor_single_scalar(out=e8i[:], in_=d8i[:, t, :], scalar=11, op=ALU.arith_shift_right),
                      reads=[B_d8], writes=[B_e8])
                kb.op("dve", lambda e: e.tensor_copy(out=e8f[:], in_=e8i[:]), reads=[B_e8], writes=[B_e8])
                kb.op("dve", lambda e: e.tensor_scalar(out=e8f[:], in0=e8f[:], scalar1=-4.0, scalar2=-4.0, op0=ALU.mult, op1=ALU.add),
                      reads=[B_e8], writes=[B_e8])
                kb.op("dve", lambda e, t=t: e.tensor_tensor(out=w8[:, t, :], in0=d8f[:], in1=e8f[:], op=ALU.add),
                      reads=[B_key, B_e8], writes=[B_w8])
                for k in range(8):
                    kb.coll(lambda g, t=t, k=k: g.indirect_dma_start(
                        out=idxtab_d[:, :], out_offset=bass.IndirectOffsetOnAxis(ap=d8i[:, t, k:k + 1], axis=0),
                        in_=tokid[:, t:t + 1], in_offset=None),
                        reads=[B_d8, B_tok, B_tab], writes=[])
            kb.dma("sp", dest_d.rearrange("t p k -> p t k"), dc8i[:], reads=[B_dc8])
            kb.dma("sp", w8_d.rearrange("t p k -> p t k"), w8[:], reads=[B_w8])
            kb.barrier()
        if stop_after == "F2":
            return nc

        with ExitStack() as ph:
            nbt = sbt(ph, "nbt", [1, NEXP], I32); B_nbt = Buf()
            kb.dma("sp", nbt[:], nblk_d[0:1, :], writes=[B_nbt])
            rowcur = sbt(ph, "rowcur", [128, 1], I32); B_rowcur = Buf()
            rowst = [sbt(ph, "rowst%d" % i, [128, 1], I32) for i in range(2)]; B_rowst = [Buf(), Buf()]
            yor = sbt(ph, "yor", [128, D]); B_yor = Buf()
            kb.op("pool", lambda e: e.iota(rowcur[:], pattern=[[0, 1]], base=NSLC, channel_multiplier=1), writes=[B_rowcur])
            kb.op("pool", lambda e: e.memset(yor[:], 0.0), writes=[B_yor])

            def flush_pending():
                kb.coll(lambda g: g.indirect_dma_start(out=slot_d[:, :], out_offset=bass.IndirectOffsetOnAxis(ap=rowcur[:, :], axis=0),
                                                       in_=yor[:, :], in_offset=None),
                        reads=[B_yor, B_rowcur], writes=[])
            ekeys = ["pe", "dve", "act", "pool", "sp"]
            NWB = 2
            NST = 6
            stg = [sbt(ph, "stg%d" % i, [128, 2048]) for i in range(NST)]; B_stg = [Buf() for _ in range(NST)]
            Wg = [sbt(ph, "Wg%d" % i, [128, 16, 512], BF16) for i in range(NWB)]
            Wu = [sbt(ph, "Wu%d" % i, [128, 16, 512], BF16) for i in range(NWB)]
            Wd = [sbt(ph, "Wd%d" % i, [128, 4, D], BF16) for i in range(NWB)]
            B_We = [Buf() for _ in range(NWB)]
            idxb = [sbt(ph, "idxb%d" % i, [128, 1], I32) for i in range(4)]; B_idx = [Buf() for _ in range(4)]
            xg = [sbt(ph, "xg%d" % i, [128, D], BF16) for i in range(2)]; B_xg = [Buf(), Buf()]
            xgT = [sbt(ph, "xgT%d" % i, [128, 16, 128], BF16) for i in range(2)]; B_xgT = [Buf(), Buf()]
            sgs = [sbt(ph, "sgs%d" % i, [128, 512]) for i in range(2)]; B_sgs = [Buf(), Buf()]
            acts = [sbt(ph, "acts%d" % i, [128, 512], BF16) for i in range(2)]; B_acts = [Buf(), Buf()]
            actT = [sbt(ph, "actT%d" % i, [128, 4, 128], BF16) for i in range(2)]; B_actT = [Buf(), Buf()]
            yo = [sbt(ph, "yo%d" % i, [128, D]) for i in range(2)]; B_yo = [Buf(), Buf()]
            psX = [pst(ph, "psX%d" % i, [128, 8, 128], BF16) for i in range(2)]; B_psX = [Buf(), Buf()]
            psg = pst(ph, "psg", [128, 512]); B_psg = Buf()
            psu = pst(ph, "psu", [128, 512]); B_psu = Buf()
            psA = pst(ph, "psA", [128, 4, 128], BF16); B_psA = Buf()
            psy = [pst(ph, "psy%d" % i, [128, 512]) for i in range(2)]; B_psy = [Buf(), Buf()]
            h2Tv = h2T_d.rearrange("k p t -> p k t")

            pctr_ = [0]

            def piece_list(e_):
                bi = e_ % NWB
                gv = ew_gate[e_].rearrange("(k p) f -> p k f", p=128)
                uv = ew_up[e_].rearrange("(k p) f -> p k f", p=128)
                dv = ew_down[e_].rearrange("(k p) d -> p k d", p=128)
                pcs = []
                for q in range(4):
                    pcs.append((gv[:, 4 * q:4 * q + 4, :], Wg[bi][:, 4 * q:4 * q + 4, :], bi))
                    pcs.append((uv[:, 4 * q:4 * q + 4, :], Wu[bi][:, 4 * q:4 * q + 4, :], bi))
                for fc in range(4):
                    pcs.append((dv[:, fc:fc + 1, :], Wd[bi][:, fc:fc + 1, :], bi))
                return pcs

            def load_piece(pc):
                srcap, dstap, bi = pc
                n = pctr_[0]
                pctr_[0] += 1
                si = n % NST
                k4 = srcap.shape[1]
                stv = stg[si][:].rearrange("p (k f) -> p k f", k=k4)
                kb.dma("sp", stv, srcap, writes=[B_stg[si]])
                kb.op("dve", lambda e: e.tensor_copy(out=dstap, in_=stv), reads=[B_stg[si]], writes=[B_We[bi]])

            bctr = [0]

            def block(e_, j, shared=False):
                n = bctr[0]
                bctr[0] += 1
                wi = e_ % NWB
                b2 = n % 2
                row0 = (e_ * 16 + j) * 128
                if shared:
                    kb.dma("sp", xgT[b2][:], h2Tv[:, :, j * 128:(j + 1) * 128], writes=[B_xgT[b2]])
                else:
                    i4 = n % 4
                    kb.dma("pool", idxb[i4][:], idxtab_d[row0:row0 + 128, :], writes=[B_idx[i4]])
                    kb.dma("pool", rowst[b2][:], rowtab_d[row0:row0 + 128, :], writes=[B_rowst[b2]])
                    kb.coll(lambda g: g.indirect_dma_start(out=xg[b2][:, :], out_offset=None, in_=h2tok_d[:, :],
                                                           in_offset=bass.IndirectOffsetOnAxis(ap=idxb[i4][:, :], axis=0)),
                            reads=[B_idx[i4]], writes=[B_xg[b2]])
                    flush_pending()
                    kb.op("dve", lambda e: e.tensor_copy(out=rowcur[:], in_=rowst[b2][:]), reads=[B_rowst[b2]], writes=[B_rowcur])
                    for half in range(2):
                        for kk in range(8):
                            k = half * 8 + kk
                            kb.op("pe", lambda e, k=k, kk=kk: e.transpose(out=psX[half][:, kk, :], in_=xg[b2][:, k * 128:(k + 1) * 128],
                                                                         identity=ident_b[:]),
                                  reads=[B_xg[b2]], writes=[B_psX[half]], inc=(kk == 7))
                        if half == 0:
                            kb.op("act", lambda e: e.copy(out=xgT[b2][:, 0:8, :], in_=psX[0][:]), reads=[B_psX[0]], writes=[B_xgT[b2]])
                        else:
                            kb.op("dve", lambda e: e.tensor_copy(out=xgT[b2][:, 8:16, :], in_=psX[1][:]), reads=[B_psX[1]],
                                  writes=[B_xgT[b2]])
                for k in range(16):
                    kb.op("pe", lambda e, k=k: e.matmul(psg[:], lhsT=xgT[b2][:, k, :], rhs=Wg[wi][:, k, :], start=(k == 0), stop=(k == 15)),
                          reads=[B_xgT[b2], B_We[wi]], writes=[B_psg], inc=(k == 15))
                for k in range(16):
                    kb.op("pe", lambda e, k=k: e.matmul(psu[:], lhsT=xgT[b2][:, k, :], rhs=Wu[wi][:, k, :], start=(k == 0), stop=(k == 15)),
                          reads=[B_xgT[b2], B_We[wi]], writes=[B_psu], inc=(k == 15))
                kb.op("act", lambda e: e.activation(out=sgs[b2][:], in_=psg[:], func=AF.Silu), reads=[B_psg], writes=[B_sgs[b2]])
                kb.op("dve", lambda e: e.tensor_tensor(out=acts[b2][:], in0=psu[:], in1=sgs[b2][:], op=ALU.mult),
                      reads=[B_psu, B_sgs[b2]], writes=[B_acts[b2]])
                for fc in range(4):
                    kb.op("pe", lambda e, fc=fc: e.transpose(out=psA[:, fc, :], in_=acts[b2][:, fc * 128:(fc + 1) * 128], identity=ident_b[:]),
                          reads=[B_acts[b2]], writes=[B_psA], inc=(fc == 3))
                kb.op("act", lambda e: e.copy(out=actT[b2][:], in_=psA[:]), reads=[B_psA], writes=[B_actT[b2]])
                for dc in range(4):
                    yi = dc % 2
                    ds_ = slice(dc * 512, (dc + 1) * 512)
                    for fc in range(4):
                        kb.op("pe", lambda e, fc=fc: e.matmul(psy[yi][:], lhsT=actT[b2][:, fc, :], rhs=Wd[wi][:, fc, ds_],
                                                              start=(fc == 0), stop=(fc == 3)),
                              reads=[B_actT[b2], B_We[wi]], writes=[B_psy[yi]], inc=(fc == 3))
                    ydst, Byd = (yo[b2], B_yo[b2]) if shared else (yor, B_yor)
                    if yi == 0:
                        kb.op("dve", lambda e: e.tensor_copy(out=ydst[:, ds_], in_=psy[yi][:]), reads=[B_psy[yi]], writes=[Byd])
                    else:
                        kb.op("act", lambda e: e.copy(out=ydst[:, ds_], in_=psy[yi][:]), reads=[B_psy[yi]], writes=[Byd])
                if shared:
                    kb.dma("act", slot_d[SHR0 + j * 128:SHR0 + (j + 1) * 128, :], yo[b2][:], reads=[B_yo[b2]])

            NE = cfg_nexp
            for pc in piece_list(0):
                load_piece(pc)
            for e_ in range(NE):
                nxt = piece_list(e_ + 1) if e_ + 1 < NE else []
                if e_ < NEXP:
                    regs = nc.alloc_registers("nbreg%d" % e_, engines=[PEe, DVEe, ACTe, POOLe, SPe])
                    for ek, r in zip(ekeys, regs):
                        kb._wait(kb.eng[ek], B_nbt.w)
                        nc.reg_load(r, nbt[0:1, e_:e_ + 1])
                    nbv = nc.snap(regs, donate=True)
                    def guarded(j):
                        snap = kb.snapshot()
                        with nc.If(nbv > j):
                            block(e_, j)
                        with nc.Else():
                            kb.compensate(snap)

                    def group(js, inner=None):
                        snap = kb.snapshot()
                        with nc.If(nbv > js[0]):
                            for j in js:
                                guarded(j)
                            if inner is not None:
                                inner()
                        with nc.Else():
                            kb.compensate(snap)

                    def pieces(lo, hi):
                        for pc in nxt[lo:hi]:
                            load_piece(pc)

                    def chain(j):
                        snap = kb.snapshot()
                        with nc.If(nbv > j):
                            block(e_, j)
                            if j + 1 < 16:
                                chain(j + 1)
                        with nc.Else():
                            kb.compensate(snap)

                    guarded(0)
                    pieces(0, 6)
                    chain(1)
                    pieces(6, 12)
                    for r in regs:
                        nc.engines[r.engine].free_register(r)
                else:
                    flush_pending()
                    for j in range(16):
                        block(e_, j, shared=True)
            kb.barrier()
        if stop_after == "G":
            return nc

        with ExitStack() as ph:
            B_m3 = Buf()
            g2b = load_bc(ph, "sp", "g2b", mod_d[5:6, :], D, B_m3)
            fgb = load_bc(ph, "sp", "fgb", final_g[0:1, :], D, B_m3)
            dst = sbt(ph, "dst", [128, NT, 8], I32); w8s = sbt(ph, "w8s", [128, NT, 8])
            kb.dma("sp", dst[:], dest_d.rearrange("t p k -> p t k"), writes=[B_m3])
            kb.dma("sp", w8s[:], w8_d.rearrange("t p k -> p t k"), writes=[B_m3])
            x1l = [sbt(ph, "x1l%d" % i, [128, D]) for i in range(2)]; B_x1l = [Buf(), Buf()]
            accA = [sbt(ph, "accA%d" % i, [128, D]) for i in range(2)]; B_accA = [Buf(), Buf()]
            gb = [sbt(ph, "gb%d" % i, [128, D]) for i in range(6)]; B_gb = [Buf() for _ in range(6)]
            junk3 = [sbt(ph, "junk3_%d" % i, [128, D], BF16) for i in range(2)]; B_j3 = [Buf(), Buf()]
            ssq3 = [sbt(ph, "ssq3_%d" % i, [128, 4]) for i in range(2)]; B_s3 = [Buf(), Buf()]
            ot = [sbt(ph, "ot%d" % i, [128, D]) for i in range(2)]; B_ot = [Buf(), Buf()]
            gctr = 0
            for t in range(NT):
                bi = t % 2
                kb.dma("sp", x1l[bi][:], x1_d[t * 128:(t + 1) * 128, :], writes=[B_x1l[bi]])
                kb.dma("sp", accA[bi][:], slot_d[SHR0 + t * 128:SHR0 + (t + 1) * 128, :], writes=[B_accA[bi]])
                for k in range(8):
                    gi = gctr % 6
                    gctr += 1
                    kb.coll(lambda g, k=k, gi=gi: g.indirect_dma_start(
                        out=gb[gi][:, :], out_offset=None, in_=slot_d[:, :],
                        in_offset=bass.IndirectOffsetOnAxis(ap=dst[:, t, k:k + 1], axis=0)), reads=[B_m3], writes=[B_gb[gi]])
                    kb.op("dve", lambda e, k=k, gi=gi: e.scalar_tensor_tensor(
                        out=accA[bi][:], in0=gb[gi][:], scalar=w8s[:, t, k:k + 1], in1=accA[bi][:], op0=ALU.mult, op1=ALU.add),
                        reads=[B_gb[gi], B_m3, B_accA[bi]], writes=[B_accA[bi]])
                kb.op("dve", lambda e: e.tensor_tensor(out=accA[bi][:], in0=accA[bi][:], in1=g2b[:], op=ALU.mult),
                      reads=[B_accA[bi], B_m3], writes=[B_accA[bi]])
                kb.op("pool", lambda e: e.tensor_tensor(out=accA[bi][:], in0=accA[bi][:], in1=x1l[bi][:], op=ALU.add),
                      reads=[B_accA[bi], B_x1l[bi]], writes=[B_accA[bi]])
                rms_mod_tile(accA[bi][:], B_accA[bi], fgb[:], None, B_m3, junk3[bi], B_j3[bi], ssq3[bi], B_s3[bi], ot[bi], B_ot[bi],
                             None, None)
                kb.dma("sp", out_d[t * 128:(t + 1) * 128, :], ot[bi][:], reads=[B_ot[bi]])
            kb.barrier()
        return nc


def _host_consts():
    t = np.arange(128)
    ident = np.eye(128, dtype=np.float32)
    tri = (t[:, None] <= t[None, :]).astype(np.float32)
    triT = (t[:, None] >= t[None, :]).astype(np.float32)
    ones = np.ones((128, 128), np.float32)
    nmf = np.where(t[None, :] >= t[:, None], 0.0, NEG).astype(np.float32)
    nmb = np.where(t[None, :] <= t[:, None], 0.0, NEG).astype(np.float32)
    return np.stack([ident, tri, triT, ones, nmf, nmb])


def _rope_tables(s):
    pos = np.arange(-128, OWN + 128) + s * OWN
    pos = np.clip(pos, 0, SEQ - 1)
    rows = pos // 64
    cols = pos % 64
    inv = (10000.0 ** (-np.arange(0, 64, 2, dtype=np.float32) / 64)).astype(np.float32)
    ar = rows.astype(np.float32)[None, :] * inv[:, None]
    ac = cols.astype(np.float32)[None, :] * inv[:, None]
    cr, sr, cc, sc = np.cos(ar), np.sin(ar), np.cos(ac), np.sin(ac)
    C = np.concatenate([cr, cr, cc, cc], 0).astype(np.float32)
    S = np.concatenate([-sr, sr, -sc, sc], 0).astype(np.float32)
    return C, S


def _masks(s):
    import ml_dtypes
    j = np.arange(128)[:, None]
    r = np.arange(128)[None, :]
    lo = np.where(j >= r, 0.0, NEG).astype(np.float32)
    hi = np.where(j <= r, 0.0, NEG).astype(np.float32)
    allneg = np.full((128, 128), NEG, np.float32)
    lo0 = allneg if s == 0 else lo
    hi15 = allneg if s == 1 else hi
    m = np.stack([np.tile(a, (1, 4)) for a in (lo, hi, lo0, hi15)])
    return m.astype(ml_dtypes.bfloat16)


def _prep_inputs(inp):
    f = lambda a: np.ascontiguousarray(np.asarray(a, dtype=np.float32))
    x = f(inp["x"]); c = f(inp["c"]); ctx = f(inp["ctx"]); c_ctx = f(inp["c_ctx"])
    w_in = f(inp["w_in"][0])
    perm1 = np.concatenate([np.arange(32, 64), np.arange(0, 32), np.arange(96, 128), np.arange(64, 96)])
    perm = np.concatenate([h * 128 + perm1 for h in range(10)])
    w_in_sw = np.ascontiguousarray(w_in[:, :1280][:, perm])
    conv_w = f(inp["conv_w"][0])
    conv_wl = np.ascontiguousarray(conv_w.reshape(5, 16, 128).transpose(2, 1, 0))
    conv_bl = np.ascontiguousarray(f(inp["conv_b"][0]).reshape(16, 128).T)
    ew_gate = np.concatenate([f(inp["expert_w_gate"][0]), f(inp["shared_w_gate"][0])[None]], 0)
    ew_up = np.concatenate([f(inp["expert_w_up"][0]), f(inp["shared_w_up"][0])[None]], 0)
    ew_down = np.concatenate([f(inp["expert_w_down"][0]), f(inp["shared_w_down"][0])[None]], 0)
    consts = _host_consts()
    ee = np.arange(NEXP, dtype=np.float32)
    ebase = np.tile(np.concatenate([ee * OWN + 1.0, (ee + 1.0) * 4.0])[None, :], (128, 1)).astype(np.float32)
    tt = np.arange(128)
    lstrict = (tt[:, None] < tt[None, :]).astype(np.float32)
    oh2 = np.zeros((128, NEXP + 1), np.float32)
    oh2[tt, tt // 2] = 1.0
    oh2[:, NEXP] = (tt % 2) * 1024.0
    shared = dict(
        w_ada=f(inp["w_ada"][0]), b_ada=f(inp["b_ada"]), norm1_g=f(inp["norm1_g"]), norm2_g=f(inp["norm2_g"]),
        w_in=w_in, w_in_sw=w_in_sw, attn_sink=f(inp["attn_sink"]), conv_wl=conv_wl, conv_bl=conv_bl,
        dt_bias=f(inp["dt_bias"]).reshape(1, 32), a_log=f(inp["a_log"]).reshape(1, 32), d_skip=f(inp["d_skip"]),
        ssd_norm_g=f(inp["ssd_norm_g"]), w_out=f(inp["w_out"][0]), router_w=f(inp["router_w"][0]),
        router_bias=f(inp["router_bias"]), ew_gate=ew_gate, ew_up=ew_up, ew_down=ew_down,
        final_g=f(inp["final_norm_g"]).reshape(1, D), consts=consts, ebase=ebase, lstrict=lstrict, oh2=oh2)
    maps = []
    for core in range(8):
        b, s = core // 2, core % 2
        xo = x[b, s * OWN:(s + 1) * OWN]
        halo = np.zeros((2, 128, D), np.float32)
        if s == 1:
            halo[0] = x[b, OWN - 128:OWN]
        else:
            halo[1] = x[b, OWN:OWN + 128]
        cl = np.concatenate([c[b].reshape(16, 128).T, c_ctx.reshape(16, 128).T], 1)
        C, S = _rope_tables(s)
        fl = np.zeros((128, 4), np.float32)
        fl[:, 0] = s; fl[:, 1] = 1 - s; fl[:, 2] = 1.0 if s == 1 else 0.0; fl[:, 3] = 1.0 if s == 0 else 0.0
        dsel = 0 if s == 1 else 1
        xoth = x[b, (1 - s) * OWN:(2 - s) * OWN]
        xadj = xo[0:128] if s == 1 else xo[OWN - 128:OWN]
        tri, triT = consts[1], consts[2]
        extra = dict(x_oth=np.ascontiguousarray(xoth), x_adj=np.ascontiguousarray(xadj),
                     w_dt_sel=np.ascontiguousarray(w_in[:, 4608 + dsel * 16:4608 + dsel * 16 + 16]),
                     dtb_sel=np.ascontiguousarray(shared["dt_bias"][:, dsel * 16:(dsel + 1) * 16]),
                     alog_sel=np.ascontiguousarray(shared["a_log"][:, dsel * 16:(dsel + 1) * 16]),
                     tri_sel=np.ascontiguousarray(tri if dsel == 0 else triT))
        m = dict(shared)
        m.update(extra)
        m.update(x_own=np.ascontiguousarray(xo), x_halo=halo, ctx_b=np.ascontiguousarray(ctx[b]),
                 c_lay=np.ascontiguousarray(cl), rope_c=C, rope_s=S, cmask=_masks(s), flags=fl)
        maps.append(m)
    return maps


def kernel(**inputs):
    maps = _prep_inputs(inputs)
    nc = build()
    res = run_bass_kernel_spmd(nc, maps, core_ids=list(range(8)))
    out = np.zeros((NB, SEQ, D), np.float32)
    for core in range(8):
        b, s = core // 2, core % 2
        out[b, s * OWN:(s + 1) * OWN] = res.results[core]["out"]
    return out
```

```python
from contextlib import ExitStack

import numpy as np
import concourse.bass as bass
import concourse.mybir as mybir
from concourse.bass_utils import run_bass_kernel_spmd

F32 = mybir.dt.float32
BF16 = mybir.dt.bfloat16
AF = mybir.ActivationFunctionType
ALU = mybir.AluOpType
AX = mybir.AxisListType

D = 2048
SEQ = 4096
NB = 4
CTX = 256
OWN = 2048
NT = 16
INW = 4640
NEXP = 64
EPS = 1e-6
NEG = -30000.0
NSLOT = 8


class Tok:
    __slots__ = ("sem", "val", "ek")

    def __init__(self, sem, val, ek):
        self.sem, self.val, self.ek = sem, val, ek


class Buf:
    def __init__(self, name=""):
        self.name = name
        self.w = None
        self.r = {}


class Eng:
    def __init__(self, key, obj, sem):
        self.key, self.obj, self.sem = key, obj, sem
        self.count = 0
        self.pending = False
        self.seen = {}
        self.slots = []
        self.nslot = 0


class KB:
    def __init__(self, nc, es):
        self.nc = nc
        self.eng = {}
        for key, obj in (("pe", nc.tensor), ("dve", nc.vector), ("act", nc.scalar),
                         ("pool", nc.gpsimd), ("sp", nc.sync)):
            sem = es.enter_context(nc.semaphore("s_" + key))
            self.eng[key] = Eng(key, obj, sem)
        for key in ("sp", "pool", "act"):
            e = self.eng[key]
            for i in range(NSLOT):
                e.slots.append([es.enter_context(nc.semaphore("d_%s%d" % (key, i))), 0])

    def _wait(self, e, tok):
        if tok is None:
            return
        sid = id(tok.sem)
        if e.seen.get(sid, 0) >= tok.val:
            return
        e.obj.wait_ge(tok.sem, tok.val)
        e.seen[sid] = tok.val

    def _deps(self, e, reads, writes):
        for b in reads:
            if b.w is not None and not (b.w.ek == e.key and e.key == "pe"):
                self._wait(e, b.w)
        for b in writes:
            if b.w is not None and not (b.w.ek == e.key and e.key == "pe"):
                self._wait(e, b.w)
            for ek, t in b.r.items():
                if ek != e.key:
                    self._wait(e, t)

    def op(self, ek, fn, reads=(), writes=(), inc=True):
        e = self.eng[ek]
        self._deps(e, reads, writes)
        ins = fn(e.obj)
        if inc:
            ins.then_inc(e.sem, 1)
            e.count += 1
            e.pending = False
            tok = Tok(e.sem, e.count, ek)
        else:
            e.pending = True
            tok = Tok(e.sem, e.count + 1, ek)
        for b in writes:
            b.w = tok
            b.r = {}
        for b in reads:
            b.r[ek] = tok
        return ins

    def dma(self, qk, out, in_, reads=(), writes=(), **kw):
        e = self.eng[qk]
        slot = e.slots[e.nslot % NSLOT]
        e.nslot += 1
        if slot[1] > 0:
            self._wait(e, Tok(slot[0], 16 * slot[1], "dma"))
        self._deps(e, reads, writes)
        ins = e.obj.dma_start(out=out, in_=in_, **kw)
        ins.then_inc(slot[0], 16)
        slot[1] += 1
        tok = Tok(slot[0], 16 * slot[1], "dma_" + qk + str(id(slot[0])))
        for b in writes:
            b.w = tok
            b.r = {}
        for b in reads:
            b.r[tok.ek] = tok
        return ins

    def coll(self, fn, reads=(), writes=()):
        e = self.eng["pool"]
        slot = e.slots[e.nslot % NSLOT]
        e.nslot += 1
        if slot[1] > 0:
            self._wait(e, Tok(slot[0], 16 * slot[1], "dma"))
        self._deps(e, reads, writes)
        ins = fn(e.obj)
        ins.then_inc(slot[0], 16)
        slot[1] += 1
        tok = Tok(slot[0], 16 * slot[1], "dma_coll")
        for b in writes:
            b.w = tok
            b.r = {}
        for b in reads:
            b.r[tok.ek] = tok
        return ins

    def snapshot(self):
        for e in self.eng.values():
            assert not e.pending
        return {k: (e.count, [s[1] for s in e.slots], dict(e.seen)) for k, e in self.eng.items()}

    def compensate(self, snap):
        for k, e in self.eng.items():
            c0, uses0, seen0 = snap[k]
            assert not e.pending
            if e.count > c0:
                if c0 > 0:
                    e.obj.wait_ge(e.sem, c0)
                e.obj.sem_inc(e.sem, e.count - c0)
            for s, u0 in zip(e.slots, uses0):
                if s[1] > u0:
                    if u0 > 0:
                        e.obj.wait_ge(s[0], 16 * u0)
                    e.obj.sem_inc(s[0], 16 * (s[1] - u0))
            e.seen = dict(seen0)

    def barrier(self):
        toks = []
        for e in self.eng.values():
            assert not e.pending, e.key
            if e.count:
                toks.append(Tok(e.sem, e.count, e.key))
            for s in e.slots:
                if s[1]:
                    toks.append(Tok(s[0], 16 * s[1], "dma"))
        for e in self.eng.values():
            for t in toks:
                if t.ek == e.key:
                    continue
                self._wait(e, t)


def bcast_row(ap_row, n=128):
    return ap_row.partition_broadcast(n)


def build(stop_after=None, dbg=(), ncores=8, cut=99, cfg_nexp=NEXP + 1):
    nc = bass.Bass("TRN2", target_bir_lowering=False)
    dbg = set(dbg)

    class _Lazy:
        def __init__(self, name, shape, dt):
            self.name, self.shape, self.dt, self._ap = name, shape, dt, None

        @property
        def ap(self):
            if self._ap is None:
                self._ap = nc.dram_tensor(self.name, list(self.shape), self.dt, kind="ExternalInput").ap()
                used_inputs.append(self.name)
            return self._ap

        def __getitem__(self, key):
            return self.ap[key]

        def rearrange(self, *a, **k):
            return self.ap.rearrange(*a, **k)

    used_inputs = []
    nc._used_inputs = used_inputs

    def din(name, shape, dt=F32):
        return _Lazy(name, shape, dt)

    def dscr(name, shape, dt=F32):
        kind = "ExternalOutput" if name in dbg else "Internal"
        return nc.dram_tensor(name, list(shape), dt, kind=kind).ap()

    x_own = din("x_own", [OWN, D])
    x_halo = din("x_halo", [2, 128, D])
    ctx_in = din("ctx_b", [CTX, D])
    c_lay = din("c_lay", [128, 32])
    w_ada = din("w_ada", [D, 6 * D])
    b_ada = din("b_ada", [1, 6 * D])
    norm1_g = din("norm1_g", [1, D])
    norm2_g = din("norm2_g", [1, D])
    w_in = din("w_in", [D, INW])
    w_in_sw = din("w_in_sw", [D, 1280])
    attn_sink = din("attn_sink", [1, 8])
    conv_wl = din("conv_wl", [128, 16, 5])
    conv_bl = din("conv_bl", [128, 16])
    dt_bias = din("dt_bias", [1, 32])
    a_log = din("a_log", [1, 32])
    d_skip = din("d_skip", [1, 16])
    ssd_norm_g = din("ssd_norm_g", [1, 1024])
    w_out = din("w_out", [D, D])
    router_w = din("router_w", [D, NEXP])
    router_bias = din("router_bias", [1, NEXP])
    ew_gate = din("ew_gate", [NEXP + 1, D, 512])
    ew_up = din("ew_up", [NEXP + 1, D, 512])
    ew_down = din("ew_down", [NEXP + 1, 512, D])
    final_g = din("final_g", [1, D])
    rope_c = din("rope_c", [128, 2304])
    rope_s = din("rope_s", [128, 2304])
    cmask = din("cmask", [4, 128, 512], BF16)
    flags = din("flags", [128, 4])
    consts = din("consts", [6, 128, 128])
    x_oth = din("x_oth", [OWN, D])
    x_adj = din("x_adj", [128, D])
    w_dt_sel = din("w_dt_sel", [D, 16])
    dtb_sel = din("dtb_sel", [1, 16])
    alog_sel = din("alog_sel", [1, 16])
    tri_sel = din("tri_sel", [128, 128])
    ebase = din("ebase", [128, 2 * NEXP])
    lstrict = din("lstrict", [128, 128])
    oh2 = din("oh2", [128, NEXP + 1])

    out_d = nc.dram_tensor("out", [OWN, D], F32, kind="ExternalOutput").ap()

    mod_d = dscr("mod_d", [8, D])
    qT_d = dscr("qT_d", [8, 128, OWN], BF16)
    kT_d = dscr("kT_d", [2, 128, 2304], BF16)
    kcT_d = dscr("kcT_d", [2, 128, CTX], BF16)
    v_d = dscr("v_d", [20, 128, 256], BF16)
    zs_d = dscr("zs_d", [NT, 128, 1024])
    dtraw_d = dscr("dtraw_d", [18, 128, 32])
    uT_d = dscr("uT_d", [16, 128, OWN], BF16)
    uTc_d = dscr("uTc_d", [16, 128, CTX], BF16)
    mixT_d = dscr("mixT_d", [16, 128, OWN], BF16)
    uT2_d = dscr("uT2_d", [12, 128, OWN], BF16)
    dtraw2_d = dscr("dtraw2_d", [NT, 128, 16])
    h0_d = dscr("h0_d", [2, NT, 128, 1024])
    xch_in = dscr("xch_in", [128, 2112])
    xch_out = dscr("xch_out", [256, 2112])
    x1_d = dscr("x1_d", [OWN, D])
    h2T_d = dscr("h2T_d", [16, 128, OWN], BF16)
    gate_d = dscr("gate_d", [NT, 128, NEXP + 1])
    I32 = mybir.dt.int32
    NSL = NEXP * OWN
    h2tok_d = dscr("h2tok_d", [OWN + 128, D], BF16)
    idxtab_d = dscr("idxtab_d", [NSL, 1], I32)
    dest_d = dscr("dest_d", [NT, 128, 8], I32)
    w8_d = dscr("w8_d", [NT, 128, 8])
    nblk_d = dscr("nblk_d", [1, NEXP], I32)
    NSLC = 192 * 128
    SHR0 = NSLC + OWN
    slot_d = dscr("slot_d", [SHR0 + OWN, D])
    cbase_d = dscr("cbase_d", [1, NEXP], I32)
    rowtab_d = dscr("rowtab_d", [NSL, 1], I32)

    es = ExitStack()
    with es:
        kb = KB(nc, es)

        def sbt(st, name, shape, dt=F32):
            return st.enter_context(nc.sbuf_tensor(name, list(shape), dt))

        def pst(st, name, shape, dt=F32):
            return st.enter_context(nc.psum_tensor(name, list(shape), dt))

        ident_f = sbt(es, "ident_f", [128, 128]); ones_f = sbt(es, "ones_f", [128, 128])
        ident_b = sbt(es, "ident_b", [128, 128], BF16); ones_b = sbt(es, "ones_b", [128, 128], BF16)
        flg = sbt(es, "flg", [128, 4])
        B_const = Buf("const")
        for i, t in ((0, ident_f), (3, ones_f)):
            kb.dma("sp", t[:], consts[i], writes=[B_const])
        kb.dma("sp", flg[:], flags[:, :], writes=[B_const])
        kb.op("dve", lambda e: e.tensor_copy(out=ident_b[:], in_=ident_f[:]), reads=[B_const], writes=[B_const])
        kb.op("dve", lambda e: e.tensor_copy(out=ones_b[:], in_=ones_f[:]), reads=[B_const], writes=[B_const])
        kb.barrier()

        with ExitStack() as ph:
            cl = sbt(ph, "cl", [128, 32]); cs = sbt(ph, "cs", [128, 32])
            LC = sbt(ph, "LC", [128, 32, 128], BF16)
            Wb = [sbt(ph, "Wa%d" % i, [128, 16, 512], BF16) for i in range(2)]
            bb = [sbt(ph, "ba%d" % i, [128, 512]) for i in range(2)]
            mo = [sbt(ph, "mo%d" % i, [128, 512]) for i in range(4)]
            psA = [pst(ph, "psA%d" % i, [128, 512]) for i in range(4)]
            B_cl, B_LC = Buf(), Buf()
            B_W = [Buf(), Buf()]; B_b = [Buf(), Buf()]; B_mo = [Buf() for _ in range(4)]
            B_ps = [Buf() for _ in range(4)]
            kb.dma("sp", cl[:], c_lay[:, :], writes=[B_cl])
            kb.op("act", lambda e: e.activation(out=cs[:], in_=cl[:], func=AF.Silu), reads=[B_cl], writes=[B_cl])
            for k in range(32):
                kb.op("dve", lambda e, k=k: e.tensor_copy(out=LC[:, k, :], in_=cs[:, k:k + 1].to_broadcast([128, 128])),
                      reads=[B_cl], writes=[B_LC])
            wv = w_ada.rearrange("(k p) n -> p k n", p=128)
            nblk = 24
            it = 0
            for j in range(nblk):
                bi = j % 2
                kb.dma("pool", Wb[bi][:], wv[:, :, j * 512:(j + 1) * 512], writes=[B_W[bi]])
                kb.dma("sp", bb[bi][:], bcast_row(b_ada[0:1, j * 512:(j + 1) * 512]), writes=[B_b[bi]])
                chunk = j // 4
                for which in range(2 if j < 8 else 1):
                    pi = it % 4
                    it += 1
                    for k in range(16):
                        kb.op("pe", lambda e, k=k, pi=pi, which=which, bi=bi: e.matmul(
                            psA[pi][:], lhsT=LC[:, which * 16 + k, :], rhs=Wb[bi][:, k, :],
                            start=(k == 0), stop=(k == 15)),
                            reads=[B_LC, B_W[bi]], writes=[B_ps[pi]], inc=(k == 15))
                    if chunk in (1, 4):
                        kb.op("dve", lambda e, pi=pi, bi=bi: e.scalar_tensor_tensor(
                            out=mo[pi][:], in0=psA[pi][:], scalar=1.0, in1=bb[bi][:], op0=ALU.add, op1=ALU.add),
                            reads=[B_ps[pi], B_b[bi]], writes=[B_mo[pi]])
                    else:
                        kb.op("dve", lambda e, pi=pi, bi=bi: e.tensor_tensor(
                            out=mo[pi][:], in0=psA[pi][:], in1=bb[bi][:], op=ALU.add),
                            reads=[B_ps[pi], B_b[bi]], writes=[B_mo[pi]])
                    row = chunk if which == 0 else 6 + chunk
                    c0 = (j % 4) * 512
                    kb.dma("sp", mod_d[row:row + 1, c0:c0 + 512], mo[pi][0:1, :], reads=[B_mo[pi]])
            kb.barrier()
        if stop_after == "A":
            return nc

        def load_bc(st, qk, name, row_ap, width, buf):
            t = sbt(st, name, [128, width])
            kb.dma(qk, t[:], bcast_row(row_ap), writes=[buf])
            return t

        def rms_mod_tile(xt, B_x, G, shv, B_mod, junk, B_junk, ssq, B_ss, tmp, B_tmp, outb, B_out, width=D):
            kb.op("act", lambda e: e.activation(out=junk[:], in_=xt, func=AF.Square, accum_out=ssq[:, 0:1]),
                  reads=[B_x], writes=[B_junk, B_ss])
            kb.op("dve", lambda e: e.tensor_scalar(out=ssq[:, 1:2], in0=ssq[:, 0:1], scalar1=1.0 / width, scalar2=EPS,
                                                   op0=ALU.mult, op1=ALU.add), reads=[B_ss], writes=[B_ss])
            kb.op("act", lambda e: e.sqrt(out=ssq[:, 3:4], in_=ssq[:, 1:2]), reads=[B_ss], writes=[B_ss])
            kb.op("dve", lambda e: e.reciprocal(out=ssq[:, 2:3], in_=ssq[:, 3:4]), reads=[B_ss], writes=[B_ss])
            kb.op("dve", lambda e: e.scalar_tensor_tensor(out=tmp[:], in0=xt, scalar=ssq[:, 2:3], in1=G,
                                                          op0=ALU.mult, op1=ALU.mult),
                  reads=[B_x, B_ss, B_mod], writes=[B_tmp])
            if shv is not None:
                kb.op("dve", lambda e: e.tensor_tensor(out=outb, in0=tmp[:], in1=shv, op=ALU.add),
                      reads=[B_tmp, B_mod], writes=[B_out])

        def phase_BC(tag, cfg):
          with ExitStack() as phBC:
              ntile = cfg["ntile"]
              hT = sbt(phBC, "hT" + tag, [128, ntile, 16, 128], BF16)
              B_hT = [Buf("hT%d" % i) for i in range(ntile)]
              with ExitStack() as ph:
                  B_mod = Buf("mod1")
                  g1n = load_bc(ph, "sp", "g1n" + tag, norm1_g[0:1, :], D, B_mod)
                  GL = load_bc(ph, "sp", "GL" + tag, mod_d[1:2, :], D, B_mod)
                  SHL = load_bc(ph, "sp", "SHL" + tag, mod_d[0:1, :], D, B_mod)
                  GC = load_bc(ph, "sp", "GC" + tag, mod_d[7:8, :], D, B_mod)
                  SHC = load_bc(ph, "sp", "SHC" + tag, mod_d[6:7, :], D, B_mod)
                  kb.op("dve", lambda e: e.tensor_tensor(out=GL[:], in0=GL[:], in1=g1n[:], op=ALU.mult),
                        reads=[B_mod], writes=[B_mod])
                  kb.op("dve", lambda e: e.tensor_tensor(out=GC[:], in0=GC[:], in1=g1n[:], op=ALU.mult),
                        reads=[B_mod], writes=[B_mod])
                  xt = [sbt(ph, "xt%d" % i + tag, [128, D]) for i in range(2)]
                  B_x = [Buf(), Buf()]
                  junk = sbt(ph, "junk" + tag, [128, D], BF16); B_junk = Buf()
                  tmp = sbt(ph, "tmpB" + tag, [128, D]); B_tmp = Buf()
                  hb = [sbt(ph, "hb%d" % i + tag, [128, D], BF16) for i in range(2)]
                  B_hb = [Buf(), Buf()]
                  ssq = [sbt(ph, "ssq%d" % i + tag, [128, 4]) for i in range(2)]
                  B_ss = [Buf(), Buf()]
                  psT = [pst(ph, "psT%d" % i + tag, [128, 8, 128], BF16) for i in range(2)]
                  B_psT = [Buf(), Buf()]

                  src_of = cfg["src_of"]

                  kb.dma("sp", xt[0][:], src_of(0), writes=[B_x[0]])
                  for t in range(ntile):
                      bi = t % 2
                      if t + 1 < ntile:
                          kb.dma("sp", xt[1 - bi][:], src_of(t + 1), writes=[B_x[1 - bi]])
                      G, SHv = (GC, SHC) if t < cfg["nctx"] else (GL, SHL)
                      rms_mod_tile(xt[bi][:], B_x[bi], G[:], SHv[:], B_mod, junk, B_junk, ssq[bi], B_ss[bi],
                                   tmp, B_tmp, hb[bi][:], B_hb[bi])
                      for half in range(2):
                          for kk in range(8):
                              k = half * 8 + kk
                              kb.op("pe", lambda e, k=k, kk=kk, half=half, bi=bi: e.transpose(
                                  out=psT[half][:, kk, :], in_=hb[bi][:, k * 128:(k + 1) * 128], identity=ident_b[:]),
                                  reads=[B_hb[bi]], writes=[B_psT[half]], inc=(kk == 7))
                          eng = "act" if half == 0 else "dve"
                          kb.op(eng, lambda e, t=t, half=half: e.tensor_copy(out=hT[:, t, half * 8:(half + 1) * 8, :],
                                                                            in_=psT[half][:]) if eng != "act" else
                                e.copy(out=hT[:, t, half * 8:(half + 1) * 8, :], in_=psT[half][:]),
                                reads=[B_psT[half]], writes=[B_hT[t]])
                  kb.barrier()
              with ExitStack() as ph:
                  wv = w_in.rearrange("(k p) n -> p k n", p=128)
                  wsv = w_in_sw.rearrange("(k p) n -> p k n", p=128)
                  Wn = [sbt(ph, "Wn%d" % i + tag, [128, 16, 512], BF16) for i in range(2)]
                  Ws = [sbt(ph, "Ws%d" % i + tag, [128, 16, 128], BF16) for i in range(2)]
                  B_Wn = [Buf(), Buf()]; B_Ws = [Buf(), Buf()]
                  B_cst = Buf()
                  rc = sbt(ph, "rc" + tag, [128, 2304]); rs = sbt(ph, "rs" + tag, [128, 2304])
                  kb.dma("sp", rc[:], rope_c[:, :], writes=[B_cst])
                  kb.dma("sp", rs[:], rope_s[:, :], writes=[B_cst])
                  cw = sbt(ph, "cw" + tag, [128, 16, 5]); cb = sbt(ph, "cb" + tag, [128, 16])
                  kb.dma("sp", cw[:], conv_wl[:, :, :], writes=[B_cst])
                  kb.dma("sp", cb[:], conv_bl[:, :], writes=[B_cst])
                  psC = [pst(ph, "psC%d" % i + tag, [128, 512]) for i in range(4)]
                  B_psC = [Buf() for _ in range(4)]
                  ev = [sbt(ph, "ev%d" % i + tag, [128, 512]) for i in range(4)]
                  B_ev = [Buf() for _ in range(4)]
                  evb = [sbt(ph, "evb%d" % i + tag, [128, 512], BF16) for i in range(2)]
                  B_evb = [Buf(), Buf()]
                  v_all = sbt(ph, "v_all" + tag, [128, 20, 256], BF16); B_vall = Buf()
                  dt_all = sbt(ph, "dt_all" + tag, [128, 18, 32]); B_dtall = Buf()
                  xraw = [sbt(ph, "xraw%d" % i + tag, [128, 2052]) for i in range(2)]
                  B_xraw = [Buf(), Buf()]
                  xrawc = [sbt(ph, "xrawc%d" % i + tag, [128, 260]) for i in range(2)]
                  B_xrawc = [Buf(), Buf()]
                  acc0 = sbt(ph, "acc0" + tag, [128, 2048])
                  acc = [acc0, acc0]
                  B_acc0 = Buf()
                  B_acc = [B_acc0, B_acc0]
                  accc = sbt(ph, "accc" + tag, [128, 256]); B_accc = Buf()
                  uo = [sbt(ph, "uo%d" % i + tag, [128, 2048], BF16) for i in range(2)]
                  B_uo = [Buf(), Buf()]
                  uoc = [sbt(ph, "uoc%d" % i + tag, [128, 256], BF16) for i in range(2)]
                  B_uoc = [Buf(), Buf()]
                  for i in range(2):
                      kb.op("pool", lambda e, i=i: e.memset(xrawc[i][:], 0.0), writes=[B_xrawc[i]])

                  jobs = cfg["jobs"]

                  def load_job(ji):
                      kind, c0, ncol, idx = jobs[ji]
                      bi = ji % 2
                      wsrc = cfg["wdt"] if (kind == "dt" and cfg.get("wdt") is not None) else wv[:, :, c0:c0 + ncol]
                      kb.dma("pool", Wn[bi][:, :, 0:ncol], wsrc, writes=[B_Wn[bi]])
                      if kind in ("q", "k"):
                          kb.dma("pool", Ws[bi][:], wsv[:, :, c0:c0 + 128], writes=[B_Ws[bi]])

                  pctr = [0]

                  def fm_mm(W, B_W, tile0, ntile, tok_lo=0, tok_hi=128):
                      pi = pctr[0] % 4
                      pctr[0] += 1
                      n = ntile * (tok_hi - tok_lo)
                      for k in range(16):
                          kb.op("pe", lambda e, k=k: e.matmul(
                              psC[pi][:, 0:n], lhsT=W[:, k, 0:128], rhs=hT[:, tile0:tile0 + ntile, k, tok_lo:tok_hi],
                              start=(k == 0), stop=(k == 15)),
                              reads=[B_W] + B_hT[tile0:tile0 + ntile], writes=[B_psC[pi]], inc=(k == 15))
                      return pi

                  def tm_mm(W, B_W, t, ncol):
                      pi = pctr[0] % 4
                      pctr[0] += 1
                      for k in range(16):
                          kb.op("pe", lambda e, k=k: e.matmul(
                              psC[pi][:, 0:ncol], lhsT=hT[:, t, k, :], rhs=W[:, k, 0:ncol],
                              start=(k == 0), stop=(k == 15)),
                              reads=[B_W, B_hT[t]], writes=[B_psC[pi]], inc=(k == 15))
                      return pi

                  ectr = [0]
                  load_job(0)
                  for ji, (kind, c0, ncol, idx) in enumerate(jobs):
                      if ji + 1 < len(jobs):
                          load_job(ji + 1)
                      bi = ji % 2
                      W, BW = Wn[bi], B_Wn[bi]
                      if kind in ("q", "k"):
                          if kind == "q":
                              groups = [(3 + 4 * g, 4, 128 + g * 512, g * 512) for g in range(4)]
                              dest = qT_d[idx]
                          else:
                              groups = [(2 + 4 * g, 4, g * 512, g * 512) for g in range(4)] + [(18, 2, 2048, 2048)]
                              dest = kT_d[idx]
                          for (t0, ntl, roff, doff) in groups:
                              n = ntl * 128
                              pa = fm_mm(W, BW, t0, ntl)
                              pb = fm_mm(Ws[bi], B_Ws[bi], t0, ntl)
                              e0, e1 = (ectr[0] % 2) * 2, (ectr[0] % 2) * 2 + 1
                              eb = ectr[0] % 2
                              ectr[0] += 1
                              kb.op("dve", lambda e: e.tensor_tensor(out=ev[e0][:, 0:n], in0=psC[pa][:, 0:n],
                                                                     in1=rc[:, roff:roff + n], op=ALU.mult),
                                    reads=[B_psC[pa], B_cst], writes=[B_ev[e0]])
                              kb.op("dve", lambda e: e.tensor_tensor(out=ev[e1][:, 0:n], in0=psC[pb][:, 0:n],
                                                                     in1=rs[:, roff:roff + n], op=ALU.mult),
                                    reads=[B_psC[pb], B_cst], writes=[B_ev[e1]])
                              kb.op("pool", lambda e: e.tensor_tensor(out=evb[eb][:, 0:n], in0=ev[e0][:, 0:n],
                                                                      in1=ev[e1][:, 0:n], op=ALU.add),
                                    reads=[B_ev[e0], B_ev[e1]], writes=[B_evb[eb]])
                              kb.dma("sp", dest[:, doff:doff + n], evb[eb][:, 0:n], reads=[B_evb[eb]])
                          if kind == "k":
                              pa = fm_mm(W, BW, 0, 2)
                              eb = ectr[0] % 2
                              ectr[0] += 1
                              kb.op("act", lambda e: e.copy(out=evb[eb][:, 0:256], in_=psC[pa][:, 0:256]),
                                    reads=[B_psC[pa]], writes=[B_evb[eb]])
                              kb.dma("sp", kcT_d[idx], evb[eb][:, 0:256], reads=[B_evb[eb]])
                      elif kind == "v":
                          for t in range(20):
                              pa = tm_mm(W, BW, t, 256)
                              kb.op("act", lambda e, t=t: e.copy(out=v_all[:, t, :], in_=psC[pa][:, 0:256]),
                                    reads=[B_psC[pa]], writes=[B_vall])
                          kb.dma("sp", v_d.rearrange("t p c -> p t c"), v_all[:], reads=[B_vall])
                      elif kind == "z":
                          for t in range(NT):
                              pa = tm_mm(W, BW, 3 + t, 512)
                              e0 = ectr[0] % 4
                              ectr[0] += 1
                              kb.op("act", lambda e: e.activation(out=ev[e0][:], in_=psC[pa][:], func=AF.Silu),
                                    reads=[B_psC[pa]], writes=[B_ev[e0]])
                              kb.dma("sp", zs_d[t][:, idx * 512:(idx + 1) * 512], ev[e0][:], reads=[B_ev[e0]])
                      elif kind == "dt":
                          dtt = cfg["dt_tiles"]
                          for i, t in enumerate(dtt):
                              pa = tm_mm(W, BW, t, ncol)
                              kb.op("act", lambda e, i=i: e.copy(out=dt_all[:, i, 0:ncol], in_=psC[pa][:, 0:ncol]),
                                    reads=[B_psC[pa]], writes=[B_dtall])
                          kb.dma("sp", cfg["dt_dst"].rearrange("t p c -> p t c"), dt_all[:, 0:len(dtt), 0:ncol], reads=[B_dtall])
                      else:
                          j = idx
                          xi = j % 2
                          xr, Bxr = xraw[xi], B_xraw[xi]
                          for g in range(4):
                              pa = fm_mm(W, BW, cfg["own0"] + 4 * g, 4)
                              kb.op("act", lambda e, g=g: e.copy(out=xr[:, 2 + g * 512:2 + (g + 1) * 512], in_=psC[pa][:]),
                                    reads=[B_psC[pa]], writes=[Bxr])
                          pa = fm_mm(W, BW, cfg["halo_lo"], 1, 126, 128)
                          kb.op("dve", lambda e: e.tensor_scalar(out=xr[:, 0:2], in0=psC[pa][:, 0:2], scalar1=cfg["fl_lo"],
                                                                 scalar2=None, op0=ALU.mult),
                                reads=[B_psC[pa]], writes=[Bxr])
                          pa = fm_mm(W, BW, cfg["halo_hi"], 1, 0, 2)
                          kb.op("dve", lambda e: e.tensor_scalar(out=xr[:, 2050:2052], in0=psC[pa][:, 0:2],
                                                                 scalar1=cfg["fl_hi"], scalar2=None, op0=ALU.mult),
                                reads=[B_psC[pa]], writes=[Bxr])
                          if cfg["nctx"]:
                              pa = fm_mm(W, BW, 0, 2)
                              kb.op("act", lambda e: e.copy(out=xrawc[xi][:, 2:258], in_=psC[pa][:, 0:256]),
                                    reads=[B_psC[pa]], writes=[B_xrawc[xi]])
                          convs = [(xr, Bxr, acc[xi], B_acc[xi], 2048, uo[xi], B_uo[xi], cfg["uT_dst"][j])]
                          if cfg["nctx"]:
                              convs.append((xrawc[xi], B_xrawc[xi], accc, B_accc, 256, uoc[xi], B_uoc[xi], uTc_d[j]))
                          for (src, Bs, dst, Bd, n, o, Bo, dd) in convs:
                              kb.op("dve", lambda e: e.tensor_scalar(out=dst[:, 0:n], in0=src[:, 0:n], scalar1=cw[:, j, 0:1],
                                                                     scalar2=None, op0=ALU.mult),
                                    reads=[Bs, B_cst], writes=[Bd])
                              for tap in range(1, 5):
                                  kb.op("dve", lambda e, tap=tap: e.scalar_tensor_tensor(
                                      out=dst[:, 0:n], in0=src[:, tap:tap + n], scalar=cw[:, j, tap:tap + 1],
                                      in1=dst[:, 0:n], op0=ALU.mult, op1=ALU.add),
                                      reads=[Bs, B_cst, Bd], writes=[Bd])
                              kb.op("act", lambda e: e.activation(out=o[:, 0:n], in_=dst[:, 0:n], func=AF.Silu,
                                                                  bias=cb[:, j:j + 1]),
                                    reads=[Bd, B_cst], writes=[Bo])
                              kb.dma("sp", dd, o[:, 0:n], reads=[Bo])
                  kb.barrier()
        def main_src(t):
            if t < 2:
                return ctx_in[t * 128:(t + 1) * 128, :]
            if t == 2:
                return x_halo[0]
            if t == 19:
                return x_halo[1]
            return x_own[(t - 3) * 128:(t - 2) * 128, :]

        jobs_main = []
        for h in range(8):
            jobs_main.append(("q", h * 128, 128, h))
        for kv in range(2):
            jobs_main.append(("k", 1024 + kv * 128, 128, kv))
        jobs_main.append(("v", 1280, 256, 0))
        jobs_main.append(("z", 1536, 512, 0))
        jobs_main.append(("z", 2048, 512, 1))
        jobs_main.append(("dt", 4608, 32, 0))
        for j in range(16):
            jobs_main.append(("x", 2560 + j * 128, 128, j))
        phase_BC("m", dict(ntile=20, nctx=2, src_of=main_src, jobs=jobs_main, own0=3, halo_lo=2, halo_hi=19,
                           fl_lo=flg[:, 2:3], fl_hi=flg[:, 3:4], uT_dst=uT_d, dt_tiles=[0, 1] + list(range(3, 19)),
                           dt_dst=dtraw_d, wdt=None))
        if stop_after == "C":
            return nc

        def oth_src(t):
            if t == 0:
                return x_adj[:, :]
            return x_oth[(t - 1) * 128:t * 128, :]

        jobs_oth = [("dt", 0, 16, 0)] + [("x", 2560 + j * 128, 128, j) for j in range(12)]
        phase_BC("o", dict(ntile=17, nctx=0, src_of=oth_src, jobs=jobs_oth, own0=1, halo_lo=0, halo_hi=0,
                           fl_lo=flg[:, 3:4], fl_hi=flg[:, 2:3], uT_dst=uT2_d, dt_tiles=list(range(1, 17)),
                           dt_dst=dtraw2_d, wdt=w_dt_sel.rearrange("(k p) n -> p k n", p=128)))
        if stop_after == "C2":
            return nc

        with ExitStack() as ph:
            qT = sbt(ph, "qT", [128, 8, OWN], BF16)
            kT = sbt(ph, "kT", [128, 2, 2304], BF16)
            kcT = sbt(ph, "kcT", [128, 2, CTX], BF16)
            vv = sbt(ph, "vv", [128, 20, 256], BF16)
            msk = sbt(ph, "msk", [128, 4, 512], BF16)
            esk = sbt(ph, "esk", [128, 8])
            B_in = Buf()
            kb.dma("sp", qT[:], qT_d.rearrange("h p t -> p h t"), writes=[B_in])
            kb.dma("sp", kT[:], kT_d.rearrange("h p t -> p h t"), writes=[B_in])
            kb.dma("sp", kcT[:], kcT_d.rearrange("h p t -> p h t"), writes=[B_in])
            kb.dma("sp", vv[:], v_d.rearrange("t p c -> p t c"), writes=[B_in])
            kb.dma("sp", msk[:], cmask.rearrange("m p c -> p m c"), writes=[B_in])
            kb.dma("sp", esk[:], bcast_row(attn_sink[0:1, :]), writes=[B_in])
            kb.op("act", lambda e: e.activation(out=esk[:], in_=esk[:], func=AF.Exp), reads=[B_in], writes=[B_in])
            psS = [pst(ph, "psS%d" % i, [128, 512]) for i in range(4)]
            psO = [pst(ph, "psO%d" % i, [128, 512]) for i in range(2)]
            psD = [pst(ph, "psD%d" % i, [128, 512]) for i in range(2)]
            B_psS = [Buf() for _ in range(4)]; B_psO = [Buf(), Buf()]; B_psD = [Buf(), Buf()]
            Eb = [sbt(ph, "Eb%d" % i, [128, 512], BF16) for i in range(6)]
            B_E = [Buf() for _ in range(6)]
            den = [sbt(ph, "den%d" % i, [128, 512]) for i in range(2)]
            B_den = [Buf(), Buf()]
            ob = [sbt(ph, "ob%d" % i, [128, 512], BF16) for i in range(2)]
            B_ob = [Buf(), Buf()]
            scale = 128 ** -0.5
            sctr = 0
            ectr_ = 0
            for i in range(NT):
                for kv in range(2):
                    pi = (i * 2 + kv) % 2
                    rhs_q = qT[:, 4 * kv:4 * kv + 4, i * 128:(i + 1) * 128]
                    keyt = []
                    for j in range(3):
                        m = None
                        if j == 0:
                            m = 2 if i == 0 else 0
                        if j == 2:
                            m = 3 if i == NT - 1 else 1
                        keyt.append((kT[:, kv, (i + j) * 128:(i + j + 1) * 128], 2 + i + j, m))
                    for cc in range(2):
                        keyt.append((kcT[:, kv, cc * 128:(cc + 1) * 128], cc, None))
                    for n, (kap, vt, m) in enumerate(keyt):
                        si = sctr % 4
                        sctr += 1
                        ei = ectr_ % 6
                        ectr_ += 1
                        kb.op("pe", lambda e: e.matmul(psS[si][:], lhsT=kap, rhs=rhs_q, start=True, stop=(m is None)),
                              reads=[B_in], writes=[B_psS[si]], inc=(m is None))
                        if m is not None:
                            kb.op("pe", lambda e: e.matmul(psS[si][:], lhsT=ident_b[:], rhs=msk[:, m, :], start=False, stop=True),
                                  reads=[B_in], writes=[B_psS[si]])
                        kb.op("act", lambda e: e.activation(out=Eb[ei][:], in_=psS[si][:], func=AF.Exp, scale=scale),
                              reads=[B_psS[si]], writes=[B_E[ei]])
                        kb.op("pe", lambda e: e.matmul(psO[pi][:], lhsT=vv[:, vt, kv * 128:(kv + 1) * 128], rhs=Eb[ei][:],
                                                       start=(n == 0), stop=(n == 4)),
                              reads=[B_in, B_E[ei]], writes=[B_psO[pi]], inc=False)
                        kb.op("pe", lambda e: e.matmul(psD[pi][:], lhsT=ones_b[:], rhs=Eb[ei][:],
                                                       start=(n == 0), stop=(n == 4)),
                              reads=[B_E[ei]], writes=[B_psD[pi]], inc=True)
                    kb.op("dve", lambda e: e.tensor_tensor(
                        out=den[pi][:].rearrange("p (h q) -> p h q", h=4),
                        in0=psD[pi][:].rearrange("p (h q) -> p h q", h=4),
                        in1=esk[:, 4 * kv:4 * kv + 4].unsqueeze(2).to_broadcast([128, 4, 128]), op=ALU.add),
                        reads=[B_psD[pi], B_in], writes=[B_den[pi]])
                    kb.op("dve", lambda e: e.reciprocal(out=den[pi][:], in_=den[pi][:]), reads=[B_den[pi]], writes=[B_den[pi]])
                    kb.op("dve", lambda e: e.tensor_tensor(out=ob[pi][:], in0=psO[pi][:], in1=den[pi][:], op=ALU.mult),
                          reads=[B_psO[pi], B_den[pi]], writes=[B_ob[pi]])
                    kb.dma("sp", mixT_d[4 * kv:4 * kv + 4, :, i * 128:(i + 1) * 128].rearrange("h p t -> p h t"),
                           ob[pi][:].rearrange("p (h q) -> p h q", h=4), reads=[B_ob[pi]])
            kb.barrier()
        if stop_after == "D":
            return nc

        with ExitStack() as phE:
            uTbc = sbt(phE, "uTbc", [128, 8, OWN], BF16)
            B_u = Buf()
            tri_f = sbt(phE, "tri_f", [128, 128]); triT_f = sbt(phE, "triT_f", [128, 128])
            nmf_f = sbt(phE, "nmf_f", [128, 128]); nmb_f = sbt(phE, "nmb_f", [128, 128])
            B_cE = Buf()
            for i, t in ((1, tri_f), (2, triT_f), (4, nmf_f), (5, nmb_f)):
                kb.dma("sp", t[:], consts[i], writes=[B_cE])
            for j in range(8):
                kb.dma("sp", uTbc[:, j, :], uT_d[8 + j], writes=[B_u])
            dtr = sbt(phE, "dtr", [128, 18, 32]); dtv = sbt(phE, "dtv", [128, 18, 32]); dtA = sbt(phE, "dtA", [128, 18, 32])
            dtb = sbt(phE, "dtb", [128, 32]); alg = sbt(phE, "alg", [128, 32]); dsk = sbt(phE, "dsk", [128, 16])
            gssd = sbt(phE, "gssd", [128, 1024])
            B_dt = Buf()
            kb.dma("sp", dtr[:], dtraw_d.rearrange("t p c -> p t c"), writes=[B_dt])
            kb.dma("sp", dtb[:], bcast_row(dt_bias[0:1, :]), writes=[B_dt])
            kb.dma("sp", alg[:], bcast_row(a_log[0:1, :]), writes=[B_dt])
            kb.dma("sp", dsk[:], bcast_row(d_skip[0:1, :]), writes=[B_dt])
            kb.dma("sp", gssd[:], bcast_row(ssd_norm_g[0:1, :]), writes=[B_dt])
            kb.op("dve", lambda e: e.tensor_tensor(out=dtr[:], in0=dtr[:], in1=dtb[:].unsqueeze(1).to_broadcast([128, 18, 32]),
                                                   op=ALU.add), reads=[B_dt, B_cE], writes=[B_dt])
            kb.op("act", lambda e: e.activation(out=dtr[:], in_=dtr[:], func=AF.Exp), reads=[B_dt], writes=[B_dt])
            kb.op("act", lambda e: e.activation(out=dtv[:], in_=dtr[:], func=AF.Ln, bias=ones_f[:, 0:1]), reads=[B_dt], writes=[B_dt])
            kb.op("act", lambda e: e.activation(out=alg[:], in_=alg[:], func=AF.Exp), reads=[B_dt], writes=[B_dt])
            kb.op("dve", lambda e: e.scalar_tensor_tensor(out=dtA[:], in0=dtv[:], scalar=-1.0,
                                                          in1=alg[:].unsqueeze(1).to_broadcast([128, 18, 32]),
                                                          op0=ALU.mult, op1=ALU.mult), reads=[B_dt], writes=[B_dt])
            xs_tok = sbt(phE, "xs_tok", [128, 18, 1024], BF16)
            B_xs = [Buf() for _ in range(18)]
            eac = sbt(phE, "eac", [128, NT, 32]); B_eac = Buf()
            Pall = sbt(phE, "Pall", [128, 2, 18, 16]); B_P = Buf()
            hc = sbt(phE, "hc", [128, 2, 1024]); B_hc = Buf()
            hinit = sbt(phE, "hinit", [128, 2, 1024]); B_hi = Buf()
            B_h0 = [[Buf() for _ in range(NT)] for _ in range(2)]

            with ExitStack() as ph:
                uTx = sbt(ph, "uTx", [128, 8, OWN], BF16)
                uTc = sbt(ph, "uTc", [128, 16, CTX], BF16)
                bm_tok = sbt(ph, "bm_tok", [128, 18, 512], BF16)
                for j in range(8):
                    kb.dma("sp", uTx[:, j, :], uT_d[j], writes=[B_u])
                kb.dma("sp", uTc[:], uTc_d.rearrange("j p t -> p j t"), writes=[B_u])
                psT1 = pst(ph, "psTE1", [128, 8, 128], BF16); B_psT1 = Buf()
                psT2 = pst(ph, "psTE2", [128, 4, 128], BF16); B_psT2 = Buf()
                sm_ps = pst(ph, "sm_ps", [128, 32]); B_smps = Buf()
                S_ps = pst(ph, "S_ps", [128, 1024]); B_Sps = Buf()
                sm = sbt(ph, "sm", [128, 64]); B_sm = Buf()
                xdd = [sbt(ph, "xdd%d" % i, [128, 16, 64], BF16) for i in range(2)]
                B_xdd = [Buf(), Buf()]
                Hb = [sbt(ph, "Hb%d" % i, [128, 1024]) for i in range(2)]
                B_H = [Buf(), Buf()]
                for ti in range(18):
                    if ti < 2:
                        srcs = lambda j, ti=ti: uTc[:, j, ti * 128:(ti + 1) * 128]
                    else:
                        srcs = lambda j, ti=ti: (uTx[:, j, (ti - 2) * 128:(ti - 1) * 128] if j < 8
                                                 else uTbc[:, j - 8, (ti - 2) * 128:(ti - 1) * 128])
                    for j in range(8):
                        kb.op("pe", lambda e, j=j: e.transpose(out=psT1[:, j, :], in_=srcs(j), identity=ident_b[:]),
                              reads=[B_u], writes=[B_psT1], inc=(j == 7))
                    kb.op("act", lambda e, ti=ti: e.copy(out=xs_tok[:, ti, :], in_=psT1[:].rearrange("p a b -> p (a b)")),
                          reads=[B_psT1], writes=[B_xs[ti]])
                    for j in range(4):
                        kb.op("pe", lambda e, j=j: e.transpose(out=psT2[:, j, :], in_=srcs(8 + j), identity=ident_b[:]),
                              reads=[B_u], writes=[B_psT2], inc=(j == 3))
                    kb.op("dve", lambda e, ti=ti: e.tensor_copy(out=bm_tok[:, ti, :], in_=psT2[:].rearrange("p a b -> p (a b)")),
                          reads=[B_psT2], writes=[B_xs[ti]])

                hctr = [0]

                def chunk_S(tr, dtA_ap, dtv_ap, xs_ap, bm_of, rd, own_c=None, d=0, tot_ap=None):
                    kb.op("pe", lambda e: e.matmul(sm_ps[:, 0:16], lhsT=tr[:], rhs=dtA_ap, start=True, stop=True),
                          reads=rd, writes=[B_smps], inc=False)
                    kb.op("pe", lambda e: e.matmul(sm_ps[:, 16:32], lhsT=ones_f[:], rhs=dtA_ap, start=True, stop=True),
                          reads=rd, writes=[B_smps])
                    kb.op("act", lambda e: e.copy(out=sm[:, 0:32], in_=sm_ps[:, 0:32]), reads=[B_smps], writes=[B_sm])
                    kb.op("dve", lambda e: e.tensor_tensor(out=sm[:, 32:48], in0=sm[:, 16:32], in1=sm[:, 0:16], op=ALU.subtract),
                          reads=[B_sm], writes=[B_sm])
                    kb.op("act", lambda e: e.activation(out=sm[:, 32:48], in_=sm[:, 32:48], func=AF.Exp), reads=[B_sm], writes=[B_sm])
                    kb.op("act", lambda e: e.activation(out=sm[:, 48:64], in_=sm[:, 16:32], func=AF.Exp), reads=[B_sm], writes=[B_sm])
                    if own_c is not None:
                        kb.op("act", lambda e: e.activation(out=eac[:, own_c, d * 16:(d + 1) * 16], in_=sm[:, 0:16], func=AF.Exp),
                              reads=[B_sm], writes=[B_eac])
                    kb.op("dve", lambda e: e.tensor_tensor(out=sm[:, 32:48], in0=sm[:, 32:48], in1=dtv_ap, op=ALU.mult),
                          reads=[B_sm] + rd, writes=[B_sm])
                    xi = hctr[0] % 2
                    hctr[0] += 1
                    kb.op("dve", lambda e: e.tensor_tensor(
                        out=xdd[xi][:], in0=xs_ap.rearrange("p (h q) -> p h q", h=16),
                        in1=sm[:, 32:48].unsqueeze(2).to_broadcast([128, 16, 64]), op=ALU.mult),
                        reads=[B_sm] + rd, writes=[B_xdd[xi]])
                    for g in range(4):
                        kb.op("pe", lambda e, g=g: e.matmul(S_ps[:, g * 256:(g + 1) * 256], lhsT=bm_of(g),
                                                            rhs=xdd[xi][:, 4 * g:4 * g + 4, :], start=True, stop=True),
                              reads=rd + [B_xdd[xi]], writes=[B_Sps], inc=(g == 3))

                def chunk_state(ti, d, Hin, B_Hin, Hout, B_Hout, own_c=None):
                    tr = tri_f if d == 0 else triT_f
                    chunk_S(tr, dtA[:, ti, d * 16:(d + 1) * 16], dtv[:, ti, d * 16:(d + 1) * 16], xs_tok[:, ti, :],
                            lambda g: bm_tok[:, ti, g * 128:(g + 1) * 128], [B_dt, B_xs[ti]], own_c=own_c, d=d)
                    if Hin is None:
                        kb.op("dve", lambda e: e.tensor_copy(out=Hout, in_=S_ps[:]), reads=[B_Sps], writes=[B_Hout])
                    else:
                        kb.op("dve", lambda e: e.tensor_tensor(
                            out=Hout.rearrange("p (h q) -> p h q", h=16), in0=Hin.rearrange("p (h q) -> p h q", h=16),
                            in1=sm[:, 48:64].unsqueeze(2).to_broadcast([128, 16, 64]), op=ALU.mult),
                            reads=[B_Hin, B_sm], writes=[B_Hout])
                        kb.op("dve", lambda e: e.tensor_tensor(out=Hout, in0=Hout, in1=S_ps[:], op=ALU.add),
                              reads=[B_Sps, B_Hout], writes=[B_Hout])

                for d in range(2):
                    order = [0, 1] if d == 0 else [1, 0]
                    chunk_state(order[0], d, None, None, Hb[0][:], B_H[0])
                    chunk_state(order[1], d, Hb[0][:], B_H[0], hc[:, d, :], B_hc)
                kb.op("pool", lambda e: e.memset(Pall[:], 1.0), writes=[B_P])
                for d in range(2):
                    order = list(range(NT)) if d == 0 else list(range(NT - 1, -1, -1))
                    kb.op("pool", lambda e: e.memset(Hb[0][:], 0.0), writes=[B_H[0]])
                    cur = 0
                    for n, c in enumerate(order):
                        kb.dma("sp", h0_d[d, c], Hb[cur][:], reads=[B_H[cur]], writes=[B_h0[d][c]])
                        if n < NT - 1:
                            chunk_state(2 + c, d, Hb[cur][:], B_H[cur], Hb[1 - cur][:], B_H[1 - cur], own_c=c)
                        else:
                            chunk_state(2 + c, d, Hb[cur][:], B_H[cur], Hb[1 - cur][:], B_H[1 - cur], own_c=c)
                        pin = Pall[:, d, c, :] if d == 0 else Pall[:, d, c + 1, :]
                        pout = Pall[:, d, c + 1, :] if d == 0 else Pall[:, d, c, :]
                        kb.op("dve", lambda e: e.tensor_tensor(out=pout, in0=pin, in1=sm[:, 48:64], op=ALU.mult),
                              reads=[B_sm, B_P], writes=[B_P])
                        cur = 1 - cur

                dt2r = sbt(ph, "dt2r", [128, NT, 16]); dt2 = sbt(ph, "dt2", [128, NT, 16]); dtA2 = sbt(ph, "dtA2", [128, NT, 16])
                dtb2 = sbt(ph, "dtb2", [128, 16]); alg2 = sbt(ph, "alg2", [128, 16]); tris = sbt(ph, "tris", [128, 128])
                B_d2 = Buf()
                kb.dma("sp", dt2r[:], dtraw2_d.rearrange("t p c -> p t c"), writes=[B_d2])
                kb.dma("sp", dtb2[:], bcast_row(dtb_sel[0:1, :]), writes=[B_d2])
                kb.dma("sp", alg2[:], bcast_row(alog_sel[0:1, :]), writes=[B_d2])
                kb.dma("sp", tris[:], tri_sel[:, :], writes=[B_d2])
                kb.op("dve", lambda e: e.tensor_tensor(out=dt2r[:], in0=dt2r[:], in1=dtb2[:].unsqueeze(1).to_broadcast([128, NT, 16]),
                                                       op=ALU.add), reads=[B_d2], writes=[B_d2])
                kb.op("act", lambda e: e.activation(out=dt2r[:], in_=dt2r[:], func=AF.Exp), reads=[B_d2], writes=[B_d2])
                kb.op("act", lambda e: e.activation(out=dt2[:], in_=dt2r[:], func=AF.Ln, bias=ones_f[:, 0:1]), reads=[B_d2], writes=[B_d2])
                kb.op("act", lambda e: e.activation(out=alg2[:], in_=alg2[:], func=AF.Exp), reads=[B_d2], writes=[B_d2])
                kb.op("dve", lambda e: e.scalar_tensor_tensor(out=dtA2[:], in0=dt2[:], scalar=-1.0,
                                                              in1=alg2[:].unsqueeze(1).to_broadcast([128, NT, 16]),
                                                              op0=ALU.mult, op1=ALU.mult), reads=[B_d2], writes=[B_d2])
                tot2_ps = pst(ph, "tot2_ps", [128, 256]); B_t2ps = Buf()
                kb.op("pe", lambda e: e.matmul(tot2_ps[:], lhsT=ones_f[:], rhs=dtA2[:], start=True, stop=True),
                      reads=[B_d2], writes=[B_t2ps])
                tot2 = sbt(ph, "tot2", [128, NT, 16]); pre = sbt(ph, "pre", [128, NT + 1, 16]); suf = sbt(ph, "suf", [128, NT + 1, 16])
                wch = sbt(ph, "wch", [128, NT + 1, 16]); B_w = Buf()
                kb.op("act", lambda e: e.copy(out=tot2[:].rearrange("p a b -> p (a b)"), in_=tot2_ps[:]), reads=[B_t2ps], writes=[B_w])
                kb.op("pool", lambda e: e.memset(pre[:], 0.0), writes=[B_w])
                kb.op("pool", lambda e: e.memset(suf[:], 0.0), writes=[B_w])
                for c in range(NT):
                    kb.op("dve", lambda e, c=c: e.tensor_tensor(out=pre[:, c + 1, :], in0=pre[:, c, :], in1=tot2[:, c, :], op=ALU.add),
                          reads=[B_w], writes=[B_w])
                for c in range(NT - 1, 0, -1):
                    kb.op("dve", lambda e, c=c: e.tensor_tensor(out=suf[:, c - 1, :], in0=suf[:, c, :], in1=tot2[:, c, :], op=ALU.add),
                          reads=[B_w], writes=[B_w])
                kb.op("dve", lambda e: e.tensor_scalar(out=wch[:], in0=suf[:], scalar1=flg[:, 0:1], scalar2=None, op0=ALU.mult),
                      reads=[B_w], writes=[B_w])
                kb.op("dve", lambda e: e.scalar_tensor_tensor(out=wch[:], in0=pre[:], scalar=flg[:, 1:2], in1=wch[:],
                                                              op0=ALU.mult, op1=ALU.add), reads=[B_w], writes=[B_w])
                kb.op("dve", lambda e: e.tensor_copy(out=wch[:, NT, :], in_=pre[:, NT, :]), reads=[B_w], writes=[B_w])
                kb.op("act", lambda e: e.activation(out=wch[:], in_=wch[:], func=AF.Exp), reads=[B_w], writes=[B_w])
                u2 = [sbt(ph, "u2_%d" % i, [128, 12, 128], BF16) for i in range(2)]
                B_u2 = [Buf(), Buf()]
                xs2 = [sbt(ph, "xs2_%d" % i, [128, 1024], BF16) for i in range(2)]
                bm2 = [sbt(ph, "bm2_%d" % i, [128, 512], BF16) for i in range(2)]
                B_x2 = [Buf(), Buf()]
                Hacc = Hb[0]; B_Hacc = B_H[0]
                stmp = Hb[1]; B_stmp = B_H[1]
                kb.op("pool", lambda e: e.memset(Hacc[:], 0.0), writes=[B_Hacc])
                u2v = uT2_d.rearrange("j p t -> p j t")
                kb.dma("sp", u2[0][:], u2v[:, :, 0:128], writes=[B_u2[0]])
                for c in range(NT):
                    bi = c % 2
                    if c + 1 < NT:
                        kb.dma("sp", u2[1 - bi][:], u2v[:, :, (c + 1) * 128:(c + 2) * 128], writes=[B_u2[1 - bi]])
                    for j in range(8):
                        kb.op("pe", lambda e, j=j: e.transpose(out=psT1[:, j, :], in_=u2[bi][:, j, :], identity=ident_b[:]),
                              reads=[B_u2[bi]], writes=[B_psT1], inc=(j == 7))
                    kb.op("act", lambda e: e.copy(out=xs2[bi][:], in_=psT1[:].rearrange("p a b -> p (a b)")),
                          reads=[B_psT1], writes=[B_x2[bi]])
                    for j in range(4):
                        kb.op("pe", lambda e, j=j: e.transpose(out=psT2[:, j, :], in_=u2[bi][:, 8 + j, :], identity=ident_b[:]),
                              reads=[B_u2[bi]], writes=[B_psT2], inc=(j == 3))
                    kb.op("dve", lambda e: e.tensor_copy(out=bm2[bi][:], in_=psT2[:].rearrange("p a b -> p (a b)")),
                          reads=[B_psT2], writes=[B_x2[bi]])
                    chunk_S(tris, dtA2[:, c, :], dt2[:, c, :], xs2[bi][:], lambda g: bm2[bi][:, g * 128:(g + 1) * 128],
                            [B_d2, B_x2[bi]])
                    kb.op("dve", lambda e: e.tensor_tensor(
                        out=stmp[:].rearrange("p (h q) -> p h q", h=16), in0=S_ps[:].rearrange("p (h q) -> p h q", h=16),
                        in1=wch[:, c, :].unsqueeze(2).to_broadcast([128, 16, 64]), op=ALU.mult),
                        reads=[B_Sps, B_w], writes=[B_stmp])
                    kb.op("pool", lambda e: e.tensor_tensor(out=Hacc[:], in0=Hacc[:], in1=stmp[:], op=ALU.add),
                          reads=[B_stmp, B_Hacc], writes=[B_Hacc])
                hsel = stmp; B_hsel = B_stmp
                kb.op("dve", lambda e: e.tensor_scalar(out=hsel[:], in0=hc[:, 0, :], scalar1=flg[:, 0:1], scalar2=None, op0=ALU.mult),
                      reads=[B_hc], writes=[B_hsel])
                kb.op("dve", lambda e: e.scalar_tensor_tensor(out=hsel[:], in0=hc[:, 1, :], scalar=flg[:, 1:2], in1=hsel[:],
                                                              op0=ALU.mult, op1=ALU.add), reads=[B_hc, B_hsel], writes=[B_hsel])
                kb.op("dve", lambda e: e.tensor_tensor(
                    out=hsel[:].rearrange("p (h q) -> p h q", h=16), in0=hsel[:].rearrange("p (h q) -> p h q", h=16),
                    in1=wch[:, NT, :].unsqueeze(2).to_broadcast([128, 16, 64]), op=ALU.mult),
                    reads=[B_hsel, B_w], writes=[B_hsel])
                kb.op("dve", lambda e: e.tensor_tensor(out=hsel[:], in0=hsel[:], in1=Hacc[:], op=ALU.add),
                      reads=[B_hsel, B_Hacc], writes=[B_hsel])
                for d in range(2):
                    fa, fb = (flg[:, 0:1], flg[:, 1:2]) if d == 0 else (flg[:, 1:2], flg[:, 0:1])
                    kb.op("dve", lambda e, d=d, fa=fa: e.tensor_scalar(out=hinit[:, d, :], in0=hsel[:], scalar1=fa, scalar2=None,
                                                                       op0=ALU.mult), reads=[B_hsel], writes=[B_hi])
                    kb.op("dve", lambda e, d=d, fb=fb: e.scalar_tensor_tensor(out=hinit[:, d, :], in0=hc[:, d, :], scalar=fb,
                                                                              in1=hinit[:, d, :], op0=ALU.mult, op1=ALU.add),
                          reads=[B_hc, B_hi], writes=[B_hi])
                kb.barrier()
            if stop_after == "E1":
                dbg_h = dscr("dbg_h", [128, 4, 1024])
                kb.dma("sp", dbg_h[:, 0:2, :], hc[:], reads=[B_hc])
                kb.dma("sp", dbg_h[:, 2:4, :], hinit[:], reads=[B_hi])
                kb.barrier()
                return nc

            with ExitStack() as ph:
                seg_ps = [pst(ph, "seg_ps%d" % i, [128, 512]) for i in range(2)]; B_seg = [Buf(), Buf()]
                cb_ps = pst(ph, "cb_ps", [128, 512]); B_cbps = Buf()
                yd_ps = pst(ph, "yd_ps", [128, 1024]); B_ydps = Buf()
                yo_ps = pst(ph, "yo_ps", [128, 1024]); B_yops = Buf()
                psT3 = pst(ph, "psTE3", [128, 8, 128], BF16); B_psT3 = Buf()
                negones = sbt(ph, "negones", [128, 128]); nm4 = sbt(ph, "nm4", [128, 2, 4, 128], BF16); B_c2 = Buf()
                kb.op("pool", lambda e: e.memset(negones[:], -1.0), writes=[B_c2])
                for d, nmx in enumerate((nmf_f, nmb_f)):
                    kb.op("dve", lambda e, d=d, nmx=nmx: e.tensor_copy(out=nm4[:, d, :, :],
                                                                       in_=nmx[:].unsqueeze(1).to_broadcast([128, 4, 128])),
                          writes=[B_c2])
                Rf = sbt(ph, "Rf", [128, 32, 128]); B_R = Buf()
                Mb = sbt(ph, "Mb", [128, 32, 128], BF16); B_M = Buf()
                LT = sbt(ph, "LT", [128, 32, 128], BF16); B_LT = Buf()
                cbT = sbt(ph, "cbT", [128, 4, 128], BF16); B_cbT = Buf()
                xd = sbt(ph, "xd", [128, 32, 64], BF16); B_xd2 = Buf()
                h0t = [sbt(ph, "h0t%d" % i, [128, 1024]) for i in range(2)]; B_h0t = [Buf(), Buf()]
                hp = [sbt(ph, "hp%d" % i, [128, 16, 64], BF16) for i in range(2)]; B_hp = [Buf(), Buf()]
                htmp = sbt(ph, "htmp", [128, 1024]); B_htmp = Buf()
                ya = sbt(ph, "ya", [128, 1024]); B_ya = Buf()
                yb = sbt(ph, "yb", [128, 1024]); B_yb = Buf()
                yy = sbt(ph, "yy", [128, 1024]); B_yy = Buf()
                zt = [sbt(ph, "zt%d" % i, [128, 1024]) for i in range(2)]; B_zt = [Buf(), Buf()]
                junk2 = sbt(ph, "junk2", [128, 1024], BF16); B_j2 = Buf()
                ssq2 = sbt(ph, "ssq2", [128, 4]); B_ss2 = Buf()
                ynb = sbt(ph, "ynb", [128, 1024], BF16); B_ynb = Buf()
                mo2 = [sbt(ph, "mo2_%d" % i, [128, 8, 128], BF16) for i in range(2)]; B_mo2 = [Buf(), Buf()]
                sctr2 = 0
                for c in range(NT):
                    ti = 2 + c
                    tk = slice(c * 128, (c + 1) * 128)
                    kb.dma("sp", zt[c % 2][:], zs_d[c], writes=[B_zt[c % 2]])
                    for d in range(2):
                        kb.dma("sp", h0t[d][:], h0_d[d, c], reads=[B_h0[d][c]], writes=[B_h0t[d]])
                    for d, tr in enumerate((tri_f, triT_f)):
                        kb.op("pool", lambda e, d=d, tr=tr: e.tensor_tensor(
                            out=Rf[:, d * 16:(d + 1) * 16, :], in0=tr[:].unsqueeze(1).to_broadcast([128, 16, 128]),
                            in1=dtA[:, ti, d * 16:(d + 1) * 16].unsqueeze(2).to_broadcast([128, 16, 128]), op=ALU.mult),
                            reads=[B_dt], writes=[B_R])
                    if cut <= 1:
                        continue
                    for g in range(4):
                        kb.op("pe", lambda e, g=g: e.matmul(cb_ps[:, g * 128:(g + 1) * 128], lhsT=uTbc[:, g, tk], rhs=uTbc[:, 4 + g, tk],
                                                            start=True, stop=True), reads=[B_u], writes=[B_cbps], inc=(g == 3))
                    kb.op("act", lambda e: e.copy(out=cbT[:].rearrange("p a b -> p (a b)"), in_=cb_ps[:]), reads=[B_cbps], writes=[B_cbT])
                    for q4 in range(8):
                        d = q4 // 4
                        si = sctr2 % 2
                        sctr2 += 1
                        kb.op("pe", lambda e: e.matmul(seg_ps[si][:], lhsT=ones_f[:], rhs=Rf[:, 4 * q4:4 * q4 + 4, :],
                                                       start=True, stop=False), reads=[B_R], writes=[B_seg[si]], inc=False)
                        for r in range(4):
                            kb.op("pe", lambda e, r=r: e.matmul(seg_ps[si][:, r * 128:(r + 1) * 128], lhsT=Rf[:, 4 * q4 + r, :],
                                                                rhs=negones[:], start=False, stop=False),
                                  reads=[B_R, B_c2], writes=[B_seg[si]], inc=False)
                        kb.op("pe", lambda e: e.matmul(seg_ps[si][:], lhsT=ident_b[:], rhs=nm4[:, d, :, :],
                                                       start=False, stop=True), reads=[B_c2], writes=[B_seg[si]])
                        kb.op("act", lambda e: e.activation(out=Mb[:, 4 * q4:4 * q4 + 4, :], in_=seg_ps[si][:], func=AF.Exp),
                              reads=[B_seg[si]], writes=[B_M])
                    if cut <= 2:
                        continue
                    for d in range(2):
                        kb.op("dve", lambda e, d=d: e.tensor_tensor(
                            out=LT[:, d * 16:(d + 1) * 16, :].rearrange("p (g r) l -> p g r l", g=4),
                            in0=Mb[:, d * 16:(d + 1) * 16, :].rearrange("p (g r) l -> p g r l", g=4),
                            in1=cbT[:].unsqueeze(2).to_broadcast([128, 4, 4, 128]), op=ALU.mult),
                            reads=[B_M, B_cbT], writes=[B_LT])
                        kb.op("pool", lambda e, d=d: e.tensor_tensor(
                            out=xd[:, d * 16:(d + 1) * 16, :], in0=xs_tok[:, ti, :].rearrange("p (h q) -> p h q", h=16),
                            in1=dtv[:, ti, d * 16:(d + 1) * 16].unsqueeze(2).to_broadcast([128, 16, 64]), op=ALU.mult),
                            reads=[B_xs[ti], B_dt], writes=[B_xd2])
                    if cut <= 3:
                        continue
                    for h in range(16):
                        for d in range(2):
                            kb.op("pe", lambda e, h=h, d=d: e.matmul(yd_ps[:, h * 64:(h + 1) * 64], lhsT=LT[:, d * 16 + h, :],
                                                                     rhs=xd[:, d * 16 + h, :], start=(d == 0), stop=(d == 1)),
                                  reads=[B_LT, B_xd2], writes=[B_ydps], inc=(h == 15 and d == 1))
                    if cut <= 4:
                        continue
                    for d in range(2):
                        pin = Pall[:, d, c, :] if d == 0 else Pall[:, d, c + 1, :]
                        kb.op("dve", lambda e, d=d, pin=pin: e.tensor_tensor(
                            out=htmp[:].rearrange("p (h q) -> p h q", h=16), in0=hinit[:, d, :].rearrange("p (h q) -> p h q", h=16),
                            in1=pin.unsqueeze(2).to_broadcast([128, 16, 64]), op=ALU.mult),
                            reads=[B_hi, B_P], writes=[B_htmp])
                        kb.op("dve", lambda e, d=d: e.tensor_tensor(out=hp[d][:].rearrange("p h q -> p (h q)"), in0=htmp[:],
                                                                    in1=h0t[d][:], op=ALU.add),
                              reads=[B_htmp, B_h0t[d]], writes=[B_hp[d]])
                        for g in range(4):
                            kb.op("pe", lambda e, g=g, d=d: e.matmul(yo_ps[:, g * 256:(g + 1) * 256], lhsT=uTbc[:, 4 + g, tk],
                                                                     rhs=hp[d][:, 4 * g:4 * g + 4, :], start=True, stop=True),
                                  reads=[B_u, B_hp[d]], writes=[B_yops], inc=(g == 3))
                        dst, Bd = (ya, B_ya) if d == 0 else (yb, B_yb)
                        kb.op("dve", lambda e, d=d, dst=dst: e.tensor_tensor(
                            out=dst[:].rearrange("p (h q) -> p h q", h=16), in0=yo_ps[:].rearrange("p (h q) -> p h q", h=16),
                            in1=eac[:, c, d * 16:(d + 1) * 16].unsqueeze(2).to_broadcast([128, 16, 64]), op=ALU.mult),
                            reads=[B_yops, B_eac], writes=[Bd])
                    if cut <= 5:
                        continue
                    kb.op("dve", lambda e: e.tensor_tensor(out=yy[:], in0=yd_ps[:], in1=ya[:], op=ALU.add),
                          reads=[B_ydps, B_ya], writes=[B_yy])
                    kb.op("pool", lambda e: e.tensor_tensor(out=yy[:], in0=yy[:], in1=yb[:], op=ALU.add),
                          reads=[B_yy, B_yb], writes=[B_yy])
                    kb.op("pool", lambda e: e.tensor_tensor(
                        out=ya[:].rearrange("p (h q) -> p h q", h=16), in0=xs_tok[:, ti, :].rearrange("p (h q) -> p h q", h=16),
                        in1=dsk[:].unsqueeze(2).to_broadcast([128, 16, 64]), op=ALU.mult),
                        reads=[B_xs[ti], B_dt], writes=[B_ya])
                    kb.op("pool", lambda e: e.tensor_tensor(out=yy[:], in0=yy[:], in1=ya[:], op=ALU.add),
                          reads=[B_yy, B_ya], writes=[B_yy])
                    if cut <= 6:
                        continue
                    kb.op("dve", lambda e: e.tensor_tensor(out=yy[:], in0=yy[:], in1=zt[c % 2][:], op=ALU.mult),
                          reads=[B_yy, B_zt[c % 2]], writes=[B_yy])
                    rms_mod_tile(yy[:], B_yy, gssd[:], None, B_dt, junk2, B_j2, ssq2, B_ss2, yb, B_yb, None, None, width=1024)
                    if cut <= 7:
                        continue
                    kb.op("act", lambda e: e.copy(out=ynb[:], in_=yb[:]), reads=[B_yb], writes=[B_ynb])
                    for j in range(8):
                        kb.op("pe", lambda e, j=j: e.transpose(out=psT3[:, j, :], in_=ynb[:, j * 128:(j + 1) * 128], identity=ident_b[:]),
                              reads=[B_ynb], writes=[B_psT3], inc=(j == 7))
                    kb.op("act", lambda e: e.copy(out=mo2[c % 2][:], in_=psT3[:]), reads=[B_psT3], writes=[B_mo2[c % 2]])
                    kb.dma("sp", mixT_d[8:16, :, tk].rearrange("h p t -> p h t"), mo2[c % 2][:], reads=[B_mo2[c % 2]])
                kb.barrier()
        if stop_after == "E":
            return nc

        with ExitStack() as ph:
            Wo = sbt(ph, "Wo", [128, 16, D], BF16); B_Wo = Buf()
            wov = w_out.rearrange("(k p) n -> p k n", p=128)
            for cbk in range(4):
                kb.dma("pool", Wo[:, :, cbk * 512:(cbk + 1) * 512], wov[:, :, cbk * 512:(cbk + 1) * 512], writes=[B_Wo])
            B_m2 = Buf()
            g1b = load_bc(ph, "sp", "g1b", mod_d[2:3, :], D, B_m2)
            G2 = load_bc(ph, "sp", "G2", mod_d[4:5, :], D, B_m2)
            SH2 = load_bc(ph, "sp", "SH2", mod_d[3:4, :], D, B_m2)
            g2n = load_bc(ph, "sp", "g2n", norm2_g[0:1, :], D, B_m2)
            kb.op("dve", lambda e: e.tensor_tensor(out=G2[:], in0=G2[:], in1=g2n[:], op=ALU.mult), reads=[B_m2], writes=[B_m2])
            rw = sbt(ph, "rw", [128, 16, NEXP]); rbias = sbt(ph, "rbias", [128, NEXP])
            kb.dma("sp", rw[:], router_w.rearrange("(k p) e -> p k e", p=128), writes=[B_m2])
            kb.dma("sp", rbias[:], bcast_row(router_bias[0:1, :]), writes=[B_m2])
            mix = [sbt(ph, "mix%d" % i, [128, 16, 128], BF16) for i in range(2)]; B_mix = [Buf(), Buf()]
            xtF = [sbt(ph, "xtF%d" % i, [128, D]) for i in range(2)]; B_xtF = [Buf(), Buf()]
            x1t = sbt(ph, "x1t", [128, D]); B_x1t = Buf()
            tmpF = sbt(ph, "tmpF", [128, D]); B_tmpF = Buf()
            h2f = g2n; B_h2f = Buf()
            h2b = sbt(ph, "h2b", [128, D], BF16); B_h2b = Buf()
            junkF = sbt(ph, "junkF", [128, D], BF16); B_junkF = Buf()
            ssqF = sbt(ph, "ssqF", [128, 4]); B_ssF = Buf()
            h2To = [sbt(ph, "h2To%d" % i, [128, 16, 128], BF16) for i in range(2)]; B_h2To = [Buf(), Buf()]
            h2Tf = sbt(ph, "h2Tf", [128, 16, 128]); B_h2Tf = Buf()
            psF = [pst(ph, "psF%d" % i, [128, 512]) for i in range(2)]; B_psF = [Buf(), Buf()]
            psTF = [pst(ph, "psTF%d" % i, [128, 8, 128], BF16) for i in range(2)]; B_psTF = [Buf(), Buf()]
            psR = [pst(ph, "psR%d" % i, [128, 4, 128]) for i in range(2)]; B_psR = [Buf(), Buf()]
            psL = pst(ph, "psL", [128, NEXP]); B_psL = Buf()
            sc_ = sbt(ph, "sc_", [128, NEXP]); sel = sbt(ph, "sel", [128, NEXP]); sel2 = sbt(ph, "sel2", [128, NEXP])
            m1 = sbt(ph, "m1", [128, 8]); m2 = sbt(ph, "m2", [128, 8]); gs = sbt(ph, "gs", [128, 8])
            cmpt = sbt(ph, "cmpt", [128, 8, 8]); rank = sbt(ph, "rank", [128, 8]); km = sbt(ph, "km", [128, 8])
            mx8 = sbt(ph, "mx8", [128, 8]); thr = sbt(ph, "thr", [128, 2])
            gate = [sbt(ph, "gate%d" % i, [128, NEXP + 1]) for i in range(2)]; B_gate = [Buf(), Buf()]
            B_r = Buf()
            for i in range(2):
                kb.op("pool", lambda e, i=i: e.memset(gate[i][:], 1.0), writes=[B_gate[i]])
            mixv = mixT_d.rearrange("k p t -> p k t")
            h2Tv = h2T_d.rearrange("k p t -> p k t")
            kb.dma("sp", mix[0][:], mixv[:, :, 0:128], writes=[B_mix[0]])
            kb.dma("sp", xtF[0][:], x_own[0:128, :], writes=[B_xtF[0]])
            pctrF = 0
            for t in range(NT):
                bi = t % 2
                tk = slice(t * 128, (t + 1) * 128)
                if t + 1 < NT:
                    kb.dma("sp", mix[1 - bi][:], mixv[:, :, (t + 1) * 128:(t + 2) * 128], writes=[B_mix[1 - bi]])
                    kb.dma("sp", xtF[1 - bi][:], x_own[(t + 1) * 128:(t + 2) * 128, :], writes=[B_xtF[1 - bi]])
                for cbk in range(4):
                    pi = pctrF % 2
                    pctrF += 1
                    cs_ = slice(cbk * 512, (cbk + 1) * 512)
                    for k in range(16):
                        kb.op("pe", lambda e, k=k: e.matmul(psF[pi][:], lhsT=mix[bi][:, k, :], rhs=Wo[:, k, cs_],
                                                            start=(k == 0), stop=(k == 15)),
                              reads=[B_mix[bi], B_Wo], writes=[B_psF[pi]], inc=(k == 15))
                    kb.op("dve", lambda e: e.tensor_tensor(out=tmpF[:, cs_], in0=psF[pi][:], in1=g1b[:, cs_], op=ALU.mult),
                          reads=[B_psF[pi], B_m2], writes=[B_tmpF])
                    kb.op("pool", lambda e: e.tensor_tensor(out=x1t[:, cs_], in0=tmpF[:, cs_], in1=xtF[bi][:, cs_], op=ALU.add),
                          reads=[B_tmpF, B_xtF[bi]], writes=[B_x1t])
                kb.dma("sp", x1_d[tk, :], x1t[:], reads=[B_x1t])
                rms_mod_tile(x1t[:], B_x1t, G2[:], SH2[:], B_m2, junkF, B_junkF, ssqF, B_ssF, tmpF, B_tmpF, h2f[:], B_h2f)
                kb.op("act", lambda e: e.copy(out=h2b[:], in_=h2f[:]), reads=[B_h2f], writes=[B_h2b])
                kb.dma("sp", h2tok_d[tk, :], h2b[:], reads=[B_h2b])
                for half in range(2):
                    for kk in range(8):
                        k = half * 8 + kk
                        kb.op("pe", lambda e, k=k, kk=kk: e.transpose(out=psTF[half][:, kk, :], in_=h2b[:, k * 128:(k + 1) * 128],
                                                                     identity=ident_b[:]),
                              reads=[B_h2b], writes=[B_psTF[half]], inc=(kk == 7))
                    if half == 0:
                        kb.op("act", lambda e: e.copy(out=h2To[bi][:, 0:8, :], in_=psTF[0][:]), reads=[B_psTF[0]], writes=[B_h2To[bi]])
                    else:
                        kb.op("dve", lambda e: e.tensor_copy(out=h2To[bi][:, 8:16, :], in_=psTF[1][:]), reads=[B_psTF[1]],
                              writes=[B_h2To[bi]])
                kb.dma("sp", h2Tv[:, :, tk], h2To[bi][:], reads=[B_h2To[bi]])
                for q in range(4):
                    ri = q % 2
                    for kk in range(4):
                        k = q * 4 + kk
                        kb.op("pe", lambda e, k=k, kk=kk: e.matmul(psR[ri][:, kk, :], lhsT=h2f[:, k * 128:(k + 1) * 128], rhs=ident_f[:],
                                                                  start=True, stop=True),
                              reads=[B_h2f], writes=[B_psR[ri]], inc=(kk == 3))
                    kb.op("act", lambda e, q=q: e.copy(out=h2Tf[:, q * 4:(q + 1) * 4, :], in_=psR[ri][:]), reads=[B_psR[ri]],
                          writes=[B_h2Tf])
                for k in range(16):
                    kb.op("pe", lambda e, k=k: e.matmul(psL[:], lhsT=h2Tf[:, k, :], rhs=rw[:, k, :], start=(k == 0), stop=(k == 15)),
                          reads=[B_h2Tf, B_m2], writes=[B_psL], inc=(k == 15))
                R_ = [B_r]
                kb.op("act", lambda e: e.activation(out=sc_[:], in_=psL[:], func=AF.Sigmoid), reads=[B_psL], writes=R_)
                kb.op("dve", lambda e: e.tensor_tensor(out=sel[:], in0=sc_[:], in1=rbias[:], op=ALU.add), reads=R_ + [B_m2], writes=R_)
                s3 = lambda a: a[:].rearrange("p (g e) -> p g e", g=8)
                kb.op("dve", lambda e: e.tensor_reduce(out=m1[:], in_=s3(sel), axis=AX.X, op=ALU.max), reads=R_, writes=R_)
                kb.op("dve", lambda e: e.tensor_tensor(out=s3(sel2), in0=s3(sel), in1=m1[:].unsqueeze(2).to_broadcast([128, 8, 8]),
                                                       op=ALU.is_equal), reads=R_, writes=R_)
                kb.op("dve", lambda e: e.scalar_tensor_tensor(out=sel2[:], in0=sel2[:], scalar=-1e9, in1=sel[:], op0=ALU.mult,
                                                              op1=ALU.add), reads=R_, writes=R_)
                kb.op("dve", lambda e: e.tensor_reduce(out=m2[:], in_=s3(sel2), axis=AX.X, op=ALU.max), reads=R_, writes=R_)
                kb.op("dve", lambda e: e.tensor_tensor(out=gs[:], in0=m1[:], in1=m2[:], op=ALU.add), reads=R_, writes=R_)
                kb.op("dve", lambda e: e.tensor_tensor(out=cmpt[:], in0=gs[:].unsqueeze(1).to_broadcast([128, 8, 8]),
                                                       in1=gs[:].unsqueeze(2).to_broadcast([128, 8, 8]), op=ALU.is_gt),
                      reads=R_, writes=R_)
                kb.op("dve", lambda e: e.tensor_reduce(out=rank[:], in_=cmpt[:], axis=AX.X, op=ALU.add), reads=R_, writes=R_)
                kb.op("dve", lambda e: e.tensor_scalar(out=km[:], in0=rank[:], scalar1=3.5, scalar2=1e9, op0=ALU.is_lt, op1=ALU.mult),
                      reads=R_, writes=R_)
                kb.op("dve", lambda e: e.tensor_scalar(out=km[:], in0=km[:], scalar1=-1e9, scalar2=None, op0=ALU.add),
                      reads=R_, writes=R_)
                kb.op("dve", lambda e: e.tensor_tensor(out=s3(sel2), in0=s3(sel), in1=km[:].unsqueeze(2).to_broadcast([128, 8, 8]),
                                                       op=ALU.add), reads=R_, writes=R_)
                kb.op("dve", lambda e: e.max(out=mx8[:], in_=sel2[:]), reads=R_, writes=R_)
                kb.op("dve", lambda e: e.tensor_reduce(out=thr[:, 0:1], in_=mx8[:], axis=AX.X, op=ALU.min), reads=R_, writes=R_)
                kb.op("dve", lambda e: e.tensor_scalar(out=sel2[:], in0=sel2[:], scalar1=thr[:, 0:1], scalar2=None, op0=ALU.is_ge),
                      reads=R_, writes=R_)
                kb.op("dve", lambda e: e.tensor_tensor(out=sel2[:], in0=sel2[:], in1=sc_[:], op=ALU.mult), reads=R_, writes=R_)
                kb.op("dve", lambda e: e.tensor_reduce(out=thr[:, 1:2], in_=sel2[:], axis=AX.X, op=ALU.add), reads=R_, writes=R_)
                kb.op("dve", lambda e: e.reciprocal(out=thr[:, 1:2], in_=thr[:, 1:2]), reads=R_, writes=R_)
                kb.op("dve", lambda e: e.tensor_scalar(out=gate[bi][:, 0:NEXP], in0=sel2[:], scalar1=thr[:, 1:2], scalar2=2.5,
                                                       op0=ALU.mult, op1=ALU.mult), reads=R_, writes=[B_gate[bi]])
                kb.dma("sp", gate_d[t], gate[bi][:], reads=[B_gate[bi]])
            kb.barrier()
        if stop_after == "F":
            return nc

        PEe, DVEe, ACTe, POOLe, SPe = (mybir.EngineType.PE, mybir.EngineType.DVE, mybir.EngineType.Activation,
                                       mybir.EngineType.Pool, mybir.EngineType.SP)
        with ExitStack() as ph:
            Gt = sbt(ph, "Gt", [128, NT, NEXP + 1]); B_G = Buf()
            kb.dma("sp", Gt[:], gate_d.rearrange("t p e -> p t e"), writes=[B_G])
            eb = sbt(ph, "eb", [128, 2 * NEXP]); lsf = sbt(ph, "lsf", [128, 128]); lsb = sbt(ph, "lsb", [128, 128], BF16)
            kb.dma("sp", eb[:], ebase[:, :], writes=[B_G])
            kb.dma("sp", lsf[:], lstrict[:, :], writes=[B_G])
            kb.op("dve", lambda e: e.tensor_copy(out=lsb[:], in_=lsf[:]), reads=[B_G], writes=[B_G])
            Mb_ = sbt(ph, "Mb_", [128, NT, NEXP], BF16)
            kb.op("dve", lambda e: e.tensor_scalar(out=Mb_[:], in0=Gt[:, :, 0:NEXP], scalar1=0.0, scalar2=None, op0=ALU.is_gt),
                  reads=[B_G], writes=[B_G])
            zrow = sbt(ph, "zrow", [128, D], BF16); B_z = Buf()
            kb.op("pool", lambda e: e.memset(zrow[:], 0.0), writes=[B_z])
            kb.dma("sp", h2tok_d[OWN:OWN + 128, :], zrow[:], reads=[B_z])
            fill = sbt(ph, "fill", [128, NSL // 128], I32); B_fill = Buf(); B_tab = Buf()
            kb.op("pool", lambda e: e.memset(fill[:], OWN), writes=[B_fill])
            kb.dma("sp", idxtab_d.rearrange("(p f) o -> p (f o)", p=128), fill[:], reads=[B_fill], writes=[B_tab])
            tokid = sbt(ph, "tokid", [128, NT], I32); B_tok = Buf()
            kb.op("pool", lambda e: e.iota(tokid[:], pattern=[[128, NT]], base=0, channel_multiplier=1), writes=[B_tok])
            cnt_ps = pst(ph, "cnt_ps", [128, NEXP]); B_cps = Buf()
            pos_ps = [pst(ph, "pos_ps%d" % i, [128, NEXP]) for i in range(2)]; B_pps = [Buf(), Buf()]
            for t in range(NT):
                kb.op("pe", lambda e, t=t: e.matmul(cnt_ps[:], lhsT=ones_b[:], rhs=Mb_[:, t, :], start=(t == 0), stop=(t == NT - 1)),
                      reads=[B_G], writes=[B_cps], inc=(t == NT - 1))
            nbf = sbt(ph, "nbf", [128, NEXP]); nbm = sbt(ph, "nbm", [128, NEXP]); nbi = sbt(ph, "nbi", [128, NEXP], I32); B_nb = Buf()
            kb.op("dve", lambda e: e.tensor_scalar(out=nbf[:], in0=cnt_ps[:], scalar1=127.0, scalar2=None, op0=ALU.add),
                  reads=[B_cps], writes=[B_nb])
            nbr = sbt(ph, "nbr", [128, NEXP], I32)
            kb.op("dve", lambda e: e.tensor_copy(out=nbr[:], in_=nbf[:]), reads=[B_nb], writes=[B_nb])
            kb.op("dve", lambda e: e.tensor_single_scalar(out=nbi[:], in_=nbr[:], scalar=7, op=ALU.arith_shift_right),
                  reads=[B_nb], writes=[B_nb])
            kb.op("dve", lambda e: e.tensor_single_scalar(out=nbr[:], in_=nbi[:], scalar=7, op=ALU.logical_shift_left),
                  reads=[B_nb], writes=[B_nb])
            kb.op("dve", lambda e: e.tensor_copy(out=nbf[:], in_=nbr[:]), reads=[B_nb], writes=[B_nb])
            cs0 = sbt(ph, "cs0", [128, NEXP]); cs1 = sbt(ph, "cs1", [128, NEXP]); cbs = sbt(ph, "cbs", [128, NEXP])
            cbi = sbt(ph, "cbi", [128, NEXP], I32)
            kb.op("dve", lambda e: e.tensor_copy(out=cs0[:], in_=nbf[:]), reads=[B_nb], writes=[B_nb])
            cur_, oth_ = cs0, cs1
            for s_ in (1, 2, 4, 8, 16, 32):
                kb.op("dve", lambda e, cur_=cur_, oth_=oth_, s_=s_: e.tensor_copy(out=oth_[:, 0:s_], in_=cur_[:, 0:s_]),
                      reads=[B_nb], writes=[B_nb])
                kb.op("dve", lambda e, cur_=cur_, oth_=oth_, s_=s_: e.tensor_tensor(out=oth_[:, s_:NEXP], in0=cur_[:, s_:NEXP],
                                                                                   in1=cur_[:, 0:NEXP - s_], op=ALU.add),
                      reads=[B_nb], writes=[B_nb])
                cur_, oth_ = oth_, cur_
            kb.op("dve", lambda e, cur_=cur_: e.tensor_tensor(out=cbs[:], in0=cur_[:], in1=nbf[:], op=ALU.subtract),
                  reads=[B_nb], writes=[B_nb])
            kb.op("dve", lambda e: e.tensor_copy(out=cbi[:], in_=cbs[:]), reads=[B_nb], writes=[B_nb])
            kb.dma("sp", cbase_d[0:1, :], cbi[0:1, :], reads=[B_nb])
            oh = sbt(ph, "oh", [128, NEXP + 1]); cbp = sbt(ph, "cbp", [128, 2]); B_oh = Buf()
            kb.dma("sp", oh[:], oh2[:, :], writes=[B_oh])
            ohj = sbt(ph, "ohj", [128, NEXP])
            kb.op("dve", lambda e: e.tensor_tensor(out=ohj[:], in0=oh[:, 0:NEXP], in1=cbs[:], op=ALU.mult), reads=[B_oh, B_nb], writes=[B_oh])
            kb.op("dve", lambda e: e.tensor_reduce(out=cbp[:, 0:1], in_=ohj[:], axis=AX.X, op=ALU.add), reads=[B_oh], writes=[B_oh])
            kb.op("dve", lambda e: e.tensor_tensor(out=cbp[:, 1:2], in0=cbp[:, 0:1], in1=oh[:, NEXP:NEXP + 1], op=ALU.add),
                  reads=[B_oh], writes=[B_oh])
            rtf = sbt(ph, "rtf", [128, NSL // 128]); rti = sbt(ph, "rti", [128, NSL // 128], I32); B_rt = Buf()
            kb.op("pool", lambda e: e.iota(rti[:], pattern=[[1, NSL // 128]], base=0, channel_multiplier=0), writes=[B_rt])
            kb.op("dve", lambda e: e.tensor_copy(out=rtf[:], in_=rti[:]), reads=[B_rt], writes=[B_rt])
            kb.op("dve", lambda e: e.tensor_scalar(out=rtf[:], in0=rtf[:], scalar1=cbp[:, 1:2], scalar2=None, op0=ALU.add),
                  reads=[B_rt, B_oh], writes=[B_rt])
            kb.op("dve", lambda e: e.tensor_copy(out=rti[:], in_=rtf[:]), reads=[B_rt], writes=[B_rt])
            kb.dma("sp", rowtab_d.rearrange("(p f) o -> p (f o)", p=128), rti[:], reads=[B_rt])
            kb.dma("sp", nblk_d[0:1, :], nbi[0:1, :], reads=[B_nb])
            dc8i = sbt(ph, "dc8i", [128, NT, 8], I32); B_dc8 = Buf()
            key = sbt(ph, "key", [128, NEXP]); B_key = Buf()
            d8f = sbt(ph, "d8f", [128, 8]); d8i = sbt(ph, "d8i", [128, NT, 8], I32); w8 = sbt(ph, "w8", [128, NT, 8])
            B_d8 = Buf(); B_w8 = Buf()
            e8i = sbt(ph, "e8i", [128, 8], I32); e8f = sbt(ph, "e8f", [128, 8]); B_e8 = Buf()
            for t in range(NT):
                pi = t % 2
                for t2 in range(t):
                    kb.op("pe", lambda e, t2=t2: e.matmul(pos_ps[pi][:], lhsT=ones_b[:], rhs=Mb_[:, t2, :], start=(t2 == 0), stop=False),
                          reads=[B_G], writes=[B_pps[pi]], inc=False)
                kb.op("pe", lambda e, t=t: e.matmul(pos_ps[pi][:], lhsT=lsb[:], rhs=Mb_[:, t, :], start=(t == 0), stop=True),
                      reads=[B_G], writes=[B_pps[pi]])
                kb.op("dve", lambda e: e.tensor_tensor(out=key[:], in0=pos_ps[pi][:], in1=eb[:, 0:NEXP], op=ALU.add),
                      reads=[B_pps[pi], B_G], writes=[B_key])
                kb.op("dve", lambda e, t=t: e.tensor_tensor(out=key[:], in0=key[:], in1=Mb_[:, t, :], op=ALU.mult),
                      reads=[B_key, B_G], writes=[B_key])
                kb.op("dve", lambda e: e.max(out=d8f[:], in_=key[:]), reads=[B_key], writes=[B_key])
                kb.op("dve", lambda e: e.tensor_scalar(out=d8f[:], in0=d8f[:], scalar1=-1.0, scalar2=None, op0=ALU.add),
                      reads=[B_key], writes=[B_key])
                kb.op("dve", lambda e, t=t: e.tensor_copy(out=d8i[:, t, :], in_=d8f[:]), reads=[B_key], writes=[B_d8])
                kb.op("dve", lambda e: e.scalar_tensor_tensor(out=key[:], in0=pos_ps[pi][:], scalar=1.0, in1=cbs[:], op0=ALU.add,
                                                              op1=ALU.add), reads=[B_pps[pi], B_nb, B_key], writes=[B_key])
                kb.op("dve", lambda e, t=t: e.tensor_tensor(out=key[:], in0=key[:], in1=Mb_[:, t, :], op=ALU.mult),
                      reads=[B_key, B_G], writes=[B_key])
                kb.op("dve", lambda e: e.max(out=d8f[:], in_=key[:]), reads=[B_key], writes=[B_key])
                kb.op("dve", lambda e: e.tensor_scalar(out=d8f[:], in0=d8f[:], scalar1=-1.0, scalar2=None, op0=ALU.add),
                      reads=[B_key], writes=[B_key])
                kb.op("dve", lambda e, t=t: e.tensor_copy(out=dc8i[:, t, :], in_=d8f[:]), reads=[B_key], writes=[B_dc8])
                kb.op("dve", lambda e, t=t: e.tensor_tensor(out=key[:], in0=Gt[:, t, 0:NEXP], in1=eb[:, NEXP:2 * NEXP], op=ALU.add),
                      reads=[B_G, B_key], writes=[B_key])
                kb.op("dve", lambda e, t=t: e.tensor_tensor(out=key[:], in0=key[:], in1=Mb_[:, t, :], op=ALU.mult),
                      reads=[B_key, B_G], writes=[B_key])
                kb.op("dve", lambda e: e.max(out=d8f[:], in_=key[:]), reads=[B_key], writes=[B_key])
                kb.op("dve", lambda e, t=t: e.tensor_single_scalar(out=e8i[:], in_=d8i[:, t, :], scalar=11, op=ALU.arith_shift_right),
                      reads=[B_d8], writes=[B_e8])
                kb.op("dve", lambda e: e.tensor_copy(out=e8f[:], in_=e8i[:]), reads=[B_e8], writes=[B_e8])
                kb.op("dve", lambda e: e.tensor_scalar(out=e8f[:], in0=e8f[:], scalar1=-4.0, scalar2=-4.0, op0=ALU.mult, op1=ALU.add),
                      reads=[B_e8], writes=[B_e8])
                kb.op("dve", lambda e, t=t: e.tensor_tensor(out=w8[:, t, :], in0=d8f[:], in1=e8f[:], op=ALU.add),
                      reads=[B_key, B_e8], writes=[B_w8])
                for k in range(8):
                    kb.coll(lambda g, t=t, k=k: g.indirect_dma_start(
                        out=idxtab_d[:, :], out_offset=bass.IndirectOffsetOnAxis(ap=d8i[:, t, k:k + 1], axis=0),
                        in_=tokid[:, t:t + 1], in_offset=None),
                        reads=[B_d8, B_tok, B_tab], writes=[])
            kb.dma("sp", dest_d.rearrange("t p k -> p t k"), dc8i[:], reads=[B_dc8])
            kb.dma("sp", w8_d.rearrange("t p k -> p t k"), w8[:], reads=[B_w8])
            kb.barrier()
        if stop_after == "F2":
            return nc

        with ExitStack() as ph:
            nbt = sbt(ph, "nbt", [1, NEXP], I32); B_nbt = Buf()
            kb.dma("sp", nbt[:], nblk_d[0:1, :], writes=[B_nbt])
            rowcur = sbt(ph, "rowcur", [128, 1], I32); B_rowcur = Buf()
            rowst = [sbt(ph, "rowst%d" % i, [128, 1], I32) for i in range(2)]; B_rowst = [Buf(), Buf()]
            yor = sbt(ph, "yor", [128, D]); B_yor = Buf()
            kb.op("pool", lambda e: e.iota(rowcur[:], pattern=[[0, 1]], base=NSLC, channel_multiplier=1), writes=[B_rowcur])
            kb.op("pool", lambda e: e.memset(yor[:], 0.0), writes=[B_yor])

            def flush_pending():
                kb.coll(lambda g: g.indirect_dma_start(out=slot_d[:, :], out_offset=bass.IndirectOffsetOnAxis(ap=rowcur[:, :], axis=0),
                                                       in_=yor[:, :], in_offset=None),
                        reads=[B_yor, B_rowcur], writes=[])
            ekeys = ["pe", "dve", "act", "pool", "sp"]
            NWB = 2
            NST = 6
            stg = [sbt(ph, "stg%d" % i, [128, 2048]) for i in range(NST)]; B_stg = [Buf() for _ in range(NST)]
            Wg = [sbt(ph, "Wg%d" % i, [128, 16, 512], BF16) for i in range(NWB)]
            Wu = [sbt(ph, "Wu%d" % i, [128, 16, 512], BF16) for i in range(NWB)]
            Wd = [sbt(ph, "Wd%d" % i, [128, 4, D], BF16) for i in range(NWB)]
            B_We = [Buf() for _ in range(NWB)]
            idxb = [sbt(ph, "idxb%d" % i, [128, 1], I32) for i in range(4)]; B_idx = [Buf() for _ in range(4)]
            xg = [sbt(ph, "xg%d" % i, [128, D], BF16) for i in range(2)]; B_xg = [Buf(), Buf()]
            xgT = [sbt(ph, "xgT%d" % i, [128, 16, 128], BF16) for i in range(2)]; B_xgT = [Buf(), Buf()]
            sgs = [sbt(ph, "sgs%d" % i, [128, 512]) for i in range(2)]; B_sgs = [Buf(), Buf()]
            acts = [sbt(ph, "acts%d" % i, [128, 512], BF16) for i in range(2)]; B_acts = [Buf(), Buf()]
            actT = [sbt(ph, "actT%d" % i, [128, 4, 128], BF16) for i in range(2)]; B_actT = [Buf(), Buf()]
            yo = [sbt(ph, "yo%d" % i, [128, D]) for i in range(2)]; B_yo = [Buf(), Buf()]
            psX = [pst(ph, "psX%d" % i, [128, 8, 128], BF16) for i in range(2)]; B_psX = [Buf(), Buf()]
            psg = pst(ph, "psg", [128, 512]); B_psg = Buf()
            psu = pst(ph, "psu", [128, 512]); B_psu = Buf()
            psA = pst(ph, "psA", [128, 4, 128], BF16); B_psA = Buf()
            psy = [pst(ph, "psy%d" % i, [128, 512]) for i in range(2)]; B_psy = [Buf(), Buf()]
            h2Tv = h2T_d.rearrange("k p t -> p k t")

            pctr_ = [0]

            def piece_list(e_):
                bi = e_ % NWB
                gv = ew_gate[e_].rearrange("(k p) f -> p k f", p=128)
                uv = ew_up[e_].rearrange("(k p) f -> p k f", p=128)
                dv = ew_down[e_].rearrange("(k p) d -> p k d", p=128)
                pcs = []
                for q in range(4):
                    pcs.append((gv[:, 4 * q:4 * q + 4, :], Wg[bi][:, 4 * q:4 * q + 4, :], bi))
                    pcs.append((uv[:, 4 * q:4 * q + 4, :], Wu[bi][:, 4 * q:4 * q + 4, :], bi))
                for fc in range(4):
                    pcs.append((dv[:, fc:fc + 1, :], Wd[bi][:, fc:fc + 1, :], bi))
                return pcs

            def load_piece(pc):
                srcap, dstap, bi = pc
                n = pctr_[0]
                pctr_[0] += 1
                si = n % NST
                k4 = srcap.shape[1]
                stv = stg[si][:].rearrange("p (k f) -> p k f", k=k4)
                kb.dma("sp", stv, srcap, writes=[B_stg[si]])
                kb.op("dve", lambda e: e.tensor_copy(out=dstap, in_=stv), reads=[B_stg[si]], writes=[B_We[bi]])

            bctr = [0]

            def block(e_, j, shared=False):
                n = bctr[0]
                bctr[0] += 1
                wi = e_ % NWB
                b2 = n % 2
                row0 = (e_ * 16 + j) * 128
                if shared:
                    kb.dma("sp", xgT[b2][:], h2Tv[:, :, j * 128:(j + 1) * 128], writes=[B_xgT[b2]])
                else:
                    i4 = n % 4
                    kb.dma("pool", idxb[i4][:], idxtab_d[row0:row0 + 128, :], writes=[B_idx[i4]])
                    kb.dma("pool", rowst[b2][:], rowtab_d[row0:row0 + 128, :], writes=[B_rowst[b2]])
                    kb.coll(lambda g: g.indirect_dma_start(out=xg[b2][:, :], out_offset=None, in_=h2tok_d[:, :],
                                                           in_offset=bass.IndirectOffsetOnAxis(ap=idxb[i4][:, :], axis=0)),
                            reads=[B_idx[i4]], writes=[B_xg[b2]])
                    flush_pending()
                    kb.op("dve", lambda e: e.tensor_copy(out=rowcur[:], in_=rowst[b2][:]), reads=[B_rowst[b2]], writes=[B_rowcur])
                    for half in range(2):
                        for kk in range(8):
                            k = half * 8 + kk
                            kb.op("pe", lambda e, k=k, kk=kk: e.transpose(out=psX[half][:, kk, :], in_=xg[b2][:, k * 128:(k + 1) * 128],
                                                                         identity=ident_b[:]),
                                  reads=[B_xg[b2]], writes=[B_psX[half]], inc=(kk == 7))
                        if half == 0:
                            kb.op("act", lambda e: e.copy(out=xgT[b2][:, 0:8, :], in_=psX[0][:]), reads=[B_psX[0]], writes=[B_xgT[b2]])
                        else:
                            kb.op("dve", lambda e: e.tensor_copy(out=xgT[b2][:, 8:16, :], in_=psX[1][:]), reads=[B_psX[1]],
                                  writes=[B_xgT[b2]])
                for k in range(16):
                    kb.op("pe", lambda e, k=k: e.matmul(psg[:], lhsT=xgT[b2][:, k, :], rhs=Wg[wi][:, k, :], start=(k == 0), stop=(k == 15)),
                          reads=[B_xgT[b2], B_We[wi]], writes=[B_psg], inc=(k == 15))
                for k in range(16):
                    kb.op("pe", lambda e, k=k: e.matmul(psu[:], lhsT=xgT[b2][:, k, :], rhs=Wu[wi][:, k, :], start=(k == 0), stop=(k == 15)),
                          reads=[B_xgT[b2], B_We[wi]], writes=[B_psu], inc=(k == 15))
                kb.op("act", lambda e: e.activation(out=sgs[b2][:], in_=psg[:], func=AF.Silu), reads=[B_psg], writes=[B_sgs[b2]])
                kb.op("dve", lambda e: e.tensor_tensor(out=acts[b2][:], in0=psu[:], in1=sgs[b2][:], op=ALU.mult),
                      reads=[B_psu, B_sgs[b2]], writes=[B_acts[b2]])
                for fc in range(4):
                    kb.op("pe", lambda e, fc=fc: e.transpose(out=psA[:, fc, :], in_=acts[b2][:, fc * 128:(fc + 1) * 128], identity=ident_b[:]),
                          reads=[B_acts[b2]], writes=[B_psA], inc=(fc == 3))
                kb.op("act", lambda e: e.copy(out=actT[b2][:], in_=psA[:]), reads=[B_psA], writes=[B_actT[b2]])
                for dc in range(4):
                    yi = dc % 2
                    ds_ = slice(dc * 512, (dc + 1) * 512)
                    for fc in range(4):
                        kb.op("pe", lambda e, fc=fc: e.matmul(psy[yi][:], lhsT=actT[b2][:, fc, :], rhs=Wd[wi][:, fc, ds_],
                                                              start=(fc == 0), stop=(fc == 3)),
                              reads=[B_actT[b2], B_We[wi]], writes=[B_psy[yi]], inc=(fc == 3))
                    ydst, Byd = (yo[b2], B_yo[b2]) if shared else (yor, B_yor)
                    if yi == 0:
                        kb.op("dve", lambda e: e.tensor_copy(out=ydst[:, ds_], in_=psy[yi][:]), reads=[B_psy[yi]], writes=[Byd])
                    else:
                        kb.op("act", lambda e: e.copy(out=ydst[:, ds_], in_=psy[yi][:]), reads=[B_psy[yi]], writes=[Byd])
                if shared:
                    kb.dma("act", slot_d[SHR0 + j * 128:SHR0 + (j + 1) * 128, :], yo[b2][:], reads=[B_yo[b2]])

            NE = cfg_nexp
            for pc in piece_list(0):
                load_piece(pc)
            for e_ in range(NE):
                nxt = piece_list(e_ + 1) if e_ + 1 < NE else []
                if e_ < NEXP:
                    regs = nc.alloc_registers("nbreg%d" % e_, engines=[PEe, DVEe, ACTe, POOLe, SPe])
                    for ek, r in zip(ekeys, regs):
                        kb._wait(kb.eng[ek], B_nbt.w)
                        nc.reg_load(r, nbt[0:1, e_:e_ + 1])
                    nbv = nc.snap(regs, donate=True)
                    def guarded(j):
                        snap = kb.snapshot()
                        with nc.If(nbv > j):
                            block(e_, j)
                        with nc.Else():
                            kb.compensate(snap)

                    def group(js, inner=None):
                        snap = kb.snapshot()
                        with nc.If(nbv > js[0]):
                            for j in js:
                                guarded(j)
                            if inner is not None:
                                inner()
                        with nc.Else():
                            kb.compensate(snap)

                    def pieces(lo, hi):
                        for pc in nxt[lo:hi]:
                            load_piece(pc)

                    def chain(j):
                        snap = kb.snapshot()
                        with nc.If(nbv > j):
                            block(e_, j)
                            if j + 1 < 16:
                                chain(j + 1)
                        with nc.Else():
                            kb.compensate(snap)

                    guarded(0)
                    pieces(0, 6)
                    chain(1)
                    pieces(6, 12)
                    for r in regs:
                        nc.engines[r.engine].free_register(r)
                else:
                    flush_pending()
                    for j in range(16):
                        block(e_, j, shared=True)
            kb.barrier()
        if stop_after == "G":
            return nc

        with ExitStack() as ph:
            B_m3 = Buf()
            g2b = load_bc(ph, "sp", "g2b", mod_d[5:6, :], D, B_m3)
            fgb = load_bc(ph, "sp", "fgb", final_g[0:1, :], D, B_m3)
            dst = sbt(ph, "dst", [128, NT, 8], I32); w8s = sbt(ph, "w8s", [128, NT, 8])
            kb.dma("sp", dst[:], dest_d.rearrange("t p k -> p t k"), writes=[B_m3])
            kb.dma("sp", w8s[:], w8_d.rearrange("t p k -> p t k"), writes=[B_m3])
            x1l = [sbt(ph, "x1l%d" % i, [128, D]) for i in range(2)]; B_x1l = [Buf(), Buf()]
            accA = [sbt(ph, "accA%d" % i, [128, D]) for i in range(2)]; B_accA = [Buf(), Buf()]
            gb = [sbt(ph, "gb%d" % i, [128, D]) for i in range(10)]; B_gb = [Buf() for _ in range(10)]
            junk3 = [sbt(ph, "junk3_%d" % i, [128, D], BF16) for i in range(2)]; B_j3 = [Buf(), Buf()]
            ssq3 = [sbt(ph, "ssq3_%d" % i, [128, 4]) for i in range(2)]; B_s3 = [Buf(), Buf()]
            ot = [sbt(ph, "ot%d" % i, [128, D]) for i in range(2)]; B_ot = [Buf(), Buf()]
            gctr = 0
            for t in range(NT):
                bi = t % 2
                kb.dma("sp", x1l[bi][:], x1_d[t * 128:(t + 1) * 128, :], writes=[B_x1l[bi]])
                kb.dma("sp", accA[bi][:], slot_d[SHR0 + t * 128:SHR0 + (t + 1) * 128, :], writes=[B_accA[bi]])
                for k in range(8):
                    gi = gctr % 10
                    gctr += 1
                    kb.coll(lambda g, k=k, gi=gi: g.indirect_dma_start(
                        out=gb[gi][:, :], out_offset=None, in_=slot_d[:, :],
                        in_offset=bass.IndirectOffsetOnAxis(ap=dst[:, t, k:k + 1], axis=0)), reads=[B_m3], writes=[B_gb[gi]])
                    kb.op("dve", lambda e, k=k, gi=gi: e.scalar_tensor_tensor(
                        out=accA[bi][:], in0=gb[gi][:], scalar=w8s[:, t, k:k + 1], in1=accA[bi][:], op0=ALU.mult, op1=ALU.add),
                        reads=[B_gb[gi], B_m3, B_accA[bi]], writes=[B_accA[bi]])
                kb.op("dve", lambda e: e.tensor_tensor(out=accA[bi][:], in0=accA[bi][:], in1=g2b[:], op=ALU.mult),
                      reads=[B_accA[bi], B_m3], writes=[B_accA[bi]])
                kb.op("pool", lambda e: e.tensor_tensor(out=accA[bi][:], in0=accA[bi][:], in1=x1l[bi][:], op=ALU.add),
                      reads=[B_accA[bi], B_x1l[bi]], writes=[B_accA[bi]])
                rms_mod_tile(accA[bi][:], B_accA[bi], fgb[:], None, B_m3, junk3[bi], B_j3[bi], ssq3[bi], B_s3[bi], ot[bi], B_ot[bi],
                             None, None)
                kb.dma("sp", out_d[t * 128:(t + 1) * 128, :], ot[bi][:], reads=[B_ot[bi]])
            kb.barrier()
        return nc


def _host_consts():
    t = np.arange(128)
    ident = np.eye(128, dtype=np.float32)
    tri = (t[:, None] <= t[None, :]).astype(np.float32)
    triT = (t[:, None] >= t[None, :]).astype(np.float32)
    ones = np.ones((128, 128), np.float32)
    nmf = np.where(t[None, :] >= t[:, None], 0.0, NEG).astype(np.float32)
    nmb = np.where(t[None, :] <= t[:, None], 0.0, NEG).astype(np.float32)
    return np.stack([ident, tri, triT, ones, nmf, nmb])


def _rope_tables(s):
    pos = np.arange(-128, OWN + 128) + s * OWN
    pos = np.clip(pos, 0, SEQ - 1)
    rows = pos // 64
    cols = pos % 64
    inv = (10000.0 ** (-np.arange(0, 64, 2, dtype=np.float32) / 64)).astype(np.float32)
    ar = rows.astype(np.float32)[None, :] * inv[:, None]
    ac = cols.astype(np.float32)[None, :] * inv[:, None]
    cr, sr, cc, sc = np.cos(ar), np.sin(ar), np.cos(ac), np.sin(ac)
    C = np.concatenate([cr, cr, cc, cc], 0).astype(np.float32)
    S = np.concatenate([-sr, sr, -sc, sc], 0).astype(np.float32)
    return C, S


def _masks(s):
    import ml_dtypes
    j = np.arange(128)[:, None]
    r = np.arange(128)[None, :]
    lo = np.where(j >= r, 0.0, NEG).astype(np.float32)
    hi = np.where(j <= r, 0.0, NEG).astype(np.float32)
    allneg = np.full((128, 128), NEG, np.float32)
    lo0 = allneg if s == 0 else lo
    hi15 = allneg if s == 1 else hi
    m = np.stack([np.tile(a, (1, 4)) for a in (lo, hi, lo0, hi15)])
    return m.astype(ml_dtypes.bfloat16)


def _prep_inputs(inp):
    f = lambda a: np.ascontiguousarray(np.asarray(a, dtype=np.float32))
    x = f(inp["x"]); c = f(inp["c"]); ctx = f(inp["ctx"]); c_ctx = f(inp["c_ctx"])
    w_in = f(inp["w_in"][0])
    perm1 = np.concatenate([np.arange(32, 64), np.arange(0, 32), np.arange(96, 128), np.arange(64, 96)])
    perm = np.concatenate([h * 128 + perm1 for h in range(10)])
    w_in_sw = np.ascontiguousarray(w_in[:, :1280][:, perm])
    conv_w = f(inp["conv_w"][0])
    conv_wl = np.ascontiguousarray(conv_w.reshape(5, 16, 128).transpose(2, 1, 0))
    conv_bl = np.ascontiguousarray(f(inp["conv_b"][0]).reshape(16, 128).T)
    ew_gate = np.concatenate([f(inp["expert_w_gate"][0]), f(inp["shared_w_gate"][0])[None]], 0)
    ew_up = np.concatenate([f(inp["expert_w_up"][0]), f(inp["shared_w_up"][0])[None]], 0)
    ew_down = np.concatenate([f(inp["expert_w_down"][0]), f(inp["shared_w_down"][0])[None]], 0)
    consts = _host_consts()
    ee = np.arange(NEXP, dtype=np.float32)
    ebase = np.tile(np.concatenate([ee * OWN + 1.0, (ee + 1.0) * 4.0])[None, :], (128, 1)).astype(np.float32)
    tt = np.arange(128)
    lstrict = (tt[:, None] < tt[None, :]).astype(np.float32)
    oh2 = np.zeros((128, NEXP + 1), np.float32)
    oh2[tt, tt // 2] = 1.0
    oh2[:, NEXP] = (tt % 2) * 1024.0
    shared = dict(
        w_ada=f(inp["w_ada"][0]), b_ada=f(inp["b_ada"]), norm1_g=f(inp["norm1_g"]), norm2_g=f(inp["norm2_g"]),
        w_in=w_in, w_in_sw=w_in_sw, attn_sink=f(inp["attn_sink"]), conv_wl=conv_wl, conv_bl=conv_bl,
        dt_bias=f(inp["dt_bias"]).reshape(1, 32), a_log=f(inp["a_log"]).reshape(1, 32), d_skip=f(inp["d_skip"]),
        ssd_norm_g=f(inp["ssd_norm_g"]), w_out=f(inp["w_out"][0]), router_w=f(inp["router_w"][0]),
        router_bias=f(inp["router_bias"]), ew_gate=ew_gate, ew_up=ew_up, ew_down=ew_down,
        final_g=f(inp["final_norm_g"]).reshape(1, D), consts=consts, ebase=ebase, lstrict=lstrict, oh2=oh2)
    maps = []
    for core in range(8):
        b, s = core // 2, core % 2
        xo = x[b, s * OWN:(s + 1) * OWN]
        halo = np.zeros((2, 128, D), np.float32)
        if s == 1:
            halo[0] = x[b, OWN - 128:OWN]
        else:
            halo[1] = x[b, OWN:OWN + 128]
        cl = np.concatenate([c[b].reshape(16, 128).T, c_ctx.reshape(16, 128).T], 1)
        C, S = _rope_tables(s)
        fl = np.zeros((128, 4), np.float32)
        fl[:, 0] = s; fl[:, 1] = 1 - s; fl[:, 2] = 1.0 if s == 1 else 0.0; fl[:, 3] = 1.0 if s == 0 else 0.0
        dsel = 0 if s == 1 else 1
        xoth = x[b, (1 - s) * OWN:(2 - s) * OWN]
        xadj = xo[0:128] if s == 1 else xo[OWN - 128:OWN]
        tri, triT = consts[1], consts[2]
        extra = dict(x_oth=np.ascontiguousarray(xoth), x_adj=np.ascontiguousarray(xadj),
                     w_dt_sel=np.ascontiguousarray(w_in[:, 4608 + dsel * 16:4608 + dsel * 16 + 16]),
                     dtb_sel=np.ascontiguousarray(shared["dt_bias"][:, dsel * 16:(dsel + 1) * 16]),
                     alog_sel=np.ascontiguousarray(shared["a_log"][:, dsel * 16:(dsel + 1) * 16]),
                     tri_sel=np.ascontiguousarray(tri if dsel == 0 else triT))
        m = dict(shared)
        m.update(extra)
        m.update(x_own=np.ascontiguousarray(xo), x_halo=halo, ctx_b=np.ascontiguousarray(ctx[b]),
                 c_lay=np.ascontiguousarray(cl), rope_c=C, rope_s=S, cmask=_masks(s), flags=fl)
        maps.append(m)
    return maps


def kernel(**inputs):
    maps = _prep_inputs(inputs)
    nc = build()
    res = run_bass_kernel_spmd(nc, maps, core_ids=list(range(8)))
    out = np.zeros((NB, SEQ, D), np.float32)
    for core in range(8):
        b, s = core // 2, core % 2
        out[b, s * OWN:(s + 1) * OWN] = res.results[core]["out"]
    return out
```

```python
from contextlib import ExitStack

import numpy as np
import concourse.bass as bass
import concourse.mybir as mybir
from concourse.bass_utils import run_bass_kernel_spmd

F32 = mybir.dt.float32
BF16 = mybir.dt.bfloat16
AF = mybir.ActivationFunctionType
ALU = mybir.AluOpType
AX = mybir.AxisListType

D = 2048
SEQ = 4096
NB = 4
CTX = 256
OWN = 2048
NT = 16
INW = 4640
NEXP = 64
EPS = 1e-6
NEG = -30000.0
NSLOT = 8


class Tok:
    __slots__ = ("sem", "val", "ek")

    def __init__(self, sem, val, ek):
        self.sem, self.val, self.ek = sem, val, ek


class Buf:
    def __init__(self, name=""):
        self.name = name
        self.w = None
        self.r = {}


class Eng:
    def __init__(self, key, obj, sem):
        self.key, self.obj, self.sem = key, obj, sem
        self.count = 0
        self.pending = False
        self.seen = {}
        self.slots = []
        self.nslot = 0


class KB:
    def __init__(self, nc, es):
        self.nc = nc
        self.eng = {}
        for key, obj in (("pe", nc.tensor), ("dve", nc.vector), ("act", nc.scalar),
                         ("pool", nc.gpsimd), ("sp", nc.sync)):
            sem = es.enter_context(nc.semaphore("s_" + key))
            self.eng[key] = Eng(key, obj, sem)
        for key in ("sp", "pool", "act"):
            e = self.eng[key]
            for i in range(NSLOT):
                e.slots.append([es.enter_context(nc.semaphore("d_%s%d" % (key, i))), 0])

    def _wait(self, e, tok):
        if tok is None:
            return
        sid = id(tok.sem)
        if e.seen.get(sid, 0) >= tok.val:
            return
        e.obj.wait_ge(tok.sem, tok.val)
        e.seen[sid] = tok.val

    def _deps(self, e, reads, writes):
        for b in reads:
            if b.w is not None and not (b.w.ek == e.key and e.key == "pe"):
                self._wait(e, b.w)
        for b in writes:
            if b.w is not None and not (b.w.ek == e.key and e.key == "pe"):
                self._wait(e, b.w)
            for ek, t in b.r.items():
                if ek != e.key:
                    self._wait(e, t)

    def op(self, ek, fn, reads=(), writes=(), inc=True):
        e = self.eng[ek]
        self._deps(e, reads, writes)
        ins = fn(e.obj)
        if inc:
            ins.then_inc(e.sem, 1)
            e.count += 1
            e.pending = False
            tok = Tok(e.sem, e.count, ek)
        else:
            e.pending = True
            tok = Tok(e.sem, e.count + 1, ek)
        for b in writes:
            b.w = tok
            b.r = {}
        for b in reads:
            b.r[ek] = tok
        return ins

    def dma(self, qk, out, in_, reads=(), writes=(), **kw):
        e = self.eng[qk]
        slot = e.slots[e.nslot % NSLOT]
        e.nslot += 1
        if slot[1] > 0:
            self._wait(e, Tok(slot[0], 16 * slot[1], "dma"))
        self._deps(e, reads, writes)
        ins = e.obj.dma_start(out=out, in_=in_, **kw)
        ins.then_inc(slot[0], 16)
        slot[1] += 1
        tok = Tok(slot[0], 16 * slot[1], "dma_" + qk + str(id(slot[0])))
        for b in writes:
            b.w = tok
            b.r = {}
        for b in reads:
            b.r[tok.ek] = tok
        return ins

    def coll(self, fn, reads=(), writes=()):
        e = self.eng["pool"]
        slot = e.slots[e.nslot % NSLOT]
        e.nslot += 1
        if slot[1] > 0:
            self._wait(e, Tok(slot[0], 16 * slot[1], "dma"))
        self._deps(e, reads, writes)
        ins = fn(e.obj)
        ins.then_inc(slot[0], 16)
        slot[1] += 1
        tok = Tok(slot[0], 16 * slot[1], "dma_coll")
        for b in writes:
            b.w = tok
            b.r = {}
        for b in reads:
            b.r[tok.ek] = tok
        return ins

    def snapshot(self):
        for e in self.eng.values():
            assert not e.pending
        return {k: (e.count, [s[1] for s in e.slots], dict(e.seen)) for k, e in self.eng.items()}

    def compensate(self, snap):
        for k, e in self.eng.items():
            c0, uses0, seen0 = snap[k]
            assert not e.pending
            if e.count > c0:
                if c0 > 0:
                    e.obj.wait_ge(e.sem, c0)
                e.obj.sem_inc(e.sem, e.count - c0)
            for s, u0 in zip(e.slots, uses0):
                if s[1] > u0:
                    if u0 > 0:
                        e.obj.wait_ge(s[0], 16 * u0)
                    e.obj.sem_inc(s[0], 16 * (s[1] - u0))
            e.seen = dict(seen0)

    def barrier(self):
        toks = []
        for e in self.eng.values():
            assert not e.pending, e.key
            if e.count:
                toks.append(Tok(e.sem, e.count, e.key))
            for s in e.slots:
                if s[1]:
                    toks.append(Tok(s[0], 16 * s[1], "dma"))
        for e in self.eng.values():
            for t in toks:
                if t.ek == e.key:
                    continue
                self._wait(e, t)


def bcast_row(ap_row, n=128):
    return ap_row.partition_broadcast(n)


def build(stop_after=None, dbg=(), ncores=8, cut=99, cfg_nexp=NEXP + 1):
    nc = bass.Bass("TRN2", target_bir_lowering=False)
    dbg = set(dbg)

    class _Lazy:
        def __init__(self, name, shape, dt):
            self.name, self.shape, self.dt, self._ap = name, shape, dt, None

        @property
        def ap(self):
            if self._ap is None:
                self._ap = nc.dram_tensor(self.name, list(self.shape), self.dt, kind="ExternalInput").ap()
                used_inputs.append(self.name)
            return self._ap

        def __getitem__(self, key):
            return self.ap[key]

        def rearrange(self, *a, **k):
            return self.ap.rearrange(*a, **k)

    used_inputs = []
    nc._used_inputs = used_inputs

    def din(name, shape, dt=F32):
        return _Lazy(name, shape, dt)

    def dscr(name, shape, dt=F32):
        kind = "ExternalOutput" if name in dbg else "Internal"
        return nc.dram_tensor(name, list(shape), dt, kind=kind).ap()

    x_own = din("x_own", [OWN, D])
    x_halo = din("x_halo", [2, 128, D])
    ctx_in = din("ctx_b", [CTX, D])
    c_lay = din("c_lay", [128, 32])
    w_ada = din("w_ada", [D, 6 * D])
    b_ada = din("b_ada", [1, 6 * D])
    norm1_g = din("norm1_g", [1, D])
    norm2_g = din("norm2_g", [1, D])
    w_in = din("w_in", [D, INW])
    w_in_sw = din("w_in_sw", [D, 1280])
    attn_sink = din("attn_sink", [1, 8])
    conv_wl = din("conv_wl", [128, 16, 5])
    conv_bl = din("conv_bl", [128, 16])
    dt_bias = din("dt_bias", [1, 32])
    a_log = din("a_log", [1, 32])
    d_skip = din("d_skip", [1, 16])
    ssd_norm_g = din("ssd_norm_g", [1, 1024])
    w_out = din("w_out", [D, D])
    router_w = din("router_w", [D, NEXP])
    router_bias = din("router_bias", [1, NEXP])
    ew_gate = din("ew_gate", [NEXP + 1, D, 512])
    ew_up = din("ew_up", [NEXP + 1, D, 512])
    ew_down = din("ew_down", [NEXP + 1, 512, D])
    final_g = din("final_g", [1, D])
    rope_c = din("rope_c", [128, 2304])
    rope_s = din("rope_s", [128, 2304])
    cmask = din("cmask", [4, 128, 512], BF16)
    flags = din("flags", [128, 4])
    consts = din("consts", [6, 128, 128])
    x_oth = din("x_oth", [OWN, D])
    x_adj = din("x_adj", [128, D])
    w_dt_sel = din("w_dt_sel", [D, 16])
    dtb_sel = din("dtb_sel", [1, 16])
    alog_sel = din("alog_sel", [1, 16])
    tri_sel = din("tri_sel", [128, 128])
    ebase = din("ebase", [128, 2 * NEXP])
    lstrict = din("lstrict", [128, 128])
    oh2 = din("oh2", [128, NEXP + 1])

    out_d = nc.dram_tensor("out", [OWN, D], F32, kind="ExternalOutput").ap()

    mod_d = dscr("mod_d", [8, D])
    qT_d = dscr("qT_d", [8, 128, OWN], BF16)
    kT_d = dscr("kT_d", [2, 128, 2304], BF16)
    kcT_d = dscr("kcT_d", [2, 128, CTX], BF16)
    v_d = dscr("v_d", [20, 128, 256], BF16)
    zs_d = dscr("zs_d", [NT, 128, 1024])
    dtraw_d = dscr("dtraw_d", [18, 128, 32])
    uT_d = dscr("uT_d", [16, 128, OWN], BF16)
    uTc_d = dscr("uTc_d", [16, 128, CTX], BF16)
    mixT_d = dscr("mixT_d", [16, 128, OWN], BF16)
    uT2_d = dscr("uT2_d", [12, 128, OWN], BF16)
    dtraw2_d = dscr("dtraw2_d", [NT, 128, 16])
    h0_d = dscr("h0_d", [2, NT, 128, 1024])
    xch_in = dscr("xch_in", [128, 2112])
    xch_out = dscr("xch_out", [256, 2112])
    x1_d = dscr("x1_d", [OWN, D])
    h2T_d = dscr("h2T_d", [16, 128, OWN], BF16)
    gate_d = dscr("gate_d", [NT, 128, NEXP + 1])
    I32 = mybir.dt.int32
    NSL = NEXP * OWN
    h2tok_d = dscr("h2tok_d", [OWN + 128, D], BF16)
    idxtab_d = dscr("idxtab_d", [NSL, 1], I32)
    dest_d = dscr("dest_d", [NT, 128, 8], I32)
    w8_d = dscr("w8_d", [NT, 128, 8])
    nblk_d = dscr("nblk_d", [1, NEXP], I32)
    NSLC = 192 * 128
    SHR0 = NSLC + OWN
    slot_d = dscr("slot_d", [SHR0 + OWN, D])
    cbase_d = dscr("cbase_d", [1, NEXP], I32)
    rowtab_d = dscr("rowtab_d", [NSL, 1], I32)

    es = ExitStack()
    with es:
        kb = KB(nc, es)

        def sbt(st, name, shape, dt=F32):
            return st.enter_context(nc.sbuf_tensor(name, list(shape), dt))

        def pst(st, name, shape, dt=F32):
            return st.enter_context(nc.psum_tensor(name, list(shape), dt))

        ident_f = sbt(es, "ident_f", [128, 128]); ones_f = sbt(es, "ones_f", [128, 128])
        ident_b = sbt(es, "ident_b", [128, 128], BF16); ones_b = sbt(es, "ones_b", [128, 128], BF16)
        flg = sbt(es, "flg", [128, 4])
        B_const = Buf("const")
        for i, t in ((0, ident_f), (3, ones_f)):
            kb.dma("sp", t[:], consts[i], writes=[B_const])
        kb.dma("sp", flg[:], flags[:, :], writes=[B_const])
        kb.op("dve", lambda e: e.tensor_copy(out=ident_b[:], in_=ident_f[:]), reads=[B_const], writes=[B_const])
        kb.op("dve", lambda e: e.tensor_copy(out=ones_b[:], in_=ones_f[:]), reads=[B_const], writes=[B_const])
        kb.barrier()

        with ExitStack() as ph:
            cl = sbt(ph, "cl", [128, 32]); cs = sbt(ph, "cs", [128, 32])
            LC = sbt(ph, "LC", [128, 32, 128], BF16)
            Wb = [sbt(ph, "Wa%d" % i, [128, 16, 512], BF16) for i in range(2)]
            bb = [sbt(ph, "ba%d" % i, [128, 512]) for i in range(2)]
            mo = [sbt(ph, "mo%d" % i, [128, 512]) for i in range(4)]
            psA = [pst(ph, "psA%d" % i, [128, 512]) for i in range(4)]
            B_cl, B_LC = Buf(), Buf()
            B_W = [Buf(), Buf()]; B_b = [Buf(), Buf()]; B_mo = [Buf() for _ in range(4)]
            B_ps = [Buf() for _ in range(4)]
            kb.dma("sp", cl[:], c_lay[:, :], writes=[B_cl])
            kb.op("act", lambda e: e.activation(out=cs[:], in_=cl[:], func=AF.Silu), reads=[B_cl], writes=[B_cl])
            for k in range(32):
                kb.op("dve", lambda e, k=k: e.tensor_copy(out=LC[:, k, :], in_=cs[:, k:k + 1].to_broadcast([128, 128])),
                      reads=[B_cl], writes=[B_LC])
            wv = w_ada.rearrange("(k p) n -> p k n", p=128)
            nblk = 24
            it = 0
            for j in range(nblk):
                bi = j % 2
                kb.dma("pool", Wb[bi][:], wv[:, :, j * 512:(j + 1) * 512], writes=[B_W[bi]])
                kb.dma("sp", bb[bi][:], bcast_row(b_ada[0:1, j * 512:(j + 1) * 512]), writes=[B_b[bi]])
                chunk = j // 4
                for which in range(2 if j < 8 else 1):
                    pi = it % 4
                    it += 1
                    for k in range(16):
                        kb.op("pe", lambda e, k=k, pi=pi, which=which, bi=bi: e.matmul(
                            psA[pi][:], lhsT=LC[:, which * 16 + k, :], rhs=Wb[bi][:, k, :],
                            start=(k == 0), stop=(k == 15)),
                            reads=[B_LC, B_W[bi]], writes=[B_ps[pi]], inc=(k == 15))
                    if chunk in (1, 4):
                        kb.op("dve", lambda e, pi=pi, bi=bi: e.scalar_tensor_tensor(
                            out=mo[pi][:], in0=psA[pi][:], scalar=1.0, in1=bb[bi][:], op0=ALU.add, op1=ALU.add),
                            reads=[B_ps[pi], B_b[bi]], writes=[B_mo[pi]])
                    else:
                        kb.op("dve", lambda e, pi=pi, bi=bi: e.tensor_tensor(
                            out=mo[pi][:], in0=psA[pi][:], in1=bb[bi][:], op=ALU.add),
                            reads=[B_ps[pi], B_b[bi]], writes=[B_mo[pi]])
                    row = chunk if which == 0 else 6 + chunk
                    c0 = (j % 4) * 512
                    kb.dma("sp", mod_d[row:row + 1, c0:c0 + 512], mo[pi][0:1, :], reads=[B_mo[pi]])
            kb.barrier()
        if stop_after == "A":
            return nc

        def load_bc(st, qk, name, row_ap, width, buf):
            t = sbt(st, name, [128, width])
            kb.dma(qk, t[:], bcast_row(row_ap), writes=[buf])
            return t

        def rms_mod_tile(xt, B_x, G, shv, B_mod, junk, B_junk, ssq, B_ss, tmp, B_tmp, outb, B_out, width=D):
            kb.op("act", lambda e: e.activation(out=junk[:], in_=xt, func=AF.Square, accum_out=ssq[:, 0:1]),
                  reads=[B_x], writes=[B_junk, B_ss])
            kb.op("dve", lambda e: e.tensor_scalar(out=ssq[:, 1:2], in0=ssq[:, 0:1], scalar1=1.0 / width, scalar2=EPS,
                                                   op0=ALU.mult, op1=ALU.add), reads=[B_ss], writes=[B_ss])
            kb.op("act", lambda e: e.sqrt(out=ssq[:, 3:4], in_=ssq[:, 1:2]), reads=[B_ss], writes=[B_ss])
            kb.op("dve", lambda e: e.reciprocal(out=ssq[:, 2:3], in_=ssq[:, 3:4]), reads=[B_ss], writes=[B_ss])
            kb.op("dve", lambda e: e.scalar_tensor_tensor(out=tmp[:], in0=xt, scalar=ssq[:, 2:3], in1=G,
                                                          op0=ALU.mult, op1=ALU.mult),
                  reads=[B_x, B_ss, B_mod], writes=[B_tmp])
            if shv is not None:
                kb.op("dve", lambda e: e.tensor_tensor(out=outb, in0=tmp[:], in1=shv, op=ALU.add),
                      reads=[B_tmp, B_mod], writes=[B_out])

        def phase_BC(tag, cfg):
          with ExitStack() as phBC:
              ntile = cfg["ntile"]
              hT = sbt(phBC, "hT" + tag, [128, ntile, 16, 128], BF16)
              B_hT = [Buf("hT%d" % i) for i in range(ntile)]
              with ExitStack() as ph:
                  B_mod = Buf("mod1")
                  g1n = load_bc(ph, "sp", "g1n" + tag, norm1_g[0:1, :], D, B_mod)
                  GL = load_bc(ph, "sp", "GL" + tag, mod_d[1:2, :], D, B_mod)
                  SHL = load_bc(ph, "sp", "SHL" + tag, mod_d[0:1, :], D, B_mod)
                  GC = load_bc(ph, "sp", "GC" + tag, mod_d[7:8, :], D, B_mod)
                  SHC = load_bc(ph, "sp", "SHC" + tag, mod_d[6:7, :], D, B_mod)
                  kb.op("dve", lambda e: e.tensor_tensor(out=GL[:], in0=GL[:], in1=g1n[:], op=ALU.mult),
                        reads=[B_mod], writes=[B_mod])
                  kb.op("dve", lambda e: e.tensor_tensor(out=GC[:], in0=GC[:], in1=g1n[:], op=ALU.mult),
                        reads=[B_mod], writes=[B_mod])
                  xt = [sbt(ph, "xt%d" % i + tag, [128, D]) for i in range(2)]
                  B_x = [Buf(), Buf()]
                  junk = sbt(ph, "junk" + tag, [128, D], BF16); B_junk = Buf()
                  tmp = sbt(ph, "tmpB" + tag, [128, D]); B_tmp = Buf()
                  hb = [sbt(ph, "hb%d" % i + tag, [128, D], BF16) for i in range(2)]
                  B_hb = [Buf(), Buf()]
                  ssq = [sbt(ph, "ssq%d" % i + tag, [128, 4]) for i in range(2)]
                  B_ss = [Buf(), Buf()]
                  psT = [pst(ph, "psT%d" % i + tag, [128, 8, 128], BF16) for i in range(2)]
                  B_psT = [Buf(), Buf()]

                  src_of = cfg["src_of"]

                  kb.dma("sp", xt[0][:], src_of(0), writes=[B_x[0]])
                  for t in range(ntile):
                      bi = t % 2
                      if t + 1 < ntile:
                          kb.dma("sp", xt[1 - bi][:], src_of(t + 1), writes=[B_x[1 - bi]])
                      G, SHv = (GC, SHC) if t < cfg["nctx"] else (GL, SHL)
                      rms_mod_tile(xt[bi][:], B_x[bi], G[:], SHv[:], B_mod, junk, B_junk, ssq[bi], B_ss[bi],
                                   tmp, B_tmp, hb[bi][:], B_hb[bi])
                      for half in range(2):
                          for kk in range(8):
                              k = half * 8 + kk
                              kb.op("pe", lambda e, k=k, kk=kk, half=half, bi=bi: e.transpose(
                                  out=psT[half][:, kk, :], in_=hb[bi][:, k * 128:(k + 1) * 128], identity=ident_b[:]),
                                  reads=[B_hb[bi]], writes=[B_psT[half]], inc=(kk == 7))
                          eng = "act" if half == 0 else "dve"
                          kb.op(eng, lambda e, t=t, half=half: e.tensor_copy(out=hT[:, t, half * 8:(half + 1) * 8, :],
                                                                            in_=psT[half][:]) if eng != "act" else
                                e.copy(out=hT[:, t, half * 8:(half + 1) * 8, :], in_=psT[half][:]),
                                reads=[B_psT[half]], writes=[B_hT[t]])
                  kb.barrier()
              with ExitStack() as ph:
                  wv = w_in.rearrange("(k p) n -> p k n", p=128)
                  wsv = w_in_sw.rearrange("(k p) n -> p k n", p=128)
                  Wn = [sbt(ph, "Wn%d" % i + tag, [128, 16, 512], BF16) for i in range(2)]
                  Ws = [sbt(ph, "Ws%d" % i + tag, [128, 16, 128], BF16) for i in range(2)]
                  B_Wn = [Buf(), Buf()]; B_Ws = [Buf(), Buf()]
                  B_cst = Buf()
                  rc = sbt(ph, "rc" + tag, [128, 2304]); rs = sbt(ph, "rs" + tag, [128, 2304])
                  kb.dma("sp", rc[:], rope_c[:, :], writes=[B_cst])
                  kb.dma("sp", rs[:], rope_s[:, :], writes=[B_cst])
                  cw = sbt(ph, "cw" + tag, [128, 16, 5]); cb = sbt(ph, "cb" + tag, [128, 16])
                  kb.dma("sp", cw[:], conv_wl[:, :, :], writes=[B_cst])
                  kb.dma("sp", cb[:], conv_bl[:, :], writes=[B_cst])
                  psC = [pst(ph, "psC%d" % i + tag, [128, 512]) for i in range(4)]
                  B_psC = [Buf() for _ in range(4)]
                  ev = [sbt(ph, "ev%d" % i + tag, [128, 512]) for i in range(4)]
                  B_ev = [Buf() for _ in range(4)]
                  evb = [sbt(ph, "evb%d" % i + tag, [128, 512], BF16) for i in range(2)]
                  B_evb = [Buf(), Buf()]
                  v_all = sbt(ph, "v_all" + tag, [128, 20, 256], BF16); B_vall = Buf()
                  dt_all = sbt(ph, "dt_all" + tag, [128, 18, 32]); B_dtall = Buf()
                  xraw = [sbt(ph, "xraw%d" % i + tag, [128, 2052]) for i in range(2)]
                  B_xraw = [Buf(), Buf()]
                  xrawc = [sbt(ph, "xrawc%d" % i + tag, [128, 260]) for i in range(2)]
                  B_xrawc = [Buf(), Buf()]
                  acc0 = sbt(ph, "acc0" + tag, [128, 2048])
                  acc = [acc0, acc0]
                  B_acc0 = Buf()
                  B_acc = [B_acc0, B_acc0]
                  accc = sbt(ph, "accc" + tag, [128, 256]); B_accc = Buf()
                  uo = [sbt(ph, "uo%d" % i + tag, [128, 2048], BF16) for i in range(2)]
                  B_uo = [Buf(), Buf()]
                  uoc = [sbt(ph, "uoc%d" % i + tag, [128, 256], BF16) for i in range(2)]
                  B_uoc = [Buf(), Buf()]
                  for i in range(2):
                      kb.op("pool", lambda e, i=i: e.memset(xrawc[i][:], 0.0), writes=[B_xrawc[i]])

                  jobs = cfg["jobs"]

                  def load_job(ji):
                      kind, c0, ncol, idx = jobs[ji]
                      bi = ji % 2
                      wsrc = cfg["wdt"] if (kind == "dt" and cfg.get("wdt") is not None) else wv[:, :, c0:c0 + ncol]
                      kb.dma("pool", Wn[bi][:, :, 0:ncol], wsrc, writes=[B_Wn[bi]])
                      if kind in ("q", "k"):
                          kb.dma("pool", Ws[bi][:], wsv[:, :, c0:c0 + 128], writes=[B_Ws[bi]])

                  pctr = [0]

                  def fm_mm(W, B_W, tile0, ntile, tok_lo=0, tok_hi=128):
                      pi = pctr[0] % 4
                      pctr[0] += 1
                      n = ntile * (tok_hi - tok_lo)
                      for k in range(16):
                          kb.op("pe", lambda e, k=k: e.matmul(
                              psC[pi][:, 0:n], lhsT=W[:, k, 0:128], rhs=hT[:, tile0:tile0 + ntile, k, tok_lo:tok_hi],
                              start=(k == 0), stop=(k == 15)),
                              reads=[B_W] + B_hT[tile0:tile0 + ntile], writes=[B_psC[pi]], inc=(k == 15))
                      return pi

                  def tm_mm(W, B_W, t, ncol):
                      pi = pctr[0] % 4
                      pctr[0] += 1
                      for k in range(16):
                          kb.op("pe", lambda e, k=k: e.matmul(
                              psC[pi][:, 0:ncol], lhsT=hT[:, t, k, :], rhs=W[:, k, 0:ncol],
                              start=(k == 0), stop=(k == 15)),
                              reads=[B_W, B_hT[t]], writes=[B_psC[pi]], inc=(k == 15))
                      return pi

                  ectr = [0]
                  load_job(0)
                  for ji, (kind, c0, ncol, idx) in enumerate(jobs):
                      if ji + 1 < len(jobs):
                          load_job(ji + 1)
                      bi = ji % 2
                      W, BW = Wn[bi], B_Wn[bi]
                      if kind in ("q", "k"):
                          if kind == "q":
                              groups = [(3 + 4 * g, 4, 128 + g * 512, g * 512) for g in range(4)]
                              dest = qT_d[idx]
                          else:
                              groups = [(2 + 4 * g, 4, g * 512, g * 512) for g in range(4)] + [(18, 2, 2048, 2048)]
                              dest = kT_d[idx]
                          for (t0, ntl, roff, doff) in groups:
                              n = ntl * 128
                              pa = fm_mm(W, BW, t0, ntl)
                              pb = fm_mm(Ws[bi], B_Ws[bi], t0, ntl)
                              e0, e1 = (ectr[0] % 2) * 2, (ectr[0] % 2) * 2 + 1
                              eb = ectr[0] % 2
                              ectr[0] += 1
                              kb.op("dve", lambda e: e.tensor_tensor(out=ev[e0][:, 0:n], in0=psC[pa][:, 0:n],
                                                                     in1=rc[:, roff:roff + n], op=ALU.mult),
                                    reads=[B_psC[pa], B_cst], writes=[B_ev[e0]])
                              kb.op("dve", lambda e: e.tensor_tensor(out=ev[e1][:, 0:n], in0=psC[pb][:, 0:n],
                                                                     in1=rs[:, roff:roff + n], op=ALU.mult),
                                    reads=[B_psC[pb], B_cst], writes=[B_ev[e1]])
                              kb.op("pool", lambda e: e.tensor_tensor(out=evb[eb][:, 0:n], in0=ev[e0][:, 0:n],
                                                                      in1=ev[e1][:, 0:n], op=ALU.add),
                                    reads=[B_ev[e0], B_ev[e1]], writes=[B_evb[eb]])
                              kb.dma("sp", dest[:, doff:doff + n], evb[eb][:, 0:n], reads=[B_evb[eb]])
                          if kind == "k":
                              pa = fm_mm(W, BW, 0, 2)
                              eb = ectr[0] % 2
                              ectr[0] += 1
                              kb.op("act", lambda e: e.copy(out=evb[eb][:, 0:256], in_=psC[pa][:, 0:256]),
                                    reads=[B_psC[pa]], writes=[B_evb[eb]])
                              kb.dma("sp", kcT_d[idx], evb[eb][:, 0:256], reads=[B_evb[eb]])
                      elif kind == "v":
                          for t in range(20):
                              pa = tm_mm(W, BW, t, 256)
                              kb.op("act", lambda e, t=t: e.copy(out=v_all[:, t, :], in_=psC[pa][:, 0:256]),
                                    reads=[B_psC[pa]], writes=[B_vall])
                          kb.dma("sp", v_d.rearrange("t p c -> p t c"), v_all[:], reads=[B_vall])
                      elif kind == "z":
                          for t in range(NT):
                              pa = tm_mm(W, BW, 3 + t, 512)
                              e0 = ectr[0] % 4
                              ectr[0] += 1
                              kb.op("act", lambda e: e.activation(out=ev[e0][:], in_=psC[pa][:], func=AF.Silu),
                                    reads=[B_psC[pa]], writes=[B_ev[e0]])
                              kb.dma("sp", zs_d[t][:, idx * 512:(idx + 1) * 512], ev[e0][:], reads=[B_ev[e0]])
                      elif kind == "dt":
                          dtt = cfg["dt_tiles"]
                          for i, t in enumerate(dtt):
                              pa = tm_mm(W, BW, t, ncol)
                              kb.op("act", lambda e, i=i: e.copy(out=dt_all[:, i, 0:ncol], in_=psC[pa][:, 0:ncol]),
                                    reads=[B_psC[pa]], writes=[B_dtall])
                          kb.dma("sp", cfg["dt_dst"].rearrange("t p c -> p t c"), dt_all[:, 0:len(dtt), 0:ncol], reads=[B_dtall])
                      else:
                          j = idx
                          xi = j % 2
                          xr, Bxr = xraw[xi], B_xraw[xi]
                          for g in range(4):
                              pa = fm_mm(W, BW, cfg["own0"] + 4 * g, 4)
                              kb.op("act", lambda e, g=g: e.copy(out=xr[:, 2 + g * 512:2 + (g + 1) * 512], in_=psC[pa][:]),
                                    reads=[B_psC[pa]], writes=[Bxr])
                          pa = fm_mm(W, BW, cfg["halo_lo"], 1, 126, 128)
                          kb.op("dve", lambda e: e.tensor_scalar(out=xr[:, 0:2], in0=psC[pa][:, 0:2], scalar1=cfg["fl_lo"],
                                                                 scalar2=None, op0=ALU.mult),
                                reads=[B_psC[pa]], writes=[Bxr])
                          pa = fm_mm(W, BW, cfg["halo_hi"], 1, 0, 2)
                          kb.op("dve", lambda e: e.tensor_scalar(out=xr[:, 2050:2052], in0=psC[pa][:, 0:2],
                                                                 scalar1=cfg["fl_hi"], scalar2=None, op0=ALU.mult),
                                reads=[B_psC[pa]], writes=[Bxr])
                          if cfg["nctx"]:
                              pa = fm_mm(W, BW, 0, 2)
                              kb.op("act", lambda e: e.copy(out=xrawc[xi][:, 2:258], in_=psC[pa][:, 0:256]),
                                    reads=[B_psC[pa]], writes=[B_xrawc[xi]])
                          convs = [(xr, Bxr, acc[xi], B_acc[xi], 2048, uo[xi], B_uo[xi], cfg["uT_dst"][j])]
                          if cfg["nctx"]:
                              convs.append((xrawc[xi], B_xrawc[xi], accc, B_accc, 256, uoc[xi], B_uoc[xi], uTc_d[j]))
                          for (src, Bs, dst, Bd, n, o, Bo, dd) in convs:
                              kb.op("dve", lambda e: e.tensor_scalar(out=dst[:, 0:n], in0=src[:, 0:n], scalar1=cw[:, j, 0:1],
                                                                     scalar2=None, op0=ALU.mult),
                                    reads=[Bs, B_cst], writes=[Bd])
                              for tap in range(1, 5):
                                  kb.op("dve", lambda e, tap=tap: e.scalar_tensor_tensor(
                                      out=dst[:, 0:n], in0=src[:, tap:tap + n], scalar=cw[:, j, tap:tap + 1],
                                      in1=dst[:, 0:n], op0=ALU.mult, op1=ALU.add),
                                      reads=[Bs, B_cst, Bd], writes=[Bd])
                              kb.op("act", lambda e: e.activation(out=o[:, 0:n], in_=dst[:, 0:n], func=AF.Silu,
                                                                  bias=cb[:, j:j + 1]),
                                    reads=[Bd, B_cst], writes=[Bo])
                              kb.dma("sp", dd, o[:, 0:n], reads=[Bo])
                  kb.barrier()
        def main_src(t):
            if t < 2:
                return ctx_in[t * 128:(t + 1) * 128, :]
            if t == 2:
                return x_halo[0]
            if t == 19:
                return x_halo[1]
            return x_own[(t - 3) * 128:(t - 2) * 128, :]

        jobs_main = []
        for h in range(8):
            jobs_main.append(("q", h * 128, 128, h))
        for kv in range(2):
            jobs_main.append(("k", 1024 + kv * 128, 128, kv))
        jobs_main.append(("v", 1280, 256, 0))
        jobs_main.append(("z", 1536, 512, 0))
        jobs_main.append(("z", 2048, 512, 1))
        jobs_main.append(("dt", 4608, 32, 0))
        for j in range(16):
            jobs_main.append(("x", 2560 + j * 128, 128, j))
        phase_BC("m", dict(ntile=20, nctx=2, src_of=main_src, jobs=jobs_main, own0=3, halo_lo=2, halo_hi=19,
                           fl_lo=flg[:, 2:3], fl_hi=flg[:, 3:4], uT_dst=uT_d, dt_tiles=[0, 1] + list(range(3, 19)),
                           dt_dst=dtraw_d, wdt=None))
        if stop_after == "C":
            return nc

        def oth_src(t):
            if t == 0:
                return x_adj[:, :]
            return x_oth[(t - 1) * 128:t * 128, :]

        jobs_oth = [("dt", 0, 16, 0)] + [("x", 2560 + j * 128, 128, j) for j in range(12)]
        phase_BC("o", dict(ntile=17, nctx=0, src_of=oth_src, jobs=jobs_oth, own0=1, halo_lo=0, halo_hi=0,
                           fl_lo=flg[:, 3:4], fl_hi=flg[:, 2:3], uT_dst=uT2_d, dt_tiles=list(range(1, 17)),
                           dt_dst=dtraw2_d, wdt=w_dt_sel.rearrange("(k p) n -> p k n", p=128)))
        if stop_after == "C2":
            return nc

        with ExitStack() as ph:
            qT = sbt(ph, "qT", [128, 8, OWN], BF16)
            kT = sbt(ph, "kT", [128, 2, 2304], BF16)
            kcT = sbt(ph, "kcT", [128, 2, CTX], BF16)
            vv = sbt(ph, "vv", [128, 20, 256], BF16)
            msk = sbt(ph, "msk", [128, 4, 512], BF16)
            esk = sbt(ph, "esk", [128, 8])
            B_in = Buf()
            kb.dma("sp", qT[:], qT_d.rearrange("h p t -> p h t"), writes=[B_in])
            kb.dma("sp", kT[:], kT_d.rearrange("h p t -> p h t"), writes=[B_in])
            kb.dma("sp", kcT[:], kcT_d.rearrange("h p t -> p h t"), writes=[B_in])
            kb.dma("sp", vv[:], v_d.rearrange("t p c -> p t c"), writes=[B_in])
            kb.dma("sp", msk[:], cmask.rearrange("m p c -> p m c"), writes=[B_in])
            kb.dma("sp", esk[:], bcast_row(attn_sink[0:1, :]), writes=[B_in])
            kb.op("act", lambda e: e.activation(out=esk[:], in_=esk[:], func=AF.Exp), reads=[B_in], writes=[B_in])
            psS = [pst(ph, "psS%d" % i, [128, 512]) for i in range(2)]
            psO = [pst(ph, "psO%d" % i, [128, 512]) for i in range(2)]
            psD = [pst(ph, "psD%d" % i, [128, 512]) for i in range(2)]
            B_psS = [Buf(), Buf()]; B_psO = [Buf(), Buf()]; B_psD = [Buf(), Buf()]
            Eb = [sbt(ph, "Eb%d" % i, [128, 512], BF16) for i in range(3)]
            B_E = [Buf() for _ in range(3)]
            den = [sbt(ph, "den%d" % i, [128, 512]) for i in range(2)]
            B_den = [Buf(), Buf()]
            ob = [sbt(ph, "ob%d" % i, [128, 512], BF16) for i in range(2)]
            B_ob = [Buf(), Buf()]
            scale = 128 ** -0.5
            sctr = 0
            ectr_ = 0
            for i in range(NT):
                for kv in range(2):
                    pi = (i * 2 + kv) % 2
                    rhs_q = qT[:, 4 * kv:4 * kv + 4, i * 128:(i + 1) * 128]
                    keyt = []
                    for j in range(3):
                        m = None
                        if j == 0:
                            m = 2 if i == 0 else 0
                        if j == 2:
                            m = 3 if i == NT - 1 else 1
                        keyt.append((kT[:, kv, (i + j) * 128:(i + j + 1) * 128], 2 + i + j, m))
                    for cc in range(2):
                        keyt.append((kcT[:, kv, cc * 128:(cc + 1) * 128], cc, None))
                    for n, (kap, vt, m) in enumerate(keyt):
                        si = sctr % 2
                        sctr += 1
                        ei = ectr_ % 3
                        ectr_ += 1
                        kb.op("pe", lambda e: e.matmul(psS[si][:], lhsT=kap, rhs=rhs_q, start=True, stop=(m is None)),
                              reads=[B_in], writes=[B_psS[si]], inc=(m is None))
                        if m is not None:
                            kb.op("pe", lambda e: e.matmul(psS[si][:], lhsT=ident_b[:], rhs=msk[:, m, :], start=False, stop=True),
                                  reads=[B_in], writes=[B_psS[si]])
                        kb.op("act", lambda e: e.activation(out=Eb[ei][:], in_=psS[si][:], func=AF.Exp, scale=scale),
                              reads=[B_psS[si]], writes=[B_E[ei]])
                        kb.op("pe", lambda e: e.matmul(psO[pi][:], lhsT=vv[:, vt, kv * 128:(kv + 1) * 128], rhs=Eb[ei][:],
                                                       start=(n == 0), stop=(n == 4)),
                              reads=[B_in, B_E[ei]], writes=[B_psO[pi]], inc=False)
                        kb.op("pe", lambda e: e.matmul(psD[pi][:], lhsT=ones_b[:], rhs=Eb[ei][:],
                                                       start=(n == 0), stop=(n == 4)),
                              reads=[B_E[ei]], writes=[B_psD[pi]], inc=True)
                    kb.op("dve", lambda e: e.tensor_tensor(
                        out=den[pi][:].rearrange("p (h q) -> p h q", h=4),
                        in0=psD[pi][:].rearrange("p (h q) -> p h q", h=4),
                        in1=esk[:, 4 * kv:4 * kv + 4].unsqueeze(2).to_broadcast([128, 4, 128]), op=ALU.add),
                        reads=[B_psD[pi], B_in], writes=[B_den[pi]])
                    kb.op("dve", lambda e: e.reciprocal(out=den[pi][:], in_=den[pi][:]), reads=[B_den[pi]], writes=[B_den[pi]])
                    kb.op("dve", lambda e: e.tensor_tensor(out=ob[pi][:], in0=psO[pi][:], in1=den[pi][:], op=ALU.mult),
                          reads=[B_psO[pi], B_den[pi]], writes=[B_ob[pi]])
                    kb.dma("sp", mixT_d[4 * kv:4 * kv + 4, :, i * 128:(i + 1) * 128].rearrange("h p t -> p h t"),
                           ob[pi][:].rearrange("p (h q) -> p h q", h=4), reads=[B_ob[pi]])
            kb.barrier()
        if stop_after == "D":
            return nc

        with ExitStack() as phE:
            uTbc = sbt(phE, "uTbc", [128, 8, OWN], BF16)
            B_u = Buf()
            tri_f = sbt(phE, "tri_f", [128, 128]); triT_f = sbt(phE, "triT_f", [128, 128])
            nmf_f = sbt(phE, "nmf_f", [128, 128]); nmb_f = sbt(phE, "nmb_f", [128, 128])
            B_cE = Buf()
            for i, t in ((1, tri_f), (2, triT_f), (4, nmf_f), (5, nmb_f)):
                kb.dma("sp", t[:], consts[i], writes=[B_cE])
            for j in range(8):
                kb.dma("sp", uTbc[:, j, :], uT_d[8 + j], writes=[B_u])
            dtr = sbt(phE, "dtr", [128, 18, 32]); dtv = sbt(phE, "dtv", [128, 18, 32]); dtA = sbt(phE, "dtA", [128, 18, 32])
            dtb = sbt(phE, "dtb", [128, 32]); alg = sbt(phE, "alg", [128, 32]); dsk = sbt(phE, "dsk", [128, 16])
            gssd = sbt(phE, "gssd", [128, 1024])
            B_dt = Buf()
            kb.dma("sp", dtr[:], dtraw_d.rearrange("t p c -> p t c"), writes=[B_dt])
            kb.dma("sp", dtb[:], bcast_row(dt_bias[0:1, :]), writes=[B_dt])
            kb.dma("sp", alg[:], bcast_row(a_log[0:1, :]), writes=[B_dt])
            kb.dma("sp", dsk[:], bcast_row(d_skip[0:1, :]), writes=[B_dt])
            kb.dma("sp", gssd[:], bcast_row(ssd_norm_g[0:1, :]), writes=[B_dt])
            kb.op("dve", lambda e: e.tensor_tensor(out=dtr[:], in0=dtr[:], in1=dtb[:].unsqueeze(1).to_broadcast([128, 18, 32]),
                                                   op=ALU.add), reads=[B_dt, B_cE], writes=[B_dt])
            kb.op("act", lambda e: e.activation(out=dtr[:], in_=dtr[:], func=AF.Exp), reads=[B_dt], writes=[B_dt])
            kb.op("act", lambda e: e.activation(out=dtv[:], in_=dtr[:], func=AF.Ln, bias=ones_f[:, 0:1]), reads=[B_dt], writes=[B_dt])
            kb.op("act", lambda e: e.activation(out=alg[:], in_=alg[:], func=AF.Exp), reads=[B_dt], writes=[B_dt])
            kb.op("dve", lambda e: e.scalar_tensor_tensor(out=dtA[:], in0=dtv[:], scalar=-1.0,
                                                          in1=alg[:].unsqueeze(1).to_broadcast([128, 18, 32]),
                                                          op0=ALU.mult, op1=ALU.mult), reads=[B_dt], writes=[B_dt])
            xs_tok = sbt(phE, "xs_tok", [128, 18, 1024], BF16)
            B_xs = [Buf() for _ in range(18)]
            eac = sbt(phE, "eac", [128, NT, 32]); B_eac = Buf()
            Pall = sbt(phE, "Pall", [128, 2, 18, 16]); B_P = Buf()
            hc = sbt(phE, "hc", [128, 2, 1024]); B_hc = Buf()
            hinit = sbt(phE, "hinit", [128, 2, 1024]); B_hi = Buf()
            B_h0 = [[Buf() for _ in range(NT)] for _ in range(2)]

            with ExitStack() as ph:
                uTx = sbt(ph, "uTx", [128, 8, OWN], BF16)
                uTc = sbt(ph, "uTc", [128, 16, CTX], BF16)
                bm_tok = sbt(ph, "bm_tok", [128, 18, 512], BF16)
                for j in range(8):
                    kb.dma("sp", uTx[:, j, :], uT_d[j], writes=[B_u])
                kb.dma("sp", uTc[:], uTc_d.rearrange("j p t -> p j t"), writes=[B_u])
                psT1 = pst(ph, "psTE1", [128, 8, 128], BF16); B_psT1 = Buf()
                psT2 = pst(ph, "psTE2", [128, 4, 128], BF16); B_psT2 = Buf()
                sm_ps = pst(ph, "sm_ps", [128, 32]); B_smps = Buf()
                S_ps = pst(ph, "S_ps", [128, 1024]); B_Sps = Buf()
                sm = sbt(ph, "sm", [128, 64]); B_sm = Buf()
                xdd = [sbt(ph, "xdd%d" % i, [128, 16, 64], BF16) for i in range(2)]
                B_xdd = [Buf(), Buf()]
                Hb = [sbt(ph, "Hb%d" % i, [128, 1024]) for i in range(2)]
                B_H = [Buf(), Buf()]
                for ti in range(18):
                    if ti < 2:
                        srcs = lambda j, ti=ti: uTc[:, j, ti * 128:(ti + 1) * 128]
                    else:
                        srcs = lambda j, ti=ti: (uTx[:, j, (ti - 2) * 128:(ti - 1) * 128] if j < 8
                                                 else uTbc[:, j - 8, (ti - 2) * 128:(ti - 1) * 128])
                    for j in range(8):
                        kb.op("pe", lambda e, j=j: e.transpose(out=psT1[:, j, :], in_=srcs(j), identity=ident_b[:]),
                              reads=[B_u], writes=[B_psT1], inc=(j == 7))
                    kb.op("act", lambda e, ti=ti: e.copy(out=xs_tok[:, ti, :], in_=psT1[:].rearrange("p a b -> p (a b)")),
                          reads=[B_psT1], writes=[B_xs[ti]])
                    for j in range(4):
                        kb.op("pe", lambda e, j=j: e.transpose(out=psT2[:, j, :], in_=srcs(8 + j), identity=ident_b[:]),
                              reads=[B_u], writes=[B_psT2], inc=(j == 3))
                    kb.op("dve", lambda e, ti=ti: e.tensor_copy(out=bm_tok[:, ti, :], in_=psT2[:].rearrange("p a b -> p (a b)")),
                          reads=[B_psT2], writes=[B_xs[ti]])

                hctr = [0]

                def chunk_S(tr, dtA_ap, dtv_ap, xs_ap, bm_of, rd, own_c=None, d=0, tot_ap=None):
                    kb.op("pe", lambda e: e.matmul(sm_ps[:, 0:16], lhsT=tr[:], rhs=dtA_ap, start=True, stop=True),
                          reads=rd, writes=[B_smps], inc=False)
                    kb.op("pe", lambda e: e.matmul(sm_ps[:, 16:32], lhsT=ones_f[:], rhs=dtA_ap, start=True, stop=True),
                          reads=rd, writes=[B_smps])
                    kb.op("act", lambda e: e.copy(out=sm[:, 0:32], in_=sm_ps[:, 0:32]), reads=[B_smps], writes=[B_sm])
                    kb.op("dve", lambda e: e.tensor_tensor(out=sm[:, 32:48], in0=sm[:, 16:32], in1=sm[:, 0:16], op=ALU.subtract),
                          reads=[B_sm], writes=[B_sm])
                    kb.op("act", lambda e: e.activation(out=sm[:, 32:48], in_=sm[:, 32:48], func=AF.Exp), reads=[B_sm], writes=[B_sm])
                    kb.op("act", lambda e: e.activation(out=sm[:, 48:64], in_=sm[:, 16:32], func=AF.Exp), reads=[B_sm], writes=[B_sm])
                    if own_c is not None:
                        kb.op("act", lambda e: e.activation(out=eac[:, own_c, d * 16:(d + 1) * 16], in_=sm[:, 0:16], func=AF.Exp),
                              reads=[B_sm], writes=[B_eac])
                    kb.op("dve", lambda e: e.tensor_tensor(out=sm[:, 32:48], in0=sm[:, 32:48], in1=dtv_ap, op=ALU.mult),
                          reads=[B_sm] + rd, writes=[B_sm])
                    xi = hctr[0] % 2
                    hctr[0] += 1
                    kb.op("dve", lambda e: e.tensor_tensor(
                        out=xdd[xi][:], in0=xs_ap.rearrange("p (h q) -> p h q", h=16),
                        in1=sm[:, 32:48].unsqueeze(2).to_broadcast([128, 16, 64]), op=ALU.mult),
                        reads=[B_sm] + rd, writes=[B_xdd[xi]])
                    for g in range(4):
                        kb.op("pe", lambda e, g=g: e.matmul(S_ps[:, g * 256:(g + 1) * 256], lhsT=bm_of(g),
                                                            rhs=xdd[xi][:, 4 * g:4 * g + 4, :], start=True, stop=True),
                              reads=rd + [B_xdd[xi]], writes=[B_Sps], inc=(g == 3))

                def chunk_state(ti, d, Hin, B_Hin, Hout, B_Hout, own_c=None):
                    tr = tri_f if d == 0 else triT_f
                    chunk_S(tr, dtA[:, ti, d * 16:(d + 1) * 16], dtv[:, ti, d * 16:(d + 1) * 16], xs_tok[:, ti, :],
                            lambda g: bm_tok[:, ti, g * 128:(g + 1) * 128], [B_dt, B_xs[ti]], own_c=own_c, d=d)
                    if Hin is None:
                        kb.op("dve", lambda e: e.tensor_copy(out=Hout, in_=S_ps[:]), reads=[B_Sps], writes=[B_Hout])
                    else:
                        kb.op("dve", lambda e: e.tensor_tensor(
                            out=Hout.rearrange("p (h q) -> p h q", h=16), in0=Hin.rearrange("p (h q) -> p h q", h=16),
                            in1=sm[:, 48:64].unsqueeze(2).to_broadcast([128, 16, 64]), op=ALU.mult),
                            reads=[B_Hin, B_sm], writes=[B_Hout])
                        kb.op("dve", lambda e: e.tensor_tensor(out=Hout, in0=Hout, in1=S_ps[:], op=ALU.add),
                              reads=[B_Sps, B_Hout], writes=[B_Hout])

                for d in range(2):
                    order = [0, 1] if d == 0 else [1, 0]
                    chunk_state(order[0], d, None, None, Hb[0][:], B_H[0])
                    chunk_state(order[1], d, Hb[0][:], B_H[0], hc[:, d, :], B_hc)
                kb.op("pool", lambda e: e.memset(Pall[:], 1.0), writes=[B_P])
                for d in range(2):
                    order = list(range(NT)) if d == 0 else list(range(NT - 1, -1, -1))
                    kb.op("pool", lambda e: e.memset(Hb[0][:], 0.0), writes=[B_H[0]])
                    cur = 0
                    for n, c in enumerate(order):
                        kb.dma("sp", h0_d[d, c], Hb[cur][:], reads=[B_H[cur]], writes=[B_h0[d][c]])
                        if n < NT - 1:
                            chunk_state(2 + c, d, Hb[cur][:], B_H[cur], Hb[1 - cur][:], B_H[1 - cur], own_c=c)
                        else:
                            chunk_state(2 + c, d, Hb[cur][:], B_H[cur], Hb[1 - cur][:], B_H[1 - cur], own_c=c)
                        pin = Pall[:, d, c, :] if d == 0 else Pall[:, d, c + 1, :]
                        pout = Pall[:, d, c + 1, :] if d == 0 else Pall[:, d, c, :]
                        kb.op("dve", lambda e: e.tensor_tensor(out=pout, in0=pin, in1=sm[:, 48:64], op=ALU.mult),
                              reads=[B_sm, B_P], writes=[B_P])
                        cur = 1 - cur

                dt2r = sbt(ph, "dt2r", [128, NT, 16]); dt2 = sbt(ph, "dt2", [128, NT, 16]); dtA2 = sbt(ph, "dtA2", [128, NT, 16])
                dtb2 = sbt(ph, "dtb2", [128, 16]); alg2 = sbt(ph, "alg2", [128, 16]); tris = sbt(ph, "tris", [128, 128])
                B_d2 = Buf()
                kb.dma("sp", dt2r[:], dtraw2_d.rearrange("t p c -> p t c"), writes=[B_d2])
                kb.dma("sp", dtb2[:], bcast_row(dtb_sel[0:1, :]), writes=[B_d2])
                kb.dma("sp", alg2[:], bcast_row(alog_sel[0:1, :]), writes=[B_d2])
                kb.dma("sp", tris[:], tri_sel[:, :], writes=[B_d2])
                kb.op("dve", lambda e: e.tensor_tensor(out=dt2r[:], in0=dt2r[:], in1=dtb2[:].unsqueeze(1).to_broadcast([128, NT, 16]),
                                                       op=ALU.add), reads=[B_d2], writes=[B_d2])
                kb.op("act", lambda e: e.activation(out=dt2r[:], in_=dt2r[:], func=AF.Exp), reads=[B_d2], writes=[B_d2])
                kb.op("act", lambda e: e.activation(out=dt2[:], in_=dt2r[:], func=AF.Ln, bias=ones_f[:, 0:1]), reads=[B_d2], writes=[B_d2])
                kb.op("act", lambda e: e.activation(out=alg2[:], in_=alg2[:], func=AF.Exp), reads=[B_d2], writes=[B_d2])
                kb.op("dve", lambda e: e.scalar_tensor_tensor(out=dtA2[:], in0=dt2[:], scalar=-1.0,
                                                              in1=alg2[:].unsqueeze(1).to_broadcast([128, NT, 16]),
                                                              op0=ALU.mult, op1=ALU.mult), reads=[B_d2], writes=[B_d2])
                tot2_ps = pst(ph, "tot2_ps", [128, 256]); B_t2ps = Buf()
                kb.op("pe", lambda e: e.matmul(tot2_ps[:], lhsT=ones_f[:], rhs=dtA2[:], start=True, stop=True),
                      reads=[B_d2], writes=[B_t2ps])
                tot2 = sbt(ph, "tot2", [128, NT, 16]); pre = sbt(ph, "pre", [128, NT + 1, 16]); suf = sbt(ph, "suf", [128, NT + 1, 16])
                wch = sbt(ph, "wch", [128, NT + 1, 16]); B_w = Buf()
                kb.op("act", lambda e: e.copy(out=tot2[:].rearrange("p a b -> p (a b)"), in_=tot2_ps[:]), reads=[B_t2ps], writes=[B_w])
                kb.op("pool", lambda e: e.memset(pre[:], 0.0), writes=[B_w])
                kb.op("pool", lambda e: e.memset(suf[:], 0.0), writes=[B_w])
                for c in range(NT):
                    kb.op("dve", lambda e, c=c: e.tensor_tensor(out=pre[:, c + 1, :], in0=pre[:, c, :], in1=tot2[:, c, :], op=ALU.add),
                          reads=[B_w], writes=[B_w])
                for c in range(NT - 1, 0, -1):
                    kb.op("dve", lambda e, c=c: e.tensor_tensor(out=suf[:, c - 1, :], in0=suf[:, c, :], in1=tot2[:, c, :], op=ALU.add),
                          reads=[B_w], writes=[B_w])
                kb.op("dve", lambda e: e.tensor_scalar(out=wch[:], in0=suf[:], scalar1=flg[:, 0:1], scalar2=None, op0=ALU.mult),
                      reads=[B_w], writes=[B_w])
                kb.op("dve", lambda e: e.scalar_tensor_tensor(out=wch[:], in0=pre[:], scalar=flg[:, 1:2], in1=wch[:],
                                                              op0=ALU.mult, op1=ALU.add), reads=[B_w], writes=[B_w])
                kb.op("dve", lambda e: e.tensor_copy(out=wch[:, NT, :], in_=pre[:, NT, :]), reads=[B_w], writes=[B_w])
                kb.op("act", lambda e: e.activation(out=wch[:], in_=wch[:], func=AF.Exp), reads=[B_w], writes=[B_w])
                u2 = [sbt(ph, "u2_%d" % i, [128, 12, 128], BF16) for i in range(2)]
                B_u2 = [Buf(), Buf()]
                xs2 = [sbt(ph, "xs2_%d" % i, [128, 1024], BF16) for i in range(2)]
                bm2 = [sbt(ph, "bm2_%d" % i, [128, 512], BF16) for i in range(2)]
                B_x2 = [Buf(), Buf()]
                Hacc = Hb[0]; B_Hacc = B_H[0]
                stmp = Hb[1]; B_stmp = B_H[1]
                kb.op("pool", lambda e: e.memset(Hacc[:], 0.0), writes=[B_Hacc])
                u2v = uT2_d.rearrange("j p t -> p j t")
                kb.dma("sp", u2[0][:], u2v[:, :, 0:128], writes=[B_u2[0]])
                for c in range(NT):
                    bi = c % 2
                    if c + 1 < NT:
                        kb.dma("sp", u2[1 - bi][:], u2v[:, :, (c + 1) * 128:(c + 2) * 128], writes=[B_u2[1 - bi]])
                    for j in range(8):
                        kb.op("pe", lambda e, j=j: e.transpose(out=psT1[:, j, :], in_=u2[bi][:, j, :], identity=ident_b[:]),
                              reads=[B_u2[bi]], writes=[B_psT1], inc=(j == 7))
                    kb.op("act", lambda e: e.copy(out=xs2[bi][:], in_=psT1[:].rearrange("p a b -> p (a b)")),
                          reads=[B_psT1], writes=[B_x2[bi]])
                    for j in range(4):
                        kb.op("pe", lambda e, j=j: e.transpose(out=psT2[:, j, :], in_=u2[bi][:, 8 + j, :], identity=ident_b[:]),
                              reads=[B_u2[bi]], writes=[B_psT2], inc=(j == 3))
                    kb.op("dve", lambda e: e.tensor_copy(out=bm2[bi][:], in_=psT2[:].rearrange("p a b -> p (a b)")),
                          reads=[B_psT2], writes=[B_x2[bi]])
                    chunk_S(tris, dtA2[:, c, :], dt2[:, c, :], xs2[bi][:], lambda g: bm2[bi][:, g * 128:(g + 1) * 128],
                            [B_d2, B_x2[bi]])
                    kb.op("dve", lambda e: e.tensor_tensor(
                        out=stmp[:].rearrange("p (h q) -> p h q", h=16), in0=S_ps[:].rearrange("p (h q) -> p h q", h=16),
                        in1=wch[:, c, :].unsqueeze(2).to_broadcast([128, 16, 64]), op=ALU.mult),
                        reads=[B_Sps, B_w], writes=[B_stmp])
                    kb.op("pool", lambda e: e.tensor_tensor(out=Hacc[:], in0=Hacc[:], in1=stmp[:], op=ALU.add),
                          reads=[B_stmp, B_Hacc], writes=[B_Hacc])
                hsel = stmp; B_hsel = B_stmp
                kb.op("dve", lambda e: e.tensor_scalar(out=hsel[:], in0=hc[:, 0, :], scalar1=flg[:, 0:1], scalar2=None, op0=ALU.mult),
                      reads=[B_hc], writes=[B_hsel])
                kb.op("dve", lambda e: e.scalar_tensor_tensor(out=hsel[:], in0=hc[:, 1, :], scalar=flg[:, 1:2], in1=hsel[:],
                                                              op0=ALU.mult, op1=ALU.add), reads=[B_hc, B_hsel], writes=[B_hsel])
                kb.op("dve", lambda e: e.tensor_tensor(
                    out=hsel[:].rearrange("p (h q) -> p h q", h=16), in0=hsel[:].rearrange("p (h q) -> p h q", h=16),
                    in1=wch[:, NT, :].unsqueeze(2).to_broadcast([128, 16, 64]), op=ALU.mult),
                    reads=[B_hsel, B_w], writes=[B_hsel])
                kb.op("dve", lambda e: e.tensor_tensor(out=hsel[:], in0=hsel[:], in1=Hacc[:], op=ALU.add),
                      reads=[B_hsel, B_Hacc], writes=[B_hsel])
                for d in range(2):
                    fa, fb = (flg[:, 0:1], flg[:, 1:2]) if d == 0 else (flg[:, 1:2], flg[:, 0:1])
                    kb.op("dve", lambda e, d=d, fa=fa: e.tensor_scalar(out=hinit[:, d, :], in0=hsel[:], scalar1=fa, scalar2=None,
                                                                       op0=ALU.mult), reads=[B_hsel], writes=[B_hi])
                    kb.op("dve", lambda e, d=d, fb=fb: e.scalar_tensor_tensor(out=hinit[:, d, :], in0=hc[:, d, :], scalar=fb,
                                                                              in1=hinit[:, d, :], op0=ALU.mult, op1=ALU.add),
                          reads=[B_hc, B_hi], writes=[B_hi])
                kb.barrier()
            if stop_after == "E1":
                dbg_h = dscr("dbg_h", [128, 4, 1024])
                kb.dma("sp", dbg_h[:, 0:2, :], hc[:], reads=[B_hc])
                kb.dma("sp", dbg_h[:, 2:4, :], hinit[:], reads=[B_hi])
                kb.barrier()
                return nc

            with ExitStack() as ph:
                seg_ps = [pst(ph, "seg_ps%d" % i, [128, 512]) for i in range(2)]; B_seg = [Buf(), Buf()]
                cb_ps = pst(ph, "cb_ps", [128, 512]); B_cbps = Buf()
                yd_ps = pst(ph, "yd_ps", [128, 1024]); B_ydps = Buf()
                yo_ps = pst(ph, "yo_ps", [128, 1024]); B_yops = Buf()
                psT3 = pst(ph, "psTE3", [128, 8, 128], BF16); B_psT3 = Buf()
                negones = sbt(ph, "negones", [128, 128]); nm4 = sbt(ph, "nm4", [128, 2, 4, 128], BF16); B_c2 = Buf()
                kb.op("pool", lambda e: e.memset(negones[:], -1.0), writes=[B_c2])
                for d, nmx in enumerate((nmf_f, nmb_f)):
                    kb.op("dve", lambda e, d=d, nmx=nmx: e.tensor_copy(out=nm4[:, d, :, :],
                                                                       in_=nmx[:].unsqueeze(1).to_broadcast([128, 4, 128])),
                          writes=[B_c2])
                Rf = sbt(ph, "Rf", [128, 32, 128]); B_R = Buf()
                Mb = sbt(ph, "Mb", [128, 32, 128], BF16); B_M = Buf()
                LT = sbt(ph, "LT", [128, 32, 128], BF16); B_LT = Buf()
                cbT = sbt(ph, "cbT", [128, 4, 128], BF16); B_cbT = Buf()
                xd = sbt(ph, "xd", [128, 32, 64], BF16); B_xd2 = Buf()
                h0t = [sbt(ph, "h0t%d" % i, [128, 1024]) for i in range(2)]; B_h0t = [Buf(), Buf()]
                hp = [sbt(ph, "hp%d" % i, [128, 16, 64], BF16) for i in range(2)]; B_hp = [Buf(), Buf()]
                htmp = sbt(ph, "htmp", [128, 1024]); B_htmp = Buf()
                ya = sbt(ph, "ya", [128, 1024]); B_ya = Buf()
                yb = sbt(ph, "yb", [128, 1024]); B_yb = Buf()
                yy = sbt(ph, "yy", [128, 1024]); B_yy = Buf()
                zt = [sbt(ph, "zt%d" % i, [128, 1024]) for i in range(2)]; B_zt = [Buf(), Buf()]
                junk2 = sbt(ph, "junk2", [128, 1024], BF16); B_j2 = Buf()
                ssq2 = sbt(ph, "ssq2", [128, 4]); B_ss2 = Buf()
                ynb = sbt(ph, "ynb", [128, 1024], BF16); B_ynb = Buf()
                mo2 = [sbt(ph, "mo2_%d" % i, [128, 8, 128], BF16) for i in range(2)]; B_mo2 = [Buf(), Buf()]
                sctr2 = 0
                for c in range(NT):
                    ti = 2 + c
                    tk = slice(c * 128, (c + 1) * 128)
                    kb.dma("sp", zt[c % 2][:], zs_d[c], writes=[B_zt[c % 2]])
                    for d in range(2):
                        kb.dma("sp", h0t[d][:], h0_d[d, c], reads=[B_h0[d][c]], writes=[B_h0t[d]])
                    for d, tr in enumerate((tri_f, triT_f)):
                        kb.op("pool", lambda e, d=d, tr=tr: e.tensor_tensor(
                            out=Rf[:, d * 16:(d + 1) * 16, :], in0=tr[:].unsqueeze(1).to_broadcast([128, 16, 128]),
                            in1=dtA[:, ti, d * 16:(d + 1) * 16].unsqueeze(2).to_broadcast([128, 16, 128]), op=ALU.mult),
                            reads=[B_dt], writes=[B_R])
                    if cut <= 1:
                        continue
                    for g in range(4):
                        kb.op("pe", lambda e, g=g: e.matmul(cb_ps[:, g * 128:(g + 1) * 128], lhsT=uTbc[:, g, tk], rhs=uTbc[:, 4 + g, tk],
                                                            start=True, stop=True), reads=[B_u], writes=[B_cbps], inc=(g == 3))
                    kb.op("act", lambda e: e.copy(out=cbT[:].rearrange("p a b -> p (a b)"), in_=cb_ps[:]), reads=[B_cbps], writes=[B_cbT])
                    for q4 in range(8):
                        d = q4 // 4
                        si = sctr2 % 2
                        sctr2 += 1
                        kb.op("pe", lambda e: e.matmul(seg_ps[si][:], lhsT=ones_f[:], rhs=Rf[:, 4 * q4:4 * q4 + 4, :],
                                                       start=True, stop=False), reads=[B_R], writes=[B_seg[si]], inc=False)
                        for r in range(4):
                            kb.op("pe", lambda e, r=r: e.matmul(seg_ps[si][:, r * 128:(r + 1) * 128], lhsT=Rf[:, 4 * q4 + r, :],
                                                                rhs=negones[:], start=False, stop=False),
                                  reads=[B_R, B_c2], writes=[B_seg[si]], inc=False)
                        kb.op("pe", lambda e: e.matmul(seg_ps[si][:], lhsT=ident_b[:], rhs=nm4[:, d, :, :],
                                                       start=False, stop=True), reads=[B_c2], writes=[B_seg[si]])
                        kb.op("act", lambda e: e.activation(out=Mb[:, 4 * q4:4 * q4 + 4, :], in_=seg_ps[si][:], func=AF.Exp),
                              reads=[B_seg[si]], writes=[B_M])
                    if cut <= 2:
                        continue
                    for d in range(2):
                        kb.op("dve", lambda e, d=d: e.tensor_tensor(
                            out=LT[:, d * 16:(d + 1) * 16, :].rearrange("p (g r) l -> p g r l", g=4),
                            in0=Mb[:, d * 16:(d + 1) * 16, :].rearrange("p (g r) l -> p g r l", g=4),
                            in1=cbT[:].unsqueeze(2).to_broadcast([128, 4, 4, 128]), op=ALU.mult),
                            reads=[B_M, B_cbT], writes=[B_LT])
                        kb.op("pool", lambda e, d=d: e.tensor_tensor(
                            out=xd[:, d * 16:(d + 1) * 16, :], in0=xs_tok[:, ti, :].rearrange("p (h q) -> p h q", h=16),
                            in1=dtv[:, ti, d * 16:(d + 1) * 16].unsqueeze(2).to_broadcast([128, 16, 64]), op=ALU.mult),
                            reads=[B_xs[ti], B_dt], writes=[B_xd2])
                    if cut <= 3:
                        continue
                    for h in range(16):
                        for d in range(2):
                            kb.op("pe", lambda e, h=h, d=d: e.matmul(yd_ps[:, h * 64:(h + 1) * 64], lhsT=LT[:, d * 16 + h, :],
                                                                     rhs=xd[:, d * 16 + h, :], start=(d == 0), stop=(d == 1)),
                                  reads=[B_LT, B_xd2], writes=[B_ydps], inc=(h == 15 and d == 1))
                    if cut <= 4:
                        continue
                    for d in range(2):
                        pin = Pall[:, d, c, :] if d == 0 else Pall[:, d, c + 1, :]
                        kb.op("dve", lambda e, d=d, pin=pin: e.tensor_tensor(
                            out=htmp[:].rearrange("p (h q) -> p h q", h=16), in0=hinit[:, d, :].rearrange("p (h q) -> p h q", h=16),
                            in1=pin.unsqueeze(2).to_broadcast([128, 16, 64]), op=ALU.mult),
                            reads=[B_hi, B_P], writes=[B_htmp])
                        kb.op("dve", lambda e, d=d: e.tensor_tensor(out=hp[d][:].rearrange("p h q -> p (h q)"), in0=htmp[:],
                                                                    in1=h0t[d][:], op=ALU.add),
                              reads=[B_htmp, B_h0t[d]], writes=[B_hp[d]])
                        for g in range(4):
                            kb.op("pe", lambda e, g=g, d=d: e.matmul(yo_ps[:, g * 256:(g + 1) * 256], lhsT=uTbc[:, 4 + g, tk],
                                                                     rhs=hp[d][:, 4 * g:4 * g + 4, :], start=True, stop=True),
                                  reads=[B_u, B_hp[d]], writes=[B_yops], inc=(g == 3))
                        dst, Bd = (ya, B_ya) if d == 0 else (yb, B_yb)
                        kb.op("dve", lambda e, d=d, dst=dst: e.tensor_tensor(
                            out=dst[:].rearrange("p (h q) -> p h q", h=16), in0=yo_ps[:].rearrange("p (h q) -> p h q", h=16),
                            in1=eac[:, c, d * 16:(d + 1) * 16].unsqueeze(2).to_broadcast([128, 16, 64]), op=ALU.mult),
                            reads=[B_yops, B_eac], writes=[Bd])
                    if cut <= 5:
                        continue
                    kb.op("dve", lambda e: e.tensor_tensor(out=yy[:], in0=yd_ps[:], in1=ya[:], op=ALU.add),
                          reads=[B_ydps, B_ya], writes=[B_yy])
                    kb.op("pool", lambda e: e.tensor_tensor(out=yy[:], in0=yy[:], in1=yb[:], op=ALU.add),
                          reads=[B_yy, B_yb], writes=[B_yy])
                    kb.op("pool", lambda e: e.tensor_tensor(
                        out=ya[:].rearrange("p (h q) -> p h q", h=16), in0=xs_tok[:, ti, :].rearrange("p (h q) -> p h q", h=16),
                        in1=dsk[:].unsqueeze(2).to_broadcast([128, 16, 64]), op=ALU.mult),
                        reads=[B_xs[ti], B_dt], writes=[B_ya])
                    kb.op("pool", lambda e: e.tensor_tensor(out=yy[:], in0=yy[:], in1=ya[:], op=ALU.add),
                          reads=[B_yy, B_ya], writes=[B_yy])
                    if cut <= 6:
                        continue
                    kb.op("dve", lambda e: e.tensor_tensor(out=yy[:], in0=yy[:], in1=zt[c % 2][:], op=ALU.mult),
                          reads=[B_yy, B_zt[c % 2]], writes=[B_yy])
                    rms_mod_tile(yy[:], B_yy, gssd[:], None, B_dt, junk2, B_j2, ssq2, B_ss2, yb, B_yb, None, None, width=1024)
                    if cut <= 7:
                        continue
                    kb.op("act", lambda e: e.copy(out=ynb[:], in_=yb[:]), reads=[B_yb], writes=[B_ynb])
                    for j in range(8):
                        kb.op("pe", lambda e, j=j: e.transpose(out=psT3[:, j, :], in_=ynb[:, j * 128:(j + 1) * 128], identity=ident_b[:]),
                              reads=[B_ynb], writes=[B_psT3], inc=(j == 7))
                    kb.op("act", lambda e: e.copy(out=mo2[c % 2][:], in_=psT3[:]), reads=[B_psT3], writes=[B_mo2[c % 2]])
                    kb.dma("sp", mixT_d[8:16, :, tk].rearrange("h p t -> p h t"), mo2[c % 2][:], reads=[B_mo2[c % 2]])
                kb.barrier()
        if stop_after == "E":
            return nc

        with ExitStack() as ph:
            Wo = sbt(ph, "Wo", [128, 16, D], BF16); B_Wo = Buf()
            wov = w_out.rearrange("(k p) n -> p k n", p=128)
            for cbk in range(4):
                kb.dma("pool", Wo[:, :, cbk * 512:(cbk + 1) * 512], wov[:, :, cbk * 512:(cbk + 1) * 512], writes=[B_Wo])
            B_m2 = Buf()
            g1b = load_bc(ph, "sp", "g1b", mod_d[2:3, :], D, B_m2)
            G2 = load_bc(ph, "sp", "G2", mod_d[4:5, :], D, B_m2)
            SH2 = load_bc(ph, "sp", "SH2", mod_d[3:4, :], D, B_m2)
            g2n = load_bc(ph, "sp", "g2n", norm2_g[0:1, :], D, B_m2)
            kb.op("dve", lambda e: e.tensor_tensor(out=G2[:], in0=G2[:], in1=g2n[:], op=ALU.mult), reads=[B_m2], writes=[B_m2])
            rw = sbt(ph, "rw", [128, 16, NEXP]); rbias = sbt(ph, "rbias", [128, NEXP])
            kb.dma("sp", rw[:], router_w.rearrange("(k p) e -> p k e", p=128), writes=[B_m2])
            kb.dma("sp", rbias[:], bcast_row(router_bias[0:1, :]), writes=[B_m2])
            mix = [sbt(ph, "mix%d" % i, [128, 16, 128], BF16) for i in range(2)]; B_mix = [Buf(), Buf()]
            xtF = [sbt(ph, "xtF%d" % i, [128, D]) for i in range(2)]; B_xtF = [Buf(), Buf()]
            x1t = sbt(ph, "x1t", [128, D]); B_x1t = Buf()
            tmpF = sbt(ph, "tmpF", [128, D]); B_tmpF = Buf()
            h2f = g2n; B_h2f = Buf()
            h2b = sbt(ph, "h2b", [128, D], BF16); B_h2b = Buf()
            junkF = sbt(ph, "junkF", [128, D], BF16); B_junkF = Buf()
            ssqF = sbt(ph, "ssqF", [128, 4]); B_ssF = Buf()
            h2To = [sbt(ph, "h2To%d" % i, [128, 16, 128], BF16) for i in range(2)]; B_h2To = [Buf(), Buf()]
            h2Tf = sbt(ph, "h2Tf", [128, 16, 128]); B_h2Tf = Buf()
            psF = [pst(ph, "psF%d" % i, [128, 512]) for i in range(2)]; B_psF = [Buf(), Buf()]
            psTF = [pst(ph, "psTF%d" % i, [128, 8, 128], BF16) for i in range(2)]; B_psTF = [Buf(), Buf()]
            psR = [pst(ph, "psR%d" % i, [128, 4, 128]) for i in range(2)]; B_psR = [Buf(), Buf()]
            psL = pst(ph, "psL", [128, NEXP]); B_psL = Buf()
            sc_ = sbt(ph, "sc_", [128, NEXP]); sel = sbt(ph, "sel", [128, NEXP]); sel2 = sbt(ph, "sel2", [128, NEXP])
            m1 = sbt(ph, "m1", [128, 8]); m2 = sbt(ph, "m2", [128, 8]); gs = sbt(ph, "gs", [128, 8])
            cmpt = sbt(ph, "cmpt", [128, 8, 8]); rank = sbt(ph, "rank", [128, 8]); km = sbt(ph, "km", [128, 8])
            mx8 = sbt(ph, "mx8", [128, 8]); thr = sbt(ph, "thr", [128, 2])
            gate = [sbt(ph, "gate%d" % i, [128, NEXP + 1]) for i in range(2)]; B_gate = [Buf(), Buf()]
            B_r = Buf()
            for i in range(2):
                kb.op("pool", lambda e, i=i: e.memset(gate[i][:], 1.0), writes=[B_gate[i]])
            mixv = mixT_d.rearrange("k p t -> p k t")
            h2Tv = h2T_d.rearrange("k p t -> p k t")
            kb.dma("sp", mix[0][:], mixv[:, :, 0:128], writes=[B_mix[0]])
            kb.dma("sp", xtF[0][:], x_own[0:128, :], writes=[B_xtF[0]])
            pctrF = 0
            for t in range(NT):
                bi = t % 2
                tk = slice(t * 128, (t + 1) * 128)
                if t + 1 < NT:
                    kb.dma("sp", mix[1 - bi][:], mixv[:, :, (t + 1) * 128:(t + 2) * 128], writes=[B_mix[1 - bi]])
                    kb.dma("sp", xtF[1 - bi][:], x_own[(t + 1) * 128:(t + 2) * 128, :], writes=[B_xtF[1 - bi]])
                for cbk in range(4):
                    pi = pctrF % 2
                    pctrF += 1
                    cs_ = slice(cbk * 512, (cbk + 1) * 512)
                    for k in range(16):
                        kb.op("pe", lambda e, k=k: e.matmul(psF[pi][:], lhsT=mix[bi][:, k, :], rhs=Wo[:, k, cs_],
                                                            start=(k == 0), stop=(k == 15)),
                              reads=[B_mix[bi], B_Wo], writes=[B_psF[pi]], inc=(k == 15))
                    kb.op("dve", lambda e: e.tensor_tensor(out=tmpF[:, cs_], in0=psF[pi][:], in1=g1b[:, cs_], op=ALU.mult),
                          reads=[B_psF[pi], B_m2], writes=[B_tmpF])
                    kb.op("pool", lambda e: e.tensor_tensor(out=x1t[:, cs_], in0=tmpF[:, cs_], in1=xtF[bi][:, cs_], op=ALU.add),
                          reads=[B_tmpF, B_xtF[bi]], writes=[B_x1t])
                kb.dma("sp", x1_d[tk, :], x1t[:], reads=[B_x1t])
                rms_mod_tile(x1t[:], B_x1t, G2[:], SH2[:], B_m2, junkF, B_junkF, ssqF, B_ssF, tmpF, B_tmpF, h2f[:], B_h2f)
                kb.op("act", lambda e: e.copy(out=h2b[:], in_=h2f[:]), reads=[B_h2f], writes=[B_h2b])
                kb.dma("sp", h2tok_d[tk, :], h2b[:], reads=[B_h2b])
                for half in range(2):
                    for kk in range(8):
                        k = half * 8 + kk
                        kb.op("pe", lambda e, k=k, kk=kk: e.transpose(out=psTF[half][:, kk, :], in_=h2b[:, k * 128:(k + 1) * 128],
                                                                     identity=ident_b[:]),
                              reads=[B_h2b], writes=[B_psTF[half]], inc=(kk == 7))
                    if half == 0:
                        kb.op("act", lambda e: e.copy(out=h2To[bi][:, 0:8, :], in_=psTF[0][:]), reads=[B_psTF[0]], writes=[B_h2To[bi]])
                    else:
                        kb.op("dve", lambda e: e.tensor_copy(out=h2To[bi][:, 8:16, :], in_=psTF[1][:]), reads=[B_psTF[1]],
                              writes=[B_h2To[bi]])
                kb.dma("sp", h2Tv[:, :, tk], h2To[bi][:], reads=[B_h2To[bi]])
                for q in range(4):
                    ri = q % 2
                    for kk in range(4):
                        k = q * 4 + kk
                        kb.op("pe", lambda e, k=k, kk=kk: e.matmul(psR[ri][:, kk, :], lhsT=h2f[:, k * 128:(k + 1) * 128], rhs=ident_f[:],
                                                                  start=True, stop=True),
                              reads=[B_h2f], writes=[B_psR[ri]], inc=(kk == 3))
                    kb.op("act", lambda e, q=q: e.copy(out=h2Tf[:, q * 4:(q + 1) * 4, :], in_=psR[ri][:]), reads=[B_psR[ri]],
                          writes=[B_h2Tf])
                for k in range(16):
                    kb.op("pe", lambda e, k=k: e.matmul(psL[:], lhsT=h2Tf[:, k, :], rhs=rw[:, k, :], start=(k == 0), stop=(k == 15)),
                          reads=[B_h2Tf, B_m2], writes=[B_psL], inc=(k == 15))
                R_ = [B_r]
                kb.op("act", lambda e: e.activation(out=sc_[:], in_=psL[:], func=AF.Sigmoid), reads=[B_psL], writes=R_)
                kb.op("dve", lambda e: e.tensor_tensor(out=sel[:], in0=sc_[:], in1=rbias[:], op=ALU.add), reads=R_ + [B_m2], writes=R_)
                s3 = lambda a: a[:].rearrange("p (g e) -> p g e", g=8)
                kb.op("dve", lambda e: e.tensor_reduce(out=m1[:], in_=s3(sel), axis=AX.X, op=ALU.max), reads=R_, writes=R_)
                kb.op("dve", lambda e: e.tensor_tensor(out=s3(sel2), in0=s3(sel), in1=m1[:].unsqueeze(2).to_broadcast([128, 8, 8]),
                                                       op=ALU.is_equal), reads=R_, writes=R_)
                kb.op("dve", lambda e: e.scalar_tensor_tensor(out=sel2[:], in0=sel2[:], scalar=-1e9, in1=sel[:], op0=ALU.mult,
                                                              op1=ALU.add), reads=R_, writes=R_)
                kb.op("dve", lambda e: e.tensor_reduce(out=m2[:], in_=s3(sel2), axis=AX.X, op=ALU.max), reads=R_, writes=R_)
                kb.op("dve", lambda e: e.tensor_tensor(out=gs[:], in0=m1[:], in1=m2[:], op=ALU.add), reads=R_, writes=R_)
                kb.op("dve", lambda e: e.tensor_tensor(out=cmpt[:], in0=gs[:].unsqueeze(1).to_broadcast([128, 8, 8]),
                                                       in1=gs[:].unsqueeze(2).to_broadcast([128, 8, 8]), op=ALU.is_gt),
                      reads=R_, writes=R_)
                kb.op("dve", lambda e: e.tensor_reduce(out=rank[:], in_=cmpt[:], axis=AX.X, op=ALU.add), reads=R_, writes=R_)
                kb.op("dve", lambda e: e.tensor_scalar(out=km[:], in0=rank[:], scalar1=3.5, scalar2=1e9, op0=ALU.is_lt, op1=ALU.mult),
                      reads=R_, writes=R_)
                kb.op("dve", lambda e: e.tensor_scalar(out=km[:], in0=km[:], scalar1=-1e9, scalar2=None, op0=ALU.add),
                      reads=R_, writes=R_)
                kb.op("dve", lambda e: e.tensor_tensor(out=s3(sel2), in0=s3(sel), in1=km[:].unsqueeze(2).to_broadcast([128, 8, 8]),
                                                       op=ALU.add), reads=R_, writes=R_)
                kb.op("dve", lambda e: e.max(out=mx8[:], in_=sel2[:]), reads=R_, writes=R_)
                kb.op("dve", lambda e: e.tensor_reduce(out=thr[:, 0:1], in_=mx8[:], axis=AX.X, op=ALU.min), reads=R_, writes=R_)
                kb.op("dve", lambda e: e.tensor_scalar(out=sel2[:], in0=sel2[:], scalar1=thr[:, 0:1], scalar2=None, op0=ALU.is_ge),
                      reads=R_, writes=R_)
                kb.op("dve", lambda e: e.tensor_tensor(out=sel2[:], in0=sel2[:], in1=sc_[:], op=ALU.mult), reads=R_, writes=R_)
                kb.op("dve", lambda e: e.tensor_reduce(out=thr[:, 1:2], in_=sel2[:], axis=AX.X, op=ALU.add), reads=R_, writes=R_)
                kb.op("dve", lambda e: e.reciprocal(out=thr[:, 1:2], in_=thr[:, 1:2]), reads=R_, writes=R_)
                kb.op("dve", lambda e: e.tensor_scalar(out=gate[bi][:, 0:NEXP], in0=sel2[:], scalar1=thr[:, 1:2], scalar2=2.5,
                                                       op0=ALU.mult, op1=ALU.mult), reads=R_, writes=[B_gate[bi]])
                kb.dma("sp", gate_d[t], gate[bi][:], reads=[B_gate[bi]])
            kb.barrier()
        if stop_after == "F":
            return nc

        PEe, DVEe, ACTe, POOLe, SPe = (mybir.EngineType.PE, mybir.EngineType.DVE, mybir.EngineType.Activation,
                                       mybir.EngineType.Pool, mybir.EngineType.SP)
        with ExitStack() as ph:
            Gt = sbt(ph, "Gt", [128, NT, NEXP + 1]); B_G = Buf()
            kb.dma("sp", Gt[:], gate_d.rearrange("t p e -> p t e"), writes=[B_G])
            eb = sbt(ph, "eb", [128, 2 * NEXP]); lsf = sbt(ph, "lsf", [128, 128]); lsb = sbt(ph, "lsb", [128, 128], BF16)
            kb.dma("sp", eb[:], ebase[:, :], writes=[B_G])
            kb.dma("sp", lsf[:], lstrict[:, :], writes=[B_G])
            kb.op("dve", lambda e: e.tensor_copy(out=lsb[:], in_=lsf[:]), reads=[B_G], writes=[B_G])
            Mb_ = sbt(ph, "Mb_", [128, NT, NEXP], BF16)
            kb.op("dve", lambda e: e.tensor_scalar(out=Mb_[:], in0=Gt[:, :, 0:NEXP], scalar1=0.0, scalar2=None, op0=ALU.is_gt),
                  reads=[B_G], writes=[B_G])
            zrow = sbt(ph, "zrow", [128, D], BF16); B_z = Buf()
            kb.op("pool", lambda e: e.memset(zrow[:], 0.0), writes=[B_z])
            kb.dma("sp", h2tok_d[OWN:OWN + 128, :], zrow[:], reads=[B_z])
            fill = sbt(ph, "fill", [128, NSL // 128], I32); B_fill = Buf(); B_tab = Buf()
            kb.op("pool", lambda e: e.memset(fill[:], OWN), writes=[B_fill])
            kb.dma("sp", idxtab_d.rearrange("(p f) o -> p (f o)", p=128), fill[:], reads=[B_fill], writes=[B_tab])
            tokid = sbt(ph, "tokid", [128, NT], I32); B_tok = Buf()
            kb.op("pool", lambda e: e.iota(tokid[:], pattern=[[128, NT]], base=0, channel_multiplier=1), writes=[B_tok])
            cnt_ps = pst(ph, "cnt_ps", [128, NEXP]); B_cps = Buf()
            pos_ps = [pst(ph, "pos_ps%d" % i, [128, NEXP]) for i in range(2)]; B_pps = [Buf(), Buf()]
            for t in range(NT):
                kb.op("pe", lambda e, t=t: e.matmul(cnt_ps[:], lhsT=ones_b[:], rhs=Mb_[:, t, :], start=(t == 0), stop=(t == NT - 1)),
                      reads=[B_G], writes=[B_cps], inc=(t == NT - 1))
            nbf = sbt(ph, "nbf", [128, NEXP]); nbm = sbt(ph, "nbm", [128, NEXP]); nbi = sbt(ph, "nbi", [128, NEXP], I32); B_nb = Buf()
            kb.op("dve", lambda e: e.tensor_scalar(out=nbf[:], in0=cnt_ps[:], scalar1=127.0, scalar2=None, op0=ALU.add),
                  reads=[B_cps], writes=[B_nb])
            nbr = sbt(ph, "nbr", [128, NEXP], I32)
            kb.op("dve", lambda e: e.tensor_copy(out=nbr[:], in_=nbf[:]), reads=[B_nb], writes=[B_nb])
            kb.op("dve", lambda e: e.tensor_single_scalar(out=nbi[:], in_=nbr[:], scalar=7, op=ALU.arith_shift_right),
                  reads=[B_nb], writes=[B_nb])
            kb.op("dve", lambda e: e.tensor_single_scalar(out=nbr[:], in_=nbi[:], scalar=7, op=ALU.logical_shift_left),
                  reads=[B_nb], writes=[B_nb])
            kb.op("dve", lambda e: e.tensor_copy(out=nbf[:], in_=nbr[:]), reads=[B_nb], writes=[B_nb])
            cs0 = sbt(ph, "cs0", [128, NEXP]); cs1 = sbt(ph, "cs1", [128, NEXP]); cbs = sbt(ph, "cbs", [128, NEXP])
            cbi = sbt(ph, "cbi", [128, NEXP], I32)
            kb.op("dve", lambda e: e.tensor_copy(out=cs0[:], in_=nbf[:]), reads=[B_nb], writes=[B_nb])
            cur_, oth_ = cs0, cs1
            for s_ in (1, 2, 4, 8, 16, 32):
                kb.op("dve", lambda e, cur_=cur_, oth_=oth_, s_=s_: e.tensor_copy(out=oth_[:, 0:s_], in_=cur_[:, 0:s_]),
                      reads=[B_nb], writes=[B_nb])
                kb.op("dve", lambda e, cur_=cur_, oth_=oth_, s_=s_: e.tensor_tensor(out=oth_[:, s_:NEXP], in0=cur_[:, s_:NEXP],
                                                                                   in1=cur_[:, 0:NEXP - s_], op=ALU.add),
                      reads=[B_nb], writes=[B_nb])
                cur_, oth_ = oth_, cur_
            kb.op("dve", lambda e, cur_=cur_: e.tensor_tensor(out=cbs[:], in0=cur_[:], in1=nbf[:], op=ALU.subtract),
                  reads=[B_nb], writes=[B_nb])
            kb.op("dve", lambda e: e.tensor_copy(out=cbi[:], in_=cbs[:]), reads=[B_nb], writes=[B_nb])
            kb.dma("sp", cbase_d[0:1, :], cbi[0:1, :], reads=[B_nb])
            oh = sbt(ph, "oh", [128, NEXP + 1]); cbp = sbt(ph, "cbp", [128, 2]); B_oh = Buf()
            kb.dma("sp", oh[:], oh2[:, :], writes=[B_oh])
            ohj = sbt(ph, "ohj", [128, NEXP])
            kb.op("dve", lambda e: e.tensor_tensor(out=ohj[:], in0=oh[:, 0:NEXP], in1=cbs[:], op=ALU.mult), reads=[B_oh, B_nb], writes=[B_oh])
            kb.op("dve", lambda e: e.tensor_reduce(out=cbp[:, 0:1], in_=ohj[:], axis=AX.X, op=ALU.add), reads=[B_oh], writes=[B_oh])
            kb.op("dve", lambda e: e.tensor_tensor(out=cbp[:, 1:2], in0=cbp[:, 0:1], in1=oh[:, NEXP:NEXP + 1], op=ALU.add),
                  reads=[B_oh], writes=[B_oh])
            rtf = sbt(ph, "rtf", [128, NSL // 128]); rti = sbt(ph, "rti", [128, NSL // 128], I32); B_rt = Buf()
            kb.op("pool", lambda e: e.iota(rti[:], pattern=[[1, NSL // 128]], base=0, channel_multiplier=0), writes=[B_rt])
            kb.op("dve", lambda e: e.tensor_copy(out=rtf[:], in_=rti[:]), reads=[B_rt], writes=[B_rt])
            kb.op("dve", lambda e: e.tensor_scalar(out=rtf[:], in0=rtf[:], scalar1=cbp[:, 1:2], scalar2=None, op0=ALU.add),
                  reads=[B_rt, B_oh], writes=[B_rt])
            kb.op("dve", lambda e: e.tensor_copy(out=rti[:], in_=rtf[:]), reads=[B_rt], writes=[B_rt])
            kb.dma("sp", rowtab_d.rearrange("(p f) o -> p (f o)", p=128), rti[:], reads=[B_rt])
            kb.dma("sp", nblk_d[0:1, :], nbi[0:1, :], reads=[B_nb])
            dc8i = sbt(ph, "dc8i", [128, NT, 8], I32); B_dc8 = Buf()
            key = sbt(ph, "key", [128, NEXP]); B_key = Buf()
            d8f = sbt(ph, "d8f", [128, 8]); d8i = sbt(ph, "d8i", [128, NT, 8], I32); w8 = sbt(ph, "w8", [128, NT, 8])
            B_d8 = Buf(); B_w8 = Buf()
            e8i = sbt(ph, "e8i", [128, 8], I32); e8f = sbt(ph, "e8f", [128, 8]); B_e8 = Buf()
            for t in range(NT):
                pi = t % 2
                for t2 in range(t):
                    kb.op("pe", lambda e, t2=t2: e.matmul(pos_ps[pi][:], lhsT=ones_b[:], rhs=Mb_[:, t2, :], start=(t2 == 0), stop=False),
                          reads=[B_G], writes=[B_pps[pi]], inc=False)
                kb.op("pe", lambda e, t=t: e.matmul(pos_ps[pi][:], lhsT=lsb[:], rhs=Mb_[:, t, :], start=(t == 0), stop=True),
                      reads=[B_G], writes=[B_pps[pi]])
                kb.op("dve", lambda e: e.tensor_tensor(out=key[:], in0=pos_ps[pi][:], in1=eb[:, 0:NEXP], op=ALU.add),
                      reads=[B_pps[pi], B_G], writes=[B_key])
                kb.op("dve", lambda e, t=t: e.tensor_tensor(out=key[:], in0=key[:], in1=Mb_[:, t, :], op=ALU.mult),
                      reads=[B_key, B_G], writes=[B_key])
                kb.op("dve", lambda e: e.max(out=d8f[:], in_=key[:]), reads=[B_key], writes=[B_key])
                kb.op("dve", lambda e: e.tensor_scalar(out=d8f[:], in0=d8f[:], scalar1=-1.0, scalar2=None, op0=ALU.add),
                      reads=[B_key], writes=[B_key])
                kb.op("dve", lambda e, t=t: e.tensor_copy(out=d8i[:, t, :], in_=d8f[:]), reads=[B_key], writes=[B_d8])
                kb.op("dve", lambda e: e.scalar_tensor_tensor(out=key[:], in0=pos_ps[pi][:], scalar=1.0, in1=cbs[:], op0=ALU.add,
                                                              op1=ALU.add), reads=[B_pps[pi], B_nb, B_key], writes=[B_key])
                kb.op("dve", lambda e, t=t: e.tensor_tensor(out=key[:], in0=key[:], in1=Mb_[:, t, :], op=ALU.mult),
                      reads=[B_key, B_G], writes=[B_key])
                kb.op("dve", lambda e: e.max(out=d8f[:], in_=key[:]), reads=[B_key], writes=[B_key])
                kb.op("dve", lambda e: e.tensor_scalar(out=d8f[:], in0=d8f[:], scalar1=-1.0, scalar2=None, op0=ALU.add),
                      reads=[B_key], writes=[B_key])
                kb.op("dve", lambda e, t=t: e.tensor_copy(out=dc8i[:, t, :], in_=d8f[:]), reads=[B_key], writes=[B_dc8])
                kb.op("dve", lambda e, t=t: e.tensor_tensor(out=key[:], in0=Gt[:, t, 0:NEXP], in1=eb[:, NEXP:2 * NEXP], op=ALU.add),
                      reads=[B_G, B_key], writes=[B_key])
                kb.op("dve", lambda e, t=t: e.tensor_tensor(out=key[:], in0=key[:], in1=Mb_[:, t, :], op=ALU.mult),
                      reads=[B_key, B_G], writes=[B_key])
                kb.op("dve", lambda e: e.max(out=d8f[:], in_=key[:]), reads=[B_key], writes=[B_key])
                kb.op("dve", lambda e, t=t: e.tensor_single_scalar(out=e8i[:], in_=d8i[:, t, :], scalar=11, op=ALU.arith_shift_right),
                      reads=[B_d8], writes=[B_e8])
                kb.op("dve", lambda e: e.tensor_copy(out=e8f[:], in_=e8i[:]), reads=[B_e8], writes=[B_e8])
                kb.op("dve", lambda e: e.tensor_scalar(out=e8f[:], in0=e8f[:], scalar1=-4.0, scalar2=-4.0, op0=ALU.mult, op1=ALU.add),
                      reads=[B_e8], writes=[B_e8])
                kb.op("dve", lambda e, t=t: e.tensor_tensor(out=w8[:, t, :], in0=d8f[:], in1=e8f[:], op=ALU.add),
                      reads=[B_key, B_e8], writes=[B_w8])
                for k in range(8):
                    kb.coll(lambda g, t=t, k=k: g.indirect_dma_start(
                        out=idxtab_d[:, :], out_offset=bass.IndirectOffsetOnAxis(ap=d8i[:, t, k:k + 1], axis=0),
                        in_=tokid[:, t:t + 1], in_offset=None),
                        reads=[B_d8, B_tok, B_tab], writes=[])
            kb.dma("sp", dest_d.rearrange("t p k -> p t k"), dc8i[:], reads=[B_dc8])
            kb.dma("sp", w8_d.rearrange("t p k -> p t k"), w8[:], reads=[B_w8])
            kb.barrier()
        if stop_after == "F2":
            return nc

        with ExitStack() as ph:
            nbt = sbt(ph, "nbt", [1, NEXP], I32); B_nbt = Buf()
            kb.dma("sp", nbt[:], nblk_d[0:1, :], writes=[B_nbt])
            rowcur = sbt(ph, "rowcur", [128, 1], I32); B_rowcur = Buf()
            rowst = [sbt(ph, "rowst%d" % i, [128, 1], I32) for i in range(2)]; B_rowst = [Buf(), Buf()]
            yor = sbt(ph, "yor", [128, D]); B_yor = Buf()
            kb.op("pool", lambda e: e.iota(rowcur[:], pattern=[[0, 1]], base=NSLC, channel_multiplier=1), writes=[B_rowcur])
            kb.op("pool", lambda e: e.memset(yor[:], 0.0), writes=[B_yor])

            def flush_pending():
                kb.coll(lambda g: g.indirect_dma_start(out=slot_d[:, :], out_offset=bass.IndirectOffsetOnAxis(ap=rowcur[:, :], axis=0),
                                                       in_=yor[:, :], in_offset=None),
                        reads=[B_yor, B_rowcur], writes=[])
            ekeys = ["pe", "dve", "act", "pool", "sp"]
            NWB = 2
            NST = 6
            stg = [sbt(ph, "stg%d" % i, [128, 2048]) for i in range(NST)]; B_stg = [Buf() for _ in range(NST)]
            Wg = [sbt(ph, "Wg%d" % i, [128, 16, 512], BF16) for i in range(NWB)]
            Wu = [sbt(ph, "Wu%d" % i, [128, 16, 512], BF16) for i in range(NWB)]
            Wd = [sbt(ph, "Wd%d" % i, [128, 4, D], BF16) for i in range(NWB)]
            B_We = [Buf() for _ in range(NWB)]
            idxb = [sbt(ph, "idxb%d" % i, [128, 1], I32) for i in range(4)]; B_idx = [Buf() for _ in range(4)]
            xg = [sbt(ph, "xg%d" % i, [128, D], BF16) for i in range(2)]; B_xg = [Buf(), Buf()]
            xgT = [sbt(ph, "xgT%d" % i, [128, 16, 128], BF16) for i in range(2)]; B_xgT = [Buf(), Buf()]
            sgs = [sbt(ph, "sgs%d" % i, [128, 512]) for i in range(2)]; B_sgs = [Buf(), Buf()]
            acts = [sbt(ph, "acts%d" % i, [128, 512], BF16) for i in range(2)]; B_acts = [Buf(), Buf()]
            actT = [sbt(ph, "actT%d" % i, [128, 4, 128], BF16) for i in range(2)]; B_actT = [Buf(), Buf()]
            yo = [sbt(ph, "yo%d" % i, [128, D]) for i in range(2)]; B_yo = [Buf(), Buf()]
            psX = [pst(ph, "psX%d" % i, [128, 8, 128], BF16) for i in range(2)]; B_psX = [Buf(), Buf()]
            psg = pst(ph, "psg", [128, 512]); B_psg = Buf()
            psu = pst(ph, "psu", [128, 512]); B_psu = Buf()
            psA = pst(ph, "psA", [128, 4, 128], BF16); B_psA = Buf()
            psy = [pst(ph, "psy%d" % i, [128, 512]) for i in range(2)]; B_psy = [Buf(), Buf()]
            h2Tv = h2T_d.rearrange("k p t -> p k t")

            pctr_ = [0]

            def piece_list(e_):
                bi = e_ % NWB
                gv = ew_gate[e_].rearrange("(k p) f -> p k f", p=128)
                uv = ew_up[e_].rearrange("(k p) f -> p k f", p=128)
                dv = ew_down[e_].rearrange("(k p) d -> p k d", p=128)
                pcs = []
                for q in range(4):
                    pcs.append((gv[:, 4 * q:4 * q + 4, :], Wg[bi][:, 4 * q:4 * q + 4, :], bi))
                    pcs.append((uv[:, 4 * q:4 * q + 4, :], Wu[bi][:, 4 * q:4 * q + 4, :], bi))
                for fc in range(4):
                    pcs.append((dv[:, fc:fc + 1, :], Wd[bi][:, fc:fc + 1, :], bi))
                return pcs

            def load_piece(pc):
                srcap, dstap, bi = pc
                n = pctr_[0]
                pctr_[0] += 1
                si = n % NST
                k4 = srcap.shape[1]
                stv = stg[si][:].rearrange("p (k f) -> p k f", k=k4)
                kb.dma("sp", stv, srcap, writes=[B_stg[si]])
                kb.op("dve", lambda e: e.tensor_copy(out=dstap, in_=stv), reads=[B_stg[si]], writes=[B_We[bi]])

            bctr = [0]

            def block(e_, j, shared=False):
                n = bctr[0]
                bctr[0] += 1
                wi = e_ % NWB
                b2 = n % 2
                row0 = (e_ * 16 + j) * 128
                if shared:
                    kb.dma("sp", xgT[b2][:], h2Tv[:, :, j * 128:(j + 1) * 128], writes=[B_xgT[b2]])
                else:
                    i4 = n % 4
                    kb.dma("pool", idxb[i4][:], idxtab_d[row0:row0 + 128, :], writes=[B_idx[i4]])
                    kb.dma("pool", rowst[b2][:], rowtab_d[row0:row0 + 128, :], writes=[B_rowst[b2]])
                    kb.coll(lambda g: g.indirect_dma_start(out=xg[b2][:, :], out_offset=None, in_=h2tok_d[:, :],
                                                           in_offset=bass.IndirectOffsetOnAxis(ap=idxb[i4][:, :], axis=0)),
                            reads=[B_idx[i4]], writes=[B_xg[b2]])
                    flush_pending()
                    kb.op("dve", lambda e: e.tensor_copy(out=rowcur[:], in_=rowst[b2][:]), reads=[B_rowst[b2]], writes=[B_rowcur])
                    for half in range(2):
                        for kk in range(8):
                            k = half * 8 + kk
                            kb.op("pe", lambda e, k=k, kk=kk: e.transpose(out=psX[half][:, kk, :], in_=xg[b2][:, k * 128:(k + 1) * 128],
                                                                         identity=ident_b[:]),
                                  reads=[B_xg[b2]], writes=[B_psX[half]], inc=(kk == 7))
                        kb.op("act", lambda e, half=half: e.copy(out=xgT[b2][:, half * 8:(half + 1) * 8, :], in_=psX[half][:]),
                              reads=[B_psX[half]], writes=[B_xgT[b2]])
                for k in range(16):
                    kb.op("pe", lambda e, k=k: e.matmul(psg[:], lhsT=xgT[b2][:, k, :], rhs=Wg[wi][:, k, :], start=(k == 0), stop=(k == 15)),
                          reads=[B_xgT[b2], B_We[wi]], writes=[B_psg], inc=(k == 15))
                for k in range(16):
                    kb.op("pe", lambda e, k=k: e.matmul(psu[:], lhsT=xgT[b2][:, k, :], rhs=Wu[wi][:, k, :], start=(k == 0), stop=(k == 15)),
                          reads=[B_xgT[b2], B_We[wi]], writes=[B_psu], inc=(k == 15))
                kb.op("act", lambda e: e.activation(out=sgs[b2][:], in_=psg[:], func=AF.Silu), reads=[B_psg], writes=[B_sgs[b2]])
                kb.op("dve", lambda e: e.tensor_tensor(out=acts[b2][:], in0=psu[:], in1=sgs[b2][:], op=ALU.mult),
                      reads=[B_psu, B_sgs[b2]], writes=[B_acts[b2]])
                for fc in range(4):
                    kb.op("pe", lambda e, fc=fc: e.transpose(out=psA[:, fc, :], in_=acts[b2][:, fc * 128:(fc + 1) * 128], identity=ident_b[:]),
                          reads=[B_acts[b2]], writes=[B_psA], inc=(fc == 3))
                kb.op("act", lambda e: e.copy(out=actT[b2][:], in_=psA[:]), reads=[B_psA], writes=[B_actT[b2]])
                for dc in range(4):
                    yi = dc % 2
                    ds_ = slice(dc * 512, (dc + 1) * 512)
                    for fc in range(4):
                        kb.op("pe", lambda e, fc=fc: e.matmul(psy[yi][:], lhsT=actT[b2][:, fc, :], rhs=Wd[wi][:, fc, ds_],
                                                              start=(fc == 0), stop=(fc == 3)),
                              reads=[B_actT[b2], B_We[wi]], writes=[B_psy[yi]], inc=(fc == 3))
                    ydst, Byd = (yo[b2], B_yo[b2]) if shared else (yor, B_yor)
                    if yi == 0:
                        kb.op("dve", lambda e: e.tensor_copy(out=ydst[:, ds_], in_=psy[yi][:]), reads=[B_psy[yi]], writes=[Byd])
                    else:
                        kb.op("act", lambda e: e.copy(out=ydst[:, ds_], in_=psy[yi][:]), reads=[B_psy[yi]], writes=[Byd])
                if shared:
                    kb.dma("act", slot_d[SHR0 + j * 128:SHR0 + (j + 1) * 128, :], yo[b2][:], reads=[B_yo[b2]])

            NE = cfg_nexp
            for pc in piece_list(0):
                load_piece(pc)
            for e_ in range(NE):
                nxt = piece_list(e_ + 1) if e_ + 1 < NE else []
                if e_ < NEXP:
                    regs = nc.alloc_registers("nbreg%d" % e_, engines=[PEe, DVEe, ACTe, POOLe, SPe])
                    for ek, r in zip(ekeys, regs):
                        kb._wait(kb.eng[ek], B_nbt.w)
                        nc.reg_load(r, nbt[0:1, e_:e_ + 1])
                    nbv = nc.snap(regs, donate=True)
                    def guarded(j):
                        snap = kb.snapshot()
                        with nc.If(nbv > j):
                            block(e_, j)
                        with nc.Else():
                            kb.compensate(snap)

                    def group(js, inner=None):
                        snap = kb.snapshot()
                        with nc.If(nbv > js[0]):
                            for j in js:
                                guarded(j)
                            if inner is not None:
                                inner()
                        with nc.Else():
                            kb.compensate(snap)

                    def pieces(lo, hi):
                        for pc in nxt[lo:hi]:
                            load_piece(pc)

                    def chain(j):
                        snap = kb.snapshot()
                        with nc.If(nbv > j):
                            block(e_, j)
                            if j + 1 < 16:
                                chain(j + 1)
                        with nc.Else():
                            kb.compensate(snap)

                    guarded(0)
                    pieces(0, 6)
                    chain(1)
                    pieces(6, 12)
                    for r in regs:
                        nc.engines[r.engine].free_register(r)
                else:
                    flush_pending()
                    for j in range(16):
                        block(e_, j, shared=True)
            kb.barrier()
        if stop_after == "G":
            return nc

        with ExitStack() as ph:
            B_m3 = Buf()
            g2b = load_bc(ph, "sp", "g2b", mod_d[5:6, :], D, B_m3)
            fgb = load_bc(ph, "sp", "fgb", final_g[0:1, :], D, B_m3)
            dst = sbt(ph, "dst", [128, NT, 8], I32); w8s = sbt(ph, "w8s", [128, NT, 8])
            kb.dma("sp", dst[:], dest_d.rearrange("t p k -> p t k"), writes=[B_m3])
            kb.dma("sp", w8s[:], w8_d.rearrange("t p k -> p t k"), writes=[B_m3])
            x1l = [sbt(ph, "x1l%d" % i, [128, D]) for i in range(2)]; B_x1l = [Buf(), Buf()]
            accA = [sbt(ph, "accA%d" % i, [128, D]) for i in range(2)]; B_accA = [Buf(), Buf()]
            gb = [sbt(ph, "gb%d" % i, [128, D]) for i in range(6)]; B_gb = [Buf() for _ in range(6)]
            junk3 = [sbt(ph, "junk3_%d" % i, [128, D], BF16) for i in range(2)]; B_j3 = [Buf(), Buf()]
            ssq3 = [sbt(ph, "ssq3_%d" % i, [128, 4]) for i in range(2)]; B_s3 = [Buf(), Buf()]
            ot = [sbt(ph, "ot%d" % i, [128, D]) for i in range(2)]; B_ot = [Buf(), Buf()]
            gctr = 0
            for t in range(NT):
                bi = t % 2
                kb.dma("sp", x1l[bi][:], x1_d[t * 128:(t + 1) * 128, :], writes=[B_x1l[bi]])
                kb.dma("sp", accA[bi][:], slot_d[SHR0 + t * 128:SHR0 + (t + 1) * 128, :], writes=[B_accA[bi]])
                for k in range(8):
                    gi = gctr % 6
                    gctr += 1
                    kb.coll(lambda g, k=k, gi=gi: g.indirect_dma_start(
                        out=gb[gi][:, :], out_offset=None, in_=slot_d[:, :],
                        in_offset=bass.IndirectOffsetOnAxis(ap=dst[:, t, k:k + 1], axis=0)), reads=[B_m3], writes=[B_gb[gi]])
                    kb.op("dve", lambda e, k=k, gi=gi: e.scalar_tensor_tensor(
                        out=accA[bi][:], in0=gb[gi][:], scalar=w8s[:, t, k:k + 1], in1=accA[bi][:], op0=ALU.mult, op1=ALU.add),
                        reads=[B_gb[gi], B_m3, B_accA[bi]], writes=[B_accA[bi]])
                kb.op("dve", lambda e: e.tensor_tensor(out=accA[bi][:], in0=accA[bi][:], in1=g2b[:], op=ALU.mult),
                      reads=[B_accA[bi], B_m3], writes=[B_accA[bi]])
                kb.op("pool", lambda e: e.tensor_tensor(out=accA[bi][:], in0=accA[bi][:], in1=x1l[bi][:], op=ALU.add),
                      reads=[B_accA[bi], B_x1l[bi]], writes=[B_accA[bi]])
                rms_mod_tile(accA[bi][:], B_accA[bi], fgb[:], None, B_m3, junk3[bi], B_j3[bi], ssq3[bi], B_s3[bi], ot[bi], B_ot[bi],
                             None, None)
                kb.dma("sp", out_d[t * 128:(t + 1) * 128, :], ot[bi][:], reads=[B_ot[bi]])
            kb.barrier()
        return nc


def _host_consts():
    t = np.arange(128)
    ident = np.eye(128, dtype=np.float32)
    tri = (t[:, None] <= t[None, :]).astype(np.float32)
    triT = (t[:, None] >= t[None, :]).astype(np.float32)
    ones = np.ones((128, 128), np.float32)
    nmf = np.where(t[None, :] >= t[:, None], 0.0, NEG).astype(np.float32)
    nmb = np.where(t[None, :] <= t[:, None], 0.0, NEG).astype(np.float32)
    return np.stack([ident, tri, triT, ones, nmf, nmb])


def _rope_tables(s):
    pos = np.arange(-128, OWN + 128) + s * OWN
    pos = np.clip(pos, 0, SEQ - 1)
    rows = pos // 64
    cols = pos % 64
    inv = (10000.0 ** (-np.arange(0, 64, 2, dtype=np.float32) / 64)).astype(np.float32)
    ar = rows.astype(np.float32)[None, :] * inv[:, None]
    ac = cols.astype(np.float32)[None, :] * inv[:, None]
    cr, sr, cc, sc = np.cos(ar), np.sin(ar), np.cos(ac), np.sin(ac)
    C = np.concatenate([cr, cr, cc, cc], 0).astype(np.float32)
    S = np.concatenate([-sr, sr, -sc, sc], 0).astype(np.float32)
    return C, S


def _masks(s):
    import ml_dtypes
    j = np.arange(128)[:, None]
    r = np.arange(128)[None, :]
    lo = np.where(j >= r, 0.0, NEG).astype(np.float32)
    hi = np.where(j <= r, 0.0, NEG).astype(np.float32)
    allneg = np.full((128, 128), NEG, np.float32)
    lo0 = allneg if s == 0 else lo
    hi15 = allneg if s == 1 else hi
    m = np.stack([np.tile(a, (1, 4)) for a in (lo, hi, lo0, hi15)])
    return m.astype(ml_dtypes.bfloat16)


def _prep_inputs(inp):
    f = lambda a: np.ascontiguousarray(np.asarray(a, dtype=np.float32))
    x = f(inp["x"]); c = f(inp["c"]); ctx = f(inp["ctx"]); c_ctx = f(inp["c_ctx"])
    w_in = f(inp["w_in"][0])
    perm1 = np.concatenate([np.arange(32, 64), np.arange(0, 32), np.arange(96, 128), np.arange(64, 96)])
    perm = np.concatenate([h * 128 + perm1 for h in range(10)])
    w_in_sw = np.ascontiguousarray(w_in[:, :1280][:, perm])
    conv_w = f(inp["conv_w"][0])
    conv_wl = np.ascontiguousarray(conv_w.reshape(5, 16, 128).transpose(2, 1, 0))
    conv_bl = np.ascontiguousarray(f(inp["conv_b"][0]).reshape(16, 128).T)
    ew_gate = np.concatenate([f(inp["expert_w_gate"][0]), f(inp["shared_w_gate"][0])[None]], 0)
    ew_up = np.concatenate([f(inp["expert_w_up"][0]), f(inp["shared_w_up"][0])[None]], 0)
    ew_down = np.concatenate([f(inp["expert_w_down"][0]), f(inp["shared_w_down"][0])[None]], 0)
    consts = _host_consts()
    ee = np.arange(NEXP, dtype=np.float32)
    ebase = np.tile(np.concatenate([ee * OWN + 1.0, (ee + 1.0) * 4.0])[None, :], (128, 1)).astype(np.float32)
    tt = np.arange(128)
    lstrict = (tt[:, None] < tt[None, :]).astype(np.float32)
    oh2 = np.zeros((128, NEXP + 1), np.float32)
    oh2[tt, tt // 2] = 1.0
    oh2[:, NEXP] = (tt % 2) * 1024.0
    shared = dict(
        w_ada=f(inp["w_ada"][0]), b_ada=f(inp["b_ada"]), norm1_g=f(inp["norm1_g"]), norm2_g=f(inp["norm2_g"]),
        w_in=w_in, w_in_sw=w_in_sw, attn_sink=f(inp["attn_sink"]), conv_wl=conv_wl, conv_bl=conv_bl,
        dt_bias=f(inp["dt_bias"]).reshape(1, 32), a_log=f(inp["a_log"]).reshape(1, 32), d_skip=f(inp["d_skip"]),
        ssd_norm_g=f(inp["ssd_norm_g"]), w_out=f(inp["w_out"][0]), router_w=f(inp["router_w"][0]),
        router_bias=f(inp["router_bias"]), ew_gate=ew_gate, ew_up=ew_up, ew_down=ew_down,
        final_g=f(inp["final_norm_g"]).reshape(1, D), consts=consts, ebase=ebase, lstrict=lstrict, oh2=oh2)
    maps = []
    for core in range(8):
        b, s = core // 2, core % 2
        xo = x[b, s * OWN:(s + 1) * OWN]
        halo = np.zeros((2, 128, D), np.float32)
        if s == 1:
            halo[0] = x[b, OWN - 128:OWN]
        else:
            halo[1] = x[b, OWN:OWN + 128]
        cl = np.concatenate([c[b].reshape(16, 128).T, c_ctx.reshape(16, 128).T], 1)
        C, S = _rope_tables(s)
        fl = np.zeros((128, 4), np.float32)
        fl[:, 0] = s; fl[:, 1] = 1 - s; fl[:, 2] = 1.0 if s == 1 else 0.0; fl[:, 3] = 1.0 if s == 0 else 0.0
        dsel = 0 if s == 1 else 1
        xoth = x[b, (1 - s) * OWN:(2 - s) * OWN]
        xadj = xo[0:128] if s == 1 else xo[OWN - 128:OWN]
        tri, triT = consts[1], consts[2]
        extra = dict(x_oth=np.ascontiguousarray(xoth), x_adj=np.ascontiguousarray(xadj),
                     w_dt_sel=np.ascontiguousarray(w_in[:, 4608 + dsel * 16:4608 + dsel * 16 + 16]),
                     dtb_sel=np.ascontiguousarray(shared["dt_bias"][:, dsel * 16:(dsel + 1) * 16]),
                     alog_sel=np.ascontiguousarray(shared["a_log"][:, dsel * 16:(dsel + 1) * 16]),
                     tri_sel=np.ascontiguousarray(tri if dsel == 0 else triT))
        m = dict(shared)
        m.update(extra)
        m.update(x_own=np.ascontiguousarray(xo), x_halo=halo, ctx_b=np.ascontiguousarray(ctx[b]),
                 c_lay=np.ascontiguousarray(cl), rope_c=C, rope_s=S, cmask=_masks(s), flags=fl)
        maps.append(m)
    return maps


def kernel(**inputs):
    maps = _prep_inputs(inputs)
    nc = build()
    res = run_bass_kernel_spmd(nc, maps, core_ids=list(range(8)))
    out = np.zeros((NB, SEQ, D), np.float32)
    for core in range(8):
        b, s = core // 2, core % 2
        out[b, s * OWN:(s + 1) * OWN] = res.results[core]["out"]
    return out
```
